# Optimizing a Trainium2 kernel written in Bass

```python
import math
import jax
import jax.numpy as jnp
from jax import lax
import numpy as np

D_MODEL = 1024
BATCH = 32
SEQ = 2048
DEPTH = 1
DEC_BATCH = 128
DEC_SEQ = 4
PAST_LEN = 8192
PAGE_SIZE = 128

HEAD_DIM = 64
N_ATT_HEADS = 8
ATT_WIDTH = N_ATT_HEADS * HEAD_DIM
ATT_SCALE = HEAD_DIM ** -0.5
DILATED_GROUPS = ((128, 1), (512, 4), (2048, 16))
WINDOW_MAX = 2048
BAND_BLOCK = 128
ROPE_THETA = 10000.0
N_RET_HEADS = 4
RET_QK_DIM = 64
RET_V_DIM = 128
RET_QK_WIDTH = N_RET_HEADS * RET_QK_DIM
RET_V_WIDTH = N_RET_HEADS * RET_V_DIM
RET_CHUNK = 128
MIX_WIDTH = RET_V_WIDTH + ATT_WIDTH
IN_WIDTHS = (RET_QK_WIDTH, RET_QK_WIDTH, RET_V_WIDTH, RET_V_WIDTH, ATT_WIDTH, ATT_WIDTH, ATT_WIDTH)
IN_WIDTH = 2 * RET_QK_WIDTH + 2 * RET_V_WIDTH + 3 * ATT_WIDTH
D_FF = -(-8 * D_MODEL // (3 * 256)) * 256
N_MOD = 6
EPS = 1e-6

kernel_name = 'hymba_retention_dilated_attn_step'


def rms_norm(x, gain=None):
    xf = x.astype(jnp.float32)
    y = xf * lax.rsqrt(jnp.mean(xf * xf, axis=-1, keepdims=True) + EPS)
    if gain is not None:
        y = y * gain.astype(jnp.float32)
    return y.astype(x.dtype)


def apply_rotary(x, pos, inv_freq):
    ang = pos.astype(jnp.float32)[:, None] * inv_freq[None, :]
    cos = jnp.cos(ang)[:, None, :]
    sin = jnp.sin(ang)[:, None, :]
    x1, x2 = jnp.split(x.astype(jnp.float32), 2, axis=-1)
    return jnp.concatenate([x1 * cos - x2 * sin, x1 * sin + x2 * cos], axis=-1).astype(x.dtype)


def rope_inv_freq(dim):
    return ROPE_THETA ** (-jnp.arange(0, dim, 2, dtype=jnp.float32) / dim)


def retnet_inv_freq(dim):
    return ROPE_THETA ** (-jnp.linspace(0.0, 1.0, dim // 2, dtype=jnp.float32))


def retention_chunked(q, k, v, state0, chunk):
    b, s, h, _ = q.shape
    dv = v.shape[-1]
    n = s // chunk
    log_g = jnp.log1p(-(2.0 ** (-5.0 - jnp.arange(h, dtype=jnp.float32))))
    idx = jnp.arange(chunk, dtype=jnp.float32)
    diff = idx[:, None] - idx[None, :]
    intra = jnp.where(diff >= 0, jnp.exp(jnp.maximum(diff, 0.0) * log_g[:, None, None]), 0.0)
    xi = jnp.exp((idx + 1.0)[None, :] * log_g[:, None])
    zeta = jnp.exp((chunk - 1.0 - idx)[None, :] * log_g[:, None])
    g_chunk = jnp.exp(chunk * log_g)

    def to_chunks(t):
        return t.astype(jnp.float32).reshape(b, n, chunk, h, -1).transpose(1, 0, 3, 2, 4)

    def step(r, qkv):
        qc, kc, vc = qkv
        sc = jnp.einsum('bhid,bhjd->bhij', qc, kc) * intra
        o = (jnp.einsum('bhij,bhjv->bhiv', sc, vc)
             + jnp.einsum('bhid,bhdv->bhiv', qc, r) * xi[None, :, :, None])
        r = r * g_chunk[None, :, None, None] + jnp.einsum('bhjd,bhjv->bhdv', kc * zeta[None, :, :, None], vc)
        return r, o

    r, o = lax.scan(step, state0.astype(jnp.float32), (to_chunks(q), to_chunks(k), to_chunks(v)))
    o = o.transpose(1, 0, 3, 2, 4).reshape(b, s, h, dv)
    return o, r


def banded_window_attention(q, k, v, n_back):
    nn_, ln, h, dh = q.shape
    blk = BAND_BLOCK
    nb = -(-ln // blk)
    lp = nb * blk
    qb = jnp.pad(q, ((0, 0), (0, lp - ln), (0, 0), (0, 0))).reshape(nn_, nb, blk, h, dh)

    def band(t):
        tp = jnp.pad(t, ((0, 0), (blk, lp - ln), (0, 0), (0, 0)))
        return jnp.concatenate([tp[:, :lp].reshape(nn_, nb, blk, h, dh),
                                tp[:, blk:].reshape(nn_, nb, blk, h, dh)], axis=2)

    kb, vb = band(k), band(v)
    s = jnp.einsum('nbihd,nbjhd->nbhij', qb, kb, preferred_element_type=jnp.float32) * ATT_SCALE
    qi = jnp.arange(nb)[:, None] * blk + jnp.arange(blk)[None, :]
    kj = jnp.arange(nb)[:, None] * blk - blk + jnp.arange(2 * blk)[None, :]
    dist = qi[:, :, None] - kj[:, None, :]
    mask = (dist >= 0) & (dist <= n_back) & (kj[:, None, :] >= 0)
    s = jnp.where(mask[None, :, None], s, -jnp.inf)
    lse = jax.nn.logsumexp(s, axis=-1)
    p = jnp.exp(s - lse[..., None])
    o = jnp.einsum('nbhij,nbjhd->nbihd', p, vb.astype(jnp.float32))
    o = o.reshape(nn_, lp, h, dh)[:, :ln]
    lse = lse.transpose(0, 1, 3, 2).reshape(nn_, lp, h)[:, :ln]
    return o, lse


def combine_by_denominator(outs, lses):
    w = jax.nn.softmax(jnp.stack(lses, axis=0), axis=0)
    return jnp.sum(w[..., None] * jnp.stack(outs, axis=0), axis=0)


def dilated_attention_prompt(q, k, v):
    b, s, h, dh = q.shape
    outs, lses = [], []
    for window, dil in DILATED_GROUPS:
        ln = s // dil

        def split(t):
            return t.reshape(b, ln, dil, h, dh).transpose(0, 2, 1, 3, 4).reshape(b * dil, ln, h, dh)

        o, lse = banded_window_attention(split(q), split(k), split(v), window // dil)
        outs.append(o.reshape(b, dil, ln, h, dh).transpose(0, 2, 1, 3, 4).reshape(b, s, h, dh))
        lses.append(lse.reshape(b, dil, ln, h).transpose(0, 2, 1, 3).reshape(b, s, h))
    return combine_by_denominator(outs, lses)


def dilated_attention_sample(q, k_all, v_all, buf_len):
    b, t, h, dh = q.shape
    outs, lses = [], []
    for window, dil in DILATED_GROUPS:
        n = window // dil
        idx = buf_len + jnp.arange(t)[:, None] - dil * jnp.arange(n + 1)[None, :]
        valid = idx >= 0
        flat = jnp.clip(idx, 0).reshape(-1)
        kg = jnp.take(k_all, flat, axis=1).reshape(b, t, n + 1, h, dh)
        vg = jnp.take(v_all, flat, axis=1).reshape(b, t, n + 1, h, dh)
        s = jnp.einsum('bthd,btmhd->bhtm', q, kg, preferred_element_type=jnp.float32) * ATT_SCALE
        s = jnp.where(valid[None, None], s, -jnp.inf)
        lse = jax.nn.logsumexp(s, axis=-1)
        p = jnp.exp(s - lse[..., None])
        outs.append(jnp.einsum('bhtm,btmhd->bthd', p, vg.astype(jnp.float32)))
        lses.append(lse.transpose(0, 2, 1))
    return combine_by_denominator(outs, lses)


def hybrid_layer(x, c, pos, ret_state0, attend, w_ada, b_ada, norm1_g, w_in, w_out,
                 norm2_g, w_gate, w_up, w_down):
    b, s, _ = x.shape
    mod = jnp.dot(jax.nn.silu(c), w_ada) + b_ada
    sh1, sc1, g1, sh2, sc2, g2 = jnp.split(mod[:, None, :], N_MOD, axis=-1)
    h = rms_norm(x, norm1_g) * (1.0 + sc1) + sh1
    z = jnp.einsum('bsd,de->bse', h, w_in)
    offs = [int(o) for o in np.cumsum(IN_WIDTHS)[:-1]]
    rq, rk, rv, rg, aq, ak, av = jnp.split(z, offs, axis=-1)
    rfreq = retnet_inv_freq(RET_QK_DIM)
    rq = apply_rotary(rq.reshape(b, s, N_RET_HEADS, RET_QK_DIM), pos, rfreq)
    rk = apply_rotary(rk.reshape(b, s, N_RET_HEADS, RET_QK_DIM), pos, rfreq) * (RET_QK_DIM ** -0.5)
    rv = rv.reshape(b, s, N_RET_HEADS, RET_V_DIM)
    ret_o, ret_state = retention_chunked(rq, rk, rv, ret_state0, math.gcd(s, RET_CHUNK))
    ret_y = rms_norm(ret_o).reshape(b, s, RET_V_WIDTH) * jax.nn.silu(rg.astype(jnp.float32))
    afreq = rope_inv_freq(HEAD_DIM)
    aq = apply_rotary(aq.reshape(b, s, N_ATT_HEADS, HEAD_DIM), pos, afreq)
    ak = apply_rotary(ak.reshape(b, s, N_ATT_HEADS, HEAD_DIM), pos, afreq)
    av = av.reshape(b, s, N_ATT_HEADS, HEAD_DIM)
    att_y = attend(aq, ak, av).reshape(b, s, ATT_WIDTH)
    mixed = jnp.concatenate([ret_y, att_y], axis=-1).astype(x.dtype)
    x = x + g1 * jnp.einsum('bse,ed->bsd', mixed, w_out)
    h = rms_norm(x, norm2_g) * (1.0 + sc2) + sh2
    ff = jax.nn.silu(jnp.einsum('bsd,df->bsf', h, w_gate)) * jnp.einsum('bsd,df->bsf', h, w_up)
    x = x + g2 * jnp.einsum('bsf,fd->bsd', ff, w_down)
    return x, ret_state.astype(x.dtype), ak, av


def setup_inputs(seed: int = 0) -> dict:
    key = jax.random.key(seed)
    ks = jax.random.split(key, 18)
    buf = min(WINDOW_MAX, PAST_LEN)

    def nrm(k, shape, scale):
        return jax.random.normal(k, shape, jnp.float32) * scale

    return {
        'x_prompt': nrm(ks[0], (BATCH, SEQ, D_MODEL), 1.0),
        'x_sample': nrm(ks[1], (DEC_BATCH, DEC_SEQ, D_MODEL), 1.0),
        'cache_attn_k': nrm(ks[2], (DEPTH, DEC_BATCH, buf, N_ATT_HEADS, HEAD_DIM), 1.0),
        'cache_attn_v': nrm(ks[3], (DEPTH, DEC_BATCH, buf, N_ATT_HEADS, HEAD_DIM), 1.0),
        'state_ret': nrm(ks[4], (DEPTH, DEC_BATCH, N_RET_HEADS, RET_QK_DIM, RET_V_DIM), 1.0),
        'c_prompt': nrm(ks[5], (BATCH, D_MODEL), 1.0),
        'c_sample': nrm(ks[6], (DEC_BATCH, D_MODEL), 1.0),
        'w_ada': nrm(ks[7], (DEPTH, D_MODEL, N_MOD * D_MODEL), 0.5 * D_MODEL ** -0.5),
        'b_ada': nrm(ks[8], (DEPTH, N_MOD * D_MODEL), 0.02),
        'norm1_g': 1.0 + nrm(ks[9], (DEPTH, D_MODEL), 0.02),
        'w_in': nrm(ks[10], (DEPTH, D_MODEL, IN_WIDTH), D_MODEL ** -0.5),
        'w_out': nrm(ks[11], (DEPTH, MIX_WIDTH, D_MODEL), MIX_WIDTH ** -0.5),
        'norm2_g': 1.0 + nrm(ks[12], (DEPTH, D_MODEL), 0.02),
        'w_gate': nrm(ks[13], (DEPTH, D_MODEL, D_FF), D_MODEL ** -0.5),
        'w_up': nrm(ks[14], (DEPTH, D_MODEL, D_FF), D_MODEL ** -0.5),
        'w_down': nrm(ks[15], (DEPTH, D_FF, D_MODEL), D_FF ** -0.5),
        'final_g': 1.0 + nrm(ks[16], (D_MODEL,), 0.02),
    }


def reference(x_prompt, x_sample, cache_attn_k, cache_attn_v, state_ret, c_prompt, c_sample,
              w_ada, b_ada, norm1_g, w_in, w_out, norm2_g, w_gate, w_up, w_down, final_g):
    seq = x_prompt.shape[1]
    dec_seq = x_sample.shape[1]
    pos_p = jnp.arange(seq)
    pos_s = PAST_LEN + jnp.arange(dec_seq)
    win_p = min(WINDOW_MAX, seq)
    xp, xs = x_prompt, x_sample
    kp_l, vp_l, rp_l, ks_l, vs_l, rs_l = [], [], [], [], [], []
    for l in range(DEPTH):
        lw = (w_ada[l], b_ada[l], norm1_g[l], w_in[l], w_out[l], norm2_g[l], w_gate[l], w_up[l], w_down[l])
        ret0 = jnp.zeros((xp.shape[0], N_RET_HEADS, RET_QK_DIM, RET_V_DIM), jnp.float32)
        xp, rp, kp, vp = hybrid_layer(xp, c_prompt, pos_p, ret0, dilated_attention_prompt, *lw)
        kp_l.append(kp[:, seq - win_p:])
        vp_l.append(vp[:, seq - win_p:])
        rp_l.append(rp)
        k_past, v_past = cache_attn_k[l], cache_attn_v[l]

        def attend_sample(q, k, v, k_past=k_past, v_past=v_past):
            return dilated_attention_sample(q, jnp.concatenate([k_past, k], axis=1),
                                            jnp.concatenate([v_past, v], axis=1), k_past.shape[1])

        xs, rs, ks_, vs_ = hybrid_layer(xs, c_sample, pos_s, state_ret[l], attend_sample, *lw)
        ks_l.append(ks_)
        vs_l.append(vs_)
        rs_l.append(rs.astype(state_ret.dtype))
    y_prompt = rms_norm(xp, final_g)
    y_sample = rms_norm(xs, final_g)
    new_k_prompt = jnp.stack(kp_l, axis=0)
    new_v_prompt = jnp.stack(vp_l, axis=0)
    new_ret_prompt = jnp.stack(rp_l, axis=0)
    new_k_sample = jnp.stack(ks_l, axis=0)
    new_v_sample = jnp.stack(vs_l, axis=0)
    new_ret_sample = jnp.stack(rs_l, axis=0)
    return (y_prompt, y_sample, new_k_prompt, new_v_prompt, new_ret_prompt, new_k_sample, new_v_sample, new_ret_sample)
```

```python
import contextlib
import math
import numpy as np
import ml_dtypes
import concourse.bass as bass
import concourse.mybir as mybir
from concourse.bass_utils import run_bass_kernel_spmd

F32 = mybir.dt.float32
BF16 = mybir.dt.bfloat16
AF = mybir.ActivationFunctionType
ALU = mybir.AluOpType

D = 1024
S = 2048
NT = S // 128
DFF = 2816
NJ = DFF // 128
EPS = 1e-6
PAST = 8192
NCORES = 8


class Buf:
    _cache = {}

    def __new__(cls, name):
        if name in cls._cache:
            return cls._cache[name]
        o = super().__new__(cls)
        o.name = name
        o.w = None
        o.r = {}
        o.dsem = None
        o.dcount = 0
        cls._cache[name] = o
        return o


class KB:
    def __init__(self, nc, stack):
        self.nc = nc
        self.stack = stack
        self.eng = {"pe": nc.tensor, "act": nc.scalar, "dve": nc.vector,
                    "pool": nc.gpsimd, "sp": nc.sync}
        self.sems = {}
        self.cnt = {}
        for k in ("pe", "act", "dve", "pool"):
            self.sems[k] = stack.enter_context(nc.semaphore("c_" + k))
            self.cnt[k] = 0
        self.seen = {k: {} for k in self.eng}
        self.pe_pending = []
        self.uid = 0
        self.dsems = {}
        self.finals = {}

    def sb(self, name, shape, dt, stack=None):
        self.uid += 1
        return (stack or self.stack).enter_context(self.nc.sbuf_tensor(f"{name}_u{self.uid}", list(shape), dt))

    def ps(self, name, shape, dt=F32):
        return self.stack.enter_context(self.nc.psum_tensor(name, list(shape), dt))

    def _deps(self, reads, writes):
        deps = {}

        def add(ev):
            if ev is None:
                return
            s, v = ev
            if deps.get(s, 0) < v:
                deps[s] = v
        for b in reads:
            add(b.w)
        for b in writes:
            add(b.w)
            for s, v in b.r.items():
                add((s, v))
        return deps

    def _wait(self, ek, deps, skip_own_pe=False):
        e = self.eng[ek]
        seen = self.seen[ek]
        for s, v in deps.items():
            if skip_own_pe and s is self.sems["pe"]:
                continue
            if seen.get(s, 0) >= v:
                continue
            e.wait_ge(s, v)
            seen[s] = v

    def _commit(self, ev, reads, writes):
        s, v = ev
        for b in reads:
            if b.r.get(s, 0) < v:
                b.r[s] = v
        for b in writes:
            b.w = ev
            b.r = {}

    def op(self, ek, fn, reads=(), writes=()):
        if KB.dead:
            return
        self._wait(ek, self._deps(reads, writes))
        inst = fn(self.eng[ek])
        self.cnt[ek] += 1
        inst.then_inc(self.sems[ek], 1)
        ev = (self.sems[ek], self.cnt[ek])
        self._commit(ev, reads, writes)
        return ev

    def _pe_done(self, inst, reads, writes, last):
        if last:
            self.cnt["pe"] += 1
            inst.then_inc(self.sems["pe"], 1)
            ev = (self.sems["pe"], self.cnt["pe"])
            for (r, w) in self.pe_pending:
                self._commit(ev, r, w)
            self.pe_pending = []
            self._commit(ev, reads, writes)
        else:
            self.pe_pending.append((list(reads), list(writes)))

    def mm(self, out, lhsT, rhs, reads, writes, start=True, stop=True, last=True):
        if KB.dead:
            return
        self._wait("pe", self._deps(reads, writes), skip_own_pe=True)
        inst = self.nc.tensor.matmul(out, lhsT, rhs, start=start, stop=stop)
        self._pe_done(inst, reads, writes, last)

    def tr(self, out, in_, ident, reads, writes, last=True):
        if KB.dead:
            return
        self._wait("pe", self._deps(reads, writes), skip_own_pe=True)
        inst = self.nc.tensor.transpose(out, in_, ident)
        self._pe_done(inst, reads, writes, last)

    def dma(self, q, out, in_, reads, writes, owner, final=False, **kw):
        if KB.dead:
            return
        self._wait(q, self._deps(reads, writes))
        if owner.dsem is None:
            self.uid += 1
            owner.dsem = self.stack.enter_context(self.nc.semaphore(f"d_{owner.name}_{self.uid}"))
            self.dsems[f"{owner.name}_{self.uid}"] = owner
        inst = self.eng[q].dma_start(out=out, in_=in_, **kw)
        owner.dcount += 16
        inst.then_inc(owner.dsem, 16)
        ev = (owner.dsem, owner.dcount)
        self._commit(ev, reads, writes)
        if final:
            self.finals[id(owner)] = owner
        return ev

    def barrier(self, force=False):
        if KB.dead and not force:
            return
        for ek in self.eng:
            e = self.eng[ek]
            seen = self.seen[ek]
            for k2 in ("pe", "act", "dve", "pool"):
                s, v = self.sems[k2], self.cnt[k2]
                if v > 0 and seen.get(s, 0) < v:
                    e.wait_ge(s, v)
                    seen[s] = v
            for o in self.dsems.values():
                if o.dcount > 0 and seen.get(o.dsem, 0) < o.dcount:
                    e.wait_ge(o.dsem, o.dcount)
                    seen[o.dsem] = o.dcount

    def finish(self):
        self.barrier(force=True)


def _tables():
    t = {}
    pos = np.arange(S, dtype=np.float64)
    afreq = (10000.0 ** (-np.arange(0, 64, 2, dtype=np.float32) / np.float32(64))).astype(np.float32).astype(np.float64)
    rfreq = (10000.0 ** (-np.linspace(0.0, 1.0, 32, dtype=np.float32))).astype(np.float32).astype(np.float64)

    def tm(fn, fr, p):
        a = (p.astype(np.float32)[:, None] * fr.astype(np.float32)[None, :]).astype(np.float32).astype(np.float64)
        v = fn(a).astype(np.float32)
        return v
    for nm, fr in (("a", afreq), ("r", rfreq)):
        c = tm(np.cos, fr, pos).reshape(NT, 128, 32).transpose(1, 0, 2)
        s = tm(np.sin, fr, pos).reshape(NT, 128, 32).transpose(1, 0, 2)
        t["cos_" + nm] = np.ascontiguousarray(c)
        t["sin_" + nm] = np.ascontiguousarray(s)
        ps = PAST + (np.arange(64) % 4).astype(np.float64)
        t["cos_s" + nm] = np.ascontiguousarray(tm(np.cos, fr, ps))
        t["sin_s" + nm] = np.ascontiguousarray(tm(np.sin, fr, ps))
    h = np.arange(4, dtype=np.float64)
    log_g = np.log1p(-(2.0 ** (-5.0 - h)))
    idx = np.arange(128, dtype=np.float64)
    zeta = np.exp((127.0 - idx)[:, None] * log_g[None, :])
    t["zeta"] = zeta.astype(np.float32)
    xi = np.exp((idx + 1.0)[None, :] * log_g[:, None])
    xit = np.zeros((128, 2, 128), np.float32)
    gc = np.zeros((128, 2), np.float32)
    for c in range(2):
        for hh in range(2):
            xit[hh * 64:(hh + 1) * 64, c, :] = xi[2 * c + hh][None, :]
            gc[hh * 64:(hh + 1) * 64, c] = np.exp(128.0 * log_g[2 * c + hh])
    t["xit"] = xit
    t["gc"] = gc
    diff = idx[None, :] - idx[:, None]
    intra = np.zeros((128, 4, 128), np.float32)
    for hd in range(4):
        intra[:, hd, :] = np.where(diff >= 0, np.exp(np.maximum(diff, 0.0) * log_g[hd]), 0.0)
    t["intra"] = intra
    tok = np.arange(64)
    tb, tt = tok // 4, tok % 4
    zs = np.exp((3.0 - tt)[:, None] * log_g[None, :])
    t["zeta_s"] = zs.astype(np.float32)
    xis = np.exp((tt + 1.0)[None, :] * log_g[:, None])
    xits = np.zeros((128, 2, 64), np.float32)
    gcs = np.zeros((128, 2), np.float32)
    for c in range(2):
        for hh in range(2):
            xits[hh * 64:(hh + 1) * 64, c, :] = xis[2 * c + hh][None, :]
            gcs[hh * 64:(hh + 1) * 64, c] = np.exp(4.0 * log_g[2 * c + hh])
    t["xit_s"] = xits
    t["gc_s"] = gcs
    ds = tt[None, :] - tt[:, None]
    same = tb[None, :] == tb[:, None]
    intras = np.zeros((64, 4, 64), np.float32)
    for hd in range(4):
        intras[:, hd, :] = np.where(same & (ds >= 0), np.exp(np.maximum(ds, 0) * log_g[hd]), 0.0)
    t["intra_s"] = intras
    onehot = np.zeros((64, 16), np.float32)
    onehot[tok, tb] = 1.0
    t["onehot_s"] = onehot
    m = np.zeros((128, 256), np.float32)
    jj = idx[:, None]
    ii = idx[None, :]
    m[:, :128] = (ii >= jj)
    m[:, 128:] = (ii <= jj)
    t["amask"] = m.astype(ml_dtypes.bfloat16)
    ms = np.zeros((128, 9, 4), np.float32)
    for tq in range(4):
        ms[:, 0, tq] = (idx >= tq)
        ms[:, 1 + tq, tq] = 1.0
        ms[:, 5 + tq, tq] = 1.0
    t["smask"] = ms.astype(ml_dtypes.bfloat16)
    mn = np.zeros((64, 16, 4), np.float32)
    for j in range(64):
        for tq in range(4):
            if tt[j] < tq:
                mn[j, tb[j], tq] = 1.0
            elif tt[j] == tq:
                mn[j, tb[j], tq] = 3.0
    t["nmask"] = mn.astype(ml_dtypes.bfloat16)
    t["ident_b"] = np.eye(128, dtype=np.float32).astype(ml_dtypes.bfloat16)
    t["ident_f"] = np.eye(128, dtype=np.float32)
    return t


_TAB_DT = {"amask": BF16, "smask": BF16, "nmask": BF16, "ident_b": BF16}


class _Stop(Exception):
    pass


def build(NB=4, NSB=16, do_sample=True, stop=99, nb_run=None):
    def chk(stage):
        if stop <= stage:
            KB.dead = True
    KB.dead = False

    Buf._cache = {}
    nc = bass.Bass("TRN2", target_bir_lowering=False)
    tabs = _tables()

    def din(name, shape, dt=F32):
        return nc.dram_tensor(name, list(shape), dt, kind="ExternalInput").ap()

    def dout(name, shape):
        return nc.dram_tensor(name, list(shape), F32, kind="ExternalOutput").ap()

    xp = din("xp", [NB, S, D])
    xs = din("xs", [64, D])
    ck = din("ck", [16, S, 512])
    cv = din("cv", [16, S, 512])
    sr = din("sr", [16, 4, 64, 128])
    call = din("call", [68, D])
    w_ada = din("w_ada", [D, 6 * D])
    b_ada = din("b_ada", [6 * D])
    n1g = din("n1g", [D])
    w_in = din("w_in", [D, 3072])
    w_out = din("w_out", [D, D])
    n2g = din("n2g", [D])
    w_gate = din("w_gate", [D, DFF])
    w_up = din("w_up", [D, DFF])
    w_down = din("w_down", [DFF, D])
    fgd = din("fg", [D])
    td = {k: din("t_" + k, v.shape, _TAB_DT.get(k, F32)) for k, v in tabs.items()}

    yp = dout("yp", [NB, S, D])
    ys = dout("ys", [64, D])
    kp = dout("kp", [NB, S, 512])
    vp = dout("vp", [NB, S, 512])
    rp = dout("rp", [NB, 4, 64, 128])
    kso = dout("kso", [64, 512])
    vso = dout("vso", [64, 512])
    rso = dout("rso", [16, 4, 64, 128])
    sc_gu = nc.dram_tensor("sc_gu", [NJ, 128, 2048], BF16, kind="Internal").ap()
    sc_d = nc.dram_tensor("sc_d", [NJ, 128, 1024], BF16, kind="Internal").ap()
    vbs = nc.dram_tensor("vbs", [NB, S, 512], BF16, kind="Internal").ap()
    sc_wi = nc.dram_tensor("sc_wi", [8, 128, 3072], BF16, kind="Internal").ap()

    with contextlib.ExitStack() as st:
        k = KB(nc, st)
        try:
            PB = [k.ps(f"pb{i}", [128, 512], F32) for i in range(8)]
            bPB = [Buf(f"pb{i}") for i in range(8)]

            def pbf(i):
                return PB[i][:].bitcast(BF16)

            bWi = Buf("Wi")
            bscwi = Buf("scwi")
            Wo = k.sb("Wo", [128, 8, 1024], BF16); bWo = Buf("Wo")
            modT = k.sb("modT", [128, 48, 68], F32); bmodT = Buf("modT")
            G1T = k.sb("G1T", [128, 8, 68], F32); bG1T = Buf("G1T")
            G2T = k.sb("G2T", [128, 8, 68], F32); bG2T = Buf("G2T")
            bgates = Buf("gates")
            bFG = Buf("FG")
            mixT = k.sb("mixT", [128, 8, S], BF16); bmixT = Buf("mixT")
            T = {}
            bT = Buf("tabs")
            for nm in ("cos_a", "sin_a", "cos_r", "sin_r", "zeta", "xit", "gc", "intra", "amask",
                       "ident_b", "ident_f"):
                T[nm] = k.sb("T_" + nm, tabs[nm].shape, _TAB_DT.get(nm, F32))
                k.dma("sp", T[nm][:], td[nm], [], [bT], bT)
            ss_t = k.sb("ss_t", [128, 8], F32); bss = Buf("ss")
            epsT = k.sb("epsT", [128, 1], F32)
            k.op("dve", lambda e: e.memset(epsT[:], EPS), [], [bT])
            Rf = k.sb("Rf", [128, 2, 128], F32); bRf = Buf("Rf")
            Rb = k.sb("Rb", [128, 2, 128], BF16); bRb = Buf("Rb")

            for dc in range(8):
                k.dma("pool", Wo[:, dc, :], w_out[dc * 128:(dc + 1) * 128, :], [], [bWo], bWo)

            def rms_stats(xt, bxt, npart, col, jk, bjk, width=1024, scale=1.0 / 1024):
                k.op("dve", lambda e: e.memset(ss_t[:npart, col:col + 1], 0.0), [], [bss])
                k.op("act", lambda e: e.activation(jk[:npart, 0:width], xt, AF.Square, accum_out=ss_t[:npart, col:col + 1]), [bxt], [bjk, bss])
                k.op("act", lambda e: e.activation(ss_t[:npart, col:col + 1], ss_t[:npart, col:col + 1], AF.Sqrt, bias=epsT[:npart, :], scale=scale), [bss, bT], [bss])
                k.op("dve", lambda e: e.reciprocal(ss_t[:npart, col:col + 1], ss_t[:npart, col:col + 1]), [bss], [bss])

            with contextlib.ExitStack() as s0:
                Wi0 = k.sb("Wi0", [128, 8, 3072], BF16, s0)
                for dc in range(8):
                    k.dma("pool", Wi0[:, dc, :], w_in[dc * 128:(dc + 1) * 128, :], [], [bWi], bWi)
                k.op("dve", lambda e: e.tensor_scalar_mul(Wi0[:, :, 256:512], Wi0[:, :, 256:512], 0.125), [bWi], [bWi])
                for dc in range(8):
                    k.dma("sp", sc_wi[dc], Wi0[:, dc, :], [bWi], [bscwi], bWi)
                cin = k.sb("cin", [68, D], F32, s0); bcin = Buf("cin")
                cT = k.sb("cT", [128, 8, 68], F32, s0); bcT = Buf("cT")
                badT = k.sb("badT", [128, 48], F32, s0); bbad = Buf("badT")
                n1T = k.sb("n1T", [128, 8], F32, s0)
                n2T = k.sb("n2T", [128, 8], F32, s0)
                wst = [k.sb(f"wst{i}", [128, 8, 512], F32, s0) for i in range(2)]
                bwst = [Buf(f"wst{i}") for i in range(2)]
                k.dma("sp", cin[:], call, [], [bcin], bcin)
                k.dma("sp", badT[:], b_ada.rearrange("(c p) -> p c", p=128), [], [bbad], bbad, allow_slow_non_contiguous=True)
                k.dma("sp", n1T[:], n1g.rearrange("(c p) -> p c", p=128), [], [bbad], bbad, allow_slow_non_contiguous=True)
                k.dma("sp", n2T[:], n2g.rearrange("(c p) -> p c", p=128), [], [bbad], bbad, allow_slow_non_contiguous=True)
                k.op("act", lambda e: e.activation(cin[:], cin[:], AF.Silu), [bcin], [bcin])
                for c in range(8):
                    pbi = c // 4
                    k.tr(PB[pbi][:, (c % 4) * 68:(c % 4 + 1) * 68], cin[:, c * 128:(c + 1) * 128], T["ident_f"][0:68, 0:68],
                         [bcin, bT], [bPB[pbi]], last=(c % 4 == 3))
                for pbi in range(2):
                    k.op("dve", lambda e, pbi=pbi: e.tensor_copy(cT[:, pbi * 4:(pbi + 1) * 4, :], PB[pbi][:, 0:4 * 68].rearrange("p (c n) -> p c n", n=68)),
                         [bPB[pbi]], [bcT])
                for blk in range(12):
                    ws, bws = wst[blk % 2], bwst[blk % 2]
                    k.dma("sp", ws[:], w_ada[:, blk * 512:(blk + 1) * 512].rearrange("(c p) n -> p c n", p=128), [], [bws], bws)
                    pb = 2 + (blk % 2)
                    for fc in range(4):
                        for dc in range(8):
                            k.mm(PB[pb][:, fc * 68:(fc + 1) * 68], ws[:, dc, fc * 128:(fc + 1) * 128], cT[:, dc, :],
                                 [bws, bcT], [bPB[pb]], start=(dc == 0), stop=(dc == 7), last=(dc == 7 and fc == 3))
                    for fc in range(4):
                        ch = blk * 4 + fc
                        k.op("act", lambda e, ch=ch, fc=fc, pb=pb: e.activation(modT[:, ch, :], PB[pb][:, fc * 68:(fc + 1) * 68], AF.Identity,
                                                                                 bias=badT[:, ch:ch + 1]), [bPB[pb], bbad], [bmodT])
                for c in range(8):
                    k.op("dve", lambda e, c=c: e.tensor_scalar(G1T[:, c, :], modT[:, 8 + c, :], 1.0, n1T[:, c:c + 1], op0=ALU.add, op1=ALU.mult),
                         [bmodT, bbad], [bG1T])
                    k.op("dve", lambda e, c=c: e.tensor_scalar(G2T[:, c, :], modT[:, 32 + c, :], 1.0, n2T[:, c:c + 1], op0=ALU.add, op1=ALU.mult),
                         [bmodT, bbad], [bG2T])
                stg = [k.sb(f"stg{i}", [128, 2, 8, 128], BF16, s0) for i in range(2)]
                std = [k.sb(f"std{i}", [128, 1024], BF16, s0) for i in range(2)]
                bstg = [Buf(f"stg{i}") for i in range(2)]
                bstd = [Buf(f"std{i}") for i in range(2)]
                bsc = [Buf(f"sc{j}") for j in range(NJ)]
                for j in range(NJ):
                    sg_, bsg_ = stg[j % 2], bstg[j % 2]
                    sd_, bsd_ = std[j % 2], bstd[j % 2]
                    k.dma("pool", sg_[:, 0, :, :], w_gate[:, j * 128:(j + 1) * 128].rearrange("(c p) n -> p c n", p=128), [], [bsg_], bsg_)
                    k.dma("pool", sg_[:, 1, :, :], w_up[:, j * 128:(j + 1) * 128].rearrange("(c p) n -> p c n", p=128), [], [bsg_], bsg_)
                    k.dma("pool", sd_[:], w_down[j * 128:(j + 1) * 128, :], [], [bsd_], bsd_)
                    k.dma("sp", sc_gu[j], sg_[:].rearrange("p a c n -> p (a c n)"), [bsg_], [bsc[j]], bsg_)
                    k.dma("sp", sc_d[j], sd_[:], [bsd_], [bsc[j]], bsd_)
                k.barrier()
            chk(0)

            def ffn_group(s1, nt, ntok, X1, bX1, h2T, bh2T, ffT, bffT, ring, bring, g2tile, bg2, out_fn, FG, yst, byst, jk, bjk):
                for j in range(NJ):
                    slot = j % len(ring)
                    gu, dd = ring[slot]
                    bsl = bring[slot]
                    k.dma("sp", gu[:].rearrange("p a c n -> p (a c n)"), sc_gu[j], [bsc[j]], [bsl], bsl)
                    for a in range(2):
                        for dc in range(8):
                            k.mm(PB[a][:, 0:ntok], gu[:, a, dc, :], h2T[:, dc, 0:ntok], [bsl, bh2T], [bPB[a]],
                                 start=(dc == 0), stop=(dc == 7), last=(dc == 7))
                    k.op("act", lambda e, j=j: e.activation(ffT[:, j, 0:ntok], PB[0][:, 0:ntok], AF.Silu), [bPB[0]], [bffT])
                    k.op("dve", lambda e, j=j: e.tensor_tensor(ffT[:, j, 0:ntok], ffT[:, j, 0:ntok], PB[1][:, 0:ntok], ALU.mult),
                         [bffT, bPB[1]], [bffT])
                for j in range(NJ):
                    slot = j % len(ring)
                    gu, dd = ring[slot]
                    bsl = bring[slot]
                    k.dma("sp", dd[:], sc_d[j], [bsc[j]], [bsl], bsl)
                    for ti in range(nt):
                        np_ = min(128, ntok - ti * 128)
                        for nb in range(2):
                            k.mm(PB[2 * ti + nb][:np_, :], ffT[:, j, ti * 128:ti * 128 + np_], dd[:, nb * 512:(nb + 1) * 512],
                                 [bsl, bffT], [bPB[2 * ti + nb]], start=(j == 0), stop=(j == NJ - 1), last=(j == NJ - 1 or (ti == nt - 1 and nb == 1)))
                for ti in range(nt):
                    np_ = min(128, ntok - ti * 128)
                    x1 = X1[ti]
                    for nb in range(2):
                        cs = slice(nb * 512, (nb + 1) * 512)
                        k.op("dve", lambda e, nb=nb, cs=cs: e.tensor_tensor(yst[:np_, cs], PB[2 * ti + nb][:np_, :], g2tile[:np_, cs], ALU.mult),
                             [bPB[2 * ti + nb], bg2], [byst])
                    k.op("dve", lambda e: e.tensor_tensor(yst[:np_, :], yst[:np_, :], x1[:np_, :], ALU.add), [byst, bX1[ti]], [byst])
                    rms_stats(yst[:np_, :], byst, np_, 3, jk, bjk)
                    k.op("dve", lambda e: e.scalar_tensor_tensor(yst[:np_, :], yst[:np_, :], ss_t[:np_, 3:4], FG[:np_, :], op0=ALU.mult, op1=ALU.mult),
                         [byst, bss, bFG], [byst])
                    out_fn(ti, yst, byst, np_)


            for b in range(NB if nb_run is None else nb_run):
                bvp = Buf(f"vp{b}")
                with contextlib.ExitStack() as sa:
                    Wi = k.sb("Wi", [128, 8, 3072], BF16, sa)
                    for dc in range(8):
                        k.dma("sp", Wi[:, dc, :], sc_wi[dc], [bscwi], [bWi], bWi)
                    Xt = [k.sb(f"Xt{i}", [128, 1024], F32, sa) for i in range(2)]
                    bXt = [Buf(f"Xt{i}") for i in range(2)]
                    xn = k.sb("xn", [128, 1024], BF16, sa); bxn = Buf("xn")
                    hTs_ = [k.sb(f"hT{i}", [128, 8, 128], BF16, sa) for i in range(2)]; bhTs_ = [Buf(f"hT{i}") for i in range(2)]
                    Rb1 = k.sb("Rb1", [128, 2, 128], BF16, sa); bRb1 = Buf("Rb1")
                    avb = k.sb("avb", [128, 512], BF16, sa); bavb = Buf("avb")
                    Rbs = [Rb, Rb1]; bRbs = [bRb, bRb1]
                    rqk = k.sb("rqk", [128, 512], BF16, sa); brqk = Buf("rqk")
                    rt = [k.sb(f"rt{i}", [128, 8, 32], F32, sa) for i in range(4)]
                    brt = Buf("rt")
                    kz = k.sb("kz", [128, 256], BF16, sa); bkz = Buf("kz")
                    rvb = k.sb("rvb", [128, 512], BF16, sa); brvb = Buf("rvb")
                    sgt = k.sb("sgt", [128, 512], BF16, sa); bsgt = Buf("sgt")
                    rqkT = k.sb("rqkT", [128, 4, 128], BF16, sa); brqkT = Buf("rqkT")
                    rqm = k.sb("rqm", [128, 2, 2, 128], BF16, sa); brqm = Buf("rqm")
                    rqxT = k.sb("rqxT", [128, 2, 2, 128], BF16, sa); brqxT = Buf("rqxT")
                    scm = k.sb("scm", [128, 512], BF16, sa); bscm = Buf("scm")
                    rety = k.sb("rety", [128, 512], BF16, sa); brety = Buf("rety")
                    aqkf = [k.sb(f"aqkf{i}", [128, 512], F32, sa) for i in range(1)]
                    baqkf = [Buf(f"aqkf{i}") for i in range(1)]
                    aqkb = k.sb("aqkb", [128, 1024], BF16, sa); baqkb = Buf("aqkb")
                    avf = [k.sb(f"avf{i}", [128, 512], F32, sa) for i in range(1)]
                    bavf = [Buf(f"avf{i}") for i in range(1)]
                    aqkT = k.sb("aqkT", [128, 8, S], BF16, sa); baqkT = Buf("aqkT")
                    Xacc = k.sb("Xacc", [128, S], F32, sa); bXacc = Buf("Xacc")
                    Va = [k.sb(f"Va{i}", [128, 128], BF16, sa) for i in range(12)]
                    bVa = [Buf(f"Va{i}") for i in range(12)]
                    Et = [k.sb(f"Et{i}", [128, 256], BF16, sa) for i in range(5)]
                    bEt = [Buf(f"Et{i}") for i in range(5)]
                    rct = k.sb("rct", [128, 256], F32, sa); brct = Buf("rct")
                    for i in range(len(Va)):
                        k.op("dve", lambda e, i=i: e.memset(Va[i][:], 1.0), [], [bVa[i]])
                    k.op("dve", lambda e: e.memset(rqm[:], 0.0), [], [brqm])
                    k.op("dve", lambda e: e.memset(Rf[:], 0.0), [], [bRf])
                    k.op("dve", lambda e: e.memset(Rb[:], 0.0), [], [bRb])
                    k.op("dve", lambda e: e.memset(Rb1[:], 0.0), [], [bRb1])

                    ZB = [0, 1, 2, 3, 4, 1]
                    STB = 5
                    TPB = 6
                    RB = 7

                    def xload(t):
                        X, bX = Xt[t % 2], bXt[t % 2]
                        k.dma("sp", X[:], xp[b, t * 128:(t + 1) * 128, :], [], [bX], bX)

                    def s1(t):
                        X, bX = Xt[t % 2], bXt[t % 2]
                        hT, bhT = hTs_[t % 2], bhTs_[t % 2]
                        rms_stats(X[:], bX, 128, 0, aqkb, baqkb)
                        k.op("dve", lambda e: e.tensor_scalar_mul(xn[:], X[:], ss_t[:, 0:1]), [bX, bss], [bxn])
                        tp = pbf(TPB)
                        for c in range(8):
                            k.tr(tp[:, c * 128:(c + 1) * 128], xn[:, c * 128:(c + 1) * 128], T["ident_b"][:], [bxn, bT], [bPB[TPB]], last=(c == 7))
                        k.op("dve", lambda e: e.tensor_tensor(hT[:], tp[:].rearrange("p (c n) -> p c n", n=128),
                                                              G1T[:, :, 64 + b:65 + b].to_broadcast([128, 8, 128]), ALU.mult), [bPB[TPB], bG1T], [bhT])
                        k.op("dve", lambda e: e.tensor_tensor(hT[:], hT[:], modT[:, 0:8, 64 + b:65 + b].to_broadcast([128, 8, 128]), ALU.add), [bhT, bmodT], [bhT])

                    def zc(t, i):
                        hT, bhT = hTs_[t % 2], bhTs_[t % 2]
                        for dc in range(8):
                            k.mm(PB[ZB[i]][:], hT[:, dc, :], Wi[:, dc, i * 512:(i + 1) * 512], [bhT, bWi], [bPB[ZB[i]]],
                                 start=(dc == 0), stop=(dc == 7), last=(dc == 7))

                    def rot(src, bsrc, dst, bdst, cosn, sinn, t):
                        zv = src.rearrange("p (h two f) -> p h two f", two=2, f=32)
                        x1v, x2v = zv[:, :, 0, :], zv[:, :, 1, :]
                        ov = dst.rearrange("p (h two f) -> p h two f", two=2, f=32)
                        cb = T[cosn][:, t, :].unsqueeze(1).to_broadcast([128, 8, 32])
                        sb_ = T[sinn][:, t, :].unsqueeze(1).to_broadcast([128, 8, 32])
                        r0, r1_, r2, r3 = (rt[i][:, 0:8, :] for i in range(4))
                        k.op("dve", lambda e: e.tensor_tensor(r0, x1v, cb, ALU.mult), [bsrc, bT], [brt])
                        k.op("dve", lambda e: e.tensor_tensor(r1_, x2v, sb_, ALU.mult), [bsrc, bT], [brt])
                        k.op("dve", lambda e: e.tensor_tensor(r2, x1v, sb_, ALU.mult), [bsrc, bT], [brt])
                        k.op("dve", lambda e: e.tensor_tensor(r3, x2v, cb, ALU.mult), [bsrc, bT], [brt])
                        k.op("pool", lambda e: e.tensor_tensor(ov[:, :, 0, :], r0, r1_, ALU.subtract), [brt], [bdst])
                        k.op("pool", lambda e: e.tensor_tensor(ov[:, :, 1, :], r2, r3, ALU.add), [brt], [bdst])

                    def r1(t):
                        rot(PB[ZB[0]][:], bPB[ZB[0]], rqk[:], brqk, "cos_r", "sin_r", t)
                        k.op("pool", lambda e: e.tensor_tensor(kz[:].rearrange("p (h f) -> p h f", f=64),
                                                               rqk[:, 256:512].rearrange("p (h f) -> p h f", f=64),
                                                               T["zeta"][:].unsqueeze(2).to_broadcast([128, 4, 64]), ALU.mult), [brqk, bT], [bkz])
                        k.op("act", lambda e: e.activation(rvb[:], PB[ZB[1]][:], AF.Copy), [bPB[ZB[1]]], [brvb])
                        k.op("act", lambda e: e.activation(sgt[:], PB[ZB[2]][:], AF.Silu), [bPB[ZB[2]]], [bsgt])
                        tp = pbf(TPB)
                        for c in range(4):
                            k.tr(tp[:, c * 128:(c + 1) * 128], rqk[:, c * 128:(c + 1) * 128], T["ident_b"][:], [brqk, bT], [bPB[TPB]], last=(c == 3))
                        k.op("act", lambda e: e.activation(rqkT[:].rearrange("p c n -> p (c n)"), tp[:, 0:512], AF.Copy), [bPB[TPB]], [brqkT])
                        k.op("act", lambda e: e.activation(rqm[0:64, 0, :, :].rearrange("p c n -> p (c n)"), tp[0:64, 0:256], AF.Copy), [bPB[TPB]], [brqm])
                        k.op("act", lambda e: e.activation(rqm[64:128, 1, :, :].rearrange("p c n -> p (c n)"), tp[64:128, 0:256], AF.Copy), [bPB[TPB]], [brqm])
                        for vv in range(2):
                            k.op("pool", lambda e, vv=vv: e.tensor_tensor(rqxT[:, vv, :, :], rqm[:, vv, :, :], T["xit"][:], ALU.mult), [brqm, bT], [brqxT])

                    def a1(t):
                        af, baf = aqkf[0], baqkf[0]
                        rot(PB[ZB[3]][:], bPB[ZB[3]], aqkb[:, 0:512], baqkb, "cos_a", "sin_a", t)
                        rot(PB[ZB[4]][:], bPB[ZB[4]], af[:, 0:512], baf, "cos_a", "sin_a", t)
                        k.dma("sp", kp[b, t * 128:(t + 1) * 128, :], af[:, 0:512], [baf], [], baf, final=True)
                        k.op("act", lambda e: e.activation(aqkb[:, 512:1024], af[:, 0:512], AF.Copy), [baf], [baqkb])
                        av, bav = avf[0], bavf[0]
                        k.op("act", lambda e: e.activation(av[:], PB[ZB[5]][:], AF.Copy), [bPB[ZB[5]]], [bav])
                        k.dma("sp", vp[b, t * 128:(t + 1) * 128, :], av[:], [bav], [], bav, final=True)
                        k.op("act", lambda e: e.activation(avb[:], PB[ZB[5]][:], AF.Copy), [bPB[ZB[5]]], [bavb])
                        k.dma("sp", vbs[b, t * 128:(t + 1) * 128, :], avb[:], [bavb], [bvp], bavb)

                    def r2(t):
                        for hd in range(4):
                            c, hh = hd // 2, hd % 2
                            k.mm(PB[RB][:, hd * 128:(hd + 1) * 128], rqkT[:, 2 + c, :], rqm[:, hh, c, :], [brqkT, brqm], [bPB[RB]], last=(hd == 3))
                        k.op("dve", lambda e: e.tensor_tensor(scm[:], PB[RB][:], T["intra"][:].rearrange("p h n -> p (h n)"), ALU.mult), [bPB[RB], bT], [bscm])

                    def r3a(t):
                        for hd in range(4):
                            c, hh = hd // 2, hd % 2
                            cs = slice(hd * 128, (hd + 1) * 128)
                            k.mm(PB[RB][:, cs], scm[:, cs], rvb[:, cs], [bscm, brvb], [bPB[RB]], start=True, stop=False, last=False)
                            k.mm(PB[RB][:, cs], rqxT[:, hh, c, :], Rbs[t % 2][:, c, :], [brqxT, bRbs[t % 2]], [bPB[RB]], start=False, stop=True, last=(hd == 3))
                        for hd in range(4):
                            cs = slice(hd * 128, (hd + 1) * 128)
                            k.op("dve", lambda e, hd=hd: e.memset(ss_t[:, 4 + hd:5 + hd], 0.0), [], [bss])
                            k.op("act", lambda e, hd=hd, cs=cs: e.activation(scm[:, cs], PB[RB][:, cs], AF.Square, accum_out=ss_t[:, 4 + hd:5 + hd]), [bPB[RB]], [bscm, bss])
                        k.op("act", lambda e: e.activation(ss_t[:, 4:8], ss_t[:, 4:8], AF.Sqrt, bias=epsT[:], scale=1.0 / 128), [bss, bT], [bss])
                        k.op("dve", lambda e: e.reciprocal(ss_t[:, 4:8], ss_t[:, 4:8]), [bss], [bss])
                        for hd in range(4):
                            cs = slice(hd * 128, (hd + 1) * 128)
                            k.op("dve", lambda e, hd=hd, cs=cs: e.scalar_tensor_tensor(rety[:, cs], PB[RB][:, cs], ss_t[:, 4 + hd:5 + hd], sgt[:, cs],
                                                                                        op0=ALU.mult, op1=ALU.mult), [bPB[RB], bss, bsgt], [brety])

                    def r3b(t):
                        for c in range(2):
                            k.mm(PB[STB][:, c * 256:(c + 1) * 256], kz[:, c * 128:(c + 1) * 128], rvb[:, c * 256:(c + 1) * 256], [bkz, brvb], [bPB[STB]], last=(c == 1))
                        for c in range(2):
                            for hh in range(2):
                                rows = slice(hh * 64, (hh + 1) * 64)
                                k.op("dve", lambda e, c=c, hh=hh, rows=rows: e.scalar_tensor_tensor(
                                    Rf[rows, c, :], Rf[rows, c, :], T["gc"][rows, c:c + 1], PB[STB][rows, c * 256 + hh * 128:c * 256 + (hh + 1) * 128],
                                    op0=ALU.mult, op1=ALU.add), [bRf, bT, bPB[STB]], [bRf])
                        k.op("act", lambda e: e.activation(Rbs[(t + 1) % 2][:], Rf[:], AF.Copy), [bRf], [bRbs[(t + 1) % 2]])

                    def r4(t):
                        tp = pbf(TPB)
                        for c in range(4):
                            k.tr(tp[:, c * 128:(c + 1) * 128], rety[:, c * 128:(c + 1) * 128], T["ident_b"][:], [brety, bT], [bPB[TPB]], last=(c == 3))
                        k.op("act", lambda e: e.activation(mixT[:, 0:4, t * 128:(t + 1) * 128], tp[:, 0:512].rearrange("p (c n) -> p c n", n=128), AF.Copy),
                             [bPB[TPB]], [bmixT])

                    def a2(t):
                        tp = pbf(TPB)
                        for c in range(8):
                            k.tr(tp[:, c * 128:(c + 1) * 128], aqkb[:, c * 128:(c + 1) * 128], T["ident_b"][:], [baqkb, bT], [bPB[TPB]], last=(c == 7))
                        k.op("act", lambda e: e.activation(aqkT[:, :, t * 128:(t + 1) * 128], tp[:].rearrange("p (c n) -> p c n", n=128), AF.Copy),
                             [bPB[TPB]], [baqkT])

                    xload(0)
                    xload(1)
                    s1(0)
                    for i in range(5):
                        zc(0, i)
                    for t in range(NT):
                        nxt = t + 1 < NT
                        if t + 2 < NT:
                            xload(t + 2)
                        if nxt:
                            s1(t + 1)
                        r1(t)
                        zc(t, 5)
                        a1(t)
                        if nxt:
                            zc(t + 1, 0)
                            zc(t + 1, 1)
                            zc(t + 1, 2)
                        r2(t)
                        if nxt:
                            zc(t + 1, 3)
                        r3a(t)
                        if nxt:
                            zc(t + 1, 4)
                        r3b(t)
                        r4(t)
                        a2(t)
                        chk(1)
                    chk(2)
                    k.dma("sp", rp[b].rearrange("(c hh) kk v -> (hh kk) c v", hh=2), Rf[:], [bRf], [], bRf, final=True)

                    its = []
                    for hd in range(8):
                        for (dil, ntile_sub) in ((16, 1), (4, 4), (1, 16)):
                            for a in range(16):
                                its.append((hd, dil, ntile_sub, a))
                    hbS = [bPB[i] for i in range(4)]
                    hbX = [bPB[4 + i] for i in range(4)]
                    LA = 3
                    LV = 2
                    assert LA + LV < len(Va) // 2 and LA + 1 < len(Et)
                    NVA = len(Va) // 2

                    def prm(i):
                        hd, dil, ntile_sub, a = its[i]
                        c, hh = hd // 2, hd % 2
                        sub, nbk = a // ntile_sub, a % ntile_sub
                        start = dil * 128 * nbk + sub
                        nq = 256 if nbk < ntile_sub - 1 else 128
                        kc = slice(start, start + 127 * dil + 1, dil)
                        qc = slice(start, start + (nq - 1) * dil + 1, dil)
                        rows = slice(hh * 64, (hh + 1) * 64)
                        vi = hh * NVA + (i % NVA)
                        return hd, dil, c, hh, nq, kc, qc, rows, vi

                    def stV(i):
                        hd, dil, c, hh, nq, kc, qc, rows, vi = prm(i)
                        k.dma("sp", Va[vi][:, hh * 64:(hh + 1) * 64], vbs[b, kc, hd * 64:(hd + 1) * 64], [bvp], [bVa[vi]], bVa[vi])

                    def stS(i):
                        hd, dil, c, hh, nq, kc, qc, rows, vi = prm(i)
                        ps_s = PB[i % 4][:, 0:nq]
                        et, bet = Et[i % len(Et)], bEt[i % len(Et)]
                        k.mm(ps_s, aqkT[rows, 4 + c, kc], aqkT[rows, c, qc], [baqkT], [hbS[i % 4]])
                        k.op("act", lambda e: e.activation(et[:, 0:nq], ps_s, AF.Exp, scale=0.125), [hbS[i % 4]], [bet])
                        k.op("pool", lambda e: e.tensor_tensor(et[:, 0:nq], et[:, 0:nq], T["amask"][:, 0:nq], ALU.mult), [bet, bT], [bet])

                    def stX(i):
                        hd, dil, c, hh, nq, kc, qc, rows, vi = prm(i)
                        ps_x = PB[4 + i % 4][:, 0:nq]
                        et, bet = Et[i % len(Et)], bEt[i % len(Et)]
                        k.mm(ps_x, Va[vi][:], et[:, 0:nq], [bVa[vi], bet], [hbX[i % 4]])
                        if dil == 16:
                            k.op("act", lambda e: e.activation(Xacc[:, qc], ps_x, AF.Copy), [hbX[i % 4]], [bXacc])
                        else:
                            k.op("dve", lambda e: e.tensor_tensor(Xacc[:, qc], Xacc[:, qc], ps_x, ALU.add), [bXacc, hbX[i % 4]], [bXacc])
                        if i % 48 == 47:
                            drows = slice((1 - hh) * 64, (2 - hh) * 64)
                            for pc in range(8):
                                cs = slice(pc * 256, (pc + 1) * 256)
                                k.op("dve", lambda e: e.reciprocal(rct[rows, :], Xacc[drows, cs]), [bXacc], [brct])
                                k.op("dve", lambda e: e.tensor_tensor(mixT[rows, 4 + c, cs], Xacc[rows, cs], rct[rows, :], ALU.mult), [bXacc, brct], [bmixT])

                    n_it = len(its)
                    for i in range(min(LV, n_it)):
                        stV(i)
                    for i in range(n_it + LA):
                        if i + LV < n_it:
                            stV(i + LV)
                        if i < n_it:
                            stS(i)
                        if i - LA >= 0:
                            stX(i - LA)
                    k.barrier()
                chk(3)

                with contextlib.ExitStack() as sc_:
                    gates = k.sb("gates", [128, 2, 1024], F32, sc_)
                    FG = k.sb("FG", [128, 1024], F32, sc_)
                    k.dma("sp", FG[:], fgd.unsqueeze(0).to_broadcast([128, 1024]), [], [bFG], bFG)
                    for gi, vec in enumerate((2, 5)):
                        for c in range(8):
                            pbi = gi * 2 + c // 4
                            k.mm(PB[pbi][:, (c % 4) * 128:(c % 4 + 1) * 128],
                                 modT[:, vec * 8 + c, 64 + b:65 + b].to_broadcast([128, 128]), T["ident_f"][:],
                                 [bmodT, bT], [bPB[pbi]], last=(c % 4 == 3))
                        for hf in range(2):
                            k.op("act", lambda e, gi=gi, hf=hf: e.activation(gates[:, gi, hf * 512:(hf + 1) * 512], PB[gi * 2 + hf][:], AF.Copy),
                                 [bPB[gi * 2 + hf]], [bgates])
                    X1 = [k.sb(f"X1_{i}", [128, 1024], F32, sc_) for i in range(8)]
                    bX1 = [Buf(f"X1_{i}") for i in range(8)]
                    Xr = [k.sb(f"Xr{i}", [128, 1024], F32, sc_) for i in range(2)]
                    bXr = [Buf(f"Xr{i}") for i in range(2)]
                    xn2 = k.sb("xn2", [128, 1024], BF16, sc_); bxn2 = Buf("xn2")
                    jk2 = k.sb("jk2", [128, 1024], BF16, sc_); bjk2 = Buf("jk2")
                    h2Ts = [k.sb(f"h2T{i}", [128, 8, 512], BF16, sc_) for i in range(2)]
                    bh2Ts = [Buf(f"h2T{i}") for i in range(2)]
                    ffT = k.sb("ffT", [128, NJ, 512], BF16, sc_); bffT = Buf("ffT")
                    ring = [(k.sb(f"gu{i}", [128, 2, 8, 128], BF16, sc_), k.sb(f"dd{i}", [128, 1024], BF16, sc_)) for i in range(3)]
                    bring = [Buf(f"ring{i}") for i in range(3)]
                    rcnt = [0]

                    def nslot():
                        i = rcnt[0] % len(ring)
                        rcnt[0] += 1
                        return ring[i][0], ring[i][1], bring[i]

                    def pro(g, ti):
                        t = g * 4 + ti
                        tcs = slice(t * 128, (t + 1) * 128)
                        tpb = 4
                        x1, bx1 = X1[(g % 2) * 4 + ti], bX1[(g % 2) * 4 + ti]
                        h2T, bh2T = h2Ts[g % 2], bh2Ts[g % 2]
                        for nb in range(2):
                            for kc_ in range(8):
                                k.mm(PB[2 + nb][:], mixT[:, kc_, tcs], Wo[:, kc_, nb * 512:(nb + 1) * 512], [bmixT, bWo], [bPB[2 + nb]],
                                     start=(kc_ == 0), stop=(kc_ == 7), last=(kc_ == 7))
                        xr, bxr = Xr[ti % 2], bXr[ti % 2]
                        k.dma("sp", xr[:], xp[b, tcs, :], [], [bxr], bxr)
                        for nb in range(2):
                            cs = slice(nb * 512, (nb + 1) * 512)
                            k.op("dve", lambda e, nb=nb, cs=cs: e.tensor_tensor(x1[:, cs], PB[2 + nb][:], gates[:, 0, cs], ALU.mult),
                                 [bPB[2 + nb], bgates], [bx1])
                        k.op("dve", lambda e: e.tensor_tensor(x1[:], x1[:], xr[:], ALU.add), [bx1, bxr], [bx1])
                        rms_stats(x1[:], bx1, 128, 1, xn2, bxn2)
                        k.op("dve", lambda e: e.tensor_scalar_mul(xn2[:], x1[:], ss_t[:, 1:2]), [bx1, bss], [bxn2])
                        tp = pbf(tpb)
                        for c in range(8):
                            k.tr(tp[:, c * 128:(c + 1) * 128], xn2[:, c * 128:(c + 1) * 128], T["ident_b"][:], [bxn2, bT], [bPB[tpb]], last=(c == 7))
                        for c in range(8):
                            k.op("act", lambda e, c=c: e.activation(h2T[:, c, ti * 128:(ti + 1) * 128], tp[:, c * 128:(c + 1) * 128], AF.Identity,
                                                                     scale=G2T[:, c, 64 + b:65 + b], bias=modT[:, 24 + c, 64 + b:65 + b]),
                                 [bPB[tpb], bG2T, bmodT], [bh2T])

                    def gu_(g, j):
                        h2T, bh2T = h2Ts[g % 2], bh2Ts[g % 2]
                        gu, dd, bsl = nslot()
                        k.dma("sp", gu[:].rearrange("p a c n -> p (a c n)"), sc_gu[j], [bsc[j]], [bsl], bsl)
                        for a in range(2):
                            for dc in range(8):
                                k.mm(PB[a][:], gu[:, a, dc, :], h2T[:, dc, :], [bsl, bh2T], [bPB[a]],
                                     start=(dc == 0), stop=(dc == 7), last=(dc == 7))
                        k.op("act", lambda e: e.activation(ffT[:, j, :], PB[0][:], AF.Silu), [bPB[0]], [bffT])
                        k.op("dve", lambda e: e.tensor_tensor(ffT[:, j, :], ffT[:, j, :], PB[1][:], ALU.mult), [bffT, bPB[1]], [bffT])

                    def dn_(g, j):
                        gu, dd, bsl = nslot()
                        k.dma("sp", dd[:], sc_d[j], [bsc[j]], [bsl], bsl)
                        for ti in range(4):
                            for nb in range(2):
                                k.mm(PB[2 * ti + nb][:], ffT[:, j, ti * 128:(ti + 1) * 128], dd[:, nb * 512:(nb + 1) * 512],
                                     [bsl, bffT], [bPB[2 * ti + nb]], start=(j == 0), stop=(j == NJ - 1), last=(j == NJ - 1 or (ti == 3 and nb == 1)))

                    def epi(g, ti):
                        t = g * 4 + ti
                        x1, bx1 = X1[(g % 2) * 4 + ti], bX1[(g % 2) * 4 + ti]
                        for nb in range(2):
                            cs = slice(nb * 512, (nb + 1) * 512)
                            pbx = PB[2 * ti + nb]
                            k.op("dve", lambda e, cs=cs, pbx=pbx: e.tensor_tensor(pbx[:], pbx[:], gates[:, 1, cs], ALU.mult), [bPB[2 * ti + nb], bgates], [bPB[2 * ti + nb]])
                            k.op("dve", lambda e, cs=cs, pbx=pbx: e.tensor_tensor(x1[:, cs], x1[:, cs], pbx[:], ALU.add), [bx1, bPB[2 * ti + nb]], [bx1])
                        rms_stats(x1[:], bx1, 128, 3, jk2, bjk2)
                        k.op("dve", lambda e: e.scalar_tensor_tensor(x1[:], x1[:], ss_t[:, 3:4], FG[:], op0=ALU.mult, op1=ALU.mult), [bx1, bss, bFG], [bx1])
                        k.dma("sp", yp[b, t * 128:(t + 1) * 128, :], x1[:], [bx1], [], bx1, final=True)

                    for ti in range(4):
                        pro(0, ti)
                    pend = []
                    for g in range(4):
                        for j in range(NJ):
                            gu_(g, j)
                            if pend:
                                epi(g - 1, pend.pop(0))
                            if g + 1 < 4 and j in (4, 9, 14, 19):
                                pro(g + 1, (j - 4) // 5)
                        for j in range(NJ):
                            dn_(g, j)
                        epi(g, 0)
                        pend = [1, 2, 3]
                        if g == 3:
                            for ti in pend:
                                epi(g, ti)
                            pend = []
                    k.barrier()

            if do_sample:
                TS = {}
                with contextlib.ExitStack() as ss_:
                    for nm in ("cos_sa", "sin_sa", "cos_sr", "sin_sr", "zeta_s", "xit_s", "gc_s", "intra_s", "onehot_s", "smask", "nmask"):
                        TS[nm] = k.sb("TS_" + nm, tabs[nm].shape, _TAB_DT.get(nm, F32), ss_)
                        k.dma("sp", TS[nm][:], td[nm], [], [bT], bT)
                    Wi = k.sb("Wi_s", [128, 8, 3072], BF16, ss_)
                    for dc in range(8):
                        k.dma("sp", Wi[:, dc, :], sc_wi[dc], [bscwi], [bWi], bWi)
                    mods = k.sb("mods", [64, 2, 1024], F32, ss_); bmods = Buf("mods")
                    Xs = k.sb("Xs", [64, 1024], F32, ss_); bXs = Buf("Xs")
                    hs = k.sb("hs", [64, 1024], BF16, ss_); bhs = Buf("hs")
                    hTs = k.sb("hTs", [128, 8, 64], BF16, ss_); bhTs = Buf("hTs")
                    rqk_s = k.sb("rqk_s", [64, 512], BF16, ss_); brqk_s = Buf("rqk_s")
                    rts = [k.sb(f"rts{i}", [64, 8, 32], F32, ss_) for i in range(4)]
                    brts = Buf("rts")
                    aqkf_s = k.sb("aqkf_s", [64, 512], F32, ss_); baqkf_s = Buf("aqkf_s")
                    aqkb_s = k.sb("aqkb_s", [64, 1024], BF16, ss_); baqkb_s = Buf("aqkb_s")
                    avf_s = k.sb("avf_s", [64, 512], F32, ss_); bavf_s = Buf("avf_s")
                    rv_s = k.sb("rv_s", [64, 512], BF16, ss_); brv_s = Buf("rv_s")
                    sg_s = k.sb("sg_s", [64, 512], BF16, ss_); bsg_s = Buf("sg_s")
                    kz_s = k.sb("kz_s", [64, 256], BF16, ss_); bkz_s = Buf("kz_s")
                    kzm = [k.sb(f"kzm{i}", [64, 256], BF16, ss_) for i in range(2)]
                    bkzm = [Buf(f"kzm{i}") for i in range(2)]
                    rqkT_s = k.sb("rqkT_s", [128, 4, 64], BF16, ss_); brqkT_s = Buf("rqkT_s")
                    rqm_s = k.sb("rqm_s", [128, 2, 2, 64], BF16, ss_); brqm_s = Buf("rqm_s")
                    rqx_s = k.sb("rqx_s", [128, 2, 2, 64], BF16, ss_); brqx_s = Buf("rqx_s")
                    scm_s = k.sb("scm_s", [64, 256], BF16, ss_); bscm_s = Buf("scm_s")
                    oT_s = k.sb("oT_s", [128, 256], F32, ss_); boT_s = Buf("oT_s")
                    rety_s = k.sb("rety_s", [64, 512], BF16, ss_); brety_s = Buf("rety_s")
                    Rsf = [k.sb(f"Rsf{i}", [128, 2, 128], F32, ss_) for i in range(2)]
                    bRsf = [Buf(f"Rsf{i}") for i in range(2)]
                    Rsb = k.sb("Rsb", [128, 2, 16, 128], BF16, ss_); bRsb = Buf("Rsb")
                    aqm_s = k.sb("aqm_s", [128, 2, 4, 64], BF16, ss_); baqm_s = Buf("aqm_s")
                    akT_s = k.sb("akT_s", [128, 4, 64], BF16, ss_); bakT_s = Buf("akT_s")
                    Ktb = [k.sb(f"Ktf{i}", [128, 512], F32, ss_) for i in range(2)]
                    bKtb = [Buf(f"Ktf{i}") for i in range(2)]
                    Vtf = [k.sb(f"Vtf{i}", [128, 512], F32, ss_) for i in range(2)]
                    bVtf = [Buf(f"Vtf{i}") for i in range(2)]
                    KTs = [k.sb(f"KTs{i}", [128, 4, 128], BF16, ss_) for i in range(2)]
                    bKTs = [Buf(f"KTs{i}") for i in range(2)]
                    vas = [k.sb(f"vas{i}", [128, 8, 128], BF16, ss_) for i in range(9)]
                    bvas = [Buf(f"vas{i}") for i in range(9)]
                    van = k.sb("van", [64, 8, 128], BF16, ss_); bvan = Buf("van")
                    Es = k.sb("Es", [128, 9, 8, 4], BF16, ss_); bEs = Buf("Es")
                    En = k.sb("En", [64, 8, 4], BF16, ss_); bEn = Buf("En")
                    rcs = k.sb("rcs", [128, 64], F32, ss_); brcs = Buf("rcs")

                    def tm_mod(dst_slot, src_fn, dst, bdst):
                        for c in range(8):
                            pbi = c // 4
                            k.tr(PB[pbi][0:64, (c % 4) * 128:(c % 4 + 1) * 128], src_fn(c), T["ident_f"][:], [bmodT, bG1T, bG2T, bT], [bPB[pbi]], last=(c % 4 == 3))
                        for pbi in range(2):
                            k.op("act", lambda e, pbi=pbi: e.activation(dst[:, dst_slot, pbi * 512:(pbi + 1) * 512], PB[pbi][0:64, :], AF.Copy), [bPB[pbi]], [bdst])
                    tm_mod(0, lambda c: modT[:, c, 0:64], mods, bmods)
                    tm_mod(1, lambda c: G1T[:, c, 0:64], mods, bmods)
                    for i in range(9):
                        k.op("dve", lambda e, i=i: e.memset(vas[i][:], 1.0), [], [bvas[i]])
                    k.op("dve", lambda e: e.memset(van[:], 1.0), [], [bvan])
                    k.op("dve", lambda e: e.memset(rqm_s[:], 0.0), [], [brqm_s])
                    k.op("dve", lambda e: e.memset(aqm_s[:], 0.0), [], [baqm_s])

                    k.dma("sp", Xs[:], xs, [], [bXs], bXs)
                    rms_stats(Xs[:], bXs, 64, 0, hs, bhs)
                    k.op("dve", lambda e: e.scalar_tensor_tensor(Xs[:], Xs[:], ss_t[0:64, 0:1], mods[:, 1, :], op0=ALU.mult, op1=ALU.mult), [bXs, bss, bmods], [bXs])
                    k.op("dve", lambda e: e.tensor_tensor(hs[:], Xs[:], mods[:, 0, :], ALU.add), [bXs, bmods], [bhs])
                    tp = pbf(7)
                    for c in range(8):
                        k.tr(tp[:, c * 64:(c + 1) * 64], hs[:, c * 128:(c + 1) * 128], T["ident_b"][0:64, 0:64], [bhs, bT], [bPB[7]], last=(c == 7))
                    k.op("act", lambda e: e.activation(hTs[:].rearrange("p c n -> p (c n)"), tp[:, 0:512], AF.Copy), [bPB[7]], [bhTs])
                    for nb in range(6):
                        for dc in range(8):
                            k.mm(PB[nb][0:64, :], hTs[:, dc, :], Wi[:, dc, nb * 512:(nb + 1) * 512], [bhTs, bWi], [bPB[nb]],
                                 start=(dc == 0), stop=(dc == 7), last=(dc == 7))

                    def rotary(src_ap, dst_ap, cosn, sinn, bsrc, bdst):
                        zv = src_ap.rearrange("p (h two f) -> p h two f", two=2, f=32)
                        x1v, x2v = zv[:, :, 0, :], zv[:, :, 1, :]
                        ov = dst_ap.rearrange("p (h two f) -> p h two f", two=2, f=32)
                        cb = TS[cosn][:].unsqueeze(1).to_broadcast([64, 8, 32])
                        sb_ = TS[sinn][:].unsqueeze(1).to_broadcast([64, 8, 32])
                        r0, r1, r2, r3 = (rts[i][:] for i in range(4))
                        k.op("dve", lambda e: e.tensor_tensor(r0, x1v, cb, ALU.mult), [bsrc, bT], [brts])
                        k.op("dve", lambda e: e.tensor_tensor(r1, x2v, sb_, ALU.mult), [bsrc, bT], [brts])
                        k.op("dve", lambda e: e.tensor_tensor(r2, x1v, sb_, ALU.mult), [bsrc, bT], [brts])
                        k.op("dve", lambda e: e.tensor_tensor(r3, x2v, cb, ALU.mult), [bsrc, bT], [brts])
                        k.op("dve", lambda e: e.tensor_tensor(ov[:, :, 0, :], r0, r1, ALU.subtract), [brts], [bdst])
                        k.op("dve", lambda e: e.tensor_tensor(ov[:, :, 1, :], r2, r3, ALU.add), [brts], [bdst])
                    rotary(PB[0][0:64, :], rqk_s[:], "cos_sr", "sin_sr", bPB[0], brqk_s)
                    rotary(PB[3][0:64, :], aqkb_s[:, 0:512], "cos_sa", "sin_sa", bPB[3], baqkb_s)
                    rotary(PB[4][0:64, :], aqkf_s[:, 0:512], "cos_sa", "sin_sa", bPB[4], baqkf_s)
                    k.dma("sp", kso, aqkf_s[:, 0:512], [baqkf_s], [], baqkf_s, final=True)
                    k.op("act", lambda e: e.activation(aqkb_s[:, 512:1024], aqkf_s[:, 0:512], AF.Copy), [baqkf_s], [baqkb_s])
                    k.op("act", lambda e: e.activation(avf_s[:], PB[5][0:64, :], AF.Copy), [bPB[5]], [bavf_s])
                    k.dma("sp", vso, avf_s[:], [bavf_s], [], bavf_s, final=True)
                    avv = avf_s[:].rearrange("p (h f) -> p h f", f=64)
                    k.op("dve", lambda e: e.tensor_copy(van[:, 0:8:2, 0:64], avv[:, 0:8:2, :]), [bavf_s], [bvan])
                    k.op("dve", lambda e: e.tensor_copy(van[:, 1:8:2, 64:128], avv[:, 1:8:2, :]), [bavf_s], [bvan])
                    k.op("act", lambda e: e.activation(rv_s[:], PB[1][0:64, :], AF.Copy), [bPB[1]], [brv_s])
                    k.op("act", lambda e: e.activation(sg_s[:], PB[2][0:64, :], AF.Silu), [bPB[2]], [bsg_s])
                    k.op("dve", lambda e: e.tensor_tensor(kz_s[:].rearrange("p (h f) -> p h f", f=64),
                                                          rqk_s[:, 256:512].rearrange("p (h f) -> p h f", f=64),
                                                          TS["zeta_s"][:].unsqueeze(2).to_broadcast([64, 4, 64]), ALU.mult), [brqk_s, bT], [bkz_s])
                    tp = pbf(7)
                    for c in range(4):
                        k.tr(tp[:, c * 64:(c + 1) * 64], rqk_s[:, c * 128:(c + 1) * 128], T["ident_b"][0:64, 0:64], [brqk_s, bT], [bPB[7]], last=(c == 3))
                    k.op("act", lambda e: e.activation(rqkT_s[:].rearrange("p c n -> p (c n)"), tp[:, 0:256], AF.Copy), [bPB[7]], [brqkT_s])
                    k.op("act", lambda e: e.activation(rqm_s[0:64, 0, :, :].rearrange("p c n -> p (c n)"), tp[0:64, 0:128], AF.Copy), [bPB[7]], [brqm_s])
                    k.op("act", lambda e: e.activation(rqm_s[64:128, 1, :, :].rearrange("p c n -> p (c n)"), tp[64:128, 0:128], AF.Copy), [bPB[7]], [brqm_s])
                    for vv in range(2):
                        k.op("dve", lambda e, vv=vv: e.tensor_tensor(rqx_s[:, vv, :, :], rqm_s[:, vv, :, :], TS["xit_s"][:], ALU.mult), [brqm_s, bT], [brqx_s])
                    tp = pbf(6)
                    for c in range(8):
                        k.tr(tp[:, c * 64:(c + 1) * 64], aqkb_s[:, c * 128:(c + 1) * 128], T["ident_b"][0:64, 0:64], [baqkb_s, bT], [bPB[6]], last=(c == 7))
                    k.op("act", lambda e: e.activation(aqm_s[0:64, 0, :, :].rearrange("p c n -> p (c n)"), tp[0:64, 0:256], AF.Copy), [bPB[6]], [baqm_s])
                    k.op("act", lambda e: e.activation(aqm_s[64:128, 1, :, :].rearrange("p c n -> p (c n)"), tp[64:128, 0:256], AF.Copy), [bPB[6]], [baqm_s])
                    k.op("act", lambda e: e.activation(akT_s[:].rearrange("p c n -> p (c n)"), tp[:, 256:512], AF.Copy), [bPB[6]], [bakT_s])

                    for hd in range(4):
                        c, hh = hd // 2, hd % 2
                        k.mm(PB[0][0:64, hd * 64:(hd + 1) * 64], rqkT_s[:, 2 + c, :], rqm_s[:, hh, c, :], [brqkT_s, brqm_s], [bPB[0]], last=(hd == 3))
                    k.op("dve", lambda e: e.tensor_tensor(scm_s[:], PB[0][0:64, 0:256], TS["intra_s"][:].rearrange("p h n -> p (h n)"), ALU.mult), [bPB[0], bT], [bscm_s])
                    srv = sr.rearrange("b (c hh) kk v -> (hh kk) c b v", hh=2)
                    for bb in range(16):
                        rf, brf = Rsf[bb % 2], bRsf[bb % 2]
                        k.dma("sp", rf[:], srv[:, :, bb, :], [], [brf], brf)
                        k.op("act", lambda e, bb=bb: e.activation(Rsb[:, :, bb, :], rf[:], AF.Copy), [brf], [bRsb])
                    for hd in range(4):
                        c, hh = hd // 2, hd % 2
                        k.mm(PB[1][:, hd * 64:(hd + 1) * 64], rv_s[:, hd * 128:(hd + 1) * 128], scm_s[:, hd * 64:(hd + 1) * 64], [brv_s, bscm_s], [bPB[1]],
                             start=True, stop=False, last=False)
                        for bb in range(16):
                            k.mm(PB[1][:, hd * 64 + bb * 4:hd * 64 + bb * 4 + 4], Rsb[:, c, bb, :], rqx_s[:, hh, c, bb * 4:(bb + 1) * 4], [bRsb, brqx_s], [bPB[1]],
                                 start=False, stop=(bb == 15), last=(hd == 3 and bb == 15))
                    k.op("act", lambda e: e.activation(oT_s[:], PB[1][:, 0:256], AF.Copy), [bPB[1]], [boT_s])
                    for hd in range(4):
                        k.tr(PB[2][0:64, hd * 128:(hd + 1) * 128], oT_s[:, hd * 64:(hd + 1) * 64], T["ident_f"][:], [boT_s, bT], [bPB[2]], last=(hd == 3))
                    for hd in range(4):
                        cs = slice(hd * 128, (hd + 1) * 128)
                        k.op("dve", lambda e, hd=hd: e.memset(ss_t[0:64, 4 + hd:5 + hd], 0.0), [], [bss])
                        k.op("act", lambda e, hd=hd, cs=cs: e.activation(rety_s[0:64, cs], PB[2][0:64, cs], AF.Square, accum_out=ss_t[0:64, 4 + hd:5 + hd]), [bPB[2]], [brety_s, bss])
                    k.op("act", lambda e: e.activation(ss_t[0:64, 4:8], ss_t[0:64, 4:8], AF.Sqrt, bias=epsT[0:64, :], scale=1.0 / 128), [bss, bT], [bss])
                    k.op("dve", lambda e: e.reciprocal(ss_t[0:64, 4:8], ss_t[0:64, 4:8]), [bss], [bss])
                    for hd in range(4):
                        cs = slice(hd * 128, (hd + 1) * 128)
                        k.op("dve", lambda e, hd=hd, cs=cs: e.scalar_tensor_tensor(rety_s[:, cs], PB[2][0:64, cs], ss_t[0:64, 4 + hd:5 + hd], sg_s[:, cs],
                                                                                    op0=ALU.mult, op1=ALU.mult), [bPB[2], bss, bsg_s], [brety_s])
                    tp = pbf(7)
                    for c in range(4):
                        k.tr(tp[:, c * 64:(c + 1) * 64], rety_s[:, c * 128:(c + 1) * 128], T["ident_b"][0:64, 0:64], [brety_s, bT], [bPB[7]], last=(c == 3))
                    k.op("act", lambda e: e.activation(mixT[:, 0:4, 0:64], tp[:, 0:256].rearrange("p (c n) -> p c n", n=64), AF.Copy), [bPB[7]], [bmixT])
                    rsov = rso.rearrange("b (c hh) kk v -> (hh kk) c b v", hh=2)
                    for bb in range(16):
                        rf, brf = Rsf[bb % 2], bRsf[bb % 2]
                        km, bkm = kzm[bb % 2], bkzm[bb % 2]
                        pbi = 3 + bb % 2
                        k.dma("sp", rf[:], srv[:, :, bb, :], [], [brf], brf)
                        k.op("dve", lambda e, bb=bb: e.tensor_scalar_mul(km[:], kz_s[:], TS["onehot_s"][:, bb:bb + 1]), [bkz_s, bT], [bkm])
                        for c in range(2):
                            k.mm(PB[pbi][:, c * 256:(c + 1) * 256], km[:, c * 128:(c + 1) * 128], rv_s[:, c * 256:(c + 1) * 256], [bkm, brv_s], [bPB[pbi]], last=(c == 1))
                        for c in range(2):
                            for hh in range(2):
                                rows = slice(hh * 64, (hh + 1) * 64)
                                k.op("dve", lambda e, c=c, hh=hh, rows=rows: e.scalar_tensor_tensor(
                                    rf[rows, c, :], rf[rows, c, :], TS["gc_s"][rows, c:c + 1], PB[pbi][rows, c * 256 + hh * 128:c * 256 + (hh + 1) * 128],
                                    op0=ALU.mult, op1=ALU.add), [brf, bT, bPB[pbi]], [brf])
                        k.dma("sp", rsov[:, :, bb, :], rf[:], [brf], [], brf, final=True)

                    for bb in range(16):
                        rowsl = [slice(1920, 2048)] + [slice(1536 + t_, 1536 + t_ + 4 * 127 + 1, 4) for t_ in range(4)] \
                            + [slice(t_, t_ + 16 * 127 + 1, 16) for t_ in range(4)]
                        for ti_, rs_ in enumerate(rowsl):
                            kt, bkt = Ktb[ti_ % 2], bKtb[ti_ % 2]
                            kT, bkT = KTs[ti_ % 2], bKTs[ti_ % 2]
                            k.dma("sp", kt[:], ck[bb, rs_, :], [], [bkt], bkt)
                            vt, bvt = Vtf[ti_ % 2], bVtf[ti_ % 2]
                            k.dma("sp", vt[:], cv[bb, rs_, :], [], [bvt], bvt)
                            vtv = vt[:].rearrange("p (h f) -> p h f", f=64)
                            k.op("pool", lambda e: e.tensor_copy(vas[ti_][:, 0:8:2, 0:64], vtv[:, 0:8:2, :]), [bvt], [bvas[ti_]])
                            k.op("pool", lambda e: e.tensor_copy(vas[ti_][:, 1:8:2, 64:128], vtv[:, 1:8:2, :]), [bvt], [bvas[ti_]])
                            tpi = 6 + ti_ % 2
                            tp = PB[tpi][:]
                            for c in range(4):
                                k.tr(tp[:, c * 128:(c + 1) * 128], kt[:, c * 128:(c + 1) * 128], T["ident_f"][:], [bkt, bT], [bPB[tpi]], last=(c == 3))
                            k.op("act", lambda e: e.activation(kT[:].rearrange("p c n -> p (c n)"), tp[:, 0:512], AF.Copy), [bPB[tpi]], [bkT])
                            for hd in range(8):
                                c, hh = hd // 2, hd % 2
                                k.mm(PB[0][:, ti_ * 32 + hd * 4:ti_ * 32 + hd * 4 + 4], kT[:, c, :], aqm_s[:, hh, c, bb * 4:(bb + 1) * 4], [bkT, baqm_s], [bPB[0]], last=(hd == 7))
                        for hd in range(8):
                            c, hh = hd // 2, hd % 2
                            k.mm(PB[1][0:64, hd * 4:hd * 4 + 4], akT_s[:, c, :], aqm_s[:, hh, c, bb * 4:(bb + 1) * 4], [bakT_s, baqm_s], [bPB[1]], last=(hd == 7))
                        k.op("act", lambda e: e.activation(Es[:].rearrange("p a h t -> p (a h t)"), PB[0][:, 0:288], AF.Exp, scale=0.125), [bPB[0]], [bEs])
                        k.op("dve", lambda e: e.tensor_tensor(Es[:], Es[:], TS["smask"][:].unsqueeze(2).to_broadcast([128, 9, 8, 4]), ALU.mult), [bEs, bT], [bEs])
                        k.op("act", lambda e: e.activation(En[:].rearrange("p h t -> p (h t)"), PB[1][0:64, 0:32], AF.Exp, scale=0.125), [bPB[1]], [bEn])
                        k.op("dve", lambda e, bb=bb: e.tensor_tensor(En[:], En[:], TS["nmask"][:, bb, :].unsqueeze(1).to_broadcast([64, 8, 4]), ALU.mult), [bEn, bT], [bEn])
                        for hd in range(8):
                            oc = slice(hd * 64 + bb * 4, hd * 64 + bb * 4 + 4)
                            for ti_ in range(9):
                                k.mm(PB[5][:, oc], vas[ti_][:, hd, :], Es[:, ti_, hd, :], [bvas[ti_], bEs], [bPB[5]], start=(ti_ == 0), stop=False, last=False)
                            k.mm(PB[5][:, oc], van[:, hd, :], En[:, hd, :], [bvan, bEn], [bPB[5]], start=False, stop=True, last=(hd == 7))
                    for hd in range(8):
                        c, hh = hd // 2, hd % 2
                        rows = slice(hh * 64, (hh + 1) * 64)
                        drows = slice((1 - hh) * 64, (2 - hh) * 64)
                        cs = slice(hd * 64, (hd + 1) * 64)
                        k.op("dve", lambda e: e.reciprocal(rcs[rows, :], PB[5][drows, cs]), [bPB[5]], [brcs])
                        k.op("dve", lambda e: e.tensor_tensor(mixT[rows, 4 + c, 0:64], PB[5][rows, cs], rcs[rows, :], ALU.mult), [bPB[5], brcs], [bmixT])
                    k.barrier()

                with contextlib.ExitStack() as sc_:
                    mods = k.sb("mods2", [64, 4, 1024], F32, sc_); bmods = Buf("mods2")
                    FG = k.sb("FGs", [128, 1024], F32, sc_)
                    k.dma("sp", FG[:], fgd.unsqueeze(0).to_broadcast([128, 1024]), [], [bFG], bFG)
                    tm_mod(0, lambda c: modT[:, 16 + c, 0:64], mods, bmods)
                    tm_mod(1, lambda c: modT[:, 24 + c, 0:64], mods, bmods)
                    tm_mod(2, lambda c: G2T[:, c, 0:64], mods, bmods)
                    tm_mod(3, lambda c: modT[:, 40 + c, 0:64], mods, bmods)
                    Xs = k.sb("Xs2", [128, 1024], F32, sc_); bXs = Buf("Xs2")
                    X1 = [k.sb("X1s", [128, 1024], F32, sc_)]; bX1 = [Buf("X1s")]
                    xn2 = k.sb("xn2s", [64, 1024], F32, sc_); bxn2 = Buf("xn2s")
                    h2s = k.sb("h2s", [64, 1024], BF16, sc_); bh2s = Buf("h2s")
                    h2T = k.sb("h2Ts", [128, 8, 64], BF16, sc_); bh2T = Buf("h2Ts")
                    ffT = k.sb("ffTs", [128, NJ, 64], BF16, sc_); bffT = Buf("ffTs")
                    ring = [(k.sb(f"gus{i}", [128, 2, 8, 128], BF16, sc_), k.sb(f"dds{i}", [128, 1024], BF16, sc_)) for i in range(2)]
                    bring = [Buf(f"rings{i}") for i in range(2)]
                    for nb in range(2):
                        for kc_ in range(8):
                            k.mm(PB[4 + nb][0:64, :], mixT[:, kc_, 0:64], Wo[:, kc_, nb * 512:(nb + 1) * 512], [bmixT, bWo], [bPB[4 + nb]],
                                 start=(kc_ == 0), stop=(kc_ == 7), last=(kc_ == 7))
                    k.dma("sp", Xs[0:64, :], xs, [], [bXs], bXs)
                    x1 = X1[0]
                    for nb in range(2):
                        cs = slice(nb * 512, (nb + 1) * 512)
                        k.op("dve", lambda e, nb=nb, cs=cs: e.tensor_tensor(x1[0:64, cs], PB[4 + nb][0:64, :], mods[:, 0, cs], ALU.mult),
                             [bPB[4 + nb], bmods], [bX1[0]])
                    k.op("dve", lambda e: e.tensor_tensor(x1[0:64, :], x1[0:64, :], Xs[0:64, :], ALU.add), [bX1[0], bXs], [bX1[0]])
                    rms_stats(x1[0:64, :], bX1[0], 64, 1, h2s, bh2s)
                    k.op("dve", lambda e: e.scalar_tensor_tensor(xn2[:], x1[0:64, :], ss_t[0:64, 1:2], mods[:, 2, :], op0=ALU.mult, op1=ALU.mult), [bX1[0], bss, bmods], [bxn2])
                    k.op("dve", lambda e: e.tensor_tensor(h2s[:], xn2[:], mods[:, 1, :], ALU.add), [bxn2, bmods], [bh2s])
                    tp = pbf(7)
                    for c in range(8):
                        k.tr(tp[:, c * 64:(c + 1) * 64], h2s[:, c * 128:(c + 1) * 128], T["ident_b"][0:64, 0:64], [bh2s, bT], [bPB[7]], last=(c == 7))
                    k.op("act", lambda e: e.activation(h2T[:].rearrange("p c n -> p (c n)"), tp[:, 0:512], AF.Copy), [bPB[7]], [bh2T])

                    def out_fn_s(ti, ytile, bytile, np_):
                        k.dma("sp", ys, ytile[0:64, :], [bytile], [], bytile, final=True)
                    ffn_group(sc_, 1, 64, X1, bX1, h2T, bh2T, ffT, bffT, ring, bring, mods[:, 3, :], bmods, out_fn_s, FG, Xs, bXs, h2s, bh2s)
                    k.barrier()

        except _Stop:
            pass
        k.finish()
    return nc, tabs


_CACHE = {}


def kernel(x_prompt, x_sample, cache_attn_k, cache_attn_v, state_ret, c_prompt, c_sample,
           w_ada, b_ada, norm1_g, w_in, w_out, norm2_g, w_gate, w_up, w_down, final_g):
    f = lambda a: np.ascontiguousarray(np.asarray(a, dtype=np.float32))
    x_prompt, x_sample = f(x_prompt), f(x_sample)
    ck, cv, srr = f(cache_attn_k)[0], f(cache_attn_v)[0], f(state_ret)[0]
    c_prompt, c_sample = f(c_prompt), f(c_sample)
    if "nc" not in _CACHE:
        _CACHE["nc"] = build()
    nc, tabs = _CACHE["nc"]
    shared = {"w_ada": f(w_ada)[0], "b_ada": f(b_ada)[0], "n1g": f(norm1_g)[0], "w_in": f(w_in)[0],
              "w_out": f(w_out)[0], "n2g": f(norm2_g)[0], "w_gate": f(w_gate)[0], "w_up": f(w_up)[0],
              "w_down": f(w_down)[0], "fg": f(final_g)}
    for kk, v in tabs.items():
        shared["t_" + kk] = v
    in_maps = []
    for c in range(NCORES):
        m = dict(shared)
        m["xp"] = x_prompt[4 * c:4 * c + 4]
        m["xs"] = x_sample[16 * c:16 * c + 16].reshape(64, D)
        m["ck"] = ck[16 * c:16 * c + 16].reshape(16, S, 512)
        m["cv"] = cv[16 * c:16 * c + 16].reshape(16, S, 512)
        m["sr"] = srr[16 * c:16 * c + 16]
        m["call"] = np.concatenate([np.repeat(c_sample[16 * c:16 * c + 16], 4, axis=0), c_prompt[4 * c:4 * c + 4]], axis=0)
        in_maps.append(m)
    res = run_bass_kernel_spmd(nc, in_maps, core_ids=list(range(NCORES)))
    R = res.results
    y_prompt = np.concatenate([r["yp"] for r in R], 0)
    y_sample = np.concatenate([r["ys"].reshape(16, 4, D) for r in R], 0)
    nkp = np.concatenate([r["kp"].reshape(4, S, 8, 64) for r in R], 0)[None]
    nvp = np.concatenate([r["vp"].reshape(4, S, 8, 64) for r in R], 0)[None]
    nrp = np.concatenate([r["rp"] for r in R], 0)[None]
    nks = np.concatenate([r["kso"].reshape(16, 4, 8, 64) for r in R], 0)[None]
    nvs = np.concatenate([r["vso"].reshape(16, 4, 8, 64) for r in R], 0)[None]
    nrs = np.concatenate([r["rso"] for r in R], 0)[None]
    return (y_prompt, y_sample, nkp, nvp, nrp, nks, nvs, nrs)
```

```python
import contextlib
import math
import numpy as np
import ml_dtypes
import concourse.bass as bass
import concourse.mybir as mybir
from concourse.bass_utils import run_bass_kernel_spmd

F32 = mybir.dt.float32
BF16 = mybir.dt.bfloat16
AF = mybir.ActivationFunctionType
ALU = mybir.AluOpType

D = 1024
S = 2048
NT = S // 128
DFF = 2816
NJ = DFF // 128
EPS = 1e-6
PAST = 8192
NCORES = 8


class Buf:
    _cache = {}

    def __new__(cls, name):
        if name in cls._cache:
            return cls._cache[name]
        o = super().__new__(cls)
        o.name = name
        o.w = None
        o.r = {}
        o.dsem = None
        o.dcount = 0
        cls._cache[name] = o
        return o


class KB:
    def __init__(self, nc, stack):
        self.nc = nc
        self.stack = stack
        self.eng = {"pe": nc.tensor, "act": nc.scalar, "dve": nc.vector,
                    "pool": nc.gpsimd, "sp": nc.sync}
        self.sems = {}
        self.cnt = {}
        for k in ("pe", "act", "dve", "pool"):
            self.sems[k] = stack.enter_context(nc.semaphore("c_" + k))
            self.cnt[k] = 0
        self.seen = {k: {} for k in self.eng}
        self.pe_pending = []
        self.uid = 0
        self.dsems = {}
        self.finals = {}

    def sb(self, name, shape, dt, stack=None):
        self.uid += 1
        return (stack or self.stack).enter_context(self.nc.sbuf_tensor(f"{name}_u{self.uid}", list(shape), dt))

    def ps(self, name, shape, dt=F32):
        return self.stack.enter_context(self.nc.psum_tensor(name, list(shape), dt))

    def _deps(self, reads, writes):
        deps = {}

        def add(ev):
            if ev is None:
                return
            s, v = ev
            if deps.get(s, 0) < v:
                deps[s] = v
        for b in reads:
            add(b.w)
        for b in writes:
            add(b.w)
            for s, v in b.r.items():
                add((s, v))
        return deps

    def _wait(self, ek, deps, skip_own_pe=False):
        e = self.eng[ek]
        seen = self.seen[ek]
        for s, v in deps.items():
            if skip_own_pe and s is self.sems["pe"]:
                continue
            if seen.get(s, 0) >= v:
                continue
            e.wait_ge(s, v)
            seen[s] = v

    def _commit(self, ev, reads, writes):
        s, v = ev
        for b in reads:
            if b.r.get(s, 0) < v:
                b.r[s] = v
        for b in writes:
            b.w = ev
            b.r = {}

    def op(self, ek, fn, reads=(), writes=()):
        if KB.dead:
            return
        self._wait(ek, self._deps(reads, writes))
        inst = fn(self.eng[ek])
        self.cnt[ek] += 1
        inst.then_inc(self.sems[ek], 1)
        ev = (self.sems[ek], self.cnt[ek])
        self._commit(ev, reads, writes)
        return ev

    def _pe_done(self, inst, reads, writes, last):
        if last:
            self.cnt["pe"] += 1
            inst.then_inc(self.sems["pe"], 1)
            ev = (self.sems["pe"], self.cnt["pe"])
            for (r, w) in self.pe_pending:
                self._commit(ev, r, w)
            self.pe_pending = []
            self._commit(ev, reads, writes)
        else:
            self.pe_pending.append((list(reads), list(writes)))

    def mm(self, out, lhsT, rhs, reads, writes, start=True, stop=True, last=True):
        if KB.dead:
            return
        self._wait("pe", self._deps(reads, writes), skip_own_pe=True)
        inst = self.nc.tensor.matmul(out, lhsT, rhs, start=start, stop=stop)
        self._pe_done(inst, reads, writes, last)

    def tr(self, out, in_, ident, reads, writes, last=True):
        if KB.dead:
            return
        self._wait("pe", self._deps(reads, writes), skip_own_pe=True)
        inst = self.nc.tensor.transpose(out, in_, ident)
        self._pe_done(inst, reads, writes, last)

    def dma(self, q, out, in_, reads, writes, owner, final=False, **kw):
        if KB.dead:
            return
        self._wait(q, self._deps(reads, writes))
        if owner.dsem is None:
            self.uid += 1
            owner.dsem = self.stack.enter_context(self.nc.semaphore(f"d_{owner.name}_{self.uid}"))
            self.dsems[f"{owner.name}_{self.uid}"] = owner
        inst = self.eng[q].dma_start(out=out, in_=in_, **kw)
        owner.dcount += 16
        inst.then_inc(owner.dsem, 16)
        ev = (owner.dsem, owner.dcount)
        self._commit(ev, reads, writes)
        if final:
            self.finals[id(owner)] = owner
        return ev

    def barrier(self, force=False):
        if KB.dead and not force:
            return
        for ek in self.eng:
            e = self.eng[ek]
            seen = self.seen[ek]
            for k2 in ("pe", "act", "dve", "pool"):
                s, v = self.sems[k2], self.cnt[k2]
                if v > 0 and seen.get(s, 0) < v:
                    e.wait_ge(s, v)
                    seen[s] = v
            for o in self.dsems.values():
                if o.dcount > 0 and seen.get(o.dsem, 0) < o.dcount:
                    e.wait_ge(o.dsem, o.dcount)
                    seen[o.dsem] = o.dcount

    def finish(self):
        self.barrier(force=True)


def _tables():
    t = {}
    pos = np.arange(S, dtype=np.float64)
    afreq = (10000.0 ** (-np.arange(0, 64, 2, dtype=np.float32) / np.float32(64))).astype(np.float32).astype(np.float64)
    rfreq = (10000.0 ** (-np.linspace(0.0, 1.0, 32, dtype=np.float32))).astype(np.float32).astype(np.float64)

    def tm(fn, fr, p):
        a = (p.astype(np.float32)[:, None] * fr.astype(np.float32)[None, :]).astype(np.float32).astype(np.float64)
        v = fn(a).astype(np.float32)
        return v
    for nm, fr in (("a", afreq), ("r", rfreq)):
        c = tm(np.cos, fr, pos).reshape(NT, 128, 32).transpose(1, 0, 2)
        s = tm(np.sin, fr, pos).reshape(NT, 128, 32).transpose(1, 0, 2)
        t["cos_" + nm] = np.ascontiguousarray(c)
        t["sin_" + nm] = np.ascontiguousarray(s)
        ps = PAST + (np.arange(64) % 4).astype(np.float64)
        t["cos_s" + nm] = np.ascontiguousarray(tm(np.cos, fr, ps))
        t["sin_s" + nm] = np.ascontiguousarray(tm(np.sin, fr, ps))
    h = np.arange(4, dtype=np.float64)
    log_g = np.log1p(-(2.0 ** (-5.0 - h)))
    idx = np.arange(128, dtype=np.float64)
    zeta = np.exp((127.0 - idx)[:, None] * log_g[None, :])
    t["zeta"] = zeta.astype(np.float32)
    xi = np.exp((idx + 1.0)[None, :] * log_g[:, None])
    xit = np.zeros((128, 2, 128), np.float32)
    gc = np.zeros((128, 2), np.float32)
    for c in range(2):
        for hh in range(2):
            xit[hh * 64:(hh + 1) * 64, c, :] = xi[2 * c + hh][None, :]
            gc[hh * 64:(hh + 1) * 64, c] = np.exp(128.0 * log_g[2 * c + hh])
    t["xit"] = xit
    t["gc"] = gc
    diff = idx[None, :] - idx[:, None]
    intra = np.zeros((128, 4, 128), np.float32)
    for hd in range(4):
        intra[:, hd, :] = np.where(diff >= 0, np.exp(np.maximum(diff, 0.0) * log_g[hd]), 0.0)
    t["intra"] = intra
    tok = np.arange(64)
    tb, tt = tok // 4, tok % 4
    zs = np.exp((3.0 - tt)[:, None] * log_g[None, :])
    t["zeta_s"] = zs.astype(np.float32)
    xis = np.exp((tt + 1.0)[None, :] * log_g[:, None])
    xits = np.zeros((128, 2, 64), np.float32)
    gcs = np.zeros((128, 2), np.float32)
    for c in range(2):
        for hh in range(2):
            xits[hh * 64:(hh + 1) * 64, c, :] = xis[2 * c + hh][None, :]
            gcs[hh * 64:(hh + 1) * 64, c] = np.exp(4.0 * log_g[2 * c + hh])
    t["xit_s"] = xits
    t["gc_s"] = gcs
    ds = tt[None, :] - tt[:, None]
    same = tb[None, :] == tb[:, None]
    intras = np.zeros((64, 4, 64), np.float32)
    for hd in range(4):
        intras[:, hd, :] = np.where(same & (ds >= 0), np.exp(np.maximum(ds, 0) * log_g[hd]), 0.0)
    t["intra_s"] = intras
    onehot = np.zeros((64, 16), np.float32)
    onehot[tok, tb] = 1.0
    t["onehot_s"] = onehot
    m = np.zeros((128, 256), np.float32)
    jj = idx[:, None]
    ii = idx[None, :]
    m[:, :128] = (ii >= jj)
    m[:, 128:] = (ii <= jj)
    t["amask"] = m.astype(ml_dtypes.bfloat16)
    ms = np.zeros((128, 9, 4), np.float32)
    for tq in range(4):
        ms[:, 0, tq] = (idx >= tq)
        ms[:, 1 + tq, tq] = 1.0
        ms[:, 5 + tq, tq] = 1.0
    t["smask"] = ms.astype(ml_dtypes.bfloat16)
    mn = np.zeros((64, 16, 4), np.float32)
    for j in range(64):
        for tq in range(4):
            if tt[j] < tq:
                mn[j, tb[j], tq] = 1.0
            elif tt[j] == tq:
                mn[j, tb[j], tq] = 3.0
    t["nmask"] = mn.astype(ml_dtypes.bfloat16)
    t["ident_b"] = np.eye(128, dtype=np.float32).astype(ml_dtypes.bfloat16)
    t["ident_f"] = np.eye(128, dtype=np.float32)
    return t


_TAB_DT = {"amask": BF16, "smask": BF16, "nmask": BF16, "ident_b": BF16}


class _Stop(Exception):
    pass


def build(NB=4, NSB=16, do_sample=True, stop=99, nb_run=None):
    def chk(stage):
        if stop <= stage:
            KB.dead = True
    KB.dead = False

    Buf._cache = {}
    nc = bass.Bass("TRN2", target_bir_lowering=False)
    tabs = _tables()

    def din(name, shape, dt=F32):
        return nc.dram_tensor(name, list(shape), dt, kind="ExternalInput").ap()

    def dout(name, shape):
        return nc.dram_tensor(name, list(shape), F32, kind="ExternalOutput").ap()

    xp = din("xp", [NB, S, D])
    xs = din("xs", [64, D])
    ck = din("ck", [16, S, 512])
    cv = din("cv", [16, S, 512])
    sr = din("sr", [16, 4, 64, 128])
    call = din("call", [68, D])
    w_ada = din("w_ada", [D, 6 * D])
    b_ada = din("b_ada", [6 * D])
    n1g = din("n1g", [D])
    w_in = din("w_in", [D, 3072])
    w_out = din("w_out", [D, D])
    n2g = din("n2g", [D])
    w_gate = din("w_gate", [D, DFF])
    w_up = din("w_up", [D, DFF])
    w_down = din("w_down", [DFF, D])
    fgd = din("fg", [D])
    td = {k: din("t_" + k, v.shape, _TAB_DT.get(k, F32)) for k, v in tabs.items()}

    yp = dout("yp", [NB, S, D])
    ys = dout("ys", [64, D])
    kp = dout("kp", [NB, S, 512])
    vp = dout("vp", [NB, S, 512])
    rp = dout("rp", [NB, 4, 64, 128])
    kso = dout("kso", [64, 512])
    vso = dout("vso", [64, 512])
    rso = dout("rso", [16, 4, 64, 128])
    sc_gu = nc.dram_tensor("sc_gu", [NJ, 128, 2048], BF16, kind="Internal").ap()
    sc_d = nc.dram_tensor("sc_d", [NJ, 128, 1024], BF16, kind="Internal").ap()
    vbs = nc.dram_tensor("vbs", [NB, S, 512], BF16, kind="Internal").ap()
    sc_wi = nc.dram_tensor("sc_wi", [8, 128, 3072], BF16, kind="Internal").ap()

    with contextlib.ExitStack() as st:
        k = KB(nc, st)
        try:
            PB = [k.ps(f"pb{i}", [128, 512], F32) for i in range(8)]
            bPB = [Buf(f"pb{i}") for i in range(8)]

            def pbf(i):
                return PB[i][:].bitcast(BF16)

            bWi = Buf("Wi")
            bscwi = Buf("scwi")
            Wo = k.sb("Wo", [128, 8, 1024], BF16); bWo = Buf("Wo")
            modT = k.sb("modT", [128, 48, 68], F32); bmodT = Buf("modT")
            G1T = k.sb("G1T", [128, 8, 68], F32); bG1T = Buf("G1T")
            G2T = k.sb("G2T", [128, 8, 68], F32); bG2T = Buf("G2T")
            bgates = Buf("gates")
            bFG = Buf("FG")
            mixT = k.sb("mixT", [128, 8, S], BF16); bmixT = Buf("mixT")
            T = {}
            bT = Buf("tabs")
            for nm in ("cos_a", "sin_a", "cos_r", "sin_r", "zeta", "xit", "gc", "intra", "amask",
                       "ident_b", "ident_f"):
                T[nm] = k.sb("T_" + nm, tabs[nm].shape, _TAB_DT.get(nm, F32))
                k.dma("sp", T[nm][:], td[nm], [], [bT], bT)
            ss_t = k.sb("ss_t", [128, 8], F32); bss = Buf("ss")
            epsT = k.sb("epsT", [128, 1], F32)
            k.op("dve", lambda e: e.memset(epsT[:], EPS), [], [bT])
            Rf = k.sb("Rf", [128, 2, 128], F32); bRf = Buf("Rf")
            Rb = k.sb("Rb", [128, 2, 128], BF16); bRb = Buf("Rb")

            for dc in range(8):
                k.dma("pool", Wo[:, dc, :], w_out[dc * 128:(dc + 1) * 128, :], [], [bWo], bWo)

            def rms_stats(xt, bxt, npart, col, jk, bjk, width=1024, scale=1.0 / 1024):
                k.op("dve", lambda e: e.memset(ss_t[:npart, col:col + 1], 0.0), [], [bss])
                k.op("act", lambda e: e.activation(jk[:npart, 0:width], xt, AF.Square, accum_out=ss_t[:npart, col:col + 1]), [bxt], [bjk, bss])
                k.op("act", lambda e: e.activation(ss_t[:npart, col:col + 1], ss_t[:npart, col:col + 1], AF.Sqrt, bias=epsT[:npart, :], scale=scale), [bss, bT], [bss])
                k.op("dve", lambda e: e.reciprocal(ss_t[:npart, col:col + 1], ss_t[:npart, col:col + 1]), [bss], [bss])

            with contextlib.ExitStack() as s0:
                Wi0 = k.sb("Wi0", [128, 8, 3072], BF16, s0)
                for dc in range(8):
                    k.dma("pool", Wi0[:, dc, :], w_in[dc * 128:(dc + 1) * 128, :], [], [bWi], bWi)
                k.op("dve", lambda e: e.tensor_scalar_mul(Wi0[:, :, 256:512], Wi0[:, :, 256:512], 0.125), [bWi], [bWi])
                for dc in range(8):
                    k.dma("sp", sc_wi[dc], Wi0[:, dc, :], [bWi], [bscwi], bWi)
                cin = k.sb("cin", [68, D], F32, s0); bcin = Buf("cin")
                cT = k.sb("cT", [128, 8, 68], F32, s0); bcT = Buf("cT")
                badT = k.sb("badT", [128, 48], F32, s0); bbad = Buf("badT")
                n1T = k.sb("n1T", [128, 8], F32, s0)
                n2T = k.sb("n2T", [128, 8], F32, s0)
                wst = [k.sb(f"wst{i}", [128, 8, 512], F32, s0) for i in range(2)]
                bwst = [Buf(f"wst{i}") for i in range(2)]
                k.dma("sp", cin[:], call, [], [bcin], bcin)
                k.dma("sp", badT[:], b_ada.rearrange("(c p) -> p c", p=128), [], [bbad], bbad, allow_slow_non_contiguous=True)
                k.dma("sp", n1T[:], n1g.rearrange("(c p) -> p c", p=128), [], [bbad], bbad, allow_slow_non_contiguous=True)
                k.dma("sp", n2T[:], n2g.rearrange("(c p) -> p c", p=128), [], [bbad], bbad, allow_slow_non_contiguous=True)
                k.op("act", lambda e: e.activation(cin[:], cin[:], AF.Silu), [bcin], [bcin])
                for c in range(8):
                    pbi = c // 4
                    k.tr(PB[pbi][:, (c % 4) * 68:(c % 4 + 1) * 68], cin[:, c * 128:(c + 1) * 128], T["ident_f"][0:68, 0:68],
                         [bcin, bT], [bPB[pbi]], last=(c % 4 == 3))
                for pbi in range(2):
                    k.op("dve", lambda e, pbi=pbi: e.tensor_copy(cT[:, pbi * 4:(pbi + 1) * 4, :], PB[pbi][:, 0:4 * 68].rearrange("p (c n) -> p c n", n=68)),
                         [bPB[pbi]], [bcT])
                for blk in range(12):
                    ws, bws = wst[blk % 2], bwst[blk % 2]
                    k.dma("sp", ws[:], w_ada[:, blk * 512:(blk + 1) * 512].rearrange("(c p) n -> p c n", p=128), [], [bws], bws)
                    pb = 2 + (blk % 2)
                    for fc in range(4):
                        for dc in range(8):
                            k.mm(PB[pb][:, fc * 68:(fc + 1) * 68], ws[:, dc, fc * 128:(fc + 1) * 128], cT[:, dc, :],
                                 [bws, bcT], [bPB[pb]], start=(dc == 0), stop=(dc == 7), last=(dc == 7 and fc == 3))
                    for fc in range(4):
                        ch = blk * 4 + fc
                        k.op("act", lambda e, ch=ch, fc=fc, pb=pb: e.activation(modT[:, ch, :], PB[pb][:, fc * 68:(fc + 1) * 68], AF.Identity,
                                                                                 bias=badT[:, ch:ch + 1]), [bPB[pb], bbad], [bmodT])
                for c in range(8):
                    k.op("dve", lambda e, c=c: e.tensor_scalar(G1T[:, c, :], modT[:, 8 + c, :], 1.0, n1T[:, c:c + 1], op0=ALU.add, op1=ALU.mult),
                         [bmodT, bbad], [bG1T])
                    k.op("dve", lambda e, c=c: e.tensor_scalar(G2T[:, c, :], modT[:, 32 + c, :], 1.0, n2T[:, c:c + 1], op0=ALU.add, op1=ALU.mult),
                         [bmodT, bbad], [bG2T])
                stg = [k.sb(f"stg{i}", [128, 2, 8, 128], BF16, s0) for i in range(2)]
                std = [k.sb(f"std{i}", [128, 1024], BF16, s0) for i in range(2)]
                bstg = [Buf(f"stg{i}") for i in range(2)]
                bstd = [Buf(f"std{i}") for i in range(2)]
                bsc = [Buf(f"sc{j}") for j in range(NJ)]
                for j in range(NJ):
                    sg_, bsg_ = stg[j % 2], bstg[j % 2]
                    sd_, bsd_ = std[j % 2], bstd[j % 2]
                    k.dma("pool", sg_[:, 0, :, :], w_gate[:, j * 128:(j + 1) * 128].rearrange("(c p) n -> p c n", p=128), [], [bsg_], bsg_)
                    k.dma("pool", sg_[:, 1, :, :], w_up[:, j * 128:(j + 1) * 128].rearrange("(c p) n -> p c n", p=128), [], [bsg_], bsg_)
                    k.dma("pool", sd_[:], w_down[j * 128:(j + 1) * 128, :], [], [bsd_], bsd_)
                    k.dma("sp", sc_gu[j], sg_[:].rearrange("p a c n -> p (a c n)"), [bsg_], [bsc[j]], bsg_)
                    k.dma("sp", sc_d[j], sd_[:], [bsd_], [bsc[j]], bsd_)
                k.barrier()
            chk(0)

            def ffn_group(s1, nt, ntok, X1, bX1, h2T, bh2T, ffT, bffT, ring, bring, g2tile, bg2, out_fn, FG, yst, byst, jk, bjk):
                for j in range(NJ):
                    slot = j % len(ring)
                    gu, dd = ring[slot]
                    bsl = bring[slot]
                    k.dma("sp", gu[:].rearrange("p a c n -> p (a c n)"), sc_gu[j], [bsc[j]], [bsl], bsl)
                    for a in range(2):
                        for dc in range(8):
                            k.mm(PB[a][:, 0:ntok], gu[:, a, dc, :], h2T[:, dc, 0:ntok], [bsl, bh2T], [bPB[a]],
                                 start=(dc == 0), stop=(dc == 7), last=(dc == 7))
                    k.op("act", lambda e, j=j: e.activation(ffT[:, j, 0:ntok], PB[0][:, 0:ntok], AF.Silu), [bPB[0]], [bffT])
                    k.op("dve", lambda e, j=j: e.tensor_tensor(ffT[:, j, 0:ntok], ffT[:, j, 0:ntok], PB[1][:, 0:ntok], ALU.mult),
                         [bffT, bPB[1]], [bffT])
                for j in range(NJ):
                    slot = j % len(ring)
                    gu, dd = ring[slot]
                    bsl = bring[slot]
                    k.dma("sp", dd[:], sc_d[j], [bsc[j]], [bsl], bsl)
                    for ti in range(nt):
                        np_ = min(128, ntok - ti * 128)
                        for nb in range(2):
                            k.mm(PB[2 * ti + nb][:np_, :], ffT[:, j, ti * 128:ti * 128 + np_], dd[:, nb * 512:(nb + 1) * 512],
                                 [bsl, bffT], [bPB[2 * ti + nb]], start=(j == 0), stop=(j == NJ - 1), last=(j == NJ - 1 or (ti == nt - 1 and nb == 1)))
                for ti in range(nt):
                    np_ = min(128, ntok - ti * 128)
                    x1 = X1[ti]
                    for nb in range(2):
                        cs = slice(nb * 512, (nb + 1) * 512)
                        k.op("dve", lambda e, nb=nb, cs=cs: e.tensor_tensor(yst[:np_, cs], PB[2 * ti + nb][:np_, :], g2tile[:np_, cs], ALU.mult),
                             [bPB[2 * ti + nb], bg2], [byst])
                    k.op("dve", lambda e: e.tensor_tensor(yst[:np_, :], yst[:np_, :], x1[:np_, :], ALU.add), [byst, bX1[ti]], [byst])
                    rms_stats(yst[:np_, :], byst, np_, 3, jk, bjk)
                    k.op("dve", lambda e: e.scalar_tensor_tensor(yst[:np_, :], yst[:np_, :], ss_t[:np_, 3:4], FG[:np_, :], op0=ALU.mult, op1=ALU.mult),
                         [byst, bss, bFG], [byst])
                    out_fn(ti, yst, byst, np_)


            for b in range(NB if nb_run is None else nb_run):
                bvp = Buf(f"vp{b}")
                with contextlib.ExitStack() as sa:
                    Wi = k.sb("Wi", [128, 8, 3072], BF16, sa)
                    for dc in range(8):
                        k.dma("sp", Wi[:, dc, :], sc_wi[dc], [bscwi], [bWi], bWi)
                    Xt = [k.sb(f"Xt{i}", [128, 1024], F32, sa) for i in range(2)]
                    bXt = [Buf(f"Xt{i}") for i in range(2)]
                    xn = k.sb("xn", [128, 1024], BF16, sa); bxn = Buf("xn")
                    hTs_ = [k.sb(f"hT{i}", [128, 8, 128], BF16, sa) for i in range(2)]; bhTs_ = [Buf(f"hT{i}") for i in range(2)]
                    Rb1 = k.sb("Rb1", [128, 2, 128], BF16, sa); bRb1 = Buf("Rb1")
                    avb = k.sb("avb", [128, 512], BF16, sa); bavb = Buf("avb")
                    Rbs = [Rb, Rb1]; bRbs = [bRb, bRb1]
                    rqk = k.sb("rqk", [128, 512], BF16, sa); brqk = Buf("rqk")
                    rt = [k.sb(f"rt{i}", [128, 8, 32], F32, sa) for i in range(4)]
                    brt = Buf("rt")
                    kz = k.sb("kz", [128, 256], BF16, sa); bkz = Buf("kz")
                    rvb = k.sb("rvb", [128, 512], BF16, sa); brvb = Buf("rvb")
                    sgt = k.sb("sgt", [128, 512], BF16, sa); bsgt = Buf("sgt")
                    rqkT = k.sb("rqkT", [128, 4, 128], BF16, sa); brqkT = Buf("rqkT")
                    rqm = k.sb("rqm", [128, 2, 2, 128], BF16, sa); brqm = Buf("rqm")
                    rqxT = k.sb("rqxT", [128, 2, 2, 128], BF16, sa); brqxT = Buf("rqxT")
                    scm = k.sb("scm", [128, 512], BF16, sa); bscm = Buf("scm")
                    rety = k.sb("rety", [128, 512], BF16, sa); brety = Buf("rety")
                    aqkf = [k.sb(f"aqkf{i}", [128, 512], F32, sa) for i in range(1)]
                    baqkf = [Buf(f"aqkf{i}") for i in range(1)]
                    aqkb = k.sb("aqkb", [128, 1024], BF16, sa); baqkb = Buf("aqkb")
                    avf = [k.sb(f"avf{i}", [128, 512], F32, sa) for i in range(1)]
                    bavf = [Buf(f"avf{i}") for i in range(1)]
                    aqkT = k.sb("aqkT", [128, 8, S], BF16, sa); baqkT = Buf("aqkT")
                    Xacc = k.sb("Xacc", [128, S], F32, sa); bXacc = Buf("Xacc")
                    Va = [k.sb(f"Va{i}", [128, 128], BF16, sa) for i in range(12)]
                    bVa = [Buf(f"Va{i}") for i in range(12)]
                    Et = [k.sb(f"Et{i}", [128, 256], BF16, sa) for i in range(5)]
                    bEt = [Buf(f"Et{i}") for i in range(5)]
                    rct = k.sb("rct", [128, 256], F32, sa); brct = Buf("rct")
                    for i in range(len(Va)):
                        k.op("dve", lambda e, i=i: e.memset(Va[i][:], 1.0), [], [bVa[i]])
                    k.op("dve", lambda e: e.memset(rqm[:], 0.0), [], [brqm])
                    k.op("dve", lambda e: e.memset(Rf[:], 0.0), [], [bRf])
                    k.op("dve", lambda e: e.memset(Rb[:], 0.0), [], [bRb])
                    k.op("dve", lambda e: e.memset(Rb1[:], 0.0), [], [bRb1])

                    ZB = [0, 1, 2, 3, 4, 1]
                    STB = 5
                    TPB = 6
                    RB = 7

                    def xload(t):
                        X, bX = Xt[t % 2], bXt[t % 2]
                        k.dma("sp", X[:], xp[b, t * 128:(t + 1) * 128, :], [], [bX], bX)

                    def s1(t):
                        X, bX = Xt[t % 2], bXt[t % 2]
                        hT, bhT = hTs_[t % 2], bhTs_[t % 2]
                        rms_stats(X[:], bX, 128, 0, aqkb, baqkb)
                        k.op("dve", lambda e: e.tensor_scalar_mul(xn[:], X[:], ss_t[:, 0:1]), [bX, bss], [bxn])
                        tp = pbf(TPB)
                        for c in range(8):
                            k.tr(tp[:, c * 128:(c + 1) * 128], xn[:, c * 128:(c + 1) * 128], T["ident_b"][:], [bxn, bT], [bPB[TPB]], last=(c == 7))
                        k.op("dve", lambda e: e.tensor_tensor(hT[:], tp[:].rearrange("p (c n) -> p c n", n=128),
                                                              G1T[:, :, 64 + b:65 + b].to_broadcast([128, 8, 128]), ALU.mult), [bPB[TPB], bG1T], [bhT])
                        k.op("dve", lambda e: e.tensor_tensor(hT[:], hT[:], modT[:, 0:8, 64 + b:65 + b].to_broadcast([128, 8, 128]), ALU.add), [bhT, bmodT], [bhT])

                    def zc(t, i):
                        hT, bhT = hTs_[t % 2], bhTs_[t % 2]
                        for dc in range(8):
                            k.mm(PB[ZB[i]][:], hT[:, dc, :], Wi[:, dc, i * 512:(i + 1) * 512], [bhT, bWi], [bPB[ZB[i]]],
                                 start=(dc == 0), stop=(dc == 7), last=(dc == 7))

                    brts_ = [Buf(f"rt{i}") for i in range(4)]

                    def rot(src, bsrc, dst, bdst, cosn, sinn, t, bdst2=None):
                        zv = src.rearrange("p (h two f) -> p h two f", two=2, f=32)
                        x1v, x2v = zv[:, :, 0, :], zv[:, :, 1, :]
                        ov = dst.rearrange("p (h two f) -> p h two f", two=2, f=32)
                        cb = T[cosn][:, t, :].unsqueeze(1).to_broadcast([128, 8, 32])
                        sb_ = T[sinn][:, t, :].unsqueeze(1).to_broadcast([128, 8, 32])
                        r0, r1_, r2, r3 = (rt[i][:, 0:8, :] for i in range(4))
                        k.op("dve", lambda e: e.tensor_tensor(r0, x1v, cb, ALU.mult), [bsrc, bT], [brts_[0]])
                        k.op("dve", lambda e: e.tensor_tensor(r1_, x2v, sb_, ALU.mult), [bsrc, bT], [brts_[1]])
                        k.op("pool", lambda e: e.tensor_tensor(ov[:, :, 0, :], r0, r1_, ALU.subtract), [brts_[0], brts_[1]], [bdst])
                        k.op("dve", lambda e: e.tensor_tensor(r2, x1v, sb_, ALU.mult), [bsrc, bT], [brts_[2]])
                        k.op("dve", lambda e: e.tensor_tensor(r3, x2v, cb, ALU.mult), [bsrc, bT], [brts_[3]])
                        k.op("pool", lambda e: e.tensor_tensor(ov[:, :, 1, :], r2, r3, ALU.add), [brts_[2], brts_[3]], [bdst2 if bdst2 is not None else bdst])

                    def r1(t):
                        rot(PB[ZB[0]][:], bPB[ZB[0]], rqk[:], brqk, "cos_r", "sin_r", t)
                        k.op("pool", lambda e: e.tensor_tensor(kz[:].rearrange("p (h f) -> p h f", f=64),
                                                               rqk[:, 256:512].rearrange("p (h f) -> p h f", f=64),
                                                               T["zeta"][:].unsqueeze(2).to_broadcast([128, 4, 64]), ALU.mult), [brqk, bT], [bkz])
                        k.op("act", lambda e: e.activation(rvb[:], PB[ZB[1]][:], AF.Copy), [bPB[ZB[1]]], [brvb])
                        k.op("act", lambda e: e.activation(sgt[:], PB[ZB[2]][:], AF.Silu), [bPB[ZB[2]]], [bsgt])
                        tp = pbf(TPB)
                        for c in range(4):
                            k.tr(tp[:, c * 128:(c + 1) * 128], rqk[:, c * 128:(c + 1) * 128], T["ident_b"][:], [brqk, bT], [bPB[TPB]], last=(c == 3))
                        k.op("act", lambda e: e.activation(rqkT[:].rearrange("p c n -> p (c n)"), tp[:, 0:512], AF.Copy), [bPB[TPB]], [brqkT])
                        k.op("act", lambda e: e.activation(rqm[0:64, 0, :, :].rearrange("p c n -> p (c n)"), tp[0:64, 0:256], AF.Copy), [bPB[TPB]], [brqm])
                        k.op("act", lambda e: e.activation(rqm[64:128, 1, :, :].rearrange("p c n -> p (c n)"), tp[64:128, 0:256], AF.Copy), [bPB[TPB]], [brqm])
                        for vv in range(2):
                            k.op("pool", lambda e, vv=vv: e.tensor_tensor(rqxT[:, vv, :, :], rqm[:, vv, :, :], T["xit"][:], ALU.mult), [brqm, bT], [brqxT])

                    def a1a(t):
                        av, bav = avf[0], bavf[0]
                        k.op("act", lambda e: e.activation(av[:], PB[ZB[5]][:], AF.Copy), [bPB[ZB[5]]], [bav])
                        k.dma("pool", vp[b, t * 128:(t + 1) * 128, :], av[:], [bav], [], bav, final=True)
                        k.op("act", lambda e: e.activation(avb[:], PB[ZB[5]][:], AF.Copy), [bPB[ZB[5]]], [bavb])
                        k.dma("pool", vbs[b, t * 128:(t + 1) * 128, :], avb[:], [bavb], [bvp], bavb)
                        rot(PB[ZB[3]][:], bPB[ZB[3]], aqkb[:, 0:512], baqkb, "cos_a", "sin_a", t)

                    def a1b(t):
                        af, baf = aqkf[0], baqkf[0]
                        rot(PB[ZB[4]][:], bPB[ZB[4]], af[:, 0:512], baf, "cos_a", "sin_a", t)
                        k.dma("pool", kp[b, t * 128:(t + 1) * 128, :], af[:, 0:512], [baf], [], baf, final=True)
                        k.op("act", lambda e: e.activation(aqkb[:, 512:1024], af[:, 0:512], AF.Copy), [baf], [baqkb])

                    def r2(t):
                        for hd in range(4):
                            c, hh = hd // 2, hd % 2
                            k.mm(PB[RB][:, hd * 128:(hd + 1) * 128], rqkT[:, 2 + c, :], rqm[:, hh, c, :], [brqkT, brqm], [bPB[RB]], last=(hd == 3))
                        k.op("dve", lambda e: e.tensor_tensor(scm[:], PB[RB][:], T["intra"][:].rearrange("p h n -> p (h n)"), ALU.mult), [bPB[RB], bT], [bscm])

                    def r3a(t):
                        for hd in range(4):
                            c, hh = hd // 2, hd % 2
                            cs = slice(hd * 128, (hd + 1) * 128)
                            k.mm(PB[RB][:, cs], scm[:, cs], rvb[:, cs], [bscm, brvb], [bPB[RB]], start=True, stop=False, last=False)
                            k.mm(PB[RB][:, cs], rqxT[:, hh, c, :], Rbs[t % 2][:, c, :], [brqxT, bRbs[t % 2]], [bPB[RB]], start=False, stop=True, last=(hd == 3))
                        for hd in range(4):
                            cs = slice(hd * 128, (hd + 1) * 128)
                            k.op("dve", lambda e, hd=hd: e.memset(ss_t[:, 4 + hd:5 + hd], 0.0), [], [bss])
                            k.op("act", lambda e, hd=hd, cs=cs: e.activation(scm[:, cs], PB[RB][:, cs], AF.Square, accum_out=ss_t[:, 4 + hd:5 + hd]), [bPB[RB]], [bscm, bss])
                        k.op("act", lambda e: e.activation(ss_t[:, 4:8], ss_t[:, 4:8], AF.Sqrt, bias=epsT[:], scale=1.0 / 128), [bss, bT], [bss])
                        k.op("dve", lambda e: e.reciprocal(ss_t[:, 4:8], ss_t[:, 4:8]), [bss], [bss])
                        for hd in range(4):
                            cs = slice(hd * 128, (hd + 1) * 128)
                            k.op("dve", lambda e, hd=hd, cs=cs: e.scalar_tensor_tensor(rety[:, cs], PB[RB][:, cs], ss_t[:, 4 + hd:5 + hd], sgt[:, cs],
                                                                                        op0=ALU.mult, op1=ALU.mult), [bPB[RB], bss, bsgt], [brety])

                    def r3b(t):
                        for c in range(2):
                            k.mm(PB[STB][:, c * 256:(c + 1) * 256], kz[:, c * 128:(c + 1) * 128], rvb[:, c * 256:(c + 1) * 256], [bkz, brvb], [bPB[STB]], last=(c == 1))
                        for c in range(2):
                            for hh in range(2):
                                rows = slice(hh * 64, (hh + 1) * 64)
                                k.op("dve", lambda e, c=c, hh=hh, rows=rows: e.scalar_tensor_tensor(
                                    Rf[rows, c, :], Rf[rows, c, :], T["gc"][rows, c:c + 1], PB[STB][rows, c * 256 + hh * 128:c * 256 + (hh + 1) * 128],
                                    op0=ALU.mult, op1=ALU.add), [bRf, bT, bPB[STB]], [bRf])
                        k.op("act", lambda e: e.activation(Rbs[(t + 1) % 2][:], Rf[:], AF.Copy), [bRf], [bRbs[(t + 1) % 2]])

                    def r4(t):
                        tp = pbf(TPB)
                        for c in range(4):
                            k.tr(tp[:, c * 128:(c + 1) * 128], rety[:, c * 128:(c + 1) * 128], T["ident_b"][:], [brety, bT], [bPB[TPB]], last=(c == 3))
                        k.op("act", lambda e: e.activation(mixT[:, 0:4, t * 128:(t + 1) * 128], tp[:, 0:512].rearrange("p (c n) -> p c n", n=128), AF.Copy),
                             [bPB[TPB]], [bmixT])

                    def a2(t):
                        tp = pbf(TPB)
                        for c in range(8):
                            k.tr(tp[:, c * 128:(c + 1) * 128], aqkb[:, c * 128:(c + 1) * 128], T["ident_b"][:], [baqkb, bT], [bPB[TPB]], last=(c == 7))
                        k.op("act", lambda e: e.activation(aqkT[:, :, t * 128:(t + 1) * 128], tp[:].rearrange("p (c n) -> p c n", n=128), AF.Copy),
                             [bPB[TPB]], [baqkT])

                    xload(0)
                    xload(1)
                    s1(0)
                    for i in range(5):
                        zc(0, i)
                    for t in range(NT):
                        nxt = t + 1 < NT
                        if t + 2 < NT:
                            xload(t + 2)
                        if nxt:
                            s1(t + 1)
                        r1(t)
                        zc(t, 5)
                        a1a(t)
                        r2(t)
                        if nxt:
                            zc(t + 1, 0)
                        a1b(t)
                        if nxt:
                            zc(t + 1, 1)
                        r3a(t)
                        if nxt:
                            zc(t + 1, 2)
                            zc(t + 1, 3)
                        r3b(t)
                        if nxt:
                            zc(t + 1, 4)
                        r4(t)
                        a2(t)
                        chk(1)
                    chk(2)
                    k.dma("sp", rp[b].rearrange("(c hh) kk v -> (hh kk) c v", hh=2), Rf[:], [bRf], [], bRf, final=True)

                    its = []
                    for hd in range(8):
                        for (dil, ntile_sub) in ((16, 1), (4, 4), (1, 16)):
                            for a in range(16):
                                its.append((hd, dil, ntile_sub, a))
                    hbS = [bPB[i] for i in range(4)]
                    hbX = [bPB[4 + i] for i in range(4)]
                    LA = 3
                    LV = 2
                    assert LA + LV < len(Va) // 2 and LA + 1 < len(Et)
                    NVA = len(Va) // 2

                    def prm(i):
                        hd, dil, ntile_sub, a = its[i]
                        c, hh = hd // 2, hd % 2
                        sub, nbk = a // ntile_sub, a % ntile_sub
                        start = dil * 128 * nbk + sub
                        nq = 256 if nbk < ntile_sub - 1 else 128
                        kc = slice(start, start + 127 * dil + 1, dil)
                        qc = slice(start, start + (nq - 1) * dil + 1, dil)
                        rows = slice(hh * 64, (hh + 1) * 64)
                        vi = hh * NVA + (i % NVA)
                        return hd, dil, c, hh, nq, kc, qc, rows, vi

                    def stV(i):
                        hd, dil, c, hh, nq, kc, qc, rows, vi = prm(i)
                        k.dma("sp", Va[vi][:, hh * 64:(hh + 1) * 64], vbs[b, kc, hd * 64:(hd + 1) * 64], [bvp], [bVa[vi]], bVa[vi])

                    def stS(i):
                        hd, dil, c, hh, nq, kc, qc, rows, vi = prm(i)
                        ps_s = PB[i % 4][:, 0:nq]
                        et, bet = Et[i % len(Et)], bEt[i % len(Et)]
                        k.mm(ps_s, aqkT[rows, 4 + c, kc], aqkT[rows, c, qc], [baqkT], [hbS[i % 4]])
                        k.op("act", lambda e: e.activation(et[:, 0:nq], ps_s, AF.Exp, scale=0.125), [hbS[i % 4]], [bet])
                        k.op("pool", lambda e: e.tensor_tensor(et[:, 0:nq], et[:, 0:nq], T["amask"][:, 0:nq], ALU.mult), [bet, bT], [bet])

                    def stX(i):
                        hd, dil, c, hh, nq, kc, qc, rows, vi = prm(i)
                        ps_x = PB[4 + i % 4][:, 0:nq]
                        et, bet = Et[i % len(Et)], bEt[i % len(Et)]
                        k.mm(ps_x, Va[vi][:], et[:, 0:nq], [bVa[vi], bet], [hbX[i % 4]])
                        if dil == 16:
                            k.op("act", lambda e: e.activation(Xacc[:, qc], ps_x, AF.Copy), [hbX[i % 4]], [bXacc])
                        else:
                            k.op("dve", lambda e: e.tensor_tensor(Xacc[:, qc], Xacc[:, qc], ps_x, ALU.add), [bXacc, hbX[i % 4]], [bXacc])
                        if i % 48 == 47:
                            drows = slice((1 - hh) * 64, (2 - hh) * 64)
                            for pc in range(8):
                                cs = slice(pc * 256, (pc + 1) * 256)
                                k.op("dve", lambda e: e.reciprocal(rct[rows, :], Xacc[drows, cs]), [bXacc], [brct])
                                k.op("dve", lambda e: e.tensor_tensor(mixT[rows, 4 + c, cs], Xacc[rows, cs], rct[rows, :], ALU.mult), [bXacc, brct], [bmixT])

                    n_it = len(its)
                    for i in range(min(LV, n_it)):
                        stV(i)
                    for i in range(n_it + LA):
                        if i + LV < n_it:
                            stV(i + LV)
                        if i < n_it:
                            stS(i)
                        if i - LA >= 0:
                            stX(i - LA)
                    k.barrier()
                chk(3)

                with contextlib.ExitStack() as sc_:
                    gates = k.sb("gates", [128, 2, 1024], F32, sc_)
                    FG = k.sb("FG", [128, 1024], F32, sc_)
                    k.dma("sp", FG[:], fgd.unsqueeze(0).to_broadcast([128, 1024]), [], [bFG], bFG)
                    for gi, vec in enumerate((2, 5)):
                        for c in range(8):
                            pbi = gi * 2 + c // 4
                            k.mm(PB[pbi][:, (c % 4) * 128:(c % 4 + 1) * 128],
                                 modT[:, vec * 8 + c, 64 + b:65 + b].to_broadcast([128, 128]), T["ident_f"][:],
                                 [bmodT, bT], [bPB[pbi]], last=(c % 4 == 3))
                        for hf in range(2):
                            k.op("act", lambda e, gi=gi, hf=hf: e.activation(gates[:, gi, hf * 512:(hf + 1) * 512], PB[gi * 2 + hf][:], AF.Copy),
                                 [bPB[gi * 2 + hf]], [bgates])
                    X1 = [k.sb(f"X1_{i}", [128, 1024], F32, sc_) for i in range(8)]
                    bX1 = [Buf(f"X1_{i}") for i in range(8)]
                    Xr = [k.sb(f"Xr{i}", [128, 1024], F32, sc_) for i in range(2)]
                    bXr = [Buf(f"Xr{i}") for i in range(2)]
                    xn2 = k.sb("xn2", [128, 1024], BF16, sc_); bxn2 = Buf("xn2")
                    jk2 = k.sb("jk2", [128, 1024], BF16, sc_); bjk2 = Buf("jk2")
                    h2Ts = [k.sb(f"h2T{i}", [128, 8, 512], BF16, sc_) for i in range(2)]
                    bh2Ts = [Buf(f"h2T{i}") for i in range(2)]
                    ffT = k.sb("ffT", [128, NJ, 512], BF16, sc_); bffT = Buf("ffT")
                    ring = [(k.sb(f"gu{i}", [128, 2, 8, 128], BF16, sc_), k.sb(f"dd{i}", [128, 1024], BF16, sc_)) for i in range(3)]
                    bring = [Buf(f"ring{i}") for i in range(3)]
                    rcnt = [0]

                    def nslot():
                        i = rcnt[0] % len(ring)
                        rcnt[0] += 1
                        return ring[i][0], ring[i][1], bring[i]

                    def pro(g, ti):
                        t = g * 4 + ti
                        tcs = slice(t * 128, (t + 1) * 128)
                        tpb = 4
                        x1, bx1 = X1[(g % 2) * 4 + ti], bX1[(g % 2) * 4 + ti]
                        h2T, bh2T = h2Ts[g % 2], bh2Ts[g % 2]
                        for nb in range(2):
                            for kc_ in range(8):
                                k.mm(PB[2 + nb][:], mixT[:, kc_, tcs], Wo[:, kc_, nb * 512:(nb + 1) * 512], [bmixT, bWo], [bPB[2 + nb]],
                                     start=(kc_ == 0), stop=(kc_ == 7), last=(kc_ == 7))
                        xr, bxr = Xr[ti % 2], bXr[ti % 2]
                        k.dma("sp", xr[:], xp[b, tcs, :], [], [bxr], bxr)
                        for nb in range(2):
                            cs = slice(nb * 512, (nb + 1) * 512)
                            k.op("dve", lambda e, nb=nb, cs=cs: e.tensor_tensor(x1[:, cs], PB[2 + nb][:], gates[:, 0, cs], ALU.mult),
                                 [bPB[2 + nb], bgates], [bx1])
                        k.op("dve", lambda e: e.tensor_tensor(x1[:], x1[:], xr[:], ALU.add), [bx1, bxr], [bx1])
                        rms_stats(x1[:], bx1, 128, 1, xn2, bxn2)
                        k.op("dve", lambda e: e.tensor_scalar_mul(xn2[:], x1[:], ss_t[:, 1:2]), [bx1, bss], [bxn2])

                    def pro_b(g, ti):
                        tpb = 4
                        h2T, bh2T = h2Ts[g % 2], bh2Ts[g % 2]
                        tp = pbf(tpb)
                        for c in range(8):
                            k.tr(tp[:, c * 128:(c + 1) * 128], xn2[:, c * 128:(c + 1) * 128], T["ident_b"][:], [bxn2, bT], [bPB[tpb]], last=(c == 7))
                        for c in range(8):
                            k.op("act", lambda e, c=c: e.activation(h2T[:, c, ti * 128:(ti + 1) * 128], tp[:, c * 128:(c + 1) * 128], AF.Identity,
                                                                     scale=G2T[:, c, 64 + b:65 + b], bias=modT[:, 24 + c, 64 + b:65 + b]),
                                 [bPB[tpb], bG2T, bmodT], [bh2T])

                    def gu_(g, j):
                        h2T, bh2T = h2Ts[g % 2], bh2Ts[g % 2]
                        gu, dd, bsl = nslot()
                        k.dma("sp", gu[:].rearrange("p a c n -> p (a c n)"), sc_gu[j], [bsc[j]], [bsl], bsl)
                        for a in range(2):
                            for dc in range(8):
                                k.mm(PB[a][:], gu[:, a, dc, :], h2T[:, dc, :], [bsl, bh2T], [bPB[a]],
                                     start=(dc == 0), stop=(dc == 7), last=(dc == 7))
                        k.op("act", lambda e: e.activation(ffT[:, j, :], PB[0][:], AF.Silu), [bPB[0]], [bffT])
                        k.op("dve", lambda e: e.tensor_tensor(ffT[:, j, :], ffT[:, j, :], PB[1][:], ALU.mult), [bffT, bPB[1]], [bffT])

                    def dn_(g, j):
                        gu, dd, bsl = nslot()
                        k.dma("sp", dd[:], sc_d[j], [bsc[j]], [bsl], bsl)
                        for ti in range(4):
                            for nb in range(2):
                                k.mm(PB[2 * ti + nb][:], ffT[:, j, ti * 128:(ti + 1) * 128], dd[:, nb * 512:(nb + 1) * 512],
                                     [bsl, bffT], [bPB[2 * ti + nb]], start=(j == 0), stop=(j == NJ - 1), last=(j == NJ - 1 or (ti == 3 and nb == 1)))

                    def epi(g, ti):
                        t = g * 4 + ti
                        x1, bx1 = X1[(g % 2) * 4 + ti], bX1[(g % 2) * 4 + ti]
                        for nb in range(2):
                            cs = slice(nb * 512, (nb + 1) * 512)
                            pbx = PB[2 * ti + nb]
                            k.op("dve", lambda e, cs=cs, pbx=pbx: e.tensor_tensor(pbx[:], pbx[:], gates[:, 1, cs], ALU.mult), [bPB[2 * ti + nb], bgates], [bPB[2 * ti + nb]])
                            k.op("dve", lambda e, cs=cs, pbx=pbx: e.tensor_tensor(x1[:, cs], x1[:, cs], pbx[:], ALU.add), [bx1, bPB[2 * ti + nb]], [bx1])
                        rms_stats(x1[:], bx1, 128, 3, jk2, bjk2)
                        k.op("dve", lambda e: e.scalar_tensor_tensor(x1[:], x1[:], ss_t[:, 3:4], FG[:], op0=ALU.mult, op1=ALU.mult), [bx1, bss, bFG], [bx1])
                        k.dma("pool", yp[b, t * 128:(t + 1) * 128, :], x1[:], [bx1], [], bx1, final=True)

                    for ti in range(4):
                        pro(0, ti)
                        pro_b(0, ti)
                    pend = []
                    for g in range(4):
                        for j in range(NJ):
                            gu_(g, j)
                            if pend:
                                epi(g - 1, pend.pop(0))
                            if g + 1 < 4 and j in (3, 8, 13, 18):
                                pro(g + 1, (j - 3) // 5)
                            if g + 1 < 4 and j in (5, 10, 15, 20):
                                pro_b(g + 1, (j - 5) // 5)
                        for j in range(NJ):
                            dn_(g, j)
                        epi(g, 0)
                        pend = [1, 2, 3]
                        if g == 3:
                            for ti in pend:
                                epi(g, ti)
                            pend = []
                    k.barrier()

            if do_sample:
                TS = {}
                with contextlib.ExitStack() as ss_:
                    for nm in ("cos_sa", "sin_sa", "cos_sr", "sin_sr", "zeta_s", "xit_s", "gc_s", "intra_s", "onehot_s", "smask", "nmask"):
                        TS[nm] = k.sb("TS_" + nm, tabs[nm].shape, _TAB_DT.get(nm, F32), ss_)
                        k.dma("sp", TS[nm][:], td[nm], [], [bT], bT)
                    Wi = k.sb("Wi_s", [128, 8, 3072], BF16, ss_)
                    for dc in range(8):
                        k.dma("sp", Wi[:, dc, :], sc_wi[dc], [bscwi], [bWi], bWi)
                    mods = k.sb("mods", [64, 2, 1024], F32, ss_); bmods = Buf("mods")
                    Xs = k.sb("Xs", [64, 1024], F32, ss_); bXs = Buf("Xs")
                    hs = k.sb("hs", [64, 1024], BF16, ss_); bhs = Buf("hs")
                    hTs = k.sb("hTs", [128, 8, 64], BF16, ss_); bhTs = Buf("hTs")
                    rqk_s = k.sb("rqk_s", [64, 512], BF16, ss_); brqk_s = Buf("rqk_s")
                    rts = [k.sb(f"rts{i}", [64, 8, 32], F32, ss_) for i in range(4)]
                    brts = Buf("rts")
                    aqkf_s = k.sb("aqkf_s", [64, 512], F32, ss_); baqkf_s = Buf("aqkf_s")
                    aqkb_s = k.sb("aqkb_s", [64, 1024], BF16, ss_); baqkb_s = Buf("aqkb_s")
                    avf_s = k.sb("avf_s", [64, 512], F32, ss_); bavf_s = Buf("avf_s")
                    rv_s = k.sb("rv_s", [64, 512], BF16, ss_); brv_s = Buf("rv_s")
                    sg_s = k.sb("sg_s", [64, 512], BF16, ss_); bsg_s = Buf("sg_s")
                    kz_s = k.sb("kz_s", [64, 256], BF16, ss_); bkz_s = Buf("kz_s")
                    kzm = [k.sb(f"kzm{i}", [64, 256], BF16, ss_) for i in range(2)]
                    bkzm = [Buf(f"kzm{i}") for i in range(2)]
                    rqkT_s = k.sb("rqkT_s", [128, 4, 64], BF16, ss_); brqkT_s = Buf("rqkT_s")
                    rqm_s = k.sb("rqm_s", [128, 2, 2, 64], BF16, ss_); brqm_s = Buf("rqm_s")
                    rqx_s = k.sb("rqx_s", [128, 2, 2, 64], BF16, ss_); brqx_s = Buf("rqx_s")
                    scm_s = k.sb("scm_s", [64, 256], BF16, ss_); bscm_s = Buf("scm_s")
                    oT_s = k.sb("oT_s", [128, 256], F32, ss_); boT_s = Buf("oT_s")
                    rety_s = k.sb("rety_s", [64, 512], BF16, ss_); brety_s = Buf("rety_s")
                    Rsf = [k.sb(f"Rsf{i}", [128, 2, 128], F32, ss_) for i in range(2)]
                    bRsf = [Buf(f"Rsf{i}") for i in range(2)]
                    Rsb = k.sb("Rsb", [128, 2, 16, 128], BF16, ss_); bRsb = Buf("Rsb")
                    aqm_s = k.sb("aqm_s", [128, 2, 4, 64], BF16, ss_); baqm_s = Buf("aqm_s")
                    akT_s = k.sb("akT_s", [128, 4, 64], BF16, ss_); bakT_s = Buf("akT_s")
                    Ktb = [k.sb(f"Ktf{i}", [128, 512], F32, ss_) for i in range(2)]
                    bKtb = [Buf(f"Ktf{i}") for i in range(2)]
                    Vtf = [k.sb(f"Vtf{i}", [128, 512], F32, ss_) for i in range(2)]
                    bVtf = [Buf(f"Vtf{i}") for i in range(2)]
                    KTs = [k.sb(f"KTs{i}", [128, 4, 128], BF16, ss_) for i in range(2)]
                    bKTs = [Buf(f"KTs{i}") for i in range(2)]
                    vas = [k.sb(f"vas{i}", [128, 8, 128], BF16, ss_) for i in range(9)]
                    bvas = [Buf(f"vas{i}") for i in range(9)]
                    van = k.sb("van", [64, 8, 128], BF16, ss_); bvan = Buf("van")
                    Es = k.sb("Es", [128, 9, 8, 4], BF16, ss_); bEs = Buf("Es")
                    En = k.sb("En", [64, 8, 4], BF16, ss_); bEn = Buf("En")
                    rcs = k.sb("rcs", [128, 64], F32, ss_); brcs = Buf("rcs")

                    def tm_mod(dst_slot, src_fn, dst, bdst):
                        for c in range(8):
                            pbi = c // 4
                            k.tr(PB[pbi][0:64, (c % 4) * 128:(c % 4 + 1) * 128], src_fn(c), T["ident_f"][:], [bmodT, bG1T, bG2T, bT], [bPB[pbi]], last=(c % 4 == 3))
                        for pbi in range(2):
                            k.op("act", lambda e, pbi=pbi: e.activation(dst[:, dst_slot, pbi * 512:(pbi + 1) * 512], PB[pbi][0:64, :], AF.Copy), [bPB[pbi]], [bdst])
                    tm_mod(0, lambda c: modT[:, c, 0:64], mods, bmods)
                    tm_mod(1, lambda c: G1T[:, c, 0:64], mods, bmods)
                    for i in range(9):
                        k.op("dve", lambda e, i=i: e.memset(vas[i][:], 1.0), [], [bvas[i]])
                    k.op("dve", lambda e: e.memset(van[:], 1.0), [], [bvan])
                    k.op("dve", lambda e: e.memset(rqm_s[:], 0.0), [], [brqm_s])
                    k.op("dve", lambda e: e.memset(aqm_s[:], 0.0), [], [baqm_s])

                    k.dma("sp", Xs[:], xs, [], [bXs], bXs)
                    rms_stats(Xs[:], bXs, 64, 0, hs, bhs)
                    k.op("dve", lambda e: e.scalar_tensor_tensor(Xs[:], Xs[:], ss_t[0:64, 0:1], mods[:, 1, :], op0=ALU.mult, op1=ALU.mult), [bXs, bss, bmods], [bXs])
                    k.op("dve", lambda e: e.tensor_tensor(hs[:], Xs[:], mods[:, 0, :], ALU.add), [bXs, bmods], [bhs])
                    tp = pbf(7)
                    for c in range(8):
                        k.tr(tp[:, c * 64:(c + 1) * 64], hs[:, c * 128:(c + 1) * 128], T["ident_b"][0:64, 0:64], [bhs, bT], [bPB[7]], last=(c == 7))
                    k.op("act", lambda e: e.activation(hTs[:].rearrange("p c n -> p (c n)"), tp[:, 0:512], AF.Copy), [bPB[7]], [bhTs])
                    for nb in range(6):
                        for dc in range(8):
                            k.mm(PB[nb][0:64, :], hTs[:, dc, :], Wi[:, dc, nb * 512:(nb + 1) * 512], [bhTs, bWi], [bPB[nb]],
                                 start=(dc == 0), stop=(dc == 7), last=(dc == 7))

                    def rotary(src_ap, dst_ap, cosn, sinn, bsrc, bdst):
                        zv = src_ap.rearrange("p (h two f) -> p h two f", two=2, f=32)
                        x1v, x2v = zv[:, :, 0, :], zv[:, :, 1, :]
                        ov = dst_ap.rearrange("p (h two f) -> p h two f", two=2, f=32)
                        cb = TS[cosn][:].unsqueeze(1).to_broadcast([64, 8, 32])
                        sb_ = TS[sinn][:].unsqueeze(1).to_broadcast([64, 8, 32])
                        r0, r1, r2, r3 = (rts[i][:] for i in range(4))
                        k.op("dve", lambda e: e.tensor_tensor(r0, x1v, cb, ALU.mult), [bsrc, bT], [brts])
                        k.op("dve", lambda e: e.tensor_tensor(r1, x2v, sb_, ALU.mult), [bsrc, bT], [brts])
                        k.op("dve", lambda e: e.tensor_tensor(r2, x1v, sb_, ALU.mult), [bsrc, bT], [brts])
                        k.op("dve", lambda e: e.tensor_tensor(r3, x2v, cb, ALU.mult), [bsrc, bT], [brts])
                        k.op("dve", lambda e: e.tensor_tensor(ov[:, :, 0, :], r0, r1, ALU.subtract), [brts], [bdst])
                        k.op("dve", lambda e: e.tensor_tensor(ov[:, :, 1, :], r2, r3, ALU.add), [brts], [bdst])
                    rotary(PB[0][0:64, :], rqk_s[:], "cos_sr", "sin_sr", bPB[0], brqk_s)
                    rotary(PB[3][0:64, :], aqkb_s[:, 0:512], "cos_sa", "sin_sa", bPB[3], baqkb_s)
                    rotary(PB[4][0:64, :], aqkf_s[:, 0:512], "cos_sa", "sin_sa", bPB[4], baqkf_s)
                    k.dma("sp", kso, aqkf_s[:, 0:512], [baqkf_s], [], baqkf_s, final=True)
                    k.op("act", lambda e: e.activation(aqkb_s[:, 512:1024], aqkf_s[:, 0:512], AF.Copy), [baqkf_s], [baqkb_s])
                    k.op("act", lambda e: e.activation(avf_s[:], PB[5][0:64, :], AF.Copy), [bPB[5]], [bavf_s])
                    k.dma("sp", vso, avf_s[:], [bavf_s], [], bavf_s, final=True)
                    avv = avf_s[:].rearrange("p (h f) -> p h f", f=64)
                    k.op("dve", lambda e: e.tensor_copy(van[:, 0:8:2, 0:64], avv[:, 0:8:2, :]), [bavf_s], [bvan])
                    k.op("dve", lambda e: e.tensor_copy(van[:, 1:8:2, 64:128], avv[:, 1:8:2, :]), [bavf_s], [bvan])
                    k.op("act", lambda e: e.activation(rv_s[:], PB[1][0:64, :], AF.Copy), [bPB[1]], [brv_s])
                    k.op("act", lambda e: e.activation(sg_s[:], PB[2][0:64, :], AF.Silu), [bPB[2]], [bsg_s])
                    k.op("dve", lambda e: e.tensor_tensor(kz_s[:].rearrange("p (h f) -> p h f", f=64),
                                                          rqk_s[:, 256:512].rearrange("p (h f) -> p h f", f=64),
                                                          TS["zeta_s"][:].unsqueeze(2).to_broadcast([64, 4, 64]), ALU.mult), [brqk_s, bT], [bkz_s])
                    tp = pbf(7)
                    for c in range(4):
                        k.tr(tp[:, c * 64:(c + 1) * 64], rqk_s[:, c * 128:(c + 1) * 128], T["ident_b"][0:64, 0:64], [brqk_s, bT], [bPB[7]], last=(c == 3))
                    k.op("act", lambda e: e.activation(rqkT_s[:].rearrange("p c n -> p (c n)"), tp[:, 0:256], AF.Copy), [bPB[7]], [brqkT_s])
                    k.op("act", lambda e: e.activation(rqm_s[0:64, 0, :, :].rearrange("p c n -> p (c n)"), tp[0:64, 0:128], AF.Copy), [bPB[7]], [brqm_s])
                    k.op("act", lambda e: e.activation(rqm_s[64:128, 1, :, :].rearrange("p c n -> p (c n)"), tp[64:128, 0:128], AF.Copy), [bPB[7]], [brqm_s])
                    for vv in range(2):
                        k.op("dve", lambda e, vv=vv: e.tensor_tensor(rqx_s[:, vv, :, :], rqm_s[:, vv, :, :], TS["xit_s"][:], ALU.mult), [brqm_s, bT], [brqx_s])
                    tp = pbf(6)
                    for c in range(8):
                        k.tr(tp[:, c * 64:(c + 1) * 64], aqkb_s[:, c * 128:(c + 1) * 128], T["ident_b"][0:64, 0:64], [baqkb_s, bT], [bPB[6]], last=(c == 7))
                    k.op("act", lambda e: e.activation(aqm_s[0:64, 0, :, :].rearrange("p c n -> p (c n)"), tp[0:64, 0:256], AF.Copy), [bPB[6]], [baqm_s])
                    k.op("act", lambda e: e.activation(aqm_s[64:128, 1, :, :].rearrange("p c n -> p (c n)"), tp[64:128, 0:256], AF.Copy), [bPB[6]], [baqm_s])
                    k.op("act", lambda e: e.activation(akT_s[:].rearrange("p c n -> p (c n)"), tp[:, 256:512], AF.Copy), [bPB[6]], [bakT_s])

                    for hd in range(4):
                        c, hh = hd // 2, hd % 2
                        k.mm(PB[0][0:64, hd * 64:(hd + 1) * 64], rqkT_s[:, 2 + c, :], rqm_s[:, hh, c, :], [brqkT_s, brqm_s], [bPB[0]], last=(hd == 3))
                    k.op("dve", lambda e: e.tensor_tensor(scm_s[:], PB[0][0:64, 0:256], TS["intra_s"][:].rearrange("p h n -> p (h n)"), ALU.mult), [bPB[0], bT], [bscm_s])
                    srv = sr.rearrange("b (c hh) kk v -> (hh kk) c b v", hh=2)
                    for bb in range(16):
                        rf, brf = Rsf[bb % 2], bRsf[bb % 2]
                        k.dma("sp", rf[:], srv[:, :, bb, :], [], [brf], brf)
                        k.op("act", lambda e, bb=bb: e.activation(Rsb[:, :, bb, :], rf[:], AF.Copy), [brf], [bRsb])
                    for hd in range(4):
                        c, hh = hd // 2, hd % 2
                        k.mm(PB[1][:, hd * 64:(hd + 1) * 64], rv_s[:, hd * 128:(hd + 1) * 128], scm_s[:, hd * 64:(hd + 1) * 64], [brv_s, bscm_s], [bPB[1]],
                             start=True, stop=False, last=False)
                        for bb in range(16):
                            k.mm(PB[1][:, hd * 64 + bb * 4:hd * 64 + bb * 4 + 4], Rsb[:, c, bb, :], rqx_s[:, hh, c, bb * 4:(bb + 1) * 4], [bRsb, brqx_s], [bPB[1]],
                                 start=False, stop=(bb == 15), last=(hd == 3 and bb == 15))
                    k.op("act", lambda e: e.activation(oT_s[:], PB[1][:, 0:256], AF.Copy), [bPB[1]], [boT_s])
                    for hd in range(4):
                        k.tr(PB[2][0:64, hd * 128:(hd + 1) * 128], oT_s[:, hd * 64:(hd + 1) * 64], T["ident_f"][:], [boT_s, bT], [bPB[2]], last=(hd == 3))
                    for hd in range(4):
                        cs = slice(hd * 128, (hd + 1) * 128)
                        k.op("dve", lambda e, hd=hd: e.memset(ss_t[0:64, 4 + hd:5 + hd], 0.0), [], [bss])
                        k.op("act", lambda e, hd=hd, cs=cs: e.activation(rety_s[0:64, cs], PB[2][0:64, cs], AF.Square, accum_out=ss_t[0:64, 4 + hd:5 + hd]), [bPB[2]], [brety_s, bss])
                    k.op("act", lambda e: e.activation(ss_t[0:64, 4:8], ss_t[0:64, 4:8], AF.Sqrt, bias=epsT[0:64, :], scale=1.0 / 128), [bss, bT], [bss])
                    k.op("dve", lambda e: e.reciprocal(ss_t[0:64, 4:8], ss_t[0:64, 4:8]), [bss], [bss])
                    for hd in range(4):
                        cs = slice(hd * 128, (hd + 1) * 128)
                        k.op("dve", lambda e, hd=hd, cs=cs: e.scalar_tensor_tensor(rety_s[:, cs], PB[2][0:64, cs], ss_t[0:64, 4 + hd:5 + hd], sg_s[:, cs],
                                                                                    op0=ALU.mult, op1=ALU.mult), [bPB[2], bss, bsg_s], [brety_s])
                    tp = pbf(7)
                    for c in range(4):
                        k.tr(tp[:, c * 64:(c + 1) * 64], rety_s[:, c * 128:(c + 1) * 128], T["ident_b"][0:64, 0:64], [brety_s, bT], [bPB[7]], last=(c == 3))
                    k.op("act", lambda e: e.activation(mixT[:, 0:4, 0:64], tp[:, 0:256].rearrange("p (c n) -> p c n", n=64), AF.Copy), [bPB[7]], [bmixT])
                    rsov = rso.rearrange("b (c hh) kk v -> (hh kk) c b v", hh=2)
                    for bb in range(16):
                        rf, brf = Rsf[bb % 2], bRsf[bb % 2]
                        km, bkm = kzm[bb % 2], bkzm[bb % 2]
                        pbi = 3 + bb % 2
                        k.dma("sp", rf[:], srv[:, :, bb, :], [], [brf], brf)
                        k.op("dve", lambda e, bb=bb: e.tensor_scalar_mul(km[:], kz_s[:], TS["onehot_s"][:, bb:bb + 1]), [bkz_s, bT], [bkm])
                        for c in range(2):
                            k.mm(PB[pbi][:, c * 256:(c + 1) * 256], km[:, c * 128:(c + 1) * 128], rv_s[:, c * 256:(c + 1) * 256], [bkm, brv_s], [bPB[pbi]], last=(c == 1))
                        for c in range(2):
                            for hh in range(2):
                                rows = slice(hh * 64, (hh + 1) * 64)
                                k.op("dve", lambda e, c=c, hh=hh, rows=rows: e.scalar_tensor_tensor(
                                    rf[rows, c, :], rf[rows, c, :], TS["gc_s"][rows, c:c + 1], PB[pbi][rows, c * 256 + hh * 128:c * 256 + (hh + 1) * 128],
                                    op0=ALU.mult, op1=ALU.add), [brf, bT, bPB[pbi]], [brf])
                        k.dma("sp", rsov[:, :, bb, :], rf[:], [brf], [], brf, final=True)

                    for bb in range(16):
                        rowsl = [slice(1920, 2048)] + [slice(1536 + t_, 1536 + t_ + 4 * 127 + 1, 4) for t_ in range(4)] \
                            + [slice(t_, t_ + 16 * 127 + 1, 16) for t_ in range(4)]
                        for ti_, rs_ in enumerate(rowsl):
                            kt, bkt = Ktb[ti_ % 2], bKtb[ti_ % 2]
                            kT, bkT = KTs[ti_ % 2], bKTs[ti_ % 2]
                            k.dma("sp", kt[:], ck[bb, rs_, :], [], [bkt], bkt)
                            vt, bvt = Vtf[ti_ % 2], bVtf[ti_ % 2]
                            k.dma("sp", vt[:], cv[bb, rs_, :], [], [bvt], bvt)
                            vtv = vt[:].rearrange("p (h f) -> p h f", f=64)
                            k.op("pool", lambda e: e.tensor_copy(vas[ti_][:, 0:8:2, 0:64], vtv[:, 0:8:2, :]), [bvt], [bvas[ti_]])
                            k.op("pool", lambda e: e.tensor_copy(vas[ti_][:, 1:8:2, 64:128], vtv[:, 1:8:2, :]), [bvt], [bvas[ti_]])
                            tpi = 6 + ti_ % 2
                            tp = PB[tpi][:]
                            for c in range(4):
                                k.tr(tp[:, c * 128:(c + 1) * 128], kt[:, c * 128:(c + 1) * 128], T["ident_f"][:], [bkt, bT], [bPB[tpi]], last=(c == 3))
                            k.op("act", lambda e: e.activation(kT[:].rearrange("p c n -> p (c n)"), tp[:, 0:512], AF.Copy), [bPB[tpi]], [bkT])
                            for hd in range(8):
                                c, hh = hd // 2, hd % 2
                                k.mm(PB[0][:, ti_ * 32 + hd * 4:ti_ * 32 + hd * 4 + 4], kT[:, c, :], aqm_s[:, hh, c, bb * 4:(bb + 1) * 4], [bkT, baqm_s], [bPB[0]], last=(hd == 7))
                        for hd in range(8):
                            c, hh = hd // 2, hd % 2
                            k.mm(PB[1][0:64, hd * 4:hd * 4 + 4], akT_s[:, c, :], aqm_s[:, hh, c, bb * 4:(bb + 1) * 4], [bakT_s, baqm_s], [bPB[1]], last=(hd == 7))
                        k.op("act", lambda e: e.activation(Es[:].rearrange("p a h t -> p (a h t)"), PB[0][:, 0:288], AF.Exp, scale=0.125), [bPB[0]], [bEs])
                        k.op("dve", lambda e: e.tensor_tensor(Es[:], Es[:], TS["smask"][:].unsqueeze(2).to_broadcast([128, 9, 8, 4]), ALU.mult), [bEs, bT], [bEs])
                        k.op("act", lambda e: e.activation(En[:].rearrange("p h t -> p (h t)"), PB[1][0:64, 0:32], AF.Exp, scale=0.125), [bPB[1]], [bEn])
                        k.op("dve", lambda e, bb=bb: e.tensor_tensor(En[:], En[:], TS["nmask"][:, bb, :].unsqueeze(1).to_broadcast([64, 8, 4]), ALU.mult), [bEn, bT], [bEn])
                        for hd in range(8):
                            oc = slice(hd * 64 + bb * 4, hd * 64 + bb * 4 + 4)
                            for ti_ in range(9):
                                k.mm(PB[5][:, oc], vas[ti_][:, hd, :], Es[:, ti_, hd, :], [bvas[ti_], bEs], [bPB[5]], start=(ti_ == 0), stop=False, last=False)
                            k.mm(PB[5][:, oc], van[:, hd, :], En[:, hd, :], [bvan, bEn], [bPB[5]], start=False, stop=True, last=(hd == 7))
                    for hd in range(8):
                        c, hh = hd // 2, hd % 2
                        rows = slice(hh * 64, (hh + 1) * 64)
                        drows = slice((1 - hh) * 64, (2 - hh) * 64)
                        cs = slice(hd * 64, (hd + 1) * 64)
                        k.op("dve", lambda e: e.reciprocal(rcs[rows, :], PB[5][drows, cs]), [bPB[5]], [brcs])
                        k.op("dve", lambda e: e.tensor_tensor(mixT[rows, 4 + c, 0:64], PB[5][rows, cs], rcs[rows, :], ALU.mult), [bPB[5], brcs], [bmixT])
                    k.barrier()

                with contextlib.ExitStack() as sc_:
                    mods = k.sb("mods2", [64, 4, 1024], F32, sc_); bmods = Buf("mods2")
                    FG = k.sb("FGs", [128, 1024], F32, sc_)
                    k.dma("sp", FG[:], fgd.unsqueeze(0).to_broadcast([128, 1024]), [], [bFG], bFG)
                    tm_mod(0, lambda c: modT[:, 16 + c, 0:64], mods, bmods)
                    tm_mod(1, lambda c: modT[:, 24 + c, 0:64], mods, bmods)
                    tm_mod(2, lambda c: G2T[:, c, 0:64], mods, bmods)
                    tm_mod(3, lambda c: modT[:, 40 + c, 0:64], mods, bmods)
                    Xs = k.sb("Xs2", [128, 1024], F32, sc_); bXs = Buf("Xs2")
                    X1 = [k.sb("X1s", [128, 1024], F32, sc_)]; bX1 = [Buf("X1s")]
                    xn2 = k.sb("xn2s", [64, 1024], F32, sc_); bxn2 = Buf("xn2s")
                    h2s = k.sb("h2s", [64, 1024], BF16, sc_); bh2s = Buf("h2s")
                    h2T = k.sb("h2Ts", [128, 8, 64], BF16, sc_); bh2T = Buf("h2Ts")
                    ffT = k.sb("ffTs", [128, NJ, 64], BF16, sc_); bffT = Buf("ffTs")
                    ring = [(k.sb(f"gus{i}", [128, 2, 8, 128], BF16, sc_), k.sb(f"dds{i}", [128, 1024], BF16, sc_)) for i in range(2)]
                    bring = [Buf(f"rings{i}") for i in range(2)]
                    for nb in range(2):
                        for kc_ in range(8):
                            k.mm(PB[4 + nb][0:64, :], mixT[:, kc_, 0:64], Wo[:, kc_, nb * 512:(nb + 1) * 512], [bmixT, bWo], [bPB[4 + nb]],
                                 start=(kc_ == 0), stop=(kc_ == 7), last=(kc_ == 7))
                    k.dma("sp", Xs[0:64, :], xs, [], [bXs], bXs)
                    x1 = X1[0]
                    for nb in range(2):
                        cs = slice(nb * 512, (nb + 1) * 512)
                        k.op("dve", lambda e, nb=nb, cs=cs: e.tensor_tensor(x1[0:64, cs], PB[4 + nb][0:64, :], mods[:, 0, cs], ALU.mult),
                             [bPB[4 + nb], bmods], [bX1[0]])
                    k.op("dve", lambda e: e.tensor_tensor(x1[0:64, :], x1[0:64, :], Xs[0:64, :], ALU.add), [bX1[0], bXs], [bX1[0]])
                    rms_stats(x1[0:64, :], bX1[0], 64, 1, h2s, bh2s)
                    k.op("dve", lambda e: e.scalar_tensor_tensor(xn2[:], x1[0:64, :], ss_t[0:64, 1:2], mods[:, 2, :], op0=ALU.mult, op1=ALU.mult), [bX1[0], bss, bmods], [bxn2])
                    k.op("dve", lambda e: e.tensor_tensor(h2s[:], xn2[:], mods[:, 1, :], ALU.add), [bxn2, bmods], [bh2s])
                    tp = pbf(7)
                    for c in range(8):
                        k.tr(tp[:, c * 64:(c + 1) * 64], h2s[:, c * 128:(c + 1) * 128], T["ident_b"][0:64, 0:64], [bh2s, bT], [bPB[7]], last=(c == 7))
                    k.op("act", lambda e: e.activation(h2T[:].rearrange("p c n -> p (c n)"), tp[:, 0:512], AF.Copy), [bPB[7]], [bh2T])

                    def out_fn_s(ti, ytile, bytile, np_):
                        k.dma("sp", ys, ytile[0:64, :], [bytile], [], bytile, final=True)
                    ffn_group(sc_, 1, 64, X1, bX1, h2T, bh2T, ffT, bffT, ring, bring, mods[:, 3, :], bmods, out_fn_s, FG, Xs, bXs, h2s, bh2s)
                    k.barrier()

        except _Stop:
            pass
        k.finish()
    return nc, tabs


_CACHE = {}


def kernel(x_prompt, x_sample, cache_attn_k, cache_attn_v, state_ret, c_prompt, c_sample,
           w_ada, b_ada, norm1_g, w_in, w_out, norm2_g, w_gate, w_up, w_down, final_g):
    f = lambda a: np.ascontiguousarray(np.asarray(a, dtype=np.float32))
    x_prompt, x_sample = f(x_prompt), f(x_sample)
    ck, cv, srr = f(cache_attn_k)[0], f(cache_attn_v)[0], f(state_ret)[0]
    c_prompt, c_sample = f(c_prompt), f(c_sample)
    if "nc" not in _CACHE:
        _CACHE["nc"] = build()
    nc, tabs = _CACHE["nc"]
    shared = {"w_ada": f(w_ada)[0], "b_ada": f(b_ada)[0], "n1g": f(norm1_g)[0], "w_in": f(w_in)[0],
              "w_out": f(w_out)[0], "n2g": f(norm2_g)[0], "w_gate": f(w_gate)[0], "w_up": f(w_up)[0],
              "w_down": f(w_down)[0], "fg": f(final_g)}
    for kk, v in tabs.items():
        shared["t_" + kk] = v
    in_maps = []
    for c in range(NCORES):
        m = dict(shared)
        m["xp"] = x_prompt[4 * c:4 * c + 4]
        m["xs"] = x_sample[16 * c:16 * c + 16].reshape(64, D)
        m["ck"] = ck[16 * c:16 * c + 16].reshape(16, S, 512)
        m["cv"] = cv[16 * c:16 * c + 16].reshape(16, S, 512)
        m["sr"] = srr[16 * c:16 * c + 16]
        m["call"] = np.concatenate([np.repeat(c_sample[16 * c:16 * c + 16], 4, axis=0), c_prompt[4 * c:4 * c + 4]], axis=0)
        in_maps.append(m)
    res = run_bass_kernel_spmd(nc, in_maps, core_ids=list(range(NCORES)))
    R = res.results
    y_prompt = np.concatenate([r["yp"] for r in R], 0)
    y_sample = np.concatenate([r["ys"].reshape(16, 4, D) for r in R], 0)
    nkp = np.concatenate([r["kp"].reshape(4, S, 8, 64) for r in R], 0)[None]
    nvp = np.concatenate([r["vp"].reshape(4, S, 8, 64) for r in R], 0)[None]
    nrp = np.concatenate([r["rp"] for r in R], 0)[None]
    nks = np.concatenate([r["kso"].reshape(16, 4, 8, 64) for r in R], 0)[None]
    nvs = np.concatenate([r["vso"].reshape(16, 4, 8, 64) for r in R], 0)[None]
    nrs = np.concatenate([r["rso"] for r in R], 0)[None]
    return (y_prompt, y_sample, nkp, nvp, nrp, nks, nvs, nrs)
```

```python
import contextlib
import math
import numpy as np
import ml_dtypes
import concourse.bass as bass
import concourse.mybir as mybir
from concourse.bass_utils import run_bass_kernel_spmd

F32 = mybir.dt.float32
BF16 = mybir.dt.bfloat16
AF = mybir.ActivationFunctionType
ALU = mybir.AluOpType

D = 1024
S = 2048
NT = S // 128
DFF = 2816
NJ = DFF // 128
EPS = 1e-6
PAST = 8192
NCORES = 8


class Buf:
    _cache = {}

    def __new__(cls, name):
        if name in cls._cache:
            return cls._cache[name]
        o = super().__new__(cls)
        o.name = name
        o.w = None
        o.r = {}
        o.dsem = None
        o.dcount = 0
        cls._cache[name] = o
        return o


class KB:
    def __init__(self, nc, stack):
        self.nc = nc
        self.stack = stack
        self.eng = {"pe": nc.tensor, "act": nc.scalar, "dve": nc.vector,
                    "pool": nc.gpsimd, "sp": nc.sync}
        self.sems = {}
        self.cnt = {}
        for k in ("pe", "act", "dve", "pool"):
            self.sems[k] = stack.enter_context(nc.semaphore("c_" + k))
            self.cnt[k] = 0
        self.seen = {k: {} for k in self.eng}
        self.pe_pending = []
        self.uid = 0
        self.dsems = {}
        self.finals = {}

    def sb(self, name, shape, dt, stack=None):
        self.uid += 1
        return (stack or self.stack).enter_context(self.nc.sbuf_tensor(f"{name}_u{self.uid}", list(shape), dt))

    def ps(self, name, shape, dt=F32):
        return self.stack.enter_context(self.nc.psum_tensor(name, list(shape), dt))

    def _deps(self, reads, writes):
        deps = {}

        def add(ev):
            if ev is None:
                return
            s, v = ev
            if deps.get(s, 0) < v:
                deps[s] = v
        for b in reads:
            add(b.w)
        for b in writes:
            add(b.w)
            for s, v in b.r.items():
                add((s, v))
        return deps

    def _wait(self, ek, deps, skip_own_pe=False):
        e = self.eng[ek]
        seen = self.seen[ek]
        for s, v in deps.items():
            if skip_own_pe and s is self.sems["pe"]:
                continue
            if seen.get(s, 0) >= v:
                continue
            e.wait_ge(s, v)
            seen[s] = v

    def _commit(self, ev, reads, writes):
        s, v = ev
        for b in reads:
            if b.r.get(s, 0) < v:
                b.r[s] = v
        for b in writes:
            b.w = ev
            b.r = {}

    def op(self, ek, fn, reads=(), writes=()):
        if KB.dead:
            return
        self._wait(ek, self._deps(reads, writes))
        inst = fn(self.eng[ek])
        self.cnt[ek] += 1
        inst.then_inc(self.sems[ek], 1)
        ev = (self.sems[ek], self.cnt[ek])
        self._commit(ev, reads, writes)
        return ev

    def _pe_done(self, inst, reads, writes, last):
        if last:
            self.cnt["pe"] += 1
            inst.then_inc(self.sems["pe"], 1)
            ev = (self.sems["pe"], self.cnt["pe"])
            for (r, w) in self.pe_pending:
                self._commit(ev, r, w)
            self.pe_pending = []
            self._commit(ev, reads, writes)
        else:
            self.pe_pending.append((list(reads), list(writes)))

    def mm(self, out, lhsT, rhs, reads, writes, start=True, stop=True, last=True):
        if KB.dead:
            return
        self._wait("pe", self._deps(reads, writes), skip_own_pe=True)
        inst = self.nc.tensor.matmul(out, lhsT, rhs, start=start, stop=stop)
        self._pe_done(inst, reads, writes, last)

    def tr(self, out, in_, ident, reads, writes, last=True):
        if KB.dead:
            return
        self._wait("pe", self._deps(reads, writes), skip_own_pe=True)
        inst = self.nc.tensor.transpose(out, in_, ident)
        self._pe_done(inst, reads, writes, last)

    def dma(self, q, out, in_, reads, writes, owner, final=False, **kw):
        if KB.dead:
            return
        self._wait(q, self._deps(reads, writes))
        if owner.dsem is None:
            self.uid += 1
            owner.dsem = self.stack.enter_context(self.nc.semaphore(f"d_{owner.name}_{self.uid}"))
            self.dsems[f"{owner.name}_{self.uid}"] = owner
        inst = self.eng[q].dma_start(out=out, in_=in_, **kw)
        owner.dcount += 16
        inst.then_inc(owner.dsem, 16)
        ev = (owner.dsem, owner.dcount)
        self._commit(ev, reads, writes)
        if final:
            self.finals[id(owner)] = owner
        return ev

    def barrier(self, force=False):
        if KB.dead and not force:
            return
        for ek in self.eng:
            e = self.eng[ek]
            seen = self.seen[ek]
            for k2 in ("pe", "act", "dve", "pool"):
                s, v = self.sems[k2], self.cnt[k2]
                if v > 0 and seen.get(s, 0) < v:
                    e.wait_ge(s, v)
                    seen[s] = v
            for o in self.dsems.values():
                if o.dcount > 0 and seen.get(o.dsem, 0) < o.dcount:
                    e.wait_ge(o.dsem, o.dcount)
                    seen[o.dsem] = o.dcount

    def finish(self):
        self.barrier(force=True)


def _tables():
    t = {}
    pos = np.arange(S, dtype=np.float64)
    afreq = (10000.0 ** (-np.arange(0, 64, 2, dtype=np.float32) / np.float32(64))).astype(np.float32).astype(np.float64)
    rfreq = (10000.0 ** (-np.linspace(0.0, 1.0, 32, dtype=np.float32))).astype(np.float32).astype(np.float64)

    def tm(fn, fr, p):
        a = (p.astype(np.float32)[:, None] * fr.astype(np.float32)[None, :]).astype(np.float32).astype(np.float64)
        v = fn(a).astype(np.float32)
        return v
    for nm, fr in (("a", afreq), ("r", rfreq)):
        c = tm(np.cos, fr, pos).reshape(NT, 128, 32).transpose(1, 0, 2)
        s = tm(np.sin, fr, pos).reshape(NT, 128, 32).transpose(1, 0, 2)
        t["cos_" + nm] = np.ascontiguousarray(c)
        t["sin_" + nm] = np.ascontiguousarray(s)
        ps = PAST + (np.arange(64) % 4).astype(np.float64)
        t["cos_s" + nm] = np.ascontiguousarray(tm(np.cos, fr, ps))
        t["sin_s" + nm] = np.ascontiguousarray(tm(np.sin, fr, ps))
    h = np.arange(4, dtype=np.float64)
    log_g = np.log1p(-(2.0 ** (-5.0 - h)))
    idx = np.arange(128, dtype=np.float64)
    zeta = np.exp((127.0 - idx)[:, None] * log_g[None, :])
    t["zeta"] = zeta.astype(np.float32)
    xi = np.exp((idx + 1.0)[None, :] * log_g[:, None])
    xit = np.zeros((128, 2, 128), np.float32)
    gc = np.zeros((128, 2), np.float32)
    for c in range(2):
        for hh in range(2):
            xit[hh * 64:(hh + 1) * 64, c, :] = xi[2 * c + hh][None, :]
            gc[hh * 64:(hh + 1) * 64, c] = np.exp(128.0 * log_g[2 * c + hh])
    t["xit"] = xit
    t["gc"] = gc
    diff = idx[None, :] - idx[:, None]
    intra = np.zeros((128, 4, 128), np.float32)
    for hd in range(4):
        intra[:, hd, :] = np.where(diff >= 0, np.exp(np.maximum(diff, 0.0) * log_g[hd]), 0.0)
    t["intra"] = intra
    tok = np.arange(64)
    tb, tt = tok // 4, tok % 4
    zs = np.exp((3.0 - tt)[:, None] * log_g[None, :])
    t["zeta_s"] = zs.astype(np.float32)
    xis = np.exp((tt + 1.0)[None, :] * log_g[:, None])
    xits = np.zeros((128, 2, 64), np.float32)
    gcs = np.zeros((128, 2), np.float32)
    for c in range(2):
        for hh in range(2):
            xits[hh * 64:(hh + 1) * 64, c, :] = xis[2 * c + hh][None, :]
            gcs[hh * 64:(hh + 1) * 64, c] = np.exp(4.0 * log_g[2 * c + hh])
    t["xit_s"] = xits
    t["gc_s"] = gcs
    ds = tt[None, :] - tt[:, None]
    same = tb[None, :] == tb[:, None]
    intras = np.zeros((64, 4, 64), np.float32)
    for hd in range(4):
        intras[:, hd, :] = np.where(same & (ds >= 0), np.exp(np.maximum(ds, 0) * log_g[hd]), 0.0)
    t["intra_s"] = intras
    onehot = np.zeros((64, 16), np.float32)
    onehot[tok, tb] = 1.0
    t["onehot_s"] = onehot
    m = np.zeros((128, 256), np.float32)
    jj = idx[:, None]
    ii = idx[None, :]
    m[:, :128] = (ii >= jj)
    m[:, 128:] = (ii <= jj)
    t["amask"] = m.astype(ml_dtypes.bfloat16)
    ms = np.zeros((128, 9, 4), np.float32)
    for tq in range(4):
        ms[:, 0, tq] = (idx >= tq)
        ms[:, 1 + tq, tq] = 1.0
        ms[:, 5 + tq, tq] = 1.0
    t["smask"] = ms.astype(ml_dtypes.bfloat16)
    mn = np.zeros((64, 16, 4), np.float32)
    for j in range(64):
        for tq in range(4):
            if tt[j] < tq:
                mn[j, tb[j], tq] = 1.0
            elif tt[j] == tq:
                mn[j, tb[j], tq] = 3.0
    t["nmask"] = mn.astype(ml_dtypes.bfloat16)
    t["ident_b"] = np.eye(128, dtype=np.float32).astype(ml_dtypes.bfloat16)
    t["ident_f"] = np.eye(128, dtype=np.float32)
    return t


_TAB_DT = {"amask": BF16, "smask": BF16, "nmask": BF16, "ident_b": BF16}


class _Stop(Exception):
    pass


def build(NB=4, NSB=16, do_sample=True, stop=99, nb_run=None):
    def chk(stage):
        if stop <= stage:
            KB.dead = True
    KB.dead = False

    Buf._cache = {}
    nc = bass.Bass("TRN2", target_bir_lowering=False)
    tabs = _tables()

    def din(name, shape, dt=F32):
        return nc.dram_tensor(name, list(shape), dt, kind="ExternalInput").ap()

    def dout(name, shape):
        return nc.dram_tensor(name, list(shape), F32, kind="ExternalOutput").ap()

    xp = din("xp", [NB, S, D])
    xs = din("xs", [64, D])
    ck = din("ck", [16, S, 512])
    cv = din("cv", [16, S, 512])
    sr = din("sr", [16, 4, 64, 128])
    call = din("call", [68, D])
    w_ada = din("w_ada", [D, 6 * D])
    b_ada = din("b_ada", [6 * D])
    n1g = din("n1g", [D])
    w_in = din("w_in", [D, 3072])
    w_out = din("w_out", [D, D])
    n2g = din("n2g", [D])
    w_gate = din("w_gate", [D, DFF])
    w_up = din("w_up", [D, DFF])
    w_down = din("w_down", [DFF, D])
    fgd = din("fg", [D])
    td = {k: din("t_" + k, v.shape, _TAB_DT.get(k, F32)) for k, v in tabs.items()}

    yp = dout("yp", [NB, S, D])
    ys = dout("ys", [64, D])
    kp = dout("kp", [NB, S, 512])
    vp = dout("vp", [NB, S, 512])
    rp = dout("rp", [NB, 4, 64, 128])
    kso = dout("kso", [64, 512])
    vso = dout("vso", [64, 512])
    rso = dout("rso", [16, 4, 64, 128])
    sc_gu = nc.dram_tensor("sc_gu", [NJ, 128, 2048], BF16, kind="Internal").ap()
    sc_d = nc.dram_tensor("sc_d", [NJ, 128, 1024], BF16, kind="Internal").ap()
    vbs = nc.dram_tensor("vbs", [NB, S, 512], BF16, kind="Internal").ap()
    sc_wi = nc.dram_tensor("sc_wi", [8, 128, 3072], BF16, kind="Internal").ap()

    with contextlib.ExitStack() as st:
        k = KB(nc, st)
        try:
            PB = [k.ps(f"pb{i}", [128, 512], F32) for i in range(8)]
            bPB = [Buf(f"pb{i}") for i in range(8)]

            def pbf(i):
                return PB[i][:].bitcast(BF16)

            bWi = Buf("Wi")
            bscwi = Buf("scwi")
            Wo = k.sb("Wo", [128, 8, 1024], BF16); bWo = Buf("Wo")
            modT = k.sb("modT", [128, 48, 68], F32); bmodT = Buf("modT")
            G1T = k.sb("G1T", [128, 8, 68], F32); bG1T = Buf("G1T")
            G2T = k.sb("G2T", [128, 8, 68], F32); bG2T = Buf("G2T")
            bgates = Buf("gates")
            bFG = Buf("FG")
            mixT = k.sb("mixT", [128, 8, S], BF16); bmixT = Buf("mixT")
            T = {}
            bT = Buf("tabs")
            for nm in ("cos_a", "sin_a", "cos_r", "sin_r", "zeta", "xit", "gc", "intra", "amask",
                       "ident_b", "ident_f"):
                T[nm] = k.sb("T_" + nm, tabs[nm].shape, _TAB_DT.get(nm, F32))
                k.dma("sp", T[nm][:], td[nm], [], [bT], bT)
            ss_t = k.sb("ss_t", [128, 8], F32); bss = Buf("ss")
            epsT = k.sb("epsT", [128, 1], F32)
            k.op("dve", lambda e: e.memset(epsT[:], EPS), [], [bT])
            Rf = k.sb("Rf", [128, 2, 128], F32); bRf = Buf("Rf")
            Rb = k.sb("Rb", [128, 2, 128], BF16); bRb = Buf("Rb")

            for dc in range(8):
                k.dma("pool", Wo[:, dc, :], w_out[dc * 128:(dc + 1) * 128, :], [], [bWo], bWo)

            def rms_stats(xt, bxt, npart, col, jk, bjk, width=1024, scale=1.0 / 1024):
                k.op("dve", lambda e: e.memset(ss_t[:npart, col:col + 1], 0.0), [], [bss])
                k.op("act", lambda e: e.activation(jk[:npart, 0:width], xt, AF.Square, accum_out=ss_t[:npart, col:col + 1]), [bxt], [bjk, bss])
                k.op("act", lambda e: e.activation(ss_t[:npart, col:col + 1], ss_t[:npart, col:col + 1], AF.Sqrt, bias=epsT[:npart, :], scale=scale), [bss, bT], [bss])
                k.op("dve", lambda e: e.reciprocal(ss_t[:npart, col:col + 1], ss_t[:npart, col:col + 1]), [bss], [bss])

            with contextlib.ExitStack() as s0:
                Wi0 = k.sb("Wi0", [128, 8, 3072], BF16, s0)
                for dc in range(8):
                    k.dma("pool", Wi0[:, dc, :], w_in[dc * 128:(dc + 1) * 128, :], [], [bWi], bWi)
                k.op("dve", lambda e: e.tensor_scalar_mul(Wi0[:, :, 256:512], Wi0[:, :, 256:512], 0.125), [bWi], [bWi])
                for dc in range(8):
                    k.dma("pool", sc_wi[dc], Wi0[:, dc, :], [bWi], [bscwi], bWi)
                cin = k.sb("cin", [68, D], F32, s0); bcin = Buf("cin")
                cT = k.sb("cT", [128, 8, 68], F32, s0); bcT = Buf("cT")
                badT = k.sb("badT", [128, 48], F32, s0); bbad = Buf("badT")
                n1T = k.sb("n1T", [128, 8], F32, s0)
                n2T = k.sb("n2T", [128, 8], F32, s0)
                wst = [k.sb(f"wst{i}", [128, 8, 512], F32, s0) for i in range(2)]
                bwst = [Buf(f"wst{i}") for i in range(2)]
                k.dma("sp", cin[:], call, [], [bcin], bcin)
                k.dma("sp", badT[:], b_ada.rearrange("(c p) -> p c", p=128), [], [bbad], bbad, allow_slow_non_contiguous=True)
                k.dma("sp", n1T[:], n1g.rearrange("(c p) -> p c", p=128), [], [bbad], bbad, allow_slow_non_contiguous=True)
                k.dma("sp", n2T[:], n2g.rearrange("(c p) -> p c", p=128), [], [bbad], bbad, allow_slow_non_contiguous=True)
                k.op("act", lambda e: e.activation(cin[:], cin[:], AF.Silu), [bcin], [bcin])
                for c in range(8):
                    pbi = c // 4
                    k.tr(PB[pbi][:, (c % 4) * 68:(c % 4 + 1) * 68], cin[:, c * 128:(c + 1) * 128], T["ident_f"][0:68, 0:68],
                         [bcin, bT], [bPB[pbi]], last=(c % 4 == 3))
                for pbi in range(2):
                    k.op("dve", lambda e, pbi=pbi: e.tensor_copy(cT[:, pbi * 4:(pbi + 1) * 4, :], PB[pbi][:, 0:4 * 68].rearrange("p (c n) -> p c n", n=68)),
                         [bPB[pbi]], [bcT])
                for blk in range(12):
                    ws, bws = wst[blk % 2], bwst[blk % 2]
                    k.dma("sp", ws[:], w_ada[:, blk * 512:(blk + 1) * 512].rearrange("(c p) n -> p c n", p=128), [], [bws], bws)
                    pb = 2 + (blk % 2)
                    for fc in range(4):
                        for dc in range(8):
                            k.mm(PB[pb][:, fc * 68:(fc + 1) * 68], ws[:, dc, fc * 128:(fc + 1) * 128], cT[:, dc, :],
                                 [bws, bcT], [bPB[pb]], start=(dc == 0), stop=(dc == 7), last=(dc == 7 and fc == 3))
                    for fc in range(4):
                        ch = blk * 4 + fc
                        k.op("act", lambda e, ch=ch, fc=fc, pb=pb: e.activation(modT[:, ch, :], PB[pb][:, fc * 68:(fc + 1) * 68], AF.Identity,
                                                                                 bias=badT[:, ch:ch + 1]), [bPB[pb], bbad], [bmodT])
                for c in range(8):
                    k.op("dve", lambda e, c=c: e.tensor_scalar(G1T[:, c, :], modT[:, 8 + c, :], 1.0, n1T[:, c:c + 1], op0=ALU.add, op1=ALU.mult),
                         [bmodT, bbad], [bG1T])
                    k.op("dve", lambda e, c=c: e.tensor_scalar(G2T[:, c, :], modT[:, 32 + c, :], 1.0, n2T[:, c:c + 1], op0=ALU.add, op1=ALU.mult),
                         [bmodT, bbad], [bG2T])
                stg = [k.sb(f"stg{i}", [128, 2, 8, 128], BF16, s0) for i in range(2)]
                std = [k.sb(f"std{i}", [128, 1024], BF16, s0) for i in range(2)]
                bstg = [Buf(f"stg{i}") for i in range(2)]
                bstd = [Buf(f"std{i}") for i in range(2)]
                bsc = [Buf(f"sc{j}") for j in range(NJ)]
                for j in range(NJ):
                    sg_, bsg_ = stg[j % 2], bstg[j % 2]
                    sd_, bsd_ = std[j % 2], bstd[j % 2]
                    k.dma("pool", sg_[:, 0, :, :], w_gate[:, j * 128:(j + 1) * 128].rearrange("(c p) n -> p c n", p=128), [], [bsg_], bsg_)
                    k.dma("pool", sg_[:, 1, :, :], w_up[:, j * 128:(j + 1) * 128].rearrange("(c p) n -> p c n", p=128), [], [bsg_], bsg_)
                    k.dma("pool", sd_[:], w_down[j * 128:(j + 1) * 128, :], [], [bsd_], bsd_)
                    k.dma("pool", sc_gu[j], sg_[:].rearrange("p a c n -> p (a c n)"), [bsg_], [bsc[j]], bsg_)
                    k.dma("pool", sc_d[j], sd_[:], [bsd_], [bsc[j]], bsd_)
                k.barrier()
            chk(0)

            def ffn_group(s1, nt, ntok, X1, bX1, h2T, bh2T, ffT, bffT, ring, bring, g2tile, bg2, out_fn, FG, yst, byst, jk, bjk):
                for j in range(NJ):
                    slot = j % len(ring)
                    gu, dd = ring[slot]
                    bsl = bring[slot]
                    k.dma("sp", gu[:].rearrange("p a c n -> p (a c n)"), sc_gu[j], [bsc[j]], [bsl], bsl)
                    for a in range(2):
                        for dc in range(8):
                            k.mm(PB[a][:, 0:ntok], gu[:, a, dc, :], h2T[:, dc, 0:ntok], [bsl, bh2T], [bPB[a]],
                                 start=(dc == 0), stop=(dc == 7), last=(dc == 7))
                    k.op("act", lambda e, j=j: e.activation(ffT[:, j, 0:ntok], PB[0][:, 0:ntok], AF.Silu), [bPB[0]], [bffT])
                    k.op("dve", lambda e, j=j: e.tensor_tensor(ffT[:, j, 0:ntok], ffT[:, j, 0:ntok], PB[1][:, 0:ntok], ALU.mult),
                         [bffT, bPB[1]], [bffT])
                for j in range(NJ):
                    slot = j % len(ring)
                    gu, dd = ring[slot]
                    bsl = bring[slot]
                    k.dma("sp", dd[:], sc_d[j], [bsc[j]], [bsl], bsl)
                    for ti in range(nt):
                        np_ = min(128, ntok - ti * 128)
                        for nb in range(2):
                            k.mm(PB[2 * ti + nb][:np_, :], ffT[:, j, ti * 128:ti * 128 + np_], dd[:, nb * 512:(nb + 1) * 512],
                                 [bsl, bffT], [bPB[2 * ti + nb]], start=(j == 0), stop=(j == NJ - 1), last=(j == NJ - 1 or (ti == nt - 1 and nb == 1)))
                for ti in range(nt):
                    np_ = min(128, ntok - ti * 128)
                    x1 = X1[ti]
                    for nb in range(2):
                        cs = slice(nb * 512, (nb + 1) * 512)
                        k.op("dve", lambda e, nb=nb, cs=cs: e.tensor_tensor(yst[:np_, cs], PB[2 * ti + nb][:np_, :], g2tile[:np_, cs], ALU.mult),
                             [bPB[2 * ti + nb], bg2], [byst])
                    k.op("dve", lambda e: e.tensor_tensor(yst[:np_, :], yst[:np_, :], x1[:np_, :], ALU.add), [byst, bX1[ti]], [byst])
                    rms_stats(yst[:np_, :], byst, np_, 3, jk, bjk)
                    k.op("dve", lambda e: e.scalar_tensor_tensor(yst[:np_, :], yst[:np_, :], ss_t[:np_, 3:4], FG[:np_, :], op0=ALU.mult, op1=ALU.mult),
                         [byst, bss, bFG], [byst])
                    out_fn(ti, yst, byst, np_)


            for b in range(NB if nb_run is None else nb_run):
                bvp = Buf(f"vp{b}")
                with contextlib.ExitStack() as sa:
                    Wi = k.sb("Wi", [128, 8, 3072], BF16, sa)
                    for dc in range(8):
                        k.dma("sp", Wi[:, dc, :], sc_wi[dc], [bscwi], [bWi], bWi)
                    Xt = [k.sb(f"Xt{i}", [128, 1024], F32, sa) for i in range(2)]
                    bXt = [Buf(f"Xt{i}") for i in range(2)]
                    xn = k.sb("xn", [128, 1024], BF16, sa); bxn = Buf("xn")
                    hTs_ = [k.sb(f"hT{i}", [128, 8, 128], BF16, sa) for i in range(2)]; bhTs_ = [Buf(f"hT{i}") for i in range(2)]
                    Rb1 = k.sb("Rb1", [128, 2, 128], BF16, sa); bRb1 = Buf("Rb1")
                    avb = k.sb("avb", [128, 512], BF16, sa); bavb = Buf("avb")
                    Rbs = [Rb, Rb1]; bRbs = [bRb, bRb1]
                    rqk = k.sb("rqk", [128, 512], BF16, sa); brqk = Buf("rqk")
                    rt = [k.sb(f"rt{i}", [128, 8, 32], F32, sa) for i in range(4)]
                    brt = Buf("rt")
                    kz = k.sb("kz", [128, 256], BF16, sa); bkz = Buf("kz")
                    rvb = k.sb("rvb", [128, 512], BF16, sa); brvb = Buf("rvb")
                    sgt = k.sb("sgt", [128, 512], BF16, sa); bsgt = Buf("sgt")
                    rqkT = k.sb("rqkT", [128, 4, 128], BF16, sa); brqkT = Buf("rqkT")
                    rqm = k.sb("rqm", [128, 2, 2, 128], BF16, sa); brqm = Buf("rqm")
                    rqxT = k.sb("rqxT", [128, 2, 2, 128], BF16, sa); brqxT = Buf("rqxT")
                    scm = k.sb("scm", [128, 512], BF16, sa); bscm = Buf("scm")
                    rety = k.sb("rety", [128, 512], BF16, sa); brety = Buf("rety")
                    aqkf = [k.sb(f"aqkf{i}", [128, 512], F32, sa) for i in range(1)]
                    baqkf = [Buf(f"aqkf{i}") for i in range(1)]
                    aqkb = k.sb("aqkb", [128, 1024], BF16, sa); baqkb = Buf("aqkb")
                    avf = [k.sb(f"avf{i}", [128, 512], F32, sa) for i in range(1)]
                    bavf = [Buf(f"avf{i}") for i in range(1)]
                    aqkT = k.sb("aqkT", [128, 8, S], BF16, sa); baqkT = Buf("aqkT")
                    Xacc = k.sb("Xacc", [128, S], F32, sa); bXacc = Buf("Xacc")
                    Va = [k.sb(f"Va{i}", [128, 128], BF16, sa) for i in range(12)]
                    bVa = [Buf(f"Va{i}") for i in range(12)]
                    Et = [k.sb(f"Et{i}", [128, 256], BF16, sa) for i in range(5)]
                    bEt = [Buf(f"Et{i}") for i in range(5)]
                    rct = k.sb("rct", [128, 256], F32, sa); brct = Buf("rct")
                    for i in range(len(Va)):
                        k.op("dve", lambda e, i=i: e.memset(Va[i][:], 1.0), [], [bVa[i]])
                    k.op("dve", lambda e: e.memset(rqm[:], 0.0), [], [brqm])
                    k.op("dve", lambda e: e.memset(Rf[:], 0.0), [], [bRf])
                    k.op("dve", lambda e: e.memset(Rb[:], 0.0), [], [bRb])
                    k.op("dve", lambda e: e.memset(Rb1[:], 0.0), [], [bRb1])

                    ZB = [0, 1, 2, 3, 4, 1]
                    STB = 5
                    TPB = 6
                    RB = 7

                    def xload(t):
                        X, bX = Xt[t % 2], bXt[t % 2]
                        k.dma("sp", X[:], xp[b, t * 128:(t + 1) * 128, :], [], [bX], bX)

                    def s1(t):
                        X, bX = Xt[t % 2], bXt[t % 2]
                        hT, bhT = hTs_[t % 2], bhTs_[t % 2]
                        rms_stats(X[:], bX, 128, 0, aqkb, baqkb)
                        k.op("dve", lambda e: e.tensor_scalar_mul(xn[:], X[:], ss_t[:, 0:1]), [bX, bss], [bxn])
                        tp = pbf(TPB)
                        for c in range(8):
                            k.tr(tp[:, c * 128:(c + 1) * 128], xn[:, c * 128:(c + 1) * 128], T["ident_b"][:], [bxn, bT], [bPB[TPB]], last=(c == 7))
                        k.op("dve", lambda e: e.tensor_tensor(hT[:], tp[:].rearrange("p (c n) -> p c n", n=128),
                                                              G1T[:, :, 64 + b:65 + b].to_broadcast([128, 8, 128]), ALU.mult), [bPB[TPB], bG1T], [bhT])
                        k.op("dve", lambda e: e.tensor_tensor(hT[:], hT[:], modT[:, 0:8, 64 + b:65 + b].to_broadcast([128, 8, 128]), ALU.add), [bhT, bmodT], [bhT])

                    def zc(t, i):
                        hT, bhT = hTs_[t % 2], bhTs_[t % 2]
                        for dc in range(8):
                            k.mm(PB[ZB[i]][:], hT[:, dc, :], Wi[:, dc, i * 512:(i + 1) * 512], [bhT, bWi], [bPB[ZB[i]]],
                                 start=(dc == 0), stop=(dc == 7), last=(dc == 7))

                    brts_ = [Buf(f"rt{i}") for i in range(4)]

                    def rot(src, bsrc, dst, bdst, cosn, sinn, t, bdst2=None):
                        zv = src.rearrange("p (h two f) -> p h two f", two=2, f=32)
                        x1v, x2v = zv[:, :, 0, :], zv[:, :, 1, :]
                        ov = dst.rearrange("p (h two f) -> p h two f", two=2, f=32)
                        cb = T[cosn][:, t, :].unsqueeze(1).to_broadcast([128, 8, 32])
                        sb_ = T[sinn][:, t, :].unsqueeze(1).to_broadcast([128, 8, 32])
                        r0, r1_, r2, r3 = (rt[i][:, 0:8, :] for i in range(4))
                        k.op("dve", lambda e: e.tensor_tensor(r0, x1v, cb, ALU.mult), [bsrc, bT], [brts_[0]])
                        k.op("dve", lambda e: e.tensor_tensor(r1_, x2v, sb_, ALU.mult), [bsrc, bT], [brts_[1]])
                        k.op("pool", lambda e: e.tensor_tensor(ov[:, :, 0, :], r0, r1_, ALU.subtract), [brts_[0], brts_[1]], [bdst])
                        k.op("dve", lambda e: e.tensor_tensor(r2, x1v, sb_, ALU.mult), [bsrc, bT], [brts_[2]])
                        k.op("dve", lambda e: e.tensor_tensor(r3, x2v, cb, ALU.mult), [bsrc, bT], [brts_[3]])
                        k.op("pool", lambda e: e.tensor_tensor(ov[:, :, 1, :], r2, r3, ALU.add), [brts_[2], brts_[3]], [bdst2 if bdst2 is not None else bdst])

                    def r1(t):
                        rot(PB[ZB[0]][:], bPB[ZB[0]], rqk[:], brqk, "cos_r", "sin_r", t)
                        k.op("pool", lambda e: e.tensor_tensor(kz[:].rearrange("p (h f) -> p h f", f=64),
                                                               rqk[:, 256:512].rearrange("p (h f) -> p h f", f=64),
                                                               T["zeta"][:].unsqueeze(2).to_broadcast([128, 4, 64]), ALU.mult), [brqk, bT], [bkz])
                        k.op("act", lambda e: e.activation(rvb[:], PB[ZB[1]][:], AF.Copy), [bPB[ZB[1]]], [brvb])
                        k.op("act", lambda e: e.activation(sgt[:], PB[ZB[2]][:], AF.Silu), [bPB[ZB[2]]], [bsgt])
                        tp = pbf(TPB)
                        for c in range(4):
                            k.tr(tp[:, c * 128:(c + 1) * 128], rqk[:, c * 128:(c + 1) * 128], T["ident_b"][:], [brqk, bT], [bPB[TPB]], last=(c == 3))
                        k.op("act", lambda e: e.activation(rqkT[:].rearrange("p c n -> p (c n)"), tp[:, 0:512], AF.Copy), [bPB[TPB]], [brqkT])
                        k.op("act", lambda e: e.activation(rqm[0:64, 0, :, :].rearrange("p c n -> p (c n)"), tp[0:64, 0:256], AF.Copy), [bPB[TPB]], [brqm])
                        k.op("act", lambda e: e.activation(rqm[64:128, 1, :, :].rearrange("p c n -> p (c n)"), tp[64:128, 0:256], AF.Copy), [bPB[TPB]], [brqm])
                        for vv in range(2):
                            k.op("pool", lambda e, vv=vv: e.tensor_tensor(rqxT[:, vv, :, :], rqm[:, vv, :, :], T["xit"][:], ALU.mult), [brqm, bT], [brqxT])

                    def a1a(t):
                        av, bav = avf[0], bavf[0]
                        k.op("act", lambda e: e.activation(av[:], PB[ZB[5]][:], AF.Copy), [bPB[ZB[5]]], [bav])
                        k.dma("pool", vp[b, t * 128:(t + 1) * 128, :], av[:], [bav], [], bav, final=True)
                        k.op("act", lambda e: e.activation(avb[:], PB[ZB[5]][:], AF.Copy), [bPB[ZB[5]]], [bavb])
                        k.dma("pool", vbs[b, t * 128:(t + 1) * 128, :], avb[:], [bavb], [bvp], bavb)
                        rot(PB[ZB[3]][:], bPB[ZB[3]], aqkb[:, 0:512], baqkb, "cos_a", "sin_a", t)

                    def a1b(t):
                        af, baf = aqkf[0], baqkf[0]
                        rot(PB[ZB[4]][:], bPB[ZB[4]], af[:, 0:512], baf, "cos_a", "sin_a", t)
                        k.dma("pool", kp[b, t * 128:(t + 1) * 128, :], af[:, 0:512], [baf], [], baf, final=True)
                        k.op("act", lambda e: e.activation(aqkb[:, 512:1024], af[:, 0:512], AF.Copy), [baf], [baqkb])

                    def r2(t):
                        for hd in range(4):
                            c, hh = hd // 2, hd % 2
                            k.mm(PB[RB][:, hd * 128:(hd + 1) * 128], rqkT[:, 2 + c, :], rqm[:, hh, c, :], [brqkT, brqm], [bPB[RB]], last=(hd == 3))
                        k.op("dve", lambda e: e.tensor_tensor(scm[:], PB[RB][:], T["intra"][:].rearrange("p h n -> p (h n)"), ALU.mult), [bPB[RB], bT], [bscm])

                    def r3a(t):
                        for hd in range(4):
                            c, hh = hd // 2, hd % 2
                            cs = slice(hd * 128, (hd + 1) * 128)
                            k.mm(PB[RB][:, cs], scm[:, cs], rvb[:, cs], [bscm, brvb], [bPB[RB]], start=True, stop=False, last=False)
                            k.mm(PB[RB][:, cs], rqxT[:, hh, c, :], Rbs[t % 2][:, c, :], [brqxT, bRbs[t % 2]], [bPB[RB]], start=False, stop=True, last=(hd == 3))
                        for hd in range(4):
                            cs = slice(hd * 128, (hd + 1) * 128)
                            k.op("dve", lambda e, hd=hd: e.memset(ss_t[:, 4 + hd:5 + hd], 0.0), [], [bss])
                            k.op("act", lambda e, hd=hd, cs=cs: e.activation(scm[:, cs], PB[RB][:, cs], AF.Square, accum_out=ss_t[:, 4 + hd:5 + hd]), [bPB[RB]], [bscm, bss])
                        k.op("act", lambda e: e.activation(ss_t[:, 4:8], ss_t[:, 4:8], AF.Sqrt, bias=epsT[:], scale=1.0 / 128), [bss, bT], [bss])
                        k.op("dve", lambda e: e.reciprocal(ss_t[:, 4:8], ss_t[:, 4:8]), [bss], [bss])
                        for hd in range(4):
                            cs = slice(hd * 128, (hd + 1) * 128)
                            k.op("dve", lambda e, hd=hd, cs=cs: e.scalar_tensor_tensor(rety[:, cs], PB[RB][:, cs], ss_t[:, 4 + hd:5 + hd], sgt[:, cs],
                                                                                        op0=ALU.mult, op1=ALU.mult), [bPB[RB], bss, bsgt], [brety])

                    def r3b(t):
                        for c in range(2):
                            k.mm(PB[STB][:, c * 256:(c + 1) * 256], kz[:, c * 128:(c + 1) * 128], rvb[:, c * 256:(c + 1) * 256], [bkz, brvb], [bPB[STB]], last=(c == 1))
                        for c in range(2):
                            for hh in range(2):
                                rows = slice(hh * 64, (hh + 1) * 64)
                                k.op("dve", lambda e, c=c, hh=hh, rows=rows: e.scalar_tensor_tensor(
                                    Rf[rows, c, :], Rf[rows, c, :], T["gc"][rows, c:c + 1], PB[STB][rows, c * 256 + hh * 128:c * 256 + (hh + 1) * 128],
                                    op0=ALU.mult, op1=ALU.add), [bRf, bT, bPB[STB]], [bRf])
                        k.op("act", lambda e: e.activation(Rbs[(t + 1) % 2][:], Rf[:], AF.Copy), [bRf], [bRbs[(t + 1) % 2]])

                    def r4(t):
                        tp = pbf(TPB)
                        for c in range(4):
                            k.tr(tp[:, c * 128:(c + 1) * 128], rety[:, c * 128:(c + 1) * 128], T["ident_b"][:], [brety, bT], [bPB[TPB]], last=(c == 3))
                        k.op("act", lambda e: e.activation(mixT[:, 0:4, t * 128:(t + 1) * 128], tp[:, 0:512].rearrange("p (c n) -> p c n", n=128), AF.Copy),
                             [bPB[TPB]], [bmixT])

                    def a2(t):
                        tp = pbf(TPB)
                        for c in range(8):
                            k.tr(tp[:, c * 128:(c + 1) * 128], aqkb[:, c * 128:(c + 1) * 128], T["ident_b"][:], [baqkb, bT], [bPB[TPB]], last=(c == 7))
                        k.op("act", lambda e: e.activation(aqkT[:, :, t * 128:(t + 1) * 128], tp[:].rearrange("p (c n) -> p c n", n=128), AF.Copy),
                             [bPB[TPB]], [baqkT])

                    xload(0)
                    xload(1)
                    s1(0)
                    for i in range(5):
                        zc(0, i)
                    for t in range(NT):
                        nxt = t + 1 < NT
                        if t + 2 < NT:
                            xload(t + 2)
                        if nxt:
                            s1(t + 1)
                        r1(t)
                        zc(t, 5)
                        a1a(t)
                        r2(t)
                        if nxt:
                            zc(t + 1, 0)
                        a1b(t)
                        if nxt:
                            zc(t + 1, 1)
                        r3a(t)
                        if nxt:
                            zc(t + 1, 2)
                            zc(t + 1, 3)
                        r3b(t)
                        if nxt:
                            zc(t + 1, 4)
                        r4(t)
                        a2(t)
                        chk(1)
                    chk(2)
                    k.dma("sp", rp[b].rearrange("(c hh) kk v -> (hh kk) c v", hh=2), Rf[:], [bRf], [], bRf, final=True)

                    its = []
                    for hd in range(8):
                        for (dil, ntile_sub) in ((16, 1), (4, 4), (1, 16)):
                            for a in range(16):
                                its.append((hd, dil, ntile_sub, a))
                    hbS = [bPB[i] for i in range(4)]
                    hbX = [bPB[4 + i] for i in range(4)]
                    LA = 3
                    LV = 2
                    assert LA + LV < len(Va) // 2 and LA + 1 < len(Et)
                    NVA = len(Va) // 2

                    def prm(i):
                        hd, dil, ntile_sub, a = its[i]
                        c, hh = hd // 2, hd % 2
                        sub, nbk = a // ntile_sub, a % ntile_sub
                        start = dil * 128 * nbk + sub
                        nq = 256 if nbk < ntile_sub - 1 else 128
                        kc = slice(start, start + 127 * dil + 1, dil)
                        qc = slice(start, start + (nq - 1) * dil + 1, dil)
                        rows = slice(hh * 64, (hh + 1) * 64)
                        vi = hh * NVA + (i % NVA)
                        return hd, dil, c, hh, nq, kc, qc, rows, vi

                    def stV(i):
                        hd, dil, c, hh, nq, kc, qc, rows, vi = prm(i)
                        k.dma("sp", Va[vi][:, hh * 64:(hh + 1) * 64], vbs[b, kc, hd * 64:(hd + 1) * 64], [bvp], [bVa[vi]], bVa[vi])

                    def stS(i):
                        hd, dil, c, hh, nq, kc, qc, rows, vi = prm(i)
                        ps_s = PB[i % 4][:, 0:nq]
                        et, bet = Et[i % len(Et)], bEt[i % len(Et)]
                        k.mm(ps_s, aqkT[rows, 4 + c, kc], aqkT[rows, c, qc], [baqkT], [hbS[i % 4]])
                        k.op("act", lambda e: e.activation(et[:, 0:nq], ps_s, AF.Exp, scale=0.125), [hbS[i % 4]], [bet])
                        k.op("pool", lambda e: e.tensor_tensor(et[:, 0:nq], et[:, 0:nq], T["amask"][:, 0:nq], ALU.mult), [bet, bT], [bet])

                    def stX(i):
                        hd, dil, c, hh, nq, kc, qc, rows, vi = prm(i)
                        ps_x = PB[4 + i % 4][:, 0:nq]
                        et, bet = Et[i % len(Et)], bEt[i % len(Et)]
                        k.mm(ps_x, Va[vi][:], et[:, 0:nq], [bVa[vi], bet], [hbX[i % 4]])
                        if dil == 16:
                            k.op("act", lambda e: e.activation(Xacc[:, qc], ps_x, AF.Copy), [hbX[i % 4]], [bXacc])
                        else:
                            k.op("dve", lambda e: e.tensor_tensor(Xacc[:, qc], Xacc[:, qc], ps_x, ALU.add), [bXacc, hbX[i % 4]], [bXacc])
                        if i % 48 == 47:
                            drows = slice((1 - hh) * 64, (2 - hh) * 64)
                            for pc in range(8):
                                cs = slice(pc * 256, (pc + 1) * 256)
                                k.op("dve", lambda e: e.reciprocal(rct[rows, :], Xacc[drows, cs]), [bXacc], [brct])
                                k.op("dve", lambda e: e.tensor_tensor(mixT[rows, 4 + c, cs], Xacc[rows, cs], rct[rows, :], ALU.mult), [bXacc, brct], [bmixT])

                    n_it = len(its)
                    for i in range(min(LV, n_it)):
                        stV(i)
                    for i in range(n_it + LA):
                        if i + LV < n_it:
                            stV(i + LV)
                        if i < n_it:
                            stS(i)
                        if i - LA >= 0:
                            stX(i - LA)
                    k.barrier()
                chk(3)

                with contextlib.ExitStack() as sc_:
                    gates = k.sb("gates", [128, 2, 1024], F32, sc_)
                    FG = k.sb("FG", [128, 1024], F32, sc_)
                    k.dma("sp", FG[:], fgd.unsqueeze(0).to_broadcast([128, 1024]), [], [bFG], bFG)
                    for gi, vec in enumerate((2, 5)):
                        for c in range(8):
                            pbi = gi * 2 + c // 4
                            k.mm(PB[pbi][:, (c % 4) * 128:(c % 4 + 1) * 128],
                                 modT[:, vec * 8 + c, 64 + b:65 + b].to_broadcast([128, 128]), T["ident_f"][:],
                                 [bmodT, bT], [bPB[pbi]], last=(c % 4 == 3))
                        for hf in range(2):
                            k.op("act", lambda e, gi=gi, hf=hf: e.activation(gates[:, gi, hf * 512:(hf + 1) * 512], PB[gi * 2 + hf][:], AF.Copy),
                                 [bPB[gi * 2 + hf]], [bgates])
                    X1 = [k.sb(f"X1_{i}", [128, 1024], F32, sc_) for i in range(8)]
                    bX1 = [Buf(f"X1_{i}") for i in range(8)]
                    Xr = [k.sb(f"Xr{i}", [128, 1024], F32, sc_) for i in range(2)]
                    bXr = [Buf(f"Xr{i}") for i in range(2)]
                    xn2 = k.sb("xn2", [128, 1024], BF16, sc_); bxn2 = Buf("xn2")
                    jk2 = k.sb("jk2", [128, 1024], BF16, sc_); bjk2 = Buf("jk2")
                    h2Ts = [k.sb(f"h2T{i}", [128, 8, 512], BF16, sc_) for i in range(2)]
                    bh2Ts = [Buf(f"h2T{i}") for i in range(2)]
                    ffT = k.sb("ffT", [128, NJ, 512], BF16, sc_); bffT = Buf("ffT")
                    ring = [(k.sb(f"gu{i}", [128, 2, 8, 128], BF16, sc_), k.sb(f"dd{i}", [128, 1024], BF16, sc_)) for i in range(3)]
                    bring = [Buf(f"ring{i}") for i in range(3)]
                    rcnt = [0]

                    def nslot():
                        i = rcnt[0] % len(ring)
                        rcnt[0] += 1
                        return ring[i][0], ring[i][1], bring[i]

                    def pro(g, ti):
                        t = g * 4 + ti
                        tcs = slice(t * 128, (t + 1) * 128)
                        tpb = 4
                        x1, bx1 = X1[(g % 2) * 4 + ti], bX1[(g % 2) * 4 + ti]
                        h2T, bh2T = h2Ts[g % 2], bh2Ts[g % 2]
                        for nb in range(2):
                            for kc_ in range(8):
                                k.mm(PB[2 + nb][:], mixT[:, kc_, tcs], Wo[:, kc_, nb * 512:(nb + 1) * 512], [bmixT, bWo], [bPB[2 + nb]],
                                     start=(kc_ == 0), stop=(kc_ == 7), last=(kc_ == 7))
                        xr, bxr = Xr[ti % 2], bXr[ti % 2]
                        k.dma("sp", xr[:], xp[b, tcs, :], [], [bxr], bxr)
                        for nb in range(2):
                            cs = slice(nb * 512, (nb + 1) * 512)
                            k.op("dve", lambda e, nb=nb, cs=cs: e.tensor_tensor(x1[:, cs], PB[2 + nb][:], gates[:, 0, cs], ALU.mult),
                                 [bPB[2 + nb], bgates], [bx1])
                        k.op("dve", lambda e: e.tensor_tensor(x1[:], x1[:], xr[:], ALU.add), [bx1, bxr], [bx1])
                        rms_stats(x1[:], bx1, 128, 1, xn2, bxn2)
                        k.op("dve", lambda e: e.tensor_scalar_mul(xn2[:], x1[:], ss_t[:, 1:2]), [bx1, bss], [bxn2])

                    def pro_b(g, ti):
                        tpb = 4
                        h2T, bh2T = h2Ts[g % 2], bh2Ts[g % 2]
                        tp = pbf(tpb)
                        for c in range(8):
                            k.tr(tp[:, c * 128:(c + 1) * 128], xn2[:, c * 128:(c + 1) * 128], T["ident_b"][:], [bxn2, bT], [bPB[tpb]], last=(c == 7))
                        for c in range(8):
                            k.op("act", lambda e, c=c: e.activation(h2T[:, c, ti * 128:(ti + 1) * 128], tp[:, c * 128:(c + 1) * 128], AF.Identity,
                                                                     scale=G2T[:, c, 64 + b:65 + b], bias=modT[:, 24 + c, 64 + b:65 + b]),
                                 [bPB[tpb], bG2T, bmodT], [bh2T])

                    def gu_(g, j):
                        h2T, bh2T = h2Ts[g % 2], bh2Ts[g % 2]
                        gu, dd, bsl = nslot()
                        k.dma("sp", gu[:].rearrange("p a c n -> p (a c n)"), sc_gu[j], [bsc[j]], [bsl], bsl)
                        for a in range(2):
                            for dc in range(8):
                                k.mm(PB[a][:], gu[:, a, dc, :], h2T[:, dc, :], [bsl, bh2T], [bPB[a]],
                                     start=(dc == 0), stop=(dc == 7), last=(dc == 7))
                        k.op("act", lambda e: e.activation(ffT[:, j, :], PB[0][:], AF.Silu), [bPB[0]], [bffT])
                        k.op("dve", lambda e: e.tensor_tensor(ffT[:, j, :], ffT[:, j, :], PB[1][:], ALU.mult), [bffT, bPB[1]], [bffT])

                    def dn_(g, j):
                        gu, dd, bsl = nslot()
                        k.dma("sp", dd[:], sc_d[j], [bsc[j]], [bsl], bsl)
                        for ti in range(4):
                            for nb in range(2):
                                k.mm(PB[2 * ti + nb][:], ffT[:, j, ti * 128:(ti + 1) * 128], dd[:, nb * 512:(nb + 1) * 512],
                                     [bsl, bffT], [bPB[2 * ti + nb]], start=(j == 0), stop=(j == NJ - 1), last=(j == NJ - 1 or (ti == 3 and nb == 1)))

                    def epi(g, ti):
                        t = g * 4 + ti
                        x1, bx1 = X1[(g % 2) * 4 + ti], bX1[(g % 2) * 4 + ti]
                        for nb in range(2):
                            cs = slice(nb * 512, (nb + 1) * 512)
                            pbx = PB[2 * ti + nb]
                            k.op("dve", lambda e, cs=cs, pbx=pbx: e.tensor_tensor(pbx[:], pbx[:], gates[:, 1, cs], ALU.mult), [bPB[2 * ti + nb], bgates], [bPB[2 * ti + nb]])
                            k.op("dve", lambda e, cs=cs, pbx=pbx: e.tensor_tensor(x1[:, cs], x1[:, cs], pbx[:], ALU.add), [bx1, bPB[2 * ti + nb]], [bx1])
                        rms_stats(x1[:], bx1, 128, 3, jk2, bjk2)
                        k.op("dve", lambda e: e.scalar_tensor_tensor(x1[:], x1[:], ss_t[:, 3:4], FG[:], op0=ALU.mult, op1=ALU.mult), [bx1, bss, bFG], [bx1])
                        k.dma("pool", yp[b, t * 128:(t + 1) * 128, :], x1[:], [bx1], [], bx1, final=True)

                    for ti in range(4):
                        pro(0, ti)
                        pro_b(0, ti)
                    pend = []
                    for g in range(4):
                        for j in range(NJ):
                            gu_(g, j)
                            if pend:
                                epi(g - 1, pend.pop(0))
                            if g + 1 < 4 and j in (3, 8, 13, 18):
                                pro(g + 1, (j - 3) // 5)
                            if g + 1 < 4 and j in (5, 10, 15, 20):
                                pro_b(g + 1, (j - 5) // 5)
                        for j in range(NJ):
                            dn_(g, j)
                        epi(g, 0)
                        pend = [1, 2, 3]
                        if g == 3:
                            for ti in pend:
                                epi(g, ti)
                            pend = []
                    k.barrier()

            if do_sample:
                TS = {}
                with contextlib.ExitStack() as ss_:
                    for nm in ("cos_sa", "sin_sa", "cos_sr", "sin_sr", "zeta_s", "xit_s", "gc_s", "intra_s", "onehot_s", "smask", "nmask"):
                        TS[nm] = k.sb("TS_" + nm, tabs[nm].shape, _TAB_DT.get(nm, F32), ss_)
                        k.dma("sp", TS[nm][:], td[nm], [], [bT], bT)
                    mods = k.sb("mods", [64, 2, 1024], F32, ss_); bmods = Buf("mods")
                    Xs = k.sb("Xs", [64, 1024], F32, ss_); bXs = Buf("Xs")
                    hs = k.sb("hs", [64, 1024], BF16, ss_); bhs = Buf("hs")
                    hTs = k.sb("hTs", [128, 8, 64], BF16, ss_); bhTs = Buf("hTs")
                    rqk_s = k.sb("rqk_s", [64, 512], BF16, ss_); brqk_s = Buf("rqk_s")
                    rts = [k.sb(f"rts{i}", [64, 8, 32], F32, ss_) for i in range(4)]
                    brts = Buf("rts")
                    aqkf_s = k.sb("aqkf_s", [64, 512], F32, ss_); baqkf_s = Buf("aqkf_s")
                    aqkb_s = k.sb("aqkb_s", [64, 1024], BF16, ss_); baqkb_s = Buf("aqkb_s")
                    avf_s = k.sb("avf_s", [64, 512], F32, ss_); bavf_s = Buf("avf_s")
                    rv_s = k.sb("rv_s", [64, 512], BF16, ss_); brv_s = Buf("rv_s")
                    sg_s = k.sb("sg_s", [64, 512], BF16, ss_); bsg_s = Buf("sg_s")
                    kz_s = k.sb("kz_s", [64, 256], BF16, ss_); bkz_s = Buf("kz_s")
                    kzm = [k.sb(f"kzm{i}", [64, 256], BF16, ss_) for i in range(2)]
                    bkzm = [Buf(f"kzm{i}") for i in range(2)]
                    rqkT_s = k.sb("rqkT_s", [128, 4, 64], BF16, ss_); brqkT_s = Buf("rqkT_s")
                    rqm_s = k.sb("rqm_s", [128, 2, 2, 64], BF16, ss_); brqm_s = Buf("rqm_s")
                    rqx_s = k.sb("rqx_s", [128, 2, 2, 64], BF16, ss_); brqx_s = Buf("rqx_s")
                    scm_s = k.sb("scm_s", [64, 256], BF16, ss_); bscm_s = Buf("scm_s")
                    oT_s = k.sb("oT_s", [128, 256], F32, ss_); boT_s = Buf("oT_s")
                    rety_s = k.sb("rety_s", [64, 512], BF16, ss_); brety_s = Buf("rety_s")
                    Rsf = [k.sb(f"Rsf{i}", [128, 2, 128], F32, ss_) for i in range(2)]
                    bRsf = [Buf(f"Rsf{i}") for i in range(2)]
                    Rsb = k.sb("Rsb", [128, 2, 16, 128], BF16, ss_); bRsb = Buf("Rsb")
                    aqm_s = k.sb("aqm_s", [128, 2, 4, 64], BF16, ss_); baqm_s = Buf("aqm_s")
                    akT_s = k.sb("akT_s", [128, 4, 64], BF16, ss_); bakT_s = Buf("akT_s")
                    van = k.sb("van", [64, 8, 128], BF16, ss_); bvan = Buf("van")
                    Es = k.sb("Es", [128, 9, 8, 4], BF16, ss_); bEs = Buf("Es")
                    En = k.sb("En", [64, 8, 4], BF16, ss_); bEn = Buf("En")
                    rcs = k.sb("rcs", [128, 64], F32, ss_); brcs = Buf("rcs")

                    def tm_mod(dst_slot, src_fn, dst, bdst):
                        for c in range(8):
                            pbi = c // 4
                            k.tr(PB[pbi][0:64, (c % 4) * 128:(c % 4 + 1) * 128], src_fn(c), T["ident_f"][:], [bmodT, bG1T, bG2T, bT], [bPB[pbi]], last=(c % 4 == 3))
                        for pbi in range(2):
                            k.op("act", lambda e, pbi=pbi: e.activation(dst[:, dst_slot, pbi * 512:(pbi + 1) * 512], PB[pbi][0:64, :], AF.Copy), [bPB[pbi]], [bdst])
                    tm_mod(0, lambda c: modT[:, c, 0:64], mods, bmods)
                    tm_mod(1, lambda c: G1T[:, c, 0:64], mods, bmods)
                    k.op("dve", lambda e: e.memset(van[:], 1.0), [], [bvan])
                    k.op("dve", lambda e: e.memset(rqm_s[:], 0.0), [], [brqm_s])
                    k.op("dve", lambda e: e.memset(aqm_s[:], 0.0), [], [baqm_s])

                    k.dma("sp", Xs[:], xs, [], [bXs], bXs)
                    rms_stats(Xs[:], bXs, 64, 0, hs, bhs)
                    k.op("dve", lambda e: e.scalar_tensor_tensor(Xs[:], Xs[:], ss_t[0:64, 0:1], mods[:, 1, :], op0=ALU.mult, op1=ALU.mult), [bXs, bss, bmods], [bXs])
                    k.op("dve", lambda e: e.tensor_tensor(hs[:], Xs[:], mods[:, 0, :], ALU.add), [bXs, bmods], [bhs])
                    tp = pbf(7)
                    for c in range(8):
                        k.tr(tp[:, c * 64:(c + 1) * 64], hs[:, c * 128:(c + 1) * 128], T["ident_b"][0:64, 0:64], [bhs, bT], [bPB[7]], last=(c == 7))
                    k.op("act", lambda e: e.activation(hTs[:].rearrange("p c n -> p (c n)"), tp[:, 0:512], AF.Copy), [bPB[7]], [bhTs])
                    with contextlib.ExitStack() as sw_:
                        Wi = k.sb("Wi_s", [128, 8, 3072], BF16, sw_)
                        for dc in range(8):
                            k.dma("sp", Wi[:, dc, :], sc_wi[dc], [bscwi], [bWi], bWi)
                        for nb in range(6):
                            for dc in range(8):
                                k.mm(PB[nb][0:64, :], hTs[:, dc, :], Wi[:, dc, nb * 512:(nb + 1) * 512], [bhTs, bWi], [bPB[nb]],
                                     start=(dc == 0), stop=(dc == 7), last=(dc == 7))
                        k.barrier()
                    NR = 4
                    Ktb = [k.sb(f"Ktf{i}", [128, 512], F32, ss_) for i in range(NR)]
                    bKtb = [Buf(f"Ktf{i}") for i in range(NR)]
                    Vtf = [k.sb(f"Vtf{i}", [128, 512], F32, ss_) for i in range(NR)]
                    bVtf = [Buf(f"Vtf{i}") for i in range(NR)]
                    Kcb = [k.sb(f"Kcb{i}", [128, 512], BF16, ss_) for i in range(NR)]
                    bKcb = [Buf(f"Kcb{i}") for i in range(NR)]
                    KTs = [k.sb(f"KTs{i}", [128, 4, 128], BF16, ss_) for i in range(NR)]
                    bKTs = [Buf(f"KTs{i}") for i in range(NR)]
                    vas = [k.sb(f"vas{i}", [128, 8, 128], BF16, ss_) for i in range(18)]
                    bvas = [Buf(f"vas{i}") for i in range(18)]
                    for i in range(18):
                        k.op("pool", lambda e, i=i: e.memset(vas[i][:], 1.0), [], [bvas[i]])

                    def rotary(src_ap, dst_ap, cosn, sinn, bsrc, bdst):
                        zv = src_ap.rearrange("p (h two f) -> p h two f", two=2, f=32)
                        x1v, x2v = zv[:, :, 0, :], zv[:, :, 1, :]
                        ov = dst_ap.rearrange("p (h two f) -> p h two f", two=2, f=32)
                        cb = TS[cosn][:].unsqueeze(1).to_broadcast([64, 8, 32])
                        sb_ = TS[sinn][:].unsqueeze(1).to_broadcast([64, 8, 32])
                        r0, r1, r2, r3 = (rts[i][:] for i in range(4))
                        k.op("dve", lambda e: e.tensor_tensor(r0, x1v, cb, ALU.mult), [bsrc, bT], [brts])
                        k.op("dve", lambda e: e.tensor_tensor(r1, x2v, sb_, ALU.mult), [bsrc, bT], [brts])
                        k.op("dve", lambda e: e.tensor_tensor(r2, x1v, sb_, ALU.mult), [bsrc, bT], [brts])
                        k.op("dve", lambda e: e.tensor_tensor(r3, x2v, cb, ALU.mult), [bsrc, bT], [brts])
                        k.op("dve", lambda e: e.tensor_tensor(ov[:, :, 0, :], r0, r1, ALU.subtract), [brts], [bdst])
                        k.op("dve", lambda e: e.tensor_tensor(ov[:, :, 1, :], r2, r3, ALU.add), [brts], [bdst])
                    rotary(PB[0][0:64, :], rqk_s[:], "cos_sr", "sin_sr", bPB[0], brqk_s)
                    rotary(PB[3][0:64, :], aqkb_s[:, 0:512], "cos_sa", "sin_sa", bPB[3], baqkb_s)
                    rotary(PB[4][0:64, :], aqkf_s[:, 0:512], "cos_sa", "sin_sa", bPB[4], baqkf_s)
                    k.dma("sp", kso, aqkf_s[:, 0:512], [baqkf_s], [], baqkf_s, final=True)
                    k.op("act", lambda e: e.activation(aqkb_s[:, 512:1024], aqkf_s[:, 0:512], AF.Copy), [baqkf_s], [baqkb_s])
                    k.op("act", lambda e: e.activation(avf_s[:], PB[5][0:64, :], AF.Copy), [bPB[5]], [bavf_s])
                    k.dma("sp", vso, avf_s[:], [bavf_s], [], bavf_s, final=True)
                    avv = avf_s[:].rearrange("p (h f) -> p h f", f=64)
                    k.op("dve", lambda e: e.tensor_copy(van[:, 0:8:2, 0:64], avv[:, 0:8:2, :]), [bavf_s], [bvan])
                    k.op("dve", lambda e: e.tensor_copy(van[:, 1:8:2, 64:128], avv[:, 1:8:2, :]), [bavf_s], [bvan])
                    k.op("act", lambda e: e.activation(rv_s[:], PB[1][0:64, :], AF.Copy), [bPB[1]], [brv_s])
                    k.op("act", lambda e: e.activation(sg_s[:], PB[2][0:64, :], AF.Silu), [bPB[2]], [bsg_s])
                    k.op("dve", lambda e: e.tensor_tensor(kz_s[:].rearrange("p (h f) -> p h f", f=64),
                                                          rqk_s[:, 256:512].rearrange("p (h f) -> p h f", f=64),
                                                          TS["zeta_s"][:].unsqueeze(2).to_broadcast([64, 4, 64]), ALU.mult), [brqk_s, bT], [bkz_s])
                    tp = pbf(7)
                    for c in range(4):
                        k.tr(tp[:, c * 64:(c + 1) * 64], rqk_s[:, c * 128:(c + 1) * 128], T["ident_b"][0:64, 0:64], [brqk_s, bT], [bPB[7]], last=(c == 3))
                    k.op("act", lambda e: e.activation(rqkT_s[:].rearrange("p c n -> p (c n)"), tp[:, 0:256], AF.Copy), [bPB[7]], [brqkT_s])
                    k.op("act", lambda e: e.activation(rqm_s[0:64, 0, :, :].rearrange("p c n -> p (c n)"), tp[0:64, 0:128], AF.Copy), [bPB[7]], [brqm_s])
                    k.op("act", lambda e: e.activation(rqm_s[64:128, 1, :, :].rearrange("p c n -> p (c n)"), tp[64:128, 0:128], AF.Copy), [bPB[7]], [brqm_s])
                    for vv in range(2):
                        k.op("dve", lambda e, vv=vv: e.tensor_tensor(rqx_s[:, vv, :, :], rqm_s[:, vv, :, :], TS["xit_s"][:], ALU.mult), [brqm_s, bT], [brqx_s])
                    tp = pbf(6)
                    for c in range(8):
                        k.tr(tp[:, c * 64:(c + 1) * 64], aqkb_s[:, c * 128:(c + 1) * 128], T["ident_b"][0:64, 0:64], [baqkb_s, bT], [bPB[6]], last=(c == 7))
                    k.op("act", lambda e: e.activation(aqm_s[0:64, 0, :, :].rearrange("p c n -> p (c n)"), tp[0:64, 0:256], AF.Copy), [bPB[6]], [baqm_s])
                    k.op("act", lambda e: e.activation(aqm_s[64:128, 1, :, :].rearrange("p c n -> p (c n)"), tp[64:128, 0:256], AF.Copy), [bPB[6]], [baqm_s])
                    k.op("act", lambda e: e.activation(akT_s[:].rearrange("p c n -> p (c n)"), tp[:, 256:512], AF.Copy), [bPB[6]], [bakT_s])

                    for hd in range(4):
                        c, hh = hd // 2, hd % 2
                        k.mm(PB[0][0:64, hd * 64:(hd + 1) * 64], rqkT_s[:, 2 + c, :], rqm_s[:, hh, c, :], [brqkT_s, brqm_s], [bPB[0]], last=(hd == 3))
                    k.op("dve", lambda e: e.tensor_tensor(scm_s[:], PB[0][0:64, 0:256], TS["intra_s"][:].rearrange("p h n -> p (h n)"), ALU.mult), [bPB[0], bT], [bscm_s])
                    srv = sr.rearrange("b (c hh) kk v -> (hh kk) c b v", hh=2)
                    for bb in range(16):
                        rf, brf = Rsf[bb % 2], bRsf[bb % 2]
                        k.dma("sp", rf[:], srv[:, :, bb, :], [], [brf], brf)
                        k.op("act", lambda e, bb=bb: e.activation(Rsb[:, :, bb, :], rf[:], AF.Copy), [brf], [bRsb])
                    for hd in range(4):
                        c, hh = hd // 2, hd % 2
                        k.mm(PB[1][:, hd * 64:(hd + 1) * 64], rv_s[:, hd * 128:(hd + 1) * 128], scm_s[:, hd * 64:(hd + 1) * 64], [brv_s, bscm_s], [bPB[1]],
                             start=True, stop=False, last=False)
                        for bb in range(16):
                            k.mm(PB[1][:, hd * 64 + bb * 4:hd * 64 + bb * 4 + 4], Rsb[:, c, bb, :], rqx_s[:, hh, c, bb * 4:(bb + 1) * 4], [bRsb, brqx_s], [bPB[1]],
                                 start=False, stop=(bb == 15), last=(hd == 3 and bb == 15))
                    k.op("act", lambda e: e.activation(oT_s[:], PB[1][:, 0:256], AF.Copy), [bPB[1]], [boT_s])
                    for hd in range(4):
                        k.tr(PB[2][0:64, hd * 128:(hd + 1) * 128], oT_s[:, hd * 64:(hd + 1) * 64], T["ident_f"][:], [boT_s, bT], [bPB[2]], last=(hd == 3))
                    for hd in range(4):
                        cs = slice(hd * 128, (hd + 1) * 128)
                        k.op("dve", lambda e, hd=hd: e.memset(ss_t[0:64, 4 + hd:5 + hd], 0.0), [], [bss])
                        k.op("act", lambda e, hd=hd, cs=cs: e.activation(rety_s[0:64, cs], PB[2][0:64, cs], AF.Square, accum_out=ss_t[0:64, 4 + hd:5 + hd]), [bPB[2]], [brety_s, bss])
                    k.op("act", lambda e: e.activation(ss_t[0:64, 4:8], ss_t[0:64, 4:8], AF.Sqrt, bias=epsT[0:64, :], scale=1.0 / 128), [bss, bT], [bss])
                    k.op("dve", lambda e: e.reciprocal(ss_t[0:64, 4:8], ss_t[0:64, 4:8]), [bss], [bss])
                    for hd in range(4):
                        cs = slice(hd * 128, (hd + 1) * 128)
                        k.op("dve", lambda e, hd=hd, cs=cs: e.scalar_tensor_tensor(rety_s[:, cs], PB[2][0:64, cs], ss_t[0:64, 4 + hd:5 + hd], sg_s[:, cs],
                                                                                    op0=ALU.mult, op1=ALU.mult), [bPB[2], bss, bsg_s], [brety_s])
                    tp = pbf(7)
                    for c in range(4):
                        k.tr(tp[:, c * 64:(c + 1) * 64], rety_s[:, c * 128:(c + 1) * 128], T["ident_b"][0:64, 0:64], [brety_s, bT], [bPB[7]], last=(c == 3))
                    k.op("act", lambda e: e.activation(mixT[:, 0:4, 0:64], tp[:, 0:256].rearrange("p (c n) -> p c n", n=64), AF.Copy), [bPB[7]], [bmixT])
                    rsov = rso.rearrange("b (c hh) kk v -> (hh kk) c b v", hh=2)
                    for bb in range(16):
                        rf, brf = Rsf[bb % 2], bRsf[bb % 2]
                        km, bkm = kzm[bb % 2], bkzm[bb % 2]
                        pbi = 3 + bb % 2
                        k.dma("sp", rf[:], srv[:, :, bb, :], [], [brf], brf)
                        k.op("dve", lambda e, bb=bb: e.tensor_scalar_mul(km[:], kz_s[:], TS["onehot_s"][:, bb:bb + 1]), [bkz_s, bT], [bkm])
                        for c in range(2):
                            k.mm(PB[pbi][:, c * 256:(c + 1) * 256], km[:, c * 128:(c + 1) * 128], rv_s[:, c * 256:(c + 1) * 256], [bkm, brv_s], [bPB[pbi]], last=(c == 1))
                        for c in range(2):
                            for hh in range(2):
                                rows = slice(hh * 64, (hh + 1) * 64)
                                k.op("dve", lambda e, c=c, hh=hh, rows=rows: e.scalar_tensor_tensor(
                                    rf[rows, c, :], rf[rows, c, :], TS["gc_s"][rows, c:c + 1], PB[pbi][rows, c * 256 + hh * 128:c * 256 + (hh + 1) * 128],
                                    op0=ALU.mult, op1=ALU.add), [brf, bT, bPB[pbi]], [brf])
                        k.dma("sp", rsov[:, :, bb, :], rf[:], [brf], [], brf, final=True)

                    for bb in range(16):
                        rowsl = [slice(1920, 2048)] + [slice(1536 + t_, 1536 + t_ + 4 * 127 + 1, 4) for t_ in range(4)] \
                            + [slice(t_, t_ + 16 * 127 + 1, 16) for t_ in range(4)]
                        for ti_, rs_ in enumerate(rowsl):
                            ri = (bb * 9 + ti_) % NR
                            vi_ = (bb % 2) * 9 + ti_
                            kt, bkt = Ktb[ri], bKtb[ri]
                            kT, bkT = KTs[ri], bKTs[ri]
                            k.dma("sp", kt[:], ck[bb, rs_, :], [], [bkt], bkt)
                            vt, bvt = Vtf[ri], bVtf[ri]
                            k.dma("sp", vt[:], cv[bb, rs_, :], [], [bvt], bvt)
                            vtv = vt[:].rearrange("p (h f) -> p h f", f=64)
                            k.op("pool", lambda e: e.tensor_copy(vas[vi_][:, 0:8:2, 0:64], vtv[:, 0:8:2, :]), [bvt], [bvas[vi_]])
                            k.op("dve", lambda e: e.tensor_copy(vas[vi_][:, 1:8:2, 64:128], vtv[:, 1:8:2, :]), [bvt], [bvas[vi_]])
                            kcb, bkcb = Kcb[ri], bKcb[ri]
                            k.op("dve", lambda e: e.tensor_copy(kcb[:], kt[:]), [bkt], [bkcb])
                            tpi = 6 + ti_ % 2
                            tp = pbf(tpi)
                            for c in range(4):
                                k.tr(tp[:, c * 128:(c + 1) * 128], kcb[:, c * 128:(c + 1) * 128], T["ident_b"][:], [bkcb, bT], [bPB[tpi]], last=(c == 3))
                            k.op("act", lambda e: e.activation(kT[:].rearrange("p c n -> p (c n)"), tp[:, 0:512], AF.Copy), [bPB[tpi]], [bkT])
                            for hd in range(8):
                                c, hh = hd // 2, hd % 2
                                k.mm(PB[0][:, ti_ * 32 + hd * 4:ti_ * 32 + hd * 4 + 4], kT[:, c, :], aqm_s[:, hh, c, bb * 4:(bb + 1) * 4], [bkT, baqm_s], [bPB[0]], last=(hd == 7))
                        for hd in range(8):
                            c, hh = hd // 2, hd % 2
                            k.mm(PB[1][0:64, hd * 4:hd * 4 + 4], akT_s[:, c, :], aqm_s[:, hh, c, bb * 4:(bb + 1) * 4], [bakT_s, baqm_s], [bPB[1]], last=(hd == 7))
                        k.op("act", lambda e: e.activation(Es[:].rearrange("p a h t -> p (a h t)"), PB[0][:, 0:288], AF.Exp, scale=0.125), [bPB[0]], [bEs])
                        k.op("dve", lambda e: e.tensor_tensor(Es[:], Es[:], TS["smask"][:].unsqueeze(2).to_broadcast([128, 9, 8, 4]), ALU.mult), [bEs, bT], [bEs])
                        k.op("act", lambda e: e.activation(En[:].rearrange("p h t -> p (h t)"), PB[1][0:64, 0:32], AF.Exp, scale=0.125), [bPB[1]], [bEn])
                        k.op("dve", lambda e, bb=bb: e.tensor_tensor(En[:], En[:], TS["nmask"][:, bb, :].unsqueeze(1).to_broadcast([64, 8, 4]), ALU.mult), [bEn, bT], [bEn])
                        for hd in range(8):
                            oc = slice(hd * 64 + bb * 4, hd * 64 + bb * 4 + 4)
                            for ti_ in range(9):
                                k.mm(PB[5][:, oc], vas[(bb % 2) * 9 + ti_][:, hd, :], Es[:, ti_, hd, :], [bvas[(bb % 2) * 9 + ti_], bEs], [bPB[5]], start=(ti_ == 0), stop=False, last=False)
                            k.mm(PB[5][:, oc], van[:, hd, :], En[:, hd, :], [bvan, bEn], [bPB[5]], start=False, stop=True, last=(hd == 7))
                    for hd in range(8):
                        c, hh = hd // 2, hd % 2
                        rows = slice(hh * 64, (hh + 1) * 64)
                        drows = slice((1 - hh) * 64, (2 - hh) * 64)
                        cs = slice(hd * 64, (hd + 1) * 64)
                        k.op("dve", lambda e: e.reciprocal(rcs[rows, :], PB[5][drows, cs]), [bPB[5]], [brcs])
                        k.op("dve", lambda e: e.tensor_tensor(mixT[rows, 4 + c, 0:64], PB[5][rows, cs], rcs[rows, :], ALU.mult), [bPB[5], brcs], [bmixT])
                    k.barrier()

                with contextlib.ExitStack() as sc_:
                    mods = k.sb("mods2", [64, 4, 1024], F32, sc_); bmods = Buf("mods2")
                    FG = k.sb("FGs", [128, 1024], F32, sc_)
                    k.dma("sp", FG[:], fgd.unsqueeze(0).to_broadcast([128, 1024]), [], [bFG], bFG)
                    tm_mod(0, lambda c: modT[:, 16 + c, 0:64], mods, bmods)
                    tm_mod(1, lambda c: modT[:, 24 + c, 0:64], mods, bmods)
                    tm_mod(2, lambda c: G2T[:, c, 0:64], mods, bmods)
                    tm_mod(3, lambda c: modT[:, 40 + c, 0:64], mods, bmods)
                    Xs = k.sb("Xs2", [128, 1024], F32, sc_); bXs = Buf("Xs2")
                    X1 = [k.sb("X1s", [128, 1024], F32, sc_)]; bX1 = [Buf("X1s")]
                    xn2 = k.sb("xn2s", [64, 1024], F32, sc_); bxn2 = Buf("xn2s")
                    h2s = k.sb("h2s", [64, 1024], BF16, sc_); bh2s = Buf("h2s")
                    h2T = k.sb("h2Ts", [128, 8, 64], BF16, sc_); bh2T = Buf("h2Ts")
                    ffT = k.sb("ffTs", [128, NJ, 64], BF16, sc_); bffT = Buf("ffTs")
                    ring = [(k.sb(f"gus{i}", [128, 2, 8, 128], BF16, sc_), k.sb(f"dds{i}", [128, 1024], BF16, sc_)) for i in range(2)]
                    bring = [Buf(f"rings{i}") for i in range(2)]
                    for nb in range(2):
                        for kc_ in range(8):
                            k.mm(PB[4 + nb][0:64, :], mixT[:, kc_, 0:64], Wo[:, kc_, nb * 512:(nb + 1) * 512], [bmixT, bWo], [bPB[4 + nb]],
                                 start=(kc_ == 0), stop=(kc_ == 7), last=(kc_ == 7))
                    k.dma("sp", Xs[0:64, :], xs, [], [bXs], bXs)
                    x1 = X1[0]
                    for nb in range(2):
                        cs = slice(nb * 512, (nb + 1) * 512)
                        k.op("dve", lambda e, nb=nb, cs=cs: e.tensor_tensor(x1[0:64, cs], PB[4 + nb][0:64, :], mods[:, 0, cs], ALU.mult),
                             [bPB[4 + nb], bmods], [bX1[0]])
                    k.op("dve", lambda e: e.tensor_tensor(x1[0:64, :], x1[0:64, :], Xs[0:64, :], ALU.add), [bX1[0], bXs], [bX1[0]])
                    rms_stats(x1[0:64, :], bX1[0], 64, 1, h2s, bh2s)
                    k.op("dve", lambda e: e.scalar_tensor_tensor(xn2[:], x1[0:64, :], ss_t[0:64, 1:2], mods[:, 2, :], op0=ALU.mult, op1=ALU.mult), [bX1[0], bss, bmods], [bxn2])
                    k.op("dve", lambda e: e.tensor_tensor(h2s[:], xn2[:], mods[:, 1, :], ALU.add), [bxn2, bmods], [bh2s])
                    tp = pbf(7)
                    for c in range(8):
                        k.tr(tp[:, c * 64:(c + 1) * 64], h2s[:, c * 128:(c + 1) * 128], T["ident_b"][0:64, 0:64], [bh2s, bT], [bPB[7]], last=(c == 7))
                    k.op("act", lambda e: e.activation(h2T[:].rearrange("p c n -> p (c n)"), tp[:, 0:512], AF.Copy), [bPB[7]], [bh2T])

                    def out_fn_s(ti, ytile, bytile, np_):
                        k.dma("sp", ys, ytile[0:64, :], [bytile], [], bytile, final=True)
                    ffn_group(sc_, 1, 64, X1, bX1, h2T, bh2T, ffT, bffT, ring, bring, mods[:, 3, :], bmods, out_fn_s, FG, Xs, bXs, h2s, bh2s)
                    k.barrier()

        except _Stop:
            pass
        k.finish()
    return nc, tabs


_CACHE = {}


def kernel(x_prompt, x_sample, cache_attn_k, cache_attn_v, state_ret, c_prompt, c_sample,
           w_ada, b_ada, norm1_g, w_in, w_out, norm2_g, w_gate, w_up, w_down, final_g):
    f = lambda a: np.ascontiguousarray(np.asarray(a, dtype=np.float32))
    x_prompt, x_sample = f(x_prompt), f(x_sample)
    ck, cv, srr = f(cache_attn_k)[0], f(cache_attn_v)[0], f(state_ret)[0]
    c_prompt, c_sample = f(c_prompt), f(c_sample)
    if "nc" not in _CACHE:
        _CACHE["nc"] = build()
    nc, tabs = _CACHE["nc"]
    shared = {"w_ada": f(w_ada)[0], "b_ada": f(b_ada)[0], "n1g": f(norm1_g)[0], "w_in": f(w_in)[0],
              "w_out": f(w_out)[0], "n2g": f(norm2_g)[0], "w_gate": f(w_gate)[0], "w_up": f(w_up)[0],
              "w_down": f(w_down)[0], "fg": f(final_g)}
    for kk, v in tabs.items():
        shared["t_" + kk] = v
    in_maps = []
    for c in range(NCORES):
        m = dict(shared)
        m["xp"] = x_prompt[4 * c:4 * c + 4]
        m["xs"] = x_sample[16 * c:16 * c + 16].reshape(64, D)
        m["ck"] = ck[16 * c:16 * c + 16].reshape(16, S, 512)
        m["cv"] = cv[16 * c:16 * c + 16].reshape(16, S, 512)
        m["sr"] = srr[16 * c:16 * c + 16]
        m["call"] = np.concatenate([np.repeat(c_sample[16 * c:16 * c + 16], 4, axis=0), c_prompt[4 * c:4 * c + 4]], axis=0)
        in_maps.append(m)
    res = run_bass_kernel_spmd(nc, in_maps, core_ids=list(range(NCORES)))
    R = res.results
    y_prompt = np.concatenate([r["yp"] for r in R], 0)
    y_sample = np.concatenate([r["ys"].reshape(16, 4, D) for r in R], 0)
    nkp = np.concatenate([r["kp"].reshape(4, S, 8, 64) for r in R], 0)[None]
    nvp = np.concatenate([r["vp"].reshape(4, S, 8, 64) for r in R], 0)[None]
    nrp = np.concatenate([r["rp"] for r in R], 0)[None]
    nks = np.concatenate([r["kso"].reshape(16, 4, 8, 64) for r in R], 0)[None]
    nvs = np.concatenate([r["vso"].reshape(16, 4, 8, 64) for r in R], 0)[None]
    nrs = np.concatenate([r["rso"] for r in R], 0)[None]
    return (y_prompt, y_sample, nkp, nvp, nrp, nks, nvs, nrs)
```

```python
import contextlib
import math
import numpy as np
import ml_dtypes
import concourse.bass as bass
import concourse.mybir as mybir
from concourse.bass_utils import run_bass_kernel_spmd

F32 = mybir.dt.float32
BF16 = mybir.dt.bfloat16
AF = mybir.ActivationFunctionType
ALU = mybir.AluOpType

D = 1024
S = 2048
NT = S // 128
DFF = 2816
NJ = DFF // 128
EPS = 1e-6
PAST = 8192
NCORES = 8


class Buf:
    _cache = {}

    def __new__(cls, name):
        if name in cls._cache:
            return cls._cache[name]
        o = super().__new__(cls)
        o.name = name
        o.w = None
        o.r = {}
        o.dsem = None
        o.dcount = 0
        cls._cache[name] = o
        return o


class KB:
    def __init__(self, nc, stack):
        self.nc = nc
        self.stack = stack
        self.eng = {"pe": nc.tensor, "act": nc.scalar, "dve": nc.vector,
                    "pool": nc.gpsimd, "sp": nc.sync}
        self.sems = {}
        self.cnt = {}
        for k in ("pe", "act", "dve", "pool"):
            self.sems[k] = stack.enter_context(nc.semaphore("c_" + k))
            self.cnt[k] = 0
        self.seen = {k: {} for k in self.eng}
        self.pe_pending = []
        self.uid = 0
        self.dsems = {}
        self.finals = {}

    def sb(self, name, shape, dt, stack=None):
        self.uid += 1
        return (stack or self.stack).enter_context(self.nc.sbuf_tensor(f"{name}_u{self.uid}", list(shape), dt))

    def ps(self, name, shape, dt=F32):
        return self.stack.enter_context(self.nc.psum_tensor(name, list(shape), dt))

    def _deps(self, reads, writes):
        deps = {}

        def add(ev):
            if ev is None:
                return
            s, v = ev
            if deps.get(s, 0) < v:
                deps[s] = v
        for b in reads:
            add(b.w)
        for b in writes:
            add(b.w)
            for s, v in b.r.items():
                add((s, v))
        return deps

    def _wait(self, ek, deps, skip_own_pe=False):
        e = self.eng[ek]
        seen = self.seen[ek]
        for s, v in deps.items():
            if skip_own_pe and s is self.sems["pe"]:
                continue
            if seen.get(s, 0) >= v:
                continue
            e.wait_ge(s, v)
            seen[s] = v

    def _commit(self, ev, reads, writes):
        s, v = ev
        for b in reads:
            if b.r.get(s, 0) < v:
                b.r[s] = v
        for b in writes:
            b.w = ev
            b.r = {}

    def op(self, ek, fn, reads=(), writes=()):
        if KB.dead:
            return
        self._wait(ek, self._deps(reads, writes))
        inst = fn(self.eng[ek])
        self.cnt[ek] += 1
        inst.then_inc(self.sems[ek], 1)
        ev = (self.sems[ek], self.cnt[ek])
        self._commit(ev, reads, writes)
        return ev

    def _pe_done(self, inst, reads, writes, last):
        if last:
            self.cnt["pe"] += 1
            inst.then_inc(self.sems["pe"], 1)
            ev = (self.sems["pe"], self.cnt["pe"])
            for (r, w) in self.pe_pending:
                self._commit(ev, r, w)
            self.pe_pending = []
            self._commit(ev, reads, writes)
        else:
            self.pe_pending.append((list(reads), list(writes)))

    def mm(self, out, lhsT, rhs, reads, writes, start=True, stop=True, last=True):
        if KB.dead:
            return
        self._wait("pe", self._deps(reads, writes), skip_own_pe=True)
        inst = self.nc.tensor.matmul(out, lhsT, rhs, start=start, stop=stop)
        self._pe_done(inst, reads, writes, last)

    def tr(self, out, in_, ident, reads, writes, last=True):
        if KB.dead:
            return
        self._wait("pe", self._deps(reads, writes), skip_own_pe=True)
        inst = self.nc.tensor.transpose(out, in_, ident)
        self._pe_done(inst, reads, writes, last)

    def dma(self, q, out, in_, reads, writes, owner, final=False, **kw):
        if KB.dead:
            return
        self._wait(q, self._deps(reads, writes))
        if owner.dsem is None:
            self.uid += 1
            owner.dsem = self.stack.enter_context(self.nc.semaphore(f"d_{owner.name}_{self.uid}"))
            self.dsems[f"{owner.name}_{self.uid}"] = owner
        inst = self.eng[q].dma_start(out=out, in_=in_, **kw)
        owner.dcount += 16
        inst.then_inc(owner.dsem, 16)
        ev = (owner.dsem, owner.dcount)
        self._commit(ev, reads, writes)
        if final:
            self.finals[id(owner)] = owner
        return ev

    def barrier(self, force=False):
        if KB.dead and not force:
            return
        for ek in self.eng:
            e = self.eng[ek]
            seen = self.seen[ek]
            for k2 in ("pe", "act", "dve", "pool"):
                s, v = self.sems[k2], self.cnt[k2]
                if v > 0 and seen.get(s, 0) < v:
                    e.wait_ge(s, v)
                    seen[s] = v
            for o in self.dsems.values():
                if o.dcount > 0 and seen.get(o.dsem, 0) < o.dcount:
                    e.wait_ge(o.dsem, o.dcount)
                    seen[o.dsem] = o.dcount

    def finish(self):
        self.barrier(force=True)


def _tables():
    t = {}
    pos = np.arange(S, dtype=np.float64)
    afreq = (10000.0 ** (-np.arange(0, 64, 2, dtype=np.float32) / np.float32(64))).astype(np.float32).astype(np.float64)
    rfreq = (10000.0 ** (-np.linspace(0.0, 1.0, 32, dtype=np.float32))).astype(np.float32).astype(np.float64)

    def tm(fn, fr, p):
        a = (p.astype(np.float32)[:, None] * fr.astype(np.float32)[None, :]).astype(np.float32).astype(np.float64)
        v = fn(a).astype(np.float32)
        return v
    for nm, fr in (("a", afreq), ("r", rfreq)):
        c = tm(np.cos, fr, pos).reshape(NT, 128, 32).transpose(1, 0, 2)
        s = tm(np.sin, fr, pos).reshape(NT, 128, 32).transpose(1, 0, 2)
        t["cos_" + nm] = np.ascontiguousarray(c)
        t["sin_" + nm] = np.ascontiguousarray(s)
        ps = PAST + (np.arange(64) % 4).astype(np.float64)
        t["cos_s" + nm] = np.ascontiguousarray(tm(np.cos, fr, ps))
        t["sin_s" + nm] = np.ascontiguousarray(tm(np.sin, fr, ps))
    h = np.arange(4, dtype=np.float64)
    log_g = np.log1p(-(2.0 ** (-5.0 - h)))
    idx = np.arange(128, dtype=np.float64)
    zeta = np.exp((127.0 - idx)[:, None] * log_g[None, :])
    t["zeta"] = zeta.astype(np.float32)
    xi = np.exp((idx + 1.0)[None, :] * log_g[:, None])
    xit = np.zeros((128, 2, 128), np.float32)
    gc = np.zeros((128, 2), np.float32)
    for c in range(2):
        for hh in range(2):
            xit[hh * 64:(hh + 1) * 64, c, :] = xi[2 * c + hh][None, :]
            gc[hh * 64:(hh + 1) * 64, c] = np.exp(128.0 * log_g[2 * c + hh])
    t["xit"] = xit
    t["gc"] = gc
    diff = idx[None, :] - idx[:, None]
    intra = np.zeros((128, 4, 128), np.float32)
    for hd in range(4):
        intra[:, hd, :] = np.where(diff >= 0, np.exp(np.maximum(diff, 0.0) * log_g[hd]), 0.0)
    t["intra"] = intra
    tok = np.arange(64)
    tb, tt = tok // 4, tok % 4
    zs = np.exp((3.0 - tt)[:, None] * log_g[None, :])
    t["zeta_s"] = zs.astype(np.float32)
    xis = np.exp((tt + 1.0)[None, :] * log_g[:, None])
    xits = np.zeros((128, 2, 64), np.float32)
    gcs = np.zeros((128, 2), np.float32)
    for c in range(2):
        for hh in range(2):
            xits[hh * 64:(hh + 1) * 64, c, :] = xis[2 * c + hh][None, :]
            gcs[hh * 64:(hh + 1) * 64, c] = np.exp(4.0 * log_g[2 * c + hh])
    t["xit_s"] = xits
    t["gc_s"] = gcs
    ds = tt[None, :] - tt[:, None]
    same = tb[None, :] == tb[:, None]
    intras = np.zeros((64, 4, 64), np.float32)
    for hd in range(4):
        intras[:, hd, :] = np.where(same & (ds >= 0), np.exp(np.maximum(ds, 0) * log_g[hd]), 0.0)
    t["intra_s"] = intras
    onehot = np.zeros((64, 16), np.float32)
    onehot[tok, tb] = 1.0
    t["onehot_s"] = onehot
    m = np.zeros((128, 256), np.float32)
    jj = idx[:, None]
    ii = idx[None, :]
    m[:, :128] = (ii >= jj)
    m[:, 128:] = (ii <= jj)
    t["amask"] = m.astype(ml_dtypes.bfloat16)
    ms = np.zeros((128, 9, 4), np.float32)
    for tq in range(4):
        ms[:, 0, tq] = (idx >= tq)
        ms[:, 1 + tq, tq] = 1.0
        ms[:, 5 + tq, tq] = 1.0
    t["smask"] = ms.astype(ml_dtypes.bfloat16)
    mn = np.zeros((64, 16, 4), np.float32)
    for j in range(64):
        for tq in range(4):
            if tt[j] < tq:
                mn[j, tb[j], tq] = 1.0
            elif tt[j] == tq:
                mn[j, tb[j], tq] = 3.0
    t["nmask"] = mn.astype(ml_dtypes.bfloat16)
    t["ident_b"] = np.eye(128, dtype=np.float32).astype(ml_dtypes.bfloat16)
    t["ident_f"] = np.eye(128, dtype=np.float32)
    return t


_TAB_DT = {"amask": BF16, "smask": BF16, "nmask": BF16, "ident_b": BF16}


class _Stop(Exception):
    pass


def build(NB=4, NSB=16, do_sample=True, stop=99, nb_run=None):
    def chk(stage):
        if stop <= stage:
            KB.dead = True
    KB.dead = False

    Buf._cache = {}
    nc = bass.Bass("TRN2", target_bir_lowering=False)
    tabs = _tables()

    def din(name, shape, dt=F32):
        return nc.dram_tensor(name, list(shape), dt, kind="ExternalInput").ap()

    def dout(name, shape):
        return nc.dram_tensor(name, list(shape), F32, kind="ExternalOutput").ap()

    xp = din("xp", [NB, S, D])
    xs = din("xs", [64, D])
    ck = din("ck", [16, S, 512])
    cv = din("cv", [16, S, 512])
    sr = din("sr", [16, 4, 64, 128])
    call = din("call", [68, D])
    w_ada = din("w_ada", [D, 6 * D])
    b_ada = din("b_ada", [6 * D])
    n1g = din("n1g", [D])
    w_in = din("w_in", [D, 3072])
    w_out = din("w_out", [D, D])
    n2g = din("n2g", [D])
    w_gate = din("w_gate", [D, DFF])
    w_up = din("w_up", [D, DFF])
    w_down = din("w_down", [DFF, D])
    fgd = din("fg", [D])
    td = {k: din("t_" + k, v.shape, _TAB_DT.get(k, F32)) for k, v in tabs.items()}

    yp = dout("yp", [NB, S, D])
    ys = dout("ys", [64, D])
    kp = dout("kp", [NB, S, 512])
    vp = dout("vp", [NB, S, 512])
    rp = dout("rp", [NB, 4, 64, 128])
    kso = dout("kso", [64, 512])
    vso = dout("vso", [64, 512])
    rso = dout("rso", [16, 4, 64, 128])
    sc_gu = nc.dram_tensor("sc_gu", [NJ, 128, 2048], BF16, kind="Internal").ap()
    sc_d = nc.dram_tensor("sc_d", [NJ, 128, 1024], BF16, kind="Internal").ap()
    vbs = nc.dram_tensor("vbs", [NB, S, 512], BF16, kind="Internal").ap()
    sc_wi = nc.dram_tensor("sc_wi", [8, 128, 3072], BF16, kind="Internal").ap()

    with contextlib.ExitStack() as st:
        k = KB(nc, st)
        try:
            PB = [k.ps(f"pb{i}", [128, 512], F32) for i in range(8)]
            bPB = [Buf(f"pb{i}") for i in range(8)]

            def pbf(i):
                return PB[i][:].bitcast(BF16)

            bWi = Buf("Wi")
            bscwi = Buf("scwi")
            Wo = k.sb("Wo", [128, 8, 1024], BF16); bWo = Buf("Wo")
            modT = k.sb("modT", [128, 48, 68], F32); bmodT = Buf("modT")
            G1T = k.sb("G1T", [128, 8, 68], F32); bG1T = Buf("G1T")
            G2T = k.sb("G2T", [128, 8, 68], F32); bG2T = Buf("G2T")
            bgates = Buf("gates")
            bFG = Buf("FG")
            mixT = k.sb("mixT", [128, 8, S], BF16); bmixT = Buf("mixT")
            T = {}
            bT = Buf("tabs")
            for nm in ("cos_a", "sin_a", "cos_r", "sin_r", "zeta", "xit", "gc", "intra", "amask",
                       "ident_b", "ident_f"):
                T[nm] = k.sb("T_" + nm, tabs[nm].shape, _TAB_DT.get(nm, F32))
                k.dma("sp", T[nm][:], td[nm], [], [bT], bT)
            ss_t = k.sb("ss_t", [128, 8], F32); bss = Buf("ss")
            epsT = k.sb("epsT", [128, 1], F32)
            k.op("dve", lambda e: e.memset(epsT[:], EPS), [], [bT])
            Rf = k.sb("Rf", [128, 2, 128], F32); bRf = Buf("Rf")
            Rb = k.sb("Rb", [128, 2, 128], BF16); bRb = Buf("Rb")

            for dc in range(8):
                k.dma("pool", Wo[:, dc, :], w_out[dc * 128:(dc + 1) * 128, :], [], [bWo], bWo)

            def rms_stats(xt, bxt, npart, col, jk, bjk, width=1024, scale=1.0 / 1024):
                k.op("dve", lambda e: e.memset(ss_t[:npart, col:col + 1], 0.0), [], [bss])
                k.op("act", lambda e: e.activation(jk[:npart, 0:width], xt, AF.Square, accum_out=ss_t[:npart, col:col + 1]), [bxt], [bjk, bss])
                k.op("act", lambda e: e.activation(ss_t[:npart, col:col + 1], ss_t[:npart, col:col + 1], AF.Sqrt, bias=epsT[:npart, :], scale=scale), [bss, bT], [bss])
                k.op("dve", lambda e: e.reciprocal(ss_t[:npart, col:col + 1], ss_t[:npart, col:col + 1]), [bss], [bss])

            with contextlib.ExitStack() as s0:
                Wi0 = k.sb("Wi0", [128, 8, 3072], BF16, s0)
                for dc in range(8):
                    k.dma("pool", Wi0[:, dc, :], w_in[dc * 128:(dc + 1) * 128, :], [], [bWi], bWi)
                k.op("dve", lambda e: e.tensor_scalar_mul(Wi0[:, :, 256:512], Wi0[:, :, 256:512], 0.125), [bWi], [bWi])
                for dc in range(8):
                    k.dma("pool", sc_wi[dc], Wi0[:, dc, :], [bWi], [bscwi], bWi)
                cin = k.sb("cin", [68, D], F32, s0); bcin = Buf("cin")
                cT = k.sb("cT", [128, 8, 68], F32, s0); bcT = Buf("cT")
                badT = k.sb("badT", [128, 48], F32, s0); bbad = Buf("badT")
                n1T = k.sb("n1T", [128, 8], F32, s0)
                n2T = k.sb("n2T", [128, 8], F32, s0)
                wst = [k.sb(f"wst{i}", [128, 8, 512], F32, s0) for i in range(2)]
                bwst = [Buf(f"wst{i}") for i in range(2)]
                k.dma("sp", cin[:], call, [], [bcin], bcin)
                k.dma("sp", badT[:], b_ada.rearrange("(c p) -> p c", p=128), [], [bbad], bbad, allow_slow_non_contiguous=True)
                k.dma("sp", n1T[:], n1g.rearrange("(c p) -> p c", p=128), [], [bbad], bbad, allow_slow_non_contiguous=True)
                k.dma("sp", n2T[:], n2g.rearrange("(c p) -> p c", p=128), [], [bbad], bbad, allow_slow_non_contiguous=True)
                k.op("act", lambda e: e.activation(cin[:], cin[:], AF.Silu), [bcin], [bcin])
                for c in range(8):
                    pbi = c // 4
                    k.tr(PB[pbi][:, (c % 4) * 68:(c % 4 + 1) * 68], cin[:, c * 128:(c + 1) * 128], T["ident_f"][0:68, 0:68],
                         [bcin, bT], [bPB[pbi]], last=(c % 4 == 3))
                for pbi in range(2):
                    k.op("dve", lambda e, pbi=pbi: e.tensor_copy(cT[:, pbi * 4:(pbi + 1) * 4, :], PB[pbi][:, 0:4 * 68].rearrange("p (c n) -> p c n", n=68)),
                         [bPB[pbi]], [bcT])
                for blk in range(12):
                    ws, bws = wst[blk % 2], bwst[blk % 2]
                    k.dma("sp", ws[:], w_ada[:, blk * 512:(blk + 1) * 512].rearrange("(c p) n -> p c n", p=128), [], [bws], bws)
                    pb = 2 + (blk % 2)
                    for fc in range(4):
                        for dc in range(8):
                            k.mm(PB[pb][:, fc * 68:(fc + 1) * 68], ws[:, dc, fc * 128:(fc + 1) * 128], cT[:, dc, :],
                                 [bws, bcT], [bPB[pb]], start=(dc == 0), stop=(dc == 7), last=(dc == 7 and fc == 3))
                    for fc in range(4):
                        ch = blk * 4 + fc
                        k.op("act", lambda e, ch=ch, fc=fc, pb=pb: e.activation(modT[:, ch, :], PB[pb][:, fc * 68:(fc + 1) * 68], AF.Identity,
                                                                                 bias=badT[:, ch:ch + 1]), [bPB[pb], bbad], [bmodT])
                for c in range(8):
                    k.op("dve", lambda e, c=c: e.tensor_scalar(G1T[:, c, :], modT[:, 8 + c, :], 1.0, n1T[:, c:c + 1], op0=ALU.add, op1=ALU.mult),
                         [bmodT, bbad], [bG1T])
                    k.op("dve", lambda e, c=c: e.tensor_scalar(G2T[:, c, :], modT[:, 32 + c, :], 1.0, n2T[:, c:c + 1], op0=ALU.add, op1=ALU.mult),
                         [bmodT, bbad], [bG2T])
                stg = [k.sb(f"stg{i}", [128, 2, 8, 128], BF16, s0) for i in range(2)]
                std = [k.sb(f"std{i}", [128, 1024], BF16, s0) for i in range(2)]
                bstg = [Buf(f"stg{i}") for i in range(2)]
                bstd = [Buf(f"std{i}") for i in range(2)]
                bsc = [Buf(f"sc{j}") for j in range(NJ)]
                for j in range(NJ):
                    sg_, bsg_ = stg[j % 2], bstg[j % 2]
                    sd_, bsd_ = std[j % 2], bstd[j % 2]
                    k.dma("pool", sg_[:, 0, :, :], w_gate[:, j * 128:(j + 1) * 128].rearrange("(c p) n -> p c n", p=128), [], [bsg_], bsg_)
                    k.dma("pool", sg_[:, 1, :, :], w_up[:, j * 128:(j + 1) * 128].rearrange("(c p) n -> p c n", p=128), [], [bsg_], bsg_)
                    k.dma("pool", sd_[:], w_down[j * 128:(j + 1) * 128, :], [], [bsd_], bsd_)
                    k.dma("pool", sc_gu[j], sg_[:].rearrange("p a c n -> p (a c n)"), [bsg_], [bsc[j]], bsg_)
                    k.dma("pool", sc_d[j], sd_[:], [bsd_], [bsc[j]], bsd_)
                k.barrier()
            chk(0)

            def ffn_group(s1, nt, ntok, X1, bX1, h2T, bh2T, ffT, bffT, ring, bring, g2tile, bg2, out_fn, FG, yst, byst, jk, bjk):
                for j in range(NJ):
                    slot = j % len(ring)
                    gu, dd = ring[slot]
                    bsl = bring[slot]
                    k.dma("sp", gu[:].rearrange("p a c n -> p (a c n)"), sc_gu[j], [bsc[j]], [bsl], bsl)
                    for a in range(2):
                        for dc in range(8):
                            k.mm(PB[a][:, 0:ntok], gu[:, a, dc, :], h2T[:, dc, 0:ntok], [bsl, bh2T], [bPB[a]],
                                 start=(dc == 0), stop=(dc == 7), last=(dc == 7))
                    k.op("act", lambda e, j=j: e.activation(ffT[:, j, 0:ntok], PB[0][:, 0:ntok], AF.Silu), [bPB[0]], [bffT])
                    k.op("dve", lambda e, j=j: e.tensor_tensor(ffT[:, j, 0:ntok], ffT[:, j, 0:ntok], PB[1][:, 0:ntok], ALU.mult),
                         [bffT, bPB[1]], [bffT])
                for j in range(NJ):
                    slot = j % len(ring)
                    gu, dd = ring[slot]
                    bsl = bring[slot]
                    k.dma("sp", dd[:], sc_d[j], [bsc[j]], [bsl], bsl)
                    for ti in range(nt):
                        np_ = min(128, ntok - ti * 128)
                        for nb in range(2):
                            k.mm(PB[2 * ti + nb][:np_, :], ffT[:, j, ti * 128:ti * 128 + np_], dd[:, nb * 512:(nb + 1) * 512],
                                 [bsl, bffT], [bPB[2 * ti + nb]], start=(j == 0), stop=(j == NJ - 1), last=(j == NJ - 1 or (ti == nt - 1 and nb == 1)))
                for ti in range(nt):
                    np_ = min(128, ntok - ti * 128)
                    x1 = X1[ti]
                    for nb in range(2):
                        cs = slice(nb * 512, (nb + 1) * 512)
                        k.op("dve", lambda e, nb=nb, cs=cs: e.tensor_tensor(yst[:np_, cs], PB[2 * ti + nb][:np_, :], g2tile[:np_, cs], ALU.mult),
                             [bPB[2 * ti + nb], bg2], [byst])
                    k.op("dve", lambda e: e.tensor_tensor(yst[:np_, :], yst[:np_, :], x1[:np_, :], ALU.add), [byst, bX1[ti]], [byst])
                    rms_stats(yst[:np_, :], byst, np_, 3, jk, bjk)
                    k.op("dve", lambda e: e.scalar_tensor_tensor(yst[:np_, :], yst[:np_, :], ss_t[:np_, 3:4], FG[:np_, :], op0=ALU.mult, op1=ALU.mult),
                         [byst, bss, bFG], [byst])
                    out_fn(ti, yst, byst, np_)


            for b in range(NB if nb_run is None else nb_run):
                bvp = Buf(f"vp{b}")
                with contextlib.ExitStack() as sa:
                    Wi = k.sb("Wi", [128, 8, 3072], BF16, sa)
                    Xt = [k.sb(f"Xt{i}", [128, 1024], F32, sa) for i in range(2)]
                    bXt = [Buf(f"Xt{i}") for i in range(2)]
                    for i in range(2):
                        k.dma("sp", Xt[i][:], xp[b, i * 128:(i + 1) * 128, :], [], [bXt[i]], bXt[i])
                    bWic = [Buf(f"Wic{i}") for i in range(6)]
                    for i in range(6):
                        k.dma("sp", Wi[:, :, i * 512:(i + 1) * 512], sc_wi[:, :, i * 512:(i + 1) * 512].rearrange("c p n -> p c n"),
                              [bscwi], [bWic[i]], bWic[i])
                    xn = k.sb("xn", [128, 1024], BF16, sa); bxn = Buf("xn")
                    hTs_ = [k.sb(f"hT{i}", [128, 8, 128], BF16, sa) for i in range(2)]; bhTs_ = [Buf(f"hT{i}") for i in range(2)]
                    Rb1 = k.sb("Rb1", [128, 2, 128], BF16, sa); bRb1 = Buf("Rb1")
                    avb = k.sb("avb", [128, 512], BF16, sa); bavb = Buf("avb")
                    Rbs = [Rb, Rb1]; bRbs = [bRb, bRb1]
                    rqk = k.sb("rqk", [128, 512], BF16, sa); brqk = Buf("rqk")
                    rt = [k.sb(f"rt{i}", [128, 8, 32], F32, sa) for i in range(4)]
                    brt = Buf("rt")
                    kz = k.sb("kz", [128, 256], BF16, sa); bkz = Buf("kz")
                    rvb = k.sb("rvb", [128, 512], BF16, sa); brvb = Buf("rvb")
                    sgt = k.sb("sgt", [128, 512], BF16, sa); bsgt = Buf("sgt")
                    rqkT = k.sb("rqkT", [128, 4, 128], BF16, sa); brqkT = Buf("rqkT")
                    rqm = k.sb("rqm", [128, 2, 2, 128], BF16, sa); brqm = Buf("rqm")
                    rqxT = k.sb("rqxT", [128, 2, 2, 128], BF16, sa); brqxT = Buf("rqxT")
                    scm = k.sb("scm", [128, 512], BF16, sa); bscm = Buf("scm")
                    rety = k.sb("rety", [128, 512], BF16, sa); brety = Buf("rety")
                    aqkf = [k.sb(f"aqkf{i}", [128, 512], F32, sa) for i in range(1)]
                    baqkf = [Buf(f"aqkf{i}") for i in range(1)]
                    aqkb = k.sb("aqkb", [128, 1024], BF16, sa); baqkb = Buf("aqkb")
                    avf = [k.sb(f"avf{i}", [128, 512], F32, sa) for i in range(1)]
                    bavf = [Buf(f"avf{i}") for i in range(1)]
                    aqkT = k.sb("aqkT", [128, 8, S], BF16, sa); baqkT = Buf("aqkT")
                    Xacc = k.sb("Xacc", [128, S], F32, sa); bXacc = Buf("Xacc")
                    Va = [k.sb(f"Va{i}", [128, 128], BF16, sa) for i in range(12)]
                    bVa = [Buf(f"Va{i}") for i in range(12)]
                    Et = [k.sb(f"Et{i}", [128, 256], BF16, sa) for i in range(5)]
                    bEt = [Buf(f"Et{i}") for i in range(5)]
                    rct = k.sb("rct", [128, 256], F32, sa); brct = Buf("rct")
                    for i in range(len(Va)):
                        k.op("dve", lambda e, i=i: e.memset(Va[i][:], 1.0), [], [bVa[i]])
                    k.op("dve", lambda e: e.memset(rqm[:], 0.0), [], [brqm])
                    k.op("dve", lambda e: e.memset(Rf[:], 0.0), [], [bRf])
                    k.op("dve", lambda e: e.memset(Rb[:], 0.0), [], [bRb])
                    k.op("dve", lambda e: e.memset(Rb1[:], 0.0), [], [bRb1])

                    ZB = [0, 1, 2, 3, 4, 1]
                    STB = 5
                    TPB = 6
                    RB = 7

                    def xload(t):
                        X, bX = Xt[t % 2], bXt[t % 2]
                        k.dma("sp", X[:], xp[b, t * 128:(t + 1) * 128, :], [], [bX], bX)

                    def s1(t):
                        X, bX = Xt[t % 2], bXt[t % 2]
                        hT, bhT = hTs_[t % 2], bhTs_[t % 2]
                        rms_stats(X[:], bX, 128, 0, aqkb, baqkb)
                        k.op("dve", lambda e: e.tensor_scalar_mul(xn[:], X[:], ss_t[:, 0:1]), [bX, bss], [bxn])
                        tp = pbf(TPB)
                        for c in range(8):
                            k.tr(tp[:, c * 128:(c + 1) * 128], xn[:, c * 128:(c + 1) * 128], T["ident_b"][:], [bxn, bT], [bPB[TPB]], last=(c == 7))
                        k.op("dve", lambda e: e.tensor_tensor(hT[:], tp[:].rearrange("p (c n) -> p c n", n=128),
                                                              G1T[:, :, 64 + b:65 + b].to_broadcast([128, 8, 128]), ALU.mult), [bPB[TPB], bG1T], [bhT])
                        k.op("dve", lambda e: e.tensor_tensor(hT[:], hT[:], modT[:, 0:8, 64 + b:65 + b].to_broadcast([128, 8, 128]), ALU.add), [bhT, bmodT], [bhT])

                    def zc(t, i):
                        hT, bhT = hTs_[t % 2], bhTs_[t % 2]
                        for dc in range(8):
                            k.mm(PB[ZB[i]][:], hT[:, dc, :], Wi[:, dc, i * 512:(i + 1) * 512], [bhT, bWic[i]], [bPB[ZB[i]]],
                                 start=(dc == 0), stop=(dc == 7), last=(dc == 7))

                    brts_ = [Buf(f"rt{i}") for i in range(4)]

                    def rot(src, bsrc, dst, bdst, cosn, sinn, t, bdst2=None):
                        zv = src.rearrange("p (h two f) -> p h two f", two=2, f=32)
                        x1v, x2v = zv[:, :, 0, :], zv[:, :, 1, :]
                        ov = dst.rearrange("p (h two f) -> p h two f", two=2, f=32)
                        cb = T[cosn][:, t, :].unsqueeze(1).to_broadcast([128, 8, 32])
                        sb_ = T[sinn][:, t, :].unsqueeze(1).to_broadcast([128, 8, 32])
                        r0, r1_, r2, r3 = (rt[i][:, 0:8, :] for i in range(4))
                        k.op("dve", lambda e: e.tensor_tensor(r0, x1v, cb, ALU.mult), [bsrc, bT], [brts_[0]])
                        k.op("dve", lambda e: e.tensor_tensor(r1_, x2v, sb_, ALU.mult), [bsrc, bT], [brts_[1]])
                        k.op("pool", lambda e: e.tensor_tensor(ov[:, :, 0, :], r0, r1_, ALU.subtract), [brts_[0], brts_[1]], [bdst])
                        k.op("dve", lambda e: e.tensor_tensor(r2, x1v, sb_, ALU.mult), [bsrc, bT], [brts_[2]])
                        k.op("dve", lambda e: e.tensor_tensor(r3, x2v, cb, ALU.mult), [bsrc, bT], [brts_[3]])
                        k.op("pool", lambda e: e.tensor_tensor(ov[:, :, 1, :], r2, r3, ALU.add), [brts_[2], brts_[3]], [bdst2 if bdst2 is not None else bdst])

                    def r1(t):
                        rot(PB[ZB[0]][:], bPB[ZB[0]], rqk[:], brqk, "cos_r", "sin_r", t)
                        k.op("pool", lambda e: e.tensor_tensor(kz[:].rearrange("p (h f) -> p h f", f=64),
                                                               rqk[:, 256:512].rearrange("p (h f) -> p h f", f=64),
                                                               T["zeta"][:].unsqueeze(2).to_broadcast([128, 4, 64]), ALU.mult), [brqk, bT], [bkz])
                        k.op("act", lambda e: e.activation(rvb[:], PB[ZB[1]][:], AF.Copy), [bPB[ZB[1]]], [brvb])
                        k.op("act", lambda e: e.activation(sgt[:], PB[ZB[2]][:], AF.Silu), [bPB[ZB[2]]], [bsgt])
                        tp = pbf(TPB)
                        for c in range(4):
                            k.tr(tp[:, c * 128:(c + 1) * 128], rqk[:, c * 128:(c + 1) * 128], T["ident_b"][:], [brqk, bT], [bPB[TPB]], last=(c == 3))
                        k.op("act", lambda e: e.activation(rqkT[:].rearrange("p c n -> p (c n)"), tp[:, 0:512], AF.Copy), [bPB[TPB]], [brqkT])
                        k.op("act", lambda e: e.activation(rqm[0:64, 0, :, :].rearrange("p c n -> p (c n)"), tp[0:64, 0:256], AF.Copy), [bPB[TPB]], [brqm])
                        k.op("act", lambda e: e.activation(rqm[64:128, 1, :, :].rearrange("p c n -> p (c n)"), tp[64:128, 0:256], AF.Copy), [bPB[TPB]], [brqm])
                        for vv in range(2):
                            k.op("pool", lambda e, vv=vv: e.tensor_tensor(rqxT[:, vv, :, :], rqm[:, vv, :, :], T["xit"][:], ALU.mult), [brqm, bT], [brqxT])

                    def a1a(t):
                        av, bav = avf[0], bavf[0]
                        k.op("act", lambda e: e.activation(av[:], PB[ZB[5]][:], AF.Copy), [bPB[ZB[5]]], [bav])
                        k.dma("pool", vp[b, t * 128:(t + 1) * 128, :], av[:], [bav], [], bav, final=True)
                        k.op("act", lambda e: e.activation(avb[:], PB[ZB[5]][:], AF.Copy), [bPB[ZB[5]]], [bavb])
                        k.dma("pool", vbs[b, t * 128:(t + 1) * 128, :], avb[:], [bavb], [bvp], bavb)
                        rot(PB[ZB[3]][:], bPB[ZB[3]], aqkb[:, 0:512], baqkb, "cos_a", "sin_a", t)

                    def a1b(t):
                        af, baf = aqkf[0], baqkf[0]
                        rot(PB[ZB[4]][:], bPB[ZB[4]], af[:, 0:512], baf, "cos_a", "sin_a", t)
                        k.dma("pool", kp[b, t * 128:(t + 1) * 128, :], af[:, 0:512], [baf], [], baf, final=True)
                        k.op("act", lambda e: e.activation(aqkb[:, 512:1024], af[:, 0:512], AF.Copy), [baf], [baqkb])

                    def r2(t):
                        for hd in range(4):
                            c, hh = hd // 2, hd % 2
                            k.mm(PB[RB][:, hd * 128:(hd + 1) * 128], rqkT[:, 2 + c, :], rqm[:, hh, c, :], [brqkT, brqm], [bPB[RB]], last=(hd == 3))
                        k.op("dve", lambda e: e.tensor_tensor(scm[:], PB[RB][:], T["intra"][:].rearrange("p h n -> p (h n)"), ALU.mult), [bPB[RB], bT], [bscm])

                    def r3a(t):
                        for hd in range(4):
                            c, hh = hd // 2, hd % 2
                            cs = slice(hd * 128, (hd + 1) * 128)
                            k.mm(PB[RB][:, cs], scm[:, cs], rvb[:, cs], [bscm, brvb], [bPB[RB]], start=True, stop=False, last=False)
                            k.mm(PB[RB][:, cs], rqxT[:, hh, c, :], Rbs[t % 2][:, c, :], [brqxT, bRbs[t % 2]], [bPB[RB]], start=False, stop=True, last=(hd == 3))
                        for hd in range(4):
                            cs = slice(hd * 128, (hd + 1) * 128)
                            k.op("dve", lambda e, hd=hd: e.memset(ss_t[:, 4 + hd:5 + hd], 0.0), [], [bss])
                            k.op("act", lambda e, hd=hd, cs=cs: e.activation(scm[:, cs], PB[RB][:, cs], AF.Square, accum_out=ss_t[:, 4 + hd:5 + hd]), [bPB[RB]], [bscm, bss])
                        k.op("act", lambda e: e.activation(ss_t[:, 4:8], ss_t[:, 4:8], AF.Sqrt, bias=epsT[:], scale=1.0 / 128), [bss, bT], [bss])
                        k.op("dve", lambda e: e.reciprocal(ss_t[:, 4:8], ss_t[:, 4:8]), [bss], [bss])
                        for hd in range(4):
                            cs = slice(hd * 128, (hd + 1) * 128)
                            k.op("dve", lambda e, hd=hd, cs=cs: e.scalar_tensor_tensor(rety[:, cs], PB[RB][:, cs], ss_t[:, 4 + hd:5 + hd], sgt[:, cs],
                                                                                        op0=ALU.mult, op1=ALU.mult), [bPB[RB], bss, bsgt], [brety])

                    def r3b(t):
                        for c in range(2):
                            k.mm(PB[STB][:, c * 256:(c + 1) * 256], kz[:, c * 128:(c + 1) * 128], rvb[:, c * 256:(c + 1) * 256], [bkz, brvb], [bPB[STB]], last=(c == 1))
                        for c in range(2):
                            for hh in range(2):
                                rows = slice(hh * 64, (hh + 1) * 64)
                                k.op("dve", lambda e, c=c, hh=hh, rows=rows: e.scalar_tensor_tensor(
                                    Rf[rows, c, :], Rf[rows, c, :], T["gc"][rows, c:c + 1], PB[STB][rows, c * 256 + hh * 128:c * 256 + (hh + 1) * 128],
                                    op0=ALU.mult, op1=ALU.add), [bRf, bT, bPB[STB]], [bRf])
                        k.op("act", lambda e: e.activation(Rbs[(t + 1) % 2][:], Rf[:], AF.Copy), [bRf], [bRbs[(t + 1) % 2]])

                    def r4(t):
                        tp = pbf(TPB)
                        for c in range(4):
                            k.tr(tp[:, c * 128:(c + 1) * 128], rety[:, c * 128:(c + 1) * 128], T["ident_b"][:], [brety, bT], [bPB[TPB]], last=(c == 3))
                        k.op("act", lambda e: e.activation(mixT[:, 0:4, t * 128:(t + 1) * 128], tp[:, 0:512].rearrange("p (c n) -> p c n", n=128), AF.Copy),
                             [bPB[TPB]], [bmixT])

                    def a2(t):
                        tp = pbf(TPB)
                        for c in range(8):
                            k.tr(tp[:, c * 128:(c + 1) * 128], aqkb[:, c * 128:(c + 1) * 128], T["ident_b"][:], [baqkb, bT], [bPB[TPB]], last=(c == 7))
                        k.op("act", lambda e: e.activation(aqkT[:, :, t * 128:(t + 1) * 128], tp[:].rearrange("p (c n) -> p c n", n=128), AF.Copy),
                             [bPB[TPB]], [baqkT])

                    s1(0)
                    for i in range(5):
                        zc(0, i)
                    for t in range(NT):
                        nxt = t + 1 < NT
                        if t + 2 < NT:
                            xload(t + 2)
                        if nxt:
                            s1(t + 1)
                        r1(t)
                        zc(t, 5)
                        a1a(t)
                        r2(t)
                        if nxt:
                            zc(t + 1, 0)
                        a1b(t)
                        if nxt:
                            zc(t + 1, 1)
                        r3a(t)
                        if nxt:
                            zc(t + 1, 2)
                            zc(t + 1, 3)
                        r3b(t)
                        if nxt:
                            zc(t + 1, 4)
                        r4(t)
                        a2(t)
                        chk(1)
                    chk(2)
                    k.dma("sp", rp[b].rearrange("(c hh) kk v -> (hh kk) c v", hh=2), Rf[:], [bRf], [], bRf, final=True)

                    its = []
                    for hd in range(8):
                        for (dil, ntile_sub) in ((16, 1), (4, 4), (1, 16)):
                            for a in range(16):
                                its.append((hd, dil, ntile_sub, a))
                    hbS = [bPB[i] for i in range(4)]
                    hbX = [bPB[4 + i] for i in range(4)]
                    LA = 3
                    LV = 2
                    assert LA + LV < len(Va) // 2 and LA + 1 < len(Et)
                    NVA = len(Va) // 2

                    def prm(i):
                        hd, dil, ntile_sub, a = its[i]
                        c, hh = hd // 2, hd % 2
                        sub, nbk = a // ntile_sub, a % ntile_sub
                        start = dil * 128 * nbk + sub
                        nq = 256 if nbk < ntile_sub - 1 else 128
                        kc = slice(start, start + 127 * dil + 1, dil)
                        qc = slice(start, start + (nq - 1) * dil + 1, dil)
                        rows = slice(hh * 64, (hh + 1) * 64)
                        vi = hh * NVA + (i % NVA)
                        return hd, dil, c, hh, nq, kc, qc, rows, vi

                    def stV(i):
                        hd, dil, c, hh, nq, kc, qc, rows, vi = prm(i)
                        k.dma("sp", Va[vi][:, hh * 64:(hh + 1) * 64], vbs[b, kc, hd * 64:(hd + 1) * 64], [bvp], [bVa[vi]], bVa[vi])

                    def stS(i):
                        hd, dil, c, hh, nq, kc, qc, rows, vi = prm(i)
                        ps_s = PB[i % 4][:, 0:nq]
                        et, bet = Et[i % len(Et)], bEt[i % len(Et)]
                        k.mm(ps_s, aqkT[rows, 4 + c, kc], aqkT[rows, c, qc], [baqkT], [hbS[i % 4]])
                        k.op("act", lambda e: e.activation(et[:, 0:nq], ps_s, AF.Exp, scale=0.125), [hbS[i % 4]], [bet])
                        k.op("pool", lambda e: e.tensor_tensor(et[:, 0:nq], et[:, 0:nq], T["amask"][:, 0:nq], ALU.mult), [bet, bT], [bet])

                    def stX(i):
                        hd, dil, c, hh, nq, kc, qc, rows, vi = prm(i)
                        ps_x = PB[4 + i % 4][:, 0:nq]
                        et, bet = Et[i % len(Et)], bEt[i % len(Et)]
                        k.mm(ps_x, Va[vi][:], et[:, 0:nq], [bVa[vi], bet], [hbX[i % 4]])
                        if dil == 16:
                            k.op("act", lambda e: e.activation(Xacc[:, qc], ps_x, AF.Copy), [hbX[i % 4]], [bXacc])
                        else:
                            k.op("dve", lambda e: e.tensor_tensor(Xacc[:, qc], Xacc[:, qc], ps_x, ALU.add), [bXacc, hbX[i % 4]], [bXacc])
                        if i % 48 == 47:
                            drows = slice((1 - hh) * 64, (2 - hh) * 64)
                            for pc in range(8):
                                cs = slice(pc * 256, (pc + 1) * 256)
                                k.op("dve", lambda e: e.reciprocal(rct[rows, :], Xacc[drows, cs]), [bXacc], [brct])
                                k.op("dve", lambda e: e.tensor_tensor(mixT[rows, 4 + c, cs], Xacc[rows, cs], rct[rows, :], ALU.mult), [bXacc, brct], [bmixT])

                    n_it = len(its)
                    for i in range(min(LV, n_it)):
                        stV(i)
                    for i in range(n_it + LA):
                        if i + LV < n_it:
                            stV(i + LV)
                        if i < n_it:
                            stS(i)
                        if i - LA >= 0:
                            stX(i - LA)
                    k.barrier()
                chk(3)

                with contextlib.ExitStack() as sc_:
                    gates = k.sb("gates", [128, 2, 1024], F32, sc_)
                    FG = k.sb("FG", [128, 1024], F32, sc_)
                    k.dma("sp", FG[:], fgd.unsqueeze(0).to_broadcast([128, 1024]), [], [bFG], bFG)
                    for gi, vec in enumerate((2, 5)):
                        for c in range(8):
                            pbi = gi * 2 + c // 4
                            k.mm(PB[pbi][:, (c % 4) * 128:(c % 4 + 1) * 128],
                                 modT[:, vec * 8 + c, 64 + b:65 + b].to_broadcast([128, 128]), T["ident_f"][:],
                                 [bmodT, bT], [bPB[pbi]], last=(c % 4 == 3))
                        for hf in range(2):
                            k.op("act", lambda e, gi=gi, hf=hf: e.activation(gates[:, gi, hf * 512:(hf + 1) * 512], PB[gi * 2 + hf][:], AF.Copy),
                                 [bPB[gi * 2 + hf]], [bgates])
                    X1 = [k.sb(f"X1_{i}", [128, 1024], F32, sc_) for i in range(8)]
                    bX1 = [Buf(f"X1_{i}") for i in range(8)]
                    Xr = [k.sb(f"Xr{i}", [128, 1024], F32, sc_) for i in range(2)]
                    bXr = [Buf(f"Xr{i}") for i in range(2)]
                    xn2 = k.sb("xn2", [128, 1024], BF16, sc_); bxn2 = Buf("xn2")
                    jk2 = k.sb("jk2", [128, 1024], BF16, sc_); bjk2 = Buf("jk2")
                    h2Ts = [k.sb(f"h2T{i}", [128, 8, 512], BF16, sc_) for i in range(2)]
                    bh2Ts = [Buf(f"h2T{i}") for i in range(2)]
                    ffT = k.sb("ffT", [128, NJ, 512], BF16, sc_); bffT = Buf("ffT")
                    ring = [(k.sb(f"gu{i}", [128, 2, 8, 128], BF16, sc_), k.sb(f"dd{i}", [128, 1024], BF16, sc_)) for i in range(3)]
                    bring = [Buf(f"ring{i}") for i in range(3)]
                    rcnt = [0]

                    def nslot():
                        i = rcnt[0] % len(ring)
                        rcnt[0] += 1
                        return ring[i][0], ring[i][1], bring[i]

                    def pro(g, ti):
                        t = g * 4 + ti
                        tcs = slice(t * 128, (t + 1) * 128)
                        tpb = 4
                        x1, bx1 = X1[(g % 2) * 4 + ti], bX1[(g % 2) * 4 + ti]
                        h2T, bh2T = h2Ts[g % 2], bh2Ts[g % 2]
                        for nb in range(2):
                            for kc_ in range(8):
                                k.mm(PB[2 + nb][:], mixT[:, kc_, tcs], Wo[:, kc_, nb * 512:(nb + 1) * 512], [bmixT, bWo], [bPB[2 + nb]],
                                     start=(kc_ == 0), stop=(kc_ == 7), last=(kc_ == 7))
                        xr, bxr = Xr[ti % 2], bXr[ti % 2]
                        k.dma("sp", xr[:], xp[b, tcs, :], [], [bxr], bxr)
                        for nb in range(2):
                            cs = slice(nb * 512, (nb + 1) * 512)
                            k.op("dve", lambda e, nb=nb, cs=cs: e.tensor_tensor(x1[:, cs], PB[2 + nb][:], gates[:, 0, cs], ALU.mult),
                                 [bPB[2 + nb], bgates], [bx1])
                        k.op("dve", lambda e: e.tensor_tensor(x1[:], x1[:], xr[:], ALU.add), [bx1, bxr], [bx1])
                        rms_stats(x1[:], bx1, 128, 1, xn2, bxn2)
                        k.op("dve", lambda e: e.tensor_scalar_mul(xn2[:], x1[:], ss_t[:, 1:2]), [bx1, bss], [bxn2])

                    def pro_b(g, ti):
                        tpb = 4
                        h2T, bh2T = h2Ts[g % 2], bh2Ts[g % 2]
                        tp = pbf(tpb)
                        for c in range(8):
                            k.tr(tp[:, c * 128:(c + 1) * 128], xn2[:, c * 128:(c + 1) * 128], T["ident_b"][:], [bxn2, bT], [bPB[tpb]], last=(c == 7))
                        for c in range(8):
                            k.op("act", lambda e, c=c: e.activation(h2T[:, c, ti * 128:(ti + 1) * 128], tp[:, c * 128:(c + 1) * 128], AF.Identity,
                                                                     scale=G2T[:, c, 64 + b:65 + b], bias=modT[:, 24 + c, 64 + b:65 + b]),
                                 [bPB[tpb], bG2T, bmodT], [bh2T])

                    def gu_(g, j):
                        h2T, bh2T = h2Ts[g % 2], bh2Ts[g % 2]
                        gu, dd, bsl = nslot()
                        k.dma("sp", gu[:].rearrange("p a c n -> p (a c n)"), sc_gu[j], [bsc[j]], [bsl], bsl)
                        for a in range(2):
                            for dc in range(8):
                                k.mm(PB[a][:], gu[:, a, dc, :], h2T[:, dc, :], [bsl, bh2T], [bPB[a]],
                                     start=(dc == 0), stop=(dc == 7), last=(dc == 7))
                        k.op("act", lambda e: e.activation(ffT[:, j, :], PB[0][:], AF.Silu), [bPB[0]], [bffT])
                        k.op("dve", lambda e: e.tensor_tensor(ffT[:, j, :], ffT[:, j, :], PB[1][:], ALU.mult), [bffT, bPB[1]], [bffT])

                    def dn_(g, j):
                        gu, dd, bsl = nslot()
                        k.dma("sp", dd[:], sc_d[j], [bsc[j]], [bsl], bsl)
                        for ti in range(4):
                            for nb in range(2):
                                k.mm(PB[2 * ti + nb][:], ffT[:, j, ti * 128:(ti + 1) * 128], dd[:, nb * 512:(nb + 1) * 512],
                                     [bsl, bffT], [bPB[2 * ti + nb]], start=(j == 0), stop=(j == NJ - 1), last=(j == NJ - 1 or (ti == 3 and nb == 1)))

                    def epi(g, ti):
                        t = g * 4 + ti
                        x1, bx1 = X1[(g % 2) * 4 + ti], bX1[(g % 2) * 4 + ti]
                        for nb in range(2):
                            cs = slice(nb * 512, (nb + 1) * 512)
                            pbx = PB[2 * ti + nb]
                            k.op("dve", lambda e, cs=cs, pbx=pbx: e.tensor_tensor(pbx[:], pbx[:], gates[:, 1, cs], ALU.mult), [bPB[2 * ti + nb], bgates], [bPB[2 * ti + nb]])
                            k.op("dve", lambda e, cs=cs, pbx=pbx: e.tensor_tensor(x1[:, cs], x1[:, cs], pbx[:], ALU.add), [bx1, bPB[2 * ti + nb]], [bx1])
                        rms_stats(x1[:], bx1, 128, 3, jk2, bjk2)
                        k.op("dve", lambda e: e.scalar_tensor_tensor(x1[:], x1[:], ss_t[:, 3:4], FG[:], op0=ALU.mult, op1=ALU.mult), [bx1, bss, bFG], [bx1])
                        k.dma("pool", yp[b, t * 128:(t + 1) * 128, :], x1[:], [bx1], [], bx1, final=True)

                    for ti in range(4):
                        pro(0, ti)
                        pro_b(0, ti)
                    pend = []
                    for g in range(4):
                        for j in range(NJ):
                            gu_(g, j)
                            if pend:
                                epi(g - 1, pend.pop(0))
                            if g + 1 < 4 and j in (3, 8, 13, 18):
                                pro(g + 1, (j - 3) // 5)
                            if g + 1 < 4 and j in (5, 10, 15, 20):
                                pro_b(g + 1, (j - 5) // 5)
                        for j in range(NJ):
                            dn_(g, j)
                        epi(g, 0)
                        pend = [1, 2, 3]
                        if g == 3:
                            for ti in pend:
                                epi(g, ti)
                            pend = []
                    k.barrier()

            if do_sample:
                TS = {}
                with contextlib.ExitStack() as ss_:
                    for nm in ("cos_sa", "sin_sa", "cos_sr", "sin_sr", "zeta_s", "xit_s", "gc_s", "intra_s", "onehot_s", "smask", "nmask"):
                        TS[nm] = k.sb("TS_" + nm, tabs[nm].shape, _TAB_DT.get(nm, F32), ss_)
                        k.dma("sp", TS[nm][:], td[nm], [], [bT], bT)
                    mods = k.sb("mods", [64, 2, 1024], F32, ss_); bmods = Buf("mods")
                    Xs = k.sb("Xs", [64, 1024], F32, ss_); bXs = Buf("Xs")
                    hs = k.sb("hs", [64, 1024], BF16, ss_); bhs = Buf("hs")
                    hTs = k.sb("hTs", [128, 8, 64], BF16, ss_); bhTs = Buf("hTs")
                    rqk_s = k.sb("rqk_s", [64, 512], BF16, ss_); brqk_s = Buf("rqk_s")
                    rts = [k.sb(f"rts{i}", [64, 8, 32], F32, ss_) for i in range(4)]
                    brts = Buf("rts")
                    aqkf_s = k.sb("aqkf_s", [64, 512], F32, ss_); baqkf_s = Buf("aqkf_s")
                    aqkb_s = k.sb("aqkb_s", [64, 1024], BF16, ss_); baqkb_s = Buf("aqkb_s")
                    avf_s = k.sb("avf_s", [64, 512], F32, ss_); bavf_s = Buf("avf_s")
                    rv_s = k.sb("rv_s", [64, 512], BF16, ss_); brv_s = Buf("rv_s")
                    sg_s = k.sb("sg_s", [64, 512], BF16, ss_); bsg_s = Buf("sg_s")
                    kz_s = k.sb("kz_s", [64, 256], BF16, ss_); bkz_s = Buf("kz_s")
                    kzm = [k.sb(f"kzm{i}", [64, 256], BF16, ss_) for i in range(2)]
                    bkzm = [Buf(f"kzm{i}") for i in range(2)]
                    rqkT_s = k.sb("rqkT_s", [128, 4, 64], BF16, ss_); brqkT_s = Buf("rqkT_s")
                    rqm_s = k.sb("rqm_s", [128, 2, 2, 64], BF16, ss_); brqm_s = Buf("rqm_s")
                    rqx_s = k.sb("rqx_s", [128, 2, 2, 64], BF16, ss_); brqx_s = Buf("rqx_s")
                    scm_s = k.sb("scm_s", [64, 256], BF16, ss_); bscm_s = Buf("scm_s")
                    oT_s = k.sb("oT_s", [128, 256], F32, ss_); boT_s = Buf("oT_s")
                    rety_s = k.sb("rety_s", [64, 512], BF16, ss_); brety_s = Buf("rety_s")
                    Rsf = [k.sb(f"Rsf{i}", [128, 2, 128], F32, ss_) for i in range(2)]
                    bRsf = [Buf(f"Rsf{i}") for i in range(2)]
                    Rsb = k.sb("Rsb", [128, 2, 16, 128], BF16, ss_); bRsb = Buf("Rsb")
                    aqm_s = k.sb("aqm_s", [128, 2, 4, 64], BF16, ss_); baqm_s = Buf("aqm_s")
                    akT_s = k.sb("akT_s", [128, 4, 64], BF16, ss_); bakT_s = Buf("akT_s")
                    van = k.sb("van", [64, 8, 128], BF16, ss_); bvan = Buf("van")
                    Es = k.sb("Es", [128, 9, 8, 4], BF16, ss_); bEs = Buf("Es")
                    En = k.sb("En", [64, 8, 4], BF16, ss_); bEn = Buf("En")
                    rcs = k.sb("rcs", [128, 64], F32, ss_); brcs = Buf("rcs")

                    def tm_mod(dst_slot, src_fn, dst, bdst):
                        for c in range(8):
                            pbi = c // 4
                            k.tr(PB[pbi][0:64, (c % 4) * 128:(c % 4 + 1) * 128], src_fn(c), T["ident_f"][:], [bmodT, bG1T, bG2T, bT], [bPB[pbi]], last=(c % 4 == 3))
                        for pbi in range(2):
                            k.op("act", lambda e, pbi=pbi: e.activation(dst[:, dst_slot, pbi * 512:(pbi + 1) * 512], PB[pbi][0:64, :], AF.Copy), [bPB[pbi]], [bdst])
                    tm_mod(0, lambda c: modT[:, c, 0:64], mods, bmods)
                    tm_mod(1, lambda c: G1T[:, c, 0:64], mods, bmods)
                    k.op("dve", lambda e: e.memset(van[:], 1.0), [], [bvan])
                    k.op("dve", lambda e: e.memset(rqm_s[:], 0.0), [], [brqm_s])
                    k.op("dve", lambda e: e.memset(aqm_s[:], 0.0), [], [baqm_s])

                    k.dma("sp", Xs[:], xs, [], [bXs], bXs)
                    rms_stats(Xs[:], bXs, 64, 0, hs, bhs)
                    k.op("dve", lambda e: e.scalar_tensor_tensor(Xs[:], Xs[:], ss_t[0:64, 0:1], mods[:, 1, :], op0=ALU.mult, op1=ALU.mult), [bXs, bss, bmods], [bXs])
                    k.op("dve", lambda e: e.tensor_tensor(hs[:], Xs[:], mods[:, 0, :], ALU.add), [bXs, bmods], [bhs])
                    tp = pbf(7)
                    for c in range(8):
                        k.tr(tp[:, c * 64:(c + 1) * 64], hs[:, c * 128:(c + 1) * 128], T["ident_b"][0:64, 0:64], [bhs, bT], [bPB[7]], last=(c == 7))
                    k.op("act", lambda e: e.activation(hTs[:].rearrange("p c n -> p (c n)"), tp[:, 0:512], AF.Copy), [bPB[7]], [bhTs])
                    with contextlib.ExitStack() as sw_:
                        Wi = k.sb("Wi_s", [128, 8, 3072], BF16, sw_)
                        for dc in range(8):
                            k.dma("sp", Wi[:, dc, :], sc_wi[dc], [bscwi], [bWi], bWi)
                        for nb in range(6):
                            for dc in range(8):
                                k.mm(PB[nb][0:64, :], hTs[:, dc, :], Wi[:, dc, nb * 512:(nb + 1) * 512], [bhTs, bWi], [bPB[nb]],
                                     start=(dc == 0), stop=(dc == 7), last=(dc == 7))
                        k.barrier()
                    NR = 4
                    Ktb = [k.sb(f"Ktf{i}", [128, 512], F32, ss_) for i in range(NR)]
                    bKtb = [Buf(f"Ktf{i}") for i in range(NR)]
                    Vtf = [k.sb(f"Vtf{i}", [128, 512], F32, ss_) for i in range(NR)]
                    bVtf = [Buf(f"Vtf{i}") for i in range(NR)]
                    Kcb = [k.sb(f"Kcb{i}", [128, 512], BF16, ss_) for i in range(NR)]
                    bKcb = [Buf(f"Kcb{i}") for i in range(NR)]
                    KTs = [k.sb(f"KTs{i}", [128, 4, 128], BF16, ss_) for i in range(NR)]
                    bKTs = [Buf(f"KTs{i}") for i in range(NR)]
                    vas = [k.sb(f"vas{i}", [128, 8, 128], BF16, ss_) for i in range(18)]
                    bvas = [Buf(f"vas{i}") for i in range(18)]
                    for i in range(18):
                        k.op("pool", lambda e, i=i: e.memset(vas[i][:], 1.0), [], [bvas[i]])

                    def rotary(src_ap, dst_ap, cosn, sinn, bsrc, bdst):
                        zv = src_ap.rearrange("p (h two f) -> p h two f", two=2, f=32)
                        x1v, x2v = zv[:, :, 0, :], zv[:, :, 1, :]
                        ov = dst_ap.rearrange("p (h two f) -> p h two f", two=2, f=32)
                        cb = TS[cosn][:].unsqueeze(1).to_broadcast([64, 8, 32])
                        sb_ = TS[sinn][:].unsqueeze(1).to_broadcast([64, 8, 32])
                        r0, r1, r2, r3 = (rts[i][:] for i in range(4))
                        k.op("dve", lambda e: e.tensor_tensor(r0, x1v, cb, ALU.mult), [bsrc, bT], [brts])
                        k.op("dve", lambda e: e.tensor_tensor(r1, x2v, sb_, ALU.mult), [bsrc, bT], [brts])
                        k.op("dve", lambda e: e.tensor_tensor(r2, x1v, sb_, ALU.mult), [bsrc, bT], [brts])
                        k.op("dve", lambda e: e.tensor_tensor(r3, x2v, cb, ALU.mult), [bsrc, bT], [brts])
                        k.op("dve", lambda e: e.tensor_tensor(ov[:, :, 0, :], r0, r1, ALU.subtract), [brts], [bdst])
                        k.op("dve", lambda e: e.tensor_tensor(ov[:, :, 1, :], r2, r3, ALU.add), [brts], [bdst])
                    rotary(PB[0][0:64, :], rqk_s[:], "cos_sr", "sin_sr", bPB[0], brqk_s)
                    rotary(PB[3][0:64, :], aqkb_s[:, 0:512], "cos_sa", "sin_sa", bPB[3], baqkb_s)
                    rotary(PB[4][0:64, :], aqkf_s[:, 0:512], "cos_sa", "sin_sa", bPB[4], baqkf_s)
                    k.dma("sp", kso, aqkf_s[:, 0:512], [baqkf_s], [], baqkf_s, final=True)
                    k.op("act", lambda e: e.activation(aqkb_s[:, 512:1024], aqkf_s[:, 0:512], AF.Copy), [baqkf_s], [baqkb_s])
                    k.op("act", lambda e: e.activation(avf_s[:], PB[5][0:64, :], AF.Copy), [bPB[5]], [bavf_s])
                    k.dma("sp", vso, avf_s[:], [bavf_s], [], bavf_s, final=True)
                    avv = avf_s[:].rearrange("p (h f) -> p h f", f=64)
                    k.op("dve", lambda e: e.tensor_copy(van[:, 0:8:2, 0:64], avv[:, 0:8:2, :]), [bavf_s], [bvan])
                    k.op("dve", lambda e: e.tensor_copy(van[:, 1:8:2, 64:128], avv[:, 1:8:2, :]), [bavf_s], [bvan])
                    k.op("act", lambda e: e.activation(rv_s[:], PB[1][0:64, :], AF.Copy), [bPB[1]], [brv_s])
                    k.op("act", lambda e: e.activation(sg_s[:], PB[2][0:64, :], AF.Silu), [bPB[2]], [bsg_s])
                    k.op("dve", lambda e: e.tensor_tensor(kz_s[:].rearrange("p (h f) -> p h f", f=64),
                                                          rqk_s[:, 256:512].rearrange("p (h f) -> p h f", f=64),
                                                          TS["zeta_s"][:].unsqueeze(2).to_broadcast([64, 4, 64]), ALU.mult), [brqk_s, bT], [bkz_s])
                    tp = pbf(7)
                    for c in range(4):
                        k.tr(tp[:, c * 64:(c + 1) * 64], rqk_s[:, c * 128:(c + 1) * 128], T["ident_b"][0:64, 0:64], [brqk_s, bT], [bPB[7]], last=(c == 3))
                    k.op("act", lambda e: e.activation(rqkT_s[:].rearrange("p c n -> p (c n)"), tp[:, 0:256], AF.Copy), [bPB[7]], [brqkT_s])
                    k.op("act", lambda e: e.activation(rqm_s[0:64, 0, :, :].rearrange("p c n -> p (c n)"), tp[0:64, 0:128], AF.Copy), [bPB[7]], [brqm_s])
                    k.op("act", lambda e: e.activation(rqm_s[64:128, 1, :, :].rearrange("p c n -> p (c n)"), tp[64:128, 0:128], AF.Copy), [bPB[7]], [brqm_s])
                    for vv in range(2):
                        k.op("dve", lambda e, vv=vv: e.tensor_tensor(rqx_s[:, vv, :, :], rqm_s[:, vv, :, :], TS["xit_s"][:], ALU.mult), [brqm_s, bT], [brqx_s])
                    tp = pbf(6)
                    for c in range(8):
                        k.tr(tp[:, c * 64:(c + 1) * 64], aqkb_s[:, c * 128:(c + 1) * 128], T["ident_b"][0:64, 0:64], [baqkb_s, bT], [bPB[6]], last=(c == 7))
                    k.op("act", lambda e: e.activation(aqm_s[0:64, 0, :, :].rearrange("p c n -> p (c n)"), tp[0:64, 0:256], AF.Copy), [bPB[6]], [baqm_s])
                    k.op("act", lambda e: e.activation(aqm_s[64:128, 1, :, :].rearrange("p c n -> p (c n)"), tp[64:128, 0:256], AF.Copy), [bPB[6]], [baqm_s])
                    k.op("act", lambda e: e.activation(akT_s[:].rearrange("p c n -> p (c n)"), tp[:, 256:512], AF.Copy), [bPB[6]], [bakT_s])

                    for hd in range(4):
                        c, hh = hd // 2, hd % 2
                        k.mm(PB[0][0:64, hd * 64:(hd + 1) * 64], rqkT_s[:, 2 + c, :], rqm_s[:, hh, c, :], [brqkT_s, brqm_s], [bPB[0]], last=(hd == 3))
                    k.op("dve", lambda e: e.tensor_tensor(scm_s[:], PB[0][0:64, 0:256], TS["intra_s"][:].rearrange("p h n -> p (h n)"), ALU.mult), [bPB[0], bT], [bscm_s])
                    srv = sr.rearrange("b (c hh) kk v -> (hh kk) c b v", hh=2)
                    for bb in range(16):
                        rf, brf = Rsf[bb % 2], bRsf[bb % 2]
                        k.dma("sp", rf[:], srv[:, :, bb, :], [], [brf], brf)
                        k.op("act", lambda e, bb=bb: e.activation(Rsb[:, :, bb, :], rf[:], AF.Copy), [brf], [bRsb])
                    for hd in range(4):
                        c, hh = hd // 2, hd % 2
                        k.mm(PB[1][:, hd * 64:(hd + 1) * 64], rv_s[:, hd * 128:(hd + 1) * 128], scm_s[:, hd * 64:(hd + 1) * 64], [brv_s, bscm_s], [bPB[1]],
                             start=True, stop=False, last=False)
                        for bb in range(16):
                            k.mm(PB[1][:, hd * 64 + bb * 4:hd * 64 + bb * 4 + 4], Rsb[:, c, bb, :], rqx_s[:, hh, c, bb * 4:(bb + 1) * 4], [bRsb, brqx_s], [bPB[1]],
                                 start=False, stop=(bb == 15), last=(hd == 3 and bb == 15))
                    k.op("act", lambda e: e.activation(oT_s[:], PB[1][:, 0:256], AF.Copy), [bPB[1]], [boT_s])
                    for hd in range(4):
                        k.tr(PB[2][0:64, hd * 128:(hd + 1) * 128], oT_s[:, hd * 64:(hd + 1) * 64], T["ident_f"][:], [boT_s, bT], [bPB[2]], last=(hd == 3))
                    for hd in range(4):
                        cs = slice(hd * 128, (hd + 1) * 128)
                        k.op("dve", lambda e, hd=hd: e.memset(ss_t[0:64, 4 + hd:5 + hd], 0.0), [], [bss])
                        k.op("act", lambda e, hd=hd, cs=cs: e.activation(rety_s[0:64, cs], PB[2][0:64, cs], AF.Square, accum_out=ss_t[0:64, 4 + hd:5 + hd]), [bPB[2]], [brety_s, bss])
                    k.op("act", lambda e: e.activation(ss_t[0:64, 4:8], ss_t[0:64, 4:8], AF.Sqrt, bias=epsT[0:64, :], scale=1.0 / 128), [bss, bT], [bss])
                    k.op("dve", lambda e: e.reciprocal(ss_t[0:64, 4:8], ss_t[0:64, 4:8]), [bss], [bss])
                    for hd in range(4):
                        cs = slice(hd * 128, (hd + 1) * 128)
                        k.op("dve", lambda e, hd=hd, cs=cs: e.scalar_tensor_tensor(rety_s[:, cs], PB[2][0:64, cs], ss_t[0:64, 4 + hd:5 + hd], sg_s[:, cs],
                                                                                    op0=ALU.mult, op1=ALU.mult), [bPB[2], bss, bsg_s], [brety_s])
                    tp = pbf(7)
                    for c in range(4):
                        k.tr(tp[:, c * 64:(c + 1) * 64], rety_s[:, c * 128:(c + 1) * 128], T["ident_b"][0:64, 0:64], [brety_s, bT], [bPB[7]], last=(c == 3))
                    k.op("act", lambda e: e.activation(mixT[:, 0:4, 0:64], tp[:, 0:256].rearrange("p (c n) -> p c n", n=64), AF.Copy), [bPB[7]], [bmixT])
                    rsov = rso.rearrange("b (c hh) kk v -> (hh kk) c b v", hh=2)
                    for bb in range(16):
                        rf, brf = Rsf[bb % 2], bRsf[bb % 2]
                        km, bkm = kzm[bb % 2], bkzm[bb % 2]
                        pbi = 3 + bb % 2
                        k.dma("sp", rf[:], srv[:, :, bb, :], [], [brf], brf)
                        k.op("dve", lambda e, bb=bb: e.tensor_scalar_mul(km[:], kz_s[:], TS["onehot_s"][:, bb:bb + 1]), [bkz_s, bT], [bkm])
                        for c in range(2):
                            k.mm(PB[pbi][:, c * 256:(c + 1) * 256], km[:, c * 128:(c + 1) * 128], rv_s[:, c * 256:(c + 1) * 256], [bkm, brv_s], [bPB[pbi]], last=(c == 1))
                        for c in range(2):
                            for hh in range(2):
                                rows = slice(hh * 64, (hh + 1) * 64)
                                k.op("dve", lambda e, c=c, hh=hh, rows=rows: e.scalar_tensor_tensor(
                                    rf[rows, c, :], rf[rows, c, :], TS["gc_s"][rows, c:c + 1], PB[pbi][rows, c * 256 + hh * 128:c * 256 + (hh + 1) * 128],
                                    op0=ALU.mult, op1=ALU.add), [brf, bT, bPB[pbi]], [brf])
                        k.dma("sp", rsov[:, :, bb, :], rf[:], [brf], [], brf, final=True)

                    for bb in range(16):
                        rowsl = [slice(1920, 2048)] + [slice(1536 + t_, 1536 + t_ + 4 * 127 + 1, 4) for t_ in range(4)] \
                            + [slice(t_, t_ + 16 * 127 + 1, 16) for t_ in range(4)]
                        for ti_, rs_ in enumerate(rowsl):
                            ri = (bb * 9 + ti_) % NR
                            vi_ = (bb % 2) * 9 + ti_
                            kt, bkt = Ktb[ri], bKtb[ri]
                            kT, bkT = KTs[ri], bKTs[ri]
                            k.dma("sp", kt[:], ck[bb, rs_, :], [], [bkt], bkt)
                            vt, bvt = Vtf[ri], bVtf[ri]
                            k.dma("sp", vt[:], cv[bb, rs_, :], [], [bvt], bvt)
                            vtv = vt[:].rearrange("p (h f) -> p h f", f=64)
                            k.op("pool", lambda e: e.tensor_copy(vas[vi_][:, 0:8:2, 0:64], vtv[:, 0:8:2, :]), [bvt], [bvas[vi_]])
                            k.op("dve", lambda e: e.tensor_copy(vas[vi_][:, 1:8:2, 64:128], vtv[:, 1:8:2, :]), [bvt], [bvas[vi_]])
                            kcb, bkcb = Kcb[ri], bKcb[ri]
                            k.op("dve", lambda e: e.tensor_copy(kcb[:], kt[:]), [bkt], [bkcb])
                            tpi = 6 + ti_ % 2
                            tp = pbf(tpi)
                            for c in range(4):
                                k.tr(tp[:, c * 128:(c + 1) * 128], kcb[:, c * 128:(c + 1) * 128], T["ident_b"][:], [bkcb, bT], [bPB[tpi]], last=(c == 3))
                            k.op("act", lambda e: e.activation(kT[:].rearrange("p c n -> p (c n)"), tp[:, 0:512], AF.Copy), [bPB[tpi]], [bkT])
                            for hd in range(8):
                                c, hh = hd // 2, hd % 2
                                k.mm(PB[0][:, ti_ * 32 + hd * 4:ti_ * 32 + hd * 4 + 4], kT[:, c, :], aqm_s[:, hh, c, bb * 4:(bb + 1) * 4], [bkT, baqm_s], [bPB[0]], last=(hd == 7))
                        for hd in range(8):
                            c, hh = hd // 2, hd % 2
                            k.mm(PB[1][0:64, hd * 4:hd * 4 + 4], akT_s[:, c, :], aqm_s[:, hh, c, bb * 4:(bb + 1) * 4], [bakT_s, baqm_s], [bPB[1]], last=(hd == 7))
                        k.op("act", lambda e: e.activation(Es[:].rearrange("p a h t -> p (a h t)"), PB[0][:, 0:288], AF.Exp, scale=0.125), [bPB[0]], [bEs])
                        k.op("dve", lambda e: e.tensor_tensor(Es[:], Es[:], TS["smask"][:].unsqueeze(2).to_broadcast([128, 9, 8, 4]), ALU.mult), [bEs, bT], [bEs])
                        k.op("act", lambda e: e.activation(En[:].rearrange("p h t -> p (h t)"), PB[1][0:64, 0:32], AF.Exp, scale=0.125), [bPB[1]], [bEn])
                        k.op("dve", lambda e, bb=bb: e.tensor_tensor(En[:], En[:], TS["nmask"][:, bb, :].unsqueeze(1).to_broadcast([64, 8, 4]), ALU.mult), [bEn, bT], [bEn])
                        for hd in range(8):
                            oc = slice(hd * 64 + bb * 4, hd * 64 + bb * 4 + 4)
                            for ti_ in range(9):
                                k.mm(PB[5][:, oc], vas[(bb % 2) * 9 + ti_][:, hd, :], Es[:, ti_, hd, :], [bvas[(bb % 2) * 9 + ti_], bEs], [bPB[5]], start=(ti_ == 0), stop=False, last=False)
                            k.mm(PB[5][:, oc], van[:, hd, :], En[:, hd, :], [bvan, bEn], [bPB[5]], start=False, stop=True, last=(hd == 7))
                    for hd in range(8):
                        c, hh = hd // 2, hd % 2
                        rows = slice(hh * 64, (hh + 1) * 64)
                        drows = slice((1 - hh) * 64, (2 - hh) * 64)
                        cs = slice(hd * 64, (hd + 1) * 64)
                        k.op("dve", lambda e: e.reciprocal(rcs[rows, :], PB[5][drows, cs]), [bPB[5]], [brcs])
                        k.op("dve", lambda e: e.tensor_tensor(mixT[rows, 4 + c, 0:64], PB[5][rows, cs], rcs[rows, :], ALU.mult), [bPB[5], brcs], [bmixT])
                    k.barrier()

                with contextlib.ExitStack() as sc_:
                    mods = k.sb("mods2", [64, 4, 1024], F32, sc_); bmods = Buf("mods2")
                    FG = k.sb("FGs", [128, 1024], F32, sc_)
                    k.dma("sp", FG[:], fgd.unsqueeze(0).to_broadcast([128, 1024]), [], [bFG], bFG)
                    tm_mod(0, lambda c: modT[:, 16 + c, 0:64], mods, bmods)
                    tm_mod(1, lambda c: modT[:, 24 + c, 0:64], mods, bmods)
                    tm_mod(2, lambda c: G2T[:, c, 0:64], mods, bmods)
                    tm_mod(3, lambda c: modT[:, 40 + c, 0:64], mods, bmods)
                    Xs = k.sb("Xs2", [128, 1024], F32, sc_); bXs = Buf("Xs2")
                    X1 = [k.sb("X1s", [128, 1024], F32, sc_)]; bX1 = [Buf("X1s")]
                    xn2 = k.sb("xn2s", [64, 1024], F32, sc_); bxn2 = Buf("xn2s")
                    h2s = k.sb("h2s", [64, 1024], BF16, sc_); bh2s = Buf("h2s")
                    h2T = k.sb("h2Ts", [128, 8, 64], BF16, sc_); bh2T = Buf("h2Ts")
                    ffT = k.sb("ffTs", [128, NJ, 64], BF16, sc_); bffT = Buf("ffTs")
                    ring = [(k.sb(f"gus{i}", [128, 2, 8, 128], BF16, sc_), k.sb(f"dds{i}", [128, 1024], BF16, sc_)) for i in range(2)]
                    bring = [Buf(f"rings{i}") for i in range(2)]
                    for nb in range(2):
                        for kc_ in range(8):
                            k.mm(PB[4 + nb][0:64, :], mixT[:, kc_, 0:64], Wo[:, kc_, nb * 512:(nb + 1) * 512], [bmixT, bWo], [bPB[4 + nb]],
                                 start=(kc_ == 0), stop=(kc_ == 7), last=(kc_ == 7))
                    k.dma("sp", Xs[0:64, :], xs, [], [bXs], bXs)
                    x1 = X1[0]
                    for nb in range(2):
                        cs = slice(nb * 512, (nb + 1) * 512)
                        k.op("dve", lambda e, nb=nb, cs=cs: e.tensor_tensor(x1[0:64, cs], PB[4 + nb][0:64, :], mods[:, 0, cs], ALU.mult),
                             [bPB[4 + nb], bmods], [bX1[0]])
                    k.op("dve", lambda e: e.tensor_tensor(x1[0:64, :], x1[0:64, :], Xs[0:64, :], ALU.add), [bX1[0], bXs], [bX1[0]])
                    rms_stats(x1[0:64, :], bX1[0], 64, 1, h2s, bh2s)
                    k.op("dve", lambda e: e.scalar_tensor_tensor(xn2[:], x1[0:64, :], ss_t[0:64, 1:2], mods[:, 2, :], op0=ALU.mult, op1=ALU.mult), [bX1[0], bss, bmods], [bxn2])
                    k.op("dve", lambda e: e.tensor_tensor(h2s[:], xn2[:], mods[:, 1, :], ALU.add), [bxn2, bmods], [bh2s])
                    tp = pbf(7)
                    for c in range(8):
                        k.tr(tp[:, c * 64:(c + 1) * 64], h2s[:, c * 128:(c + 1) * 128], T["ident_b"][0:64, 0:64], [bh2s, bT], [bPB[7]], last=(c == 7))
                    k.op("act", lambda e: e.activation(h2T[:].rearrange("p c n -> p (c n)"), tp[:, 0:512], AF.Copy), [bPB[7]], [bh2T])

                    def out_fn_s(ti, ytile, bytile, np_):
                        k.dma("sp", ys, ytile[0:64, :], [bytile], [], bytile, final=True)
                    ffn_group(sc_, 1, 64, X1, bX1, h2T, bh2T, ffT, bffT, ring, bring, mods[:, 3, :], bmods, out_fn_s, FG, Xs, bXs, h2s, bh2s)
                    k.barrier()

        except _Stop:
            pass
        k.finish()
    return nc, tabs


_CACHE = {}


def kernel(x_prompt, x_sample, cache_attn_k, cache_attn_v, state_ret, c_prompt, c_sample,
           w_ada, b_ada, norm1_g, w_in, w_out, norm2_g, w_gate, w_up, w_down, final_g):
    f = lambda a: np.ascontiguousarray(np.asarray(a, dtype=np.float32))
    x_prompt, x_sample = f(x_prompt), f(x_sample)
    ck, cv, srr = f(cache_attn_k)[0], f(cache_attn_v)[0], f(state_ret)[0]
    c_prompt, c_sample = f(c_prompt), f(c_sample)
    if "nc" not in _CACHE:
        _CACHE["nc"] = build()
    nc, tabs = _CACHE["nc"]
    shared = {"w_ada": f(w_ada)[0], "b_ada": f(b_ada)[0], "n1g": f(norm1_g)[0], "w_in": f(w_in)[0],
              "w_out": f(w_out)[0], "n2g": f(norm2_g)[0], "w_gate": f(w_gate)[0], "w_up": f(w_up)[0],
              "w_down": f(w_down)[0], "fg": f(final_g)}
    for kk, v in tabs.items():
        shared["t_" + kk] = v
    in_maps = []
    for c in range(NCORES):
        m = dict(shared)
        m["xp"] = x_prompt[4 * c:4 * c + 4]
        m["xs"] = x_sample[16 * c:16 * c + 16].reshape(64, D)
        m["ck"] = ck[16 * c:16 * c + 16].reshape(16, S, 512)
        m["cv"] = cv[16 * c:16 * c + 16].reshape(16, S, 512)
        m["sr"] = srr[16 * c:16 * c + 16]
        m["call"] = np.concatenate([np.repeat(c_sample[16 * c:16 * c + 16], 4, axis=0), c_prompt[4 * c:4 * c + 4]], axis=0)
        in_maps.append(m)
    res = run_bass_kernel_spmd(nc, in_maps, core_ids=list(range(NCORES)))
    R = res.results
    y_prompt = np.concatenate([r["yp"] for r in R], 0)
    y_sample = np.concatenate([r["ys"].reshape(16, 4, D) for r in R], 0)
    nkp = np.concatenate([r["kp"].reshape(4, S, 8, 64) for r in R], 0)[None]
    nvp = np.concatenate([r["vp"].reshape(4, S, 8, 64) for r in R], 0)[None]
    nrp = np.concatenate([r["rp"] for r in R], 0)[None]
    nks = np.concatenate([r["kso"].reshape(16, 4, 8, 64) for r in R], 0)[None]
    nvs = np.concatenate([r["vso"].reshape(16, 4, 8, 64) for r in R], 0)[None]
    nrs = np.concatenate([r["rso"] for r in R], 0)[None]
    return (y_prompt, y_sample, nkp, nvp, nrp, nks, nvs, nrs)
```

```python
import contextlib
import math
import numpy as np
import ml_dtypes
import concourse.bass as bass
import concourse.mybir as mybir
from concourse.bass_utils import run_bass_kernel_spmd

F32 = mybir.dt.float32
BF16 = mybir.dt.bfloat16
AF = mybir.ActivationFunctionType
ALU = mybir.AluOpType

D = 1024
S = 2048
NT = S // 128
DFF = 2816
NJ = DFF // 128
EPS = 1e-6
PAST = 8192
NCORES = 8


class Buf:
    _cache = {}

    def __new__(cls, name):
        if name in cls._cache:
            return cls._cache[name]
        o = super().__new__(cls)
        o.name = name
        o.w = None
        o.r = {}
        o.dsem = None
        o.dcount = 0
        cls._cache[name] = o
        return o


class KB:
    def __init__(self, nc, stack):
        self.nc = nc
        self.stack = stack
        self.eng = {"pe": nc.tensor, "act": nc.scalar, "dve": nc.vector,
                    "pool": nc.gpsimd, "sp": nc.sync}
        self.sems = {}
        self.cnt = {}
        for k in ("pe", "act", "dve", "pool"):
            self.sems[k] = stack.enter_context(nc.semaphore("c_" + k))
            self.cnt[k] = 0
        self.seen = {k: {} for k in self.eng}
        self.pe_pending = []
        self.uid = 0
        self.dsems = {}
        self.finals = {}

    def sb(self, name, shape, dt, stack=None):
        self.uid += 1
        return (stack or self.stack).enter_context(self.nc.sbuf_tensor(f"{name}_u{self.uid}", list(shape), dt))

    def ps(self, name, shape, dt=F32):
        return self.stack.enter_context(self.nc.psum_tensor(name, list(shape), dt))

    def _deps(self, reads, writes):
        deps = {}

        def add(ev):
            if ev is None:
                return
            s, v = ev
            if deps.get(s, 0) < v:
                deps[s] = v
        for b in reads:
            add(b.w)
        for b in writes:
            add(b.w)
            for s, v in b.r.items():
                add((s, v))
        return deps

    def _wait(self, ek, deps, skip_own_pe=False):
        e = self.eng[ek]
        seen = self.seen[ek]
        for s, v in deps.items():
            if skip_own_pe and s is self.sems["pe"]:
                continue
            if seen.get(s, 0) >= v:
                continue
            e.wait_ge(s, v)
            seen[s] = v

    def _commit(self, ev, reads, writes):
        s, v = ev
        for b in reads:
            if b.r.get(s, 0) < v:
                b.r[s] = v
        for b in writes:
            b.w = ev
            b.r = {}

    def op(self, ek, fn, reads=(), writes=()):
        if KB.dead:
            return
        self._wait(ek, self._deps(reads, writes))
        inst = fn(self.eng[ek])
        self.cnt[ek] += 1
        inst.then_inc(self.sems[ek], 1)
        ev = (self.sems[ek], self.cnt[ek])
        self._commit(ev, reads, writes)
        return ev

    def _pe_done(self, inst, reads, writes, last):
        if last:
            self.cnt["pe"] += 1
            inst.then_inc(self.sems["pe"], 1)
            ev = (self.sems["pe"], self.cnt["pe"])
            for (r, w) in self.pe_pending:
                self._commit(ev, r, w)
            self.pe_pending = []
            self._commit(ev, reads, writes)
        else:
            self.pe_pending.append((list(reads), list(writes)))

    def mm(self, out, lhsT, rhs, reads, writes, start=True, stop=True, last=True):
        if KB.dead:
            return
        self._wait("pe", self._deps(reads, writes), skip_own_pe=True)
        inst = self.nc.tensor.matmul(out, lhsT, rhs, start=start, stop=stop)
        self._pe_done(inst, reads, writes, last)

    def tr(self, out, in_, ident, reads, writes, last=True):
        if KB.dead:
            return
        self._wait("pe", self._deps(reads, writes), skip_own_pe=True)
        inst = self.nc.tensor.transpose(out, in_, ident)
        self._pe_done(inst, reads, writes, last)

    def dma(self, q, out, in_, reads, writes, owner, final=False, **kw):
        if KB.dead:
            return
        self._wait(q, self._deps(reads, writes))
        if owner.dsem is None:
            self.uid += 1
            owner.dsem = self.stack.enter_context(self.nc.semaphore(f"d_{owner.name}_{self.uid}"))
            self.dsems[f"{owner.name}_{self.uid}"] = owner
        inst = self.eng[q].dma_start(out=out, in_=in_, **kw)
        owner.dcount += 16
        inst.then_inc(owner.dsem, 16)
        ev = (owner.dsem, owner.dcount)
        self._commit(ev, reads, writes)
        if final:
            self.finals[id(owner)] = owner
        return ev

    def barrier(self, force=False):
        if KB.dead and not force:
            return
        for ek in self.eng:
            e = self.eng[ek]
            seen = self.seen[ek]
            for k2 in ("pe", "act", "dve", "pool"):
                s, v = self.sems[k2], self.cnt[k2]
                if v > 0 and seen.get(s, 0) < v:
                    e.wait_ge(s, v)
                    seen[s] = v
            for o in self.dsems.values():
                if o.dcount > 0 and seen.get(o.dsem, 0) < o.dcount:
                    e.wait_ge(o.dsem, o.dcount)
                    seen[o.dsem] = o.dcount

    def finish(self):
        self.barrier(force=True)


def _tables():
    t = {}
    pos = np.arange(S, dtype=np.float64)
    afreq = (10000.0 ** (-np.arange(0, 64, 2, dtype=np.float32) / np.float32(64))).astype(np.float32).astype(np.float64)
    rfreq = (10000.0 ** (-np.linspace(0.0, 1.0, 32, dtype=np.float32))).astype(np.float32).astype(np.float64)

    def tm(fn, fr, p):
        a = (p.astype(np.float32)[:, None] * fr.astype(np.float32)[None, :]).astype(np.float32).astype(np.float64)
        v = fn(a).astype(np.float32)
        return v
    for nm, fr in (("a", afreq), ("r", rfreq)):
        c = tm(np.cos, fr, pos).reshape(NT, 128, 32).transpose(1, 0, 2)
        s = tm(np.sin, fr, pos).reshape(NT, 128, 32).transpose(1, 0, 2)
        t["cos_" + nm] = np.ascontiguousarray(c)
        t["sin_" + nm] = np.ascontiguousarray(s)
        ps = PAST + (np.arange(64) % 4).astype(np.float64)
        t["cos_s" + nm] = np.ascontiguousarray(tm(np.cos, fr, ps))
        t["sin_s" + nm] = np.ascontiguousarray(tm(np.sin, fr, ps))
    h = np.arange(4, dtype=np.float64)
    log_g = np.log1p(-(2.0 ** (-5.0 - h)))
    idx = np.arange(128, dtype=np.float64)
    zeta = np.exp((127.0 - idx)[:, None] * log_g[None, :])
    t["zeta"] = zeta.astype(np.float32)
    xi = np.exp((idx + 1.0)[None, :] * log_g[:, None])
    xit = np.zeros((128, 2, 128), np.float32)
    gc = np.zeros((128, 2), np.float32)
    for c in range(2):
        for hh in range(2):
            xit[hh * 64:(hh + 1) * 64, c, :] = xi[2 * c + hh][None, :]
            gc[hh * 64:(hh + 1) * 64, c] = np.exp(128.0 * log_g[2 * c + hh])
    t["xit"] = xit
    t["gc"] = gc
    diff = idx[None, :] - idx[:, None]
    intra = np.zeros((128, 4, 128), np.float32)
    for hd in range(4):
        intra[:, hd, :] = np.where(diff >= 0, np.exp(np.maximum(diff, 0.0) * log_g[hd]), 0.0)
    t["intra"] = intra
    tok = np.arange(64)
    tb, tt = tok // 4, tok % 4
    zs = np.exp((3.0 - tt)[:, None] * log_g[None, :])
    t["zeta_s"] = zs.astype(np.float32)
    xis = np.exp((tt + 1.0)[None, :] * log_g[:, None])
    xits = np.zeros((128, 2, 64), np.float32)
    gcs = np.zeros((128, 2), np.float32)
    for c in range(2):
        for hh in range(2):
            xits[hh * 64:(hh + 1) * 64, c, :] = xis[2 * c + hh][None, :]
            gcs[hh * 64:(hh + 1) * 64, c] = np.exp(4.0 * log_g[2 * c + hh])
    t["xit_s"] = xits
    t["gc_s"] = gcs
    ds = tt[None, :] - tt[:, None]
    same = tb[None, :] == tb[:, None]
    intras = np.zeros((64, 4, 64), np.float32)
    for hd in range(4):
        intras[:, hd, :] = np.where(same & (ds >= 0), np.exp(np.maximum(ds, 0) * log_g[hd]), 0.0)
    t["intra_s"] = intras
    onehot = np.zeros((64, 16), np.float32)
    onehot[tok, tb] = 1.0
    t["onehot_s"] = onehot
    m = np.zeros((128, 256), np.float32)
    jj = idx[:, None]
    ii = idx[None, :]
    m[:, :128] = (ii >= jj)
    m[:, 128:] = (ii <= jj)
    t["amask"] = m.astype(ml_dtypes.bfloat16)
    ms = np.zeros((128, 9, 4), np.float32)
    for tq in range(4):
        ms[:, 0, tq] = (idx >= tq)
        ms[:, 1 + tq, tq] = 1.0
        ms[:, 5 + tq, tq] = 1.0
    t["smask"] = ms.astype(ml_dtypes.bfloat16)
    mn = np.zeros((64, 16, 4), np.float32)
    for j in range(64):
        for tq in range(4):
            if tt[j] < tq:
                mn[j, tb[j], tq] = 1.0
            elif tt[j] == tq:
                mn[j, tb[j], tq] = 3.0
    t["nmask"] = mn.astype(ml_dtypes.bfloat16)
    t["ident_b"] = np.eye(128, dtype=np.float32).astype(ml_dtypes.bfloat16)
    t["ident_f"] = np.eye(128, dtype=np.float32)
    return t


_TAB_DT = {"amask": BF16, "smask": BF16, "nmask": BF16, "ident_b": BF16}


class _Stop(Exception):
    pass


def build(NB=4, NSB=16, do_sample=True, stop=99, nb_run=None):
    def chk(stage):
        if stop <= stage:
            KB.dead = True
    KB.dead = False

    Buf._cache = {}
    nc = bass.Bass("TRN2", target_bir_lowering=False)
    tabs = _tables()

    def din(name, shape, dt=F32):
        return nc.dram_tensor(name, list(shape), dt, kind="ExternalInput").ap()

    def dout(name, shape):
        return nc.dram_tensor(name, list(shape), F32, kind="ExternalOutput").ap()

    xp = din("xp", [NB, S, D])
    xs = din("xs", [64, D])
    ck = din("ck", [16, S, 512])
    cv = din("cv", [16, S, 512])
    sr = din("sr", [16, 4, 64, 128])
    call = din("call", [68, D])
    w_ada = din("w_ada", [D, 6 * D])
    b_ada = din("b_ada", [6 * D])
    n1g = din("n1g", [D])
    w_in = din("w_in", [D, 3072])
    w_out = din("w_out", [D, D])
    n2g = din("n2g", [D])
    w_gate = din("w_gate", [D, DFF])
    w_up = din("w_up", [D, DFF])
    w_down = din("w_down", [DFF, D])
    fgd = din("fg", [D])
    td = {k: din("t_" + k, v.shape, _TAB_DT.get(k, F32)) for k, v in tabs.items()}

    yp = dout("yp", [NB, S, D])
    ys = dout("ys", [64, D])
    kp = dout("kp", [NB, S, 512])
    vp = dout("vp", [NB, S, 512])
    rp = dout("rp", [NB, 4, 64, 128])
    kso = dout("kso", [64, 512])
    vso = dout("vso", [64, 512])
    rso = dout("rso", [16, 4, 64, 128])
    sc_gu = nc.dram_tensor("sc_gu", [NJ, 128, 2048], BF16, kind="Internal").ap()
    sc_d = nc.dram_tensor("sc_d", [NJ, 128, 1024], BF16, kind="Internal").ap()
    vbs = nc.dram_tensor("vbs", [NB, S, 512], BF16, kind="Internal").ap()
    sc_wi = nc.dram_tensor("sc_wi", [8, 128, 3072], BF16, kind="Internal").ap()

    with contextlib.ExitStack() as st:
        k = KB(nc, st)
        try:
            PB = [k.ps(f"pb{i}", [128, 512], F32) for i in range(8)]
            bPB = [Buf(f"pb{i}") for i in range(8)]

            def pbf(i):
                return PB[i][:].bitcast(BF16)

            bWi = Buf("Wi")
            bscwi = Buf("scwi")
            Wo = k.sb("Wo", [128, 8, 1024], BF16); bWo = Buf("Wo")
            modT = k.sb("modT", [128, 48, 68], F32); bmodT = Buf("modT")
            G1T = k.sb("G1T", [128, 8, 68], F32); bG1T = Buf("G1T")
            G2T = k.sb("G2T", [128, 8, 68], F32); bG2T = Buf("G2T")
            bgates = Buf("gates")
            bFG = Buf("FG")
            mixT = k.sb("mixT", [128, 8, S], BF16); bmixT = Buf("mixT")
            T = {}
            bT = Buf("tabs")
            for nm in ("cos_a", "sin_a", "cos_r", "sin_r", "zeta", "xit", "gc", "intra", "amask",
                       "ident_b", "ident_f"):
                T[nm] = k.sb("T_" + nm, tabs[nm].shape, _TAB_DT.get(nm, F32))
                k.dma("sp", T[nm][:], td[nm], [], [bT], bT)
            ss_t = k.sb("ss_t", [128, 8], F32); bss = Buf("ss")
            epsT = k.sb("epsT", [128, 1], F32)
            k.op("dve", lambda e: e.memset(epsT[:], EPS), [], [bT])
            Rf = k.sb("Rf", [128, 2, 128], F32); bRf = Buf("Rf")
            Rb = k.sb("Rb", [128, 2, 128], BF16); bRb = Buf("Rb")

            for dc in range(8):
                k.dma("pool", Wo[:, dc, :], w_out[dc * 128:(dc + 1) * 128, :], [], [bWo], bWo)

            def rms_stats(xt, bxt, npart, col, jk, bjk, width=1024, scale=1.0 / 1024):
                k.op("dve", lambda e: e.memset(ss_t[:npart, col:col + 1], 0.0), [], [bss])
                k.op("act", lambda e: e.activation(jk[:npart, 0:width], xt, AF.Square, accum_out=ss_t[:npart, col:col + 1]), [bxt], [bjk, bss])
                k.op("act", lambda e: e.activation(ss_t[:npart, col:col + 1], ss_t[:npart, col:col + 1], AF.Sqrt, bias=epsT[:npart, :], scale=scale), [bss, bT], [bss])
                k.op("dve", lambda e: e.reciprocal(ss_t[:npart, col:col + 1], ss_t[:npart, col:col + 1]), [bss], [bss])

            with contextlib.ExitStack() as s0:
                Wi0 = k.sb("Wi0", [128, 8, 3072], BF16, s0)
                for dc in range(8):
                    k.dma("pool", Wi0[:, dc, :], w_in[dc * 128:(dc + 1) * 128, :], [], [bWi], bWi)
                k.op("dve", lambda e: e.tensor_scalar_mul(Wi0[:, :, 256:512], Wi0[:, :, 256:512], 0.125), [bWi], [bWi])
                for dc in range(8):
                    k.dma("pool", sc_wi[dc], Wi0[:, dc, :], [bWi], [bscwi], bWi)
                cin = k.sb("cin", [68, D], F32, s0); bcin = Buf("cin")
                cT = k.sb("cT", [128, 8, 68], F32, s0); bcT = Buf("cT")
                badT = k.sb("badT", [128, 48], F32, s0); bbad = Buf("badT")
                n1T = k.sb("n1T", [128, 8], F32, s0)
                n2T = k.sb("n2T", [128, 8], F32, s0)
                wst = [k.sb(f"wst{i}", [128, 8, 512], F32, s0) for i in range(2)]
                bwst = [Buf(f"wst{i}") for i in range(2)]
                k.dma("sp", cin[:], call, [], [bcin], bcin)
                k.dma("sp", badT[:], b_ada.rearrange("(c p) -> p c", p=128), [], [bbad], bbad, allow_slow_non_contiguous=True)
                k.dma("sp", n1T[:], n1g.rearrange("(c p) -> p c", p=128), [], [bbad], bbad, allow_slow_non_contiguous=True)
                k.dma("sp", n2T[:], n2g.rearrange("(c p) -> p c", p=128), [], [bbad], bbad, allow_slow_non_contiguous=True)
                k.op("act", lambda e: e.activation(cin[:], cin[:], AF.Silu), [bcin], [bcin])
                for c in range(8):
                    pbi = c // 4
                    k.tr(PB[pbi][:, (c % 4) * 68:(c % 4 + 1) * 68], cin[:, c * 128:(c + 1) * 128], T["ident_f"][0:68, 0:68],
                         [bcin, bT], [bPB[pbi]], last=(c % 4 == 3))
                for pbi in range(2):
                    k.op("dve", lambda e, pbi=pbi: e.tensor_copy(cT[:, pbi * 4:(pbi + 1) * 4, :], PB[pbi][:, 0:4 * 68].rearrange("p (c n) -> p c n", n=68)),
                         [bPB[pbi]], [bcT])
                for blk in range(12):
                    ws, bws = wst[blk % 2], bwst[blk % 2]
                    k.dma("sp", ws[:], w_ada[:, blk * 512:(blk + 1) * 512].rearrange("(c p) n -> p c n", p=128), [], [bws], bws)
                    pb = 2 + (blk % 2)
                    for fc in range(4):
                        for dc in range(8):
                            k.mm(PB[pb][:, fc * 68:(fc + 1) * 68], ws[:, dc, fc * 128:(fc + 1) * 128], cT[:, dc, :],
                                 [bws, bcT], [bPB[pb]], start=(dc == 0), stop=(dc == 7), last=(dc == 7 and fc == 3))
                    for fc in range(4):
                        ch = blk * 4 + fc
                        k.op("act", lambda e, ch=ch, fc=fc, pb=pb: e.activation(modT[:, ch, :], PB[pb][:, fc * 68:(fc + 1) * 68], AF.Identity,
                                                                                 bias=badT[:, ch:ch + 1]), [bPB[pb], bbad], [bmodT])
                for c in range(8):
                    k.op("dve", lambda e, c=c: e.tensor_scalar(G1T[:, c, :], modT[:, 8 + c, :], 1.0, n1T[:, c:c + 1], op0=ALU.add, op1=ALU.mult),
                         [bmodT, bbad], [bG1T])
                    k.op("dve", lambda e, c=c: e.tensor_scalar(G2T[:, c, :], modT[:, 32 + c, :], 1.0, n2T[:, c:c + 1], op0=ALU.add, op1=ALU.mult),
                         [bmodT, bbad], [bG2T])
                stg = [k.sb(f"stg{i}", [128, 2, 8, 128], BF16, s0) for i in range(2)]
                std = [k.sb(f"std{i}", [128, 1024], BF16, s0) for i in range(2)]
                bstg = [Buf(f"stg{i}") for i in range(2)]
                bstd = [Buf(f"std{i}") for i in range(2)]
                bsc = [Buf(f"sc{j}") for j in range(NJ)]
                for j in range(NJ):
                    sg_, bsg_ = stg[j % 2], bstg[j % 2]
                    sd_, bsd_ = std[j % 2], bstd[j % 2]
                    k.dma("pool", sg_[:, 0, :, :], w_gate[:, j * 128:(j + 1) * 128].rearrange("(c p) n -> p c n", p=128), [], [bsg_], bsg_)
                    k.dma("pool", sg_[:, 1, :, :], w_up[:, j * 128:(j + 1) * 128].rearrange("(c p) n -> p c n", p=128), [], [bsg_], bsg_)
                    k.dma("pool", sd_[:], w_down[j * 128:(j + 1) * 128, :], [], [bsd_], bsd_)
                    k.dma("pool", sc_gu[j], sg_[:].rearrange("p a c n -> p (a c n)"), [bsg_], [bsc[j]], bsg_)
                    k.dma("pool", sc_d[j], sd_[:], [bsd_], [bsc[j]], bsd_)
                k.barrier()
            chk(0)

            def ffn_group(s1, nt, ntok, X1, bX1, h2T, bh2T, ffT, bffT, ring, bring, g2tile, bg2, out_fn, FG, yst, byst, jk, bjk):
                for j in range(NJ):
                    slot = j % len(ring)
                    gu, dd = ring[slot]
                    bsl = bring[slot]
                    k.dma("sp", gu[:].rearrange("p a c n -> p (a c n)"), sc_gu[j], [bsc[j]], [bsl], bsl)
                    for a in range(2):
                        for dc in range(8):
                            k.mm(PB[a][:, 0:ntok], gu[:, a, dc, :], h2T[:, dc, 0:ntok], [bsl, bh2T], [bPB[a]],
                                 start=(dc == 0), stop=(dc == 7), last=(dc == 7))
                    k.op("act", lambda e, j=j: e.activation(ffT[:, j, 0:ntok], PB[0][:, 0:ntok], AF.Silu), [bPB[0]], [bffT])
                    k.op("dve", lambda e, j=j: e.tensor_tensor(ffT[:, j, 0:ntok], ffT[:, j, 0:ntok], PB[1][:, 0:ntok], ALU.mult),
                         [bffT, bPB[1]], [bffT])
                for j in range(NJ):
                    slot = j % len(ring)
                    gu, dd = ring[slot]
                    bsl = bring[slot]
                    k.dma("sp", dd[:], sc_d[j], [bsc[j]], [bsl], bsl)
                    for ti in range(nt):
                        np_ = min(128, ntok - ti * 128)
                        for nb in range(2):
                            k.mm(PB[2 * ti + nb][:np_, :], ffT[:, j, ti * 128:ti * 128 + np_], dd[:, nb * 512:(nb + 1) * 512],
                                 [bsl, bffT], [bPB[2 * ti + nb]], start=(j == 0), stop=(j == NJ - 1), last=(j == NJ - 1 or (ti == nt - 1 and nb == 1)))
                for ti in range(nt):
                    np_ = min(128, ntok - ti * 128)
                    x1 = X1[ti]
                    for nb in range(2):
                        cs = slice(nb * 512, (nb + 1) * 512)
                        k.op("dve", lambda e, nb=nb, cs=cs: e.tensor_tensor(yst[:np_, cs], PB[2 * ti + nb][:np_, :], g2tile[:np_, cs], ALU.mult),
                             [bPB[2 * ti + nb], bg2], [byst])
                    k.op("dve", lambda e: e.tensor_tensor(yst[:np_, :], yst[:np_, :], x1[:np_, :], ALU.add), [byst, bX1[ti]], [byst])
                    rms_stats(yst[:np_, :], byst, np_, 3, jk, bjk)
                    k.op("dve", lambda e: e.scalar_tensor_tensor(yst[:np_, :], yst[:np_, :], ss_t[:np_, 3:4], FG[:np_, :], op0=ALU.mult, op1=ALU.mult),
                         [byst, bss, bFG], [byst])
                    out_fn(ti, yst, byst, np_)


            for b in range(NB if nb_run is None else nb_run):
                bvp = Buf(f"vp{b}")
                with contextlib.ExitStack() as sa:
                    Wi = k.sb("Wi", [128, 8, 3072], BF16, sa)
                    Xt = [k.sb(f"Xt{i}", [128, 1024], F32, sa) for i in range(2)]
                    bXt = [Buf(f"Xt{i}") for i in range(2)]
                    for i in range(2):
                        k.dma("sp", Xt[i][:], xp[b, i * 128:(i + 1) * 128, :], [], [bXt[i]], bXt[i])
                    bWic = [Buf(f"Wic{i}") for i in range(6)]
                    for i in range(6):
                        k.dma("sp", Wi[:, :, i * 512:(i + 1) * 512], sc_wi[:, :, i * 512:(i + 1) * 512].rearrange("c p n -> p c n"),
                              [bscwi], [bWic[i]], bWic[i])
                    xn = k.sb("xn", [128, 1024], BF16, sa); bxn = Buf("xn")
                    hTs_ = [k.sb(f"hT{i}", [128, 8, 128], BF16, sa) for i in range(2)]; bhTs_ = [Buf(f"hT{i}") for i in range(2)]
                    Rb1 = k.sb("Rb1", [128, 2, 128], BF16, sa); bRb1 = Buf("Rb1")
                    avb = k.sb("avb", [128, 512], BF16, sa); bavb = Buf("avb")
                    Rbs = [Rb, Rb1]; bRbs = [bRb, bRb1]
                    rqk = k.sb("rqk", [128, 512], BF16, sa); brqk = Buf("rqk")
                    rt = [k.sb(f"rt{i}", [128, 8, 32], F32, sa) for i in range(4)]
                    brt = Buf("rt")
                    kz = k.sb("kz", [128, 256], BF16, sa); bkz = Buf("kz")
                    rvb = k.sb("rvb", [128, 512], BF16, sa); brvb = Buf("rvb")
                    sgt = k.sb("sgt", [128, 512], BF16, sa); bsgt = Buf("sgt")
                    rqkT = k.sb("rqkT", [128, 4, 128], BF16, sa); brqkT = Buf("rqkT")
                    rqm = k.sb("rqm", [128, 2, 2, 128], BF16, sa); brqm = Buf("rqm")
                    rqxT = k.sb("rqxT", [128, 2, 2, 128], BF16, sa); brqxT = Buf("rqxT")
                    scm = k.sb("scm", [128, 512], BF16, sa); bscm = Buf("scm")
                    rety = k.sb("rety", [128, 512], BF16, sa); brety = Buf("rety")
                    aqkf = [k.sb(f"aqkf{i}", [128, 512], F32, sa) for i in range(1)]
                    baqkf = [Buf(f"aqkf{i}") for i in range(1)]
                    aqkb = k.sb("aqkb", [128, 1024], BF16, sa); baqkb = Buf("aqkb")
                    avf = [k.sb(f"avf{i}", [128, 512], F32, sa) for i in range(1)]
                    bavf = [Buf(f"avf{i}") for i in range(1)]
                    aqkT = k.sb("aqkT", [128, 8, S], BF16, sa); baqkT = Buf("aqkT")
                    Xacc = k.sb("Xacc", [128, S], F32, sa); bXacc = Buf("Xacc")
                    Va = [k.sb(f"Va{i}", [128, 128], BF16, sa) for i in range(12)]
                    bVa = [Buf(f"Va{i}") for i in range(12)]
                    Et = [k.sb(f"Et{i}", [128, 256], BF16, sa) for i in range(5)]
                    bEt = [Buf(f"Et{i}") for i in range(5)]
                    rct = k.sb("rct", [128, 256], F32, sa); brct = Buf("rct")
                    for i in range(len(Va)):
                        k.op("dve", lambda e, i=i: e.memset(Va[i][:], 1.0), [], [bVa[i]])
                    k.op("dve", lambda e: e.memset(rqm[:], 0.0), [], [brqm])
                    k.op("dve", lambda e: e.memset(Rf[:], 0.0), [], [bRf])
                    k.op("dve", lambda e: e.memset(Rb[:], 0.0), [], [bRb])
                    k.op("dve", lambda e: e.memset(Rb1[:], 0.0), [], [bRb1])

                    ZB = [0, 1, 2, 3, 4, 1]
                    STB = 5
                    TPB = 6
                    RB = 7

                    def xload(t):
                        X, bX = Xt[t % 2], bXt[t % 2]
                        k.dma("sp", X[:], xp[b, t * 128:(t + 1) * 128, :], [], [bX], bX)

                    def s1(t):
                        X, bX = Xt[t % 2], bXt[t % 2]
                        hT, bhT = hTs_[t % 2], bhTs_[t % 2]
                        rms_stats(X[:], bX, 128, 0, aqkb, baqkb)
                        k.op("dve", lambda e: e.tensor_scalar_mul(xn[:], X[:], ss_t[:, 0:1]), [bX, bss], [bxn])
                        tp = pbf(TPB)
                        for c in range(8):
                            k.tr(tp[:, c * 128:(c + 1) * 128], xn[:, c * 128:(c + 1) * 128], T["ident_b"][:], [bxn, bT], [bPB[TPB]], last=(c == 7))
                        k.op("dve", lambda e: e.tensor_tensor(hT[:], tp[:].rearrange("p (c n) -> p c n", n=128),
                                                              G1T[:, :, 64 + b:65 + b].to_broadcast([128, 8, 128]), ALU.mult), [bPB[TPB], bG1T], [bhT])
                        k.op("dve", lambda e: e.tensor_tensor(hT[:], hT[:], modT[:, 0:8, 64 + b:65 + b].to_broadcast([128, 8, 128]), ALU.add), [bhT, bmodT], [bhT])

                    def zc(t, i):
                        hT, bhT = hTs_[t % 2], bhTs_[t % 2]
                        for dc in range(8):
                            k.mm(PB[ZB[i]][:], hT[:, dc, :], Wi[:, dc, i * 512:(i + 1) * 512], [bhT, bWic[i]], [bPB[ZB[i]]],
                                 start=(dc == 0), stop=(dc == 7), last=(dc == 7))

                    brts_ = [Buf(f"rt{i}") for i in range(4)]

                    def rot(src, bsrc, dst, bdst, cosn, sinn, t, bdst2=None):
                        zv = src.rearrange("p (h two f) -> p h two f", two=2, f=32)
                        x1v, x2v = zv[:, :, 0, :], zv[:, :, 1, :]
                        ov = dst.rearrange("p (h two f) -> p h two f", two=2, f=32)
                        cb = T[cosn][:, t, :].unsqueeze(1).to_broadcast([128, 8, 32])
                        sb_ = T[sinn][:, t, :].unsqueeze(1).to_broadcast([128, 8, 32])
                        r0, r1_, r2, r3 = (rt[i][:, 0:8, :] for i in range(4))
                        k.op("dve", lambda e: e.tensor_tensor(r0, x1v, cb, ALU.mult), [bsrc, bT], [brts_[0]])
                        k.op("dve", lambda e: e.tensor_tensor(r1_, x2v, sb_, ALU.mult), [bsrc, bT], [brts_[1]])
                        k.op("pool", lambda e: e.tensor_tensor(ov[:, :, 0, :], r0, r1_, ALU.subtract), [brts_[0], brts_[1]], [bdst])
                        k.op("dve", lambda e: e.tensor_tensor(r2, x1v, sb_, ALU.mult), [bsrc, bT], [brts_[2]])
                        k.op("dve", lambda e: e.tensor_tensor(r3, x2v, cb, ALU.mult), [bsrc, bT], [brts_[3]])
                        k.op("pool", lambda e: e.tensor_tensor(ov[:, :, 1, :], r2, r3, ALU.add), [brts_[2], brts_[3]], [bdst2 if bdst2 is not None else bdst])

                    def r1(t):
                        rot(PB[ZB[0]][:], bPB[ZB[0]], rqk[:], brqk, "cos_r", "sin_r", t)
                        k.op("pool", lambda e: e.tensor_tensor(kz[:].rearrange("p (h f) -> p h f", f=64),
                                                               rqk[:, 256:512].rearrange("p (h f) -> p h f", f=64),
                                                               T["zeta"][:].unsqueeze(2).to_broadcast([128, 4, 64]), ALU.mult), [brqk, bT], [bkz])
                        k.op("act", lambda e: e.activation(rvb[:], PB[ZB[1]][:], AF.Copy), [bPB[ZB[1]]], [brvb])
                        k.op("act", lambda e: e.activation(sgt[:], PB[ZB[2]][:], AF.Silu), [bPB[ZB[2]]], [bsgt])
                        tp = pbf(TPB)
                        for c in range(4):
                            k.tr(tp[:, c * 128:(c + 1) * 128], rqk[:, c * 128:(c + 1) * 128], T["ident_b"][:], [brqk, bT], [bPB[TPB]], last=(c == 3))
                        k.op("act", lambda e: e.activation(rqkT[:].rearrange("p c n -> p (c n)"), tp[:, 0:512], AF.Copy), [bPB[TPB]], [brqkT])
                        k.op("act", lambda e: e.activation(rqm[0:64, 0, :, :].rearrange("p c n -> p (c n)"), tp[0:64, 0:256], AF.Copy), [bPB[TPB]], [brqm])
                        k.op("act", lambda e: e.activation(rqm[64:128, 1, :, :].rearrange("p c n -> p (c n)"), tp[64:128, 0:256], AF.Copy), [bPB[TPB]], [brqm])
                        for vv in range(2):
                            k.op("pool", lambda e, vv=vv: e.tensor_tensor(rqxT[:, vv, :, :], rqm[:, vv, :, :], T["xit"][:], ALU.mult), [brqm, bT], [brqxT])

                    def a1a(t):
                        av, bav = avf[0], bavf[0]
                        k.op("act", lambda e: e.activation(av[:], PB[ZB[5]][:], AF.Copy), [bPB[ZB[5]]], [bav])
                        k.dma("pool", vp[b, t * 128:(t + 1) * 128, :], av[:], [bav], [], bav, final=True)
                        k.op("act", lambda e: e.activation(avb[:], PB[ZB[5]][:], AF.Copy), [bPB[ZB[5]]], [bavb])
                        k.dma("pool", vbs[b, t * 128:(t + 1) * 128, :], avb[:], [bavb], [bvp], bavb)
                        rot(PB[ZB[3]][:], bPB[ZB[3]], aqkb[:, 0:512], baqkb, "cos_a", "sin_a", t)

                    def a1b(t):
                        af, baf = aqkf[0], baqkf[0]
                        rot(PB[ZB[4]][:], bPB[ZB[4]], af[:, 0:512], baf, "cos_a", "sin_a", t)
                        k.dma("pool", kp[b, t * 128:(t + 1) * 128, :], af[:, 0:512], [baf], [], baf, final=True)
                        k.op("act", lambda e: e.activation(aqkb[:, 512:1024], af[:, 0:512], AF.Copy), [baf], [baqkb])

                    def r2(t):
                        for hd in range(4):
                            c, hh = hd // 2, hd % 2
                            k.mm(PB[RB][:, hd * 128:(hd + 1) * 128], rqkT[:, 2 + c, :], rqm[:, hh, c, :], [brqkT, brqm], [bPB[RB]], last=(hd == 3))
                        k.op("dve", lambda e: e.tensor_tensor(scm[:], PB[RB][:], T["intra"][:].rearrange("p h n -> p (h n)"), ALU.mult), [bPB[RB], bT], [bscm])

                    def r3a(t):
                        for hd in range(4):
                            c, hh = hd // 2, hd % 2
                            cs = slice(hd * 128, (hd + 1) * 128)
                            k.mm(PB[RB][:, cs], scm[:, cs], rvb[:, cs], [bscm, brvb], [bPB[RB]], start=True, stop=False, last=False)
                            k.mm(PB[RB][:, cs], rqxT[:, hh, c, :], Rbs[t % 2][:, c, :], [brqxT, bRbs[t % 2]], [bPB[RB]], start=False, stop=True, last=(hd == 3))
                        for hd in range(4):
                            cs = slice(hd * 128, (hd + 1) * 128)
                            k.op("dve", lambda e, hd=hd: e.memset(ss_t[:, 4 + hd:5 + hd], 0.0), [], [bss])
                            k.op("act", lambda e, hd=hd, cs=cs: e.activation(scm[:, cs], PB[RB][:, cs], AF.Square, accum_out=ss_t[:, 4 + hd:5 + hd]), [bPB[RB]], [bscm, bss])
                        k.op("act", lambda e: e.activation(ss_t[:, 4:8], ss_t[:, 4:8], AF.Sqrt, bias=epsT[:], scale=1.0 / 128), [bss, bT], [bss])
                        k.op("dve", lambda e: e.reciprocal(ss_t[:, 4:8], ss_t[:, 4:8]), [bss], [bss])
                        for hd in range(4):
                            cs = slice(hd * 128, (hd + 1) * 128)
                            k.op("dve", lambda e, hd=hd, cs=cs: e.scalar_tensor_tensor(rety[:, cs], PB[RB][:, cs], ss_t[:, 4 + hd:5 + hd], sgt[:, cs],
                                                                                        op0=ALU.mult, op1=ALU.mult), [bPB[RB], bss, bsgt], [brety])

                    def r3b(t):
                        for c in range(2):
                            k.mm(PB[STB][:, c * 256:(c + 1) * 256], kz[:, c * 128:(c + 1) * 128], rvb[:, c * 256:(c + 1) * 256], [bkz, brvb], [bPB[STB]], last=(c == 1))
                        for c in range(2):
                            for hh in range(2):
                                rows = slice(hh * 64, (hh + 1) * 64)
                                k.op("dve", lambda e, c=c, hh=hh, rows=rows: e.scalar_tensor_tensor(
                                    Rf[rows, c, :], Rf[rows, c, :], T["gc"][rows, c:c + 1], PB[STB][rows, c * 256 + hh * 128:c * 256 + (hh + 1) * 128],
                                    op0=ALU.mult, op1=ALU.add), [bRf, bT, bPB[STB]], [bRf])
                        k.op("act", lambda e: e.activation(Rbs[(t + 1) % 2][:], Rf[:], AF.Copy), [bRf], [bRbs[(t + 1) % 2]])

                    def r4(t):
                        tp = pbf(TPB)
                        for c in range(4):
                            k.tr(tp[:, c * 128:(c + 1) * 128], rety[:, c * 128:(c + 1) * 128], T["ident_b"][:], [brety, bT], [bPB[TPB]], last=(c == 3))
                        k.op("act", lambda e: e.activation(mixT[:, 0:4, t * 128:(t + 1) * 128], tp[:, 0:512].rearrange("p (c n) -> p c n", n=128), AF.Copy),
                             [bPB[TPB]], [bmixT])

                    def a2(t):
                        tp = pbf(TPB)
                        for c in range(8):
                            k.tr(tp[:, c * 128:(c + 1) * 128], aqkb[:, c * 128:(c + 1) * 128], T["ident_b"][:], [baqkb, bT], [bPB[TPB]], last=(c == 7))
                        k.op("act", lambda e: e.activation(aqkT[:, :, t * 128:(t + 1) * 128], tp[:].rearrange("p (c n) -> p c n", n=128), AF.Copy),
                             [bPB[TPB]], [baqkT])

                    s1(0)
                    for i in range(5):
                        zc(0, i)
                    for t in range(NT):
                        nxt = t + 1 < NT
                        if t + 2 < NT:
                            xload(t + 2)
                        if nxt:
                            s1(t + 1)
                        r1(t)
                        zc(t, 5)
                        a1a(t)
                        r2(t)
                        if nxt:
                            zc(t + 1, 0)
                        a1b(t)
                        if nxt:
                            zc(t + 1, 1)
                        r3a(t)
                        if nxt:
                            zc(t + 1, 2)
                            zc(t + 1, 3)
                        r3b(t)
                        if nxt:
                            zc(t + 1, 4)
                        r4(t)
                        a2(t)
                        chk(1)
                    chk(2)
                    k.dma("sp", rp[b].rearrange("(c hh) kk v -> (hh kk) c v", hh=2), Rf[:], [bRf], [], bRf, final=True)

                    its = []
                    for hd in range(8):
                        for (dil, ntile_sub) in ((16, 1), (4, 4), (1, 16)):
                            for a in range(16):
                                its.append((hd, dil, ntile_sub, a))
                    hbS = [bPB[i] for i in range(4)]
                    hbX = [bPB[4 + i] for i in range(4)]
                    LA = 3
                    LV = 2
                    assert LA + LV < len(Va) // 2 and LA + 1 < len(Et)
                    NVA = len(Va) // 2

                    def prm(i):
                        hd, dil, ntile_sub, a = its[i]
                        c, hh = hd // 2, hd % 2
                        sub, nbk = a // ntile_sub, a % ntile_sub
                        start = dil * 128 * nbk + sub
                        nq = 256 if nbk < ntile_sub - 1 else 128
                        kc = slice(start, start + 127 * dil + 1, dil)
                        qc = slice(start, start + (nq - 1) * dil + 1, dil)
                        rows = slice(hh * 64, (hh + 1) * 64)
                        vi = hh * NVA + (i % NVA)
                        return hd, dil, c, hh, nq, kc, qc, rows, vi

                    def stV(i):
                        hd, dil, c, hh, nq, kc, qc, rows, vi = prm(i)
                        k.dma("sp", Va[vi][:, hh * 64:(hh + 1) * 64], vbs[b, kc, hd * 64:(hd + 1) * 64], [bvp], [bVa[vi]], bVa[vi])

                    def stS(i):
                        hd, dil, c, hh, nq, kc, qc, rows, vi = prm(i)
                        ps_s = PB[i % 4][:, 0:nq]
                        et, bet = Et[i % len(Et)], bEt[i % len(Et)]
                        k.mm(ps_s, aqkT[rows, 4 + c, kc], aqkT[rows, c, qc], [baqkT], [hbS[i % 4]])
                        k.op("act", lambda e: e.activation(et[:, 0:nq], ps_s, AF.Exp, scale=0.125), [hbS[i % 4]], [bet])
                        k.op("pool", lambda e: e.tensor_tensor(et[:, 0:nq], et[:, 0:nq], T["amask"][:, 0:nq], ALU.mult), [bet, bT], [bet])

                    def stX(i):
                        hd, dil, c, hh, nq, kc, qc, rows, vi = prm(i)
                        ps_x = PB[4 + i % 4][:, 0:nq]
                        et, bet = Et[i % len(Et)], bEt[i % len(Et)]
                        k.mm(ps_x, Va[vi][:], et[:, 0:nq], [bVa[vi], bet], [hbX[i % 4]])
                        if dil == 16:
                            k.op("act", lambda e: e.activation(Xacc[:, qc], ps_x, AF.Copy), [hbX[i % 4]], [bXacc])
                        else:
                            k.op("dve", lambda e: e.tensor_tensor(Xacc[:, qc], Xacc[:, qc], ps_x, ALU.add), [bXacc, hbX[i % 4]], [bXacc])
                        if i % 48 == 47:
                            drows = slice((1 - hh) * 64, (2 - hh) * 64)
                            for pc in range(8):
                                cs = slice(pc * 256, (pc + 1) * 256)
                                k.op("dve", lambda e: e.reciprocal(rct[rows, :], Xacc[drows, cs]), [bXacc], [brct])
                                k.op("dve", lambda e: e.tensor_tensor(mixT[rows, 4 + c, cs], Xacc[rows, cs], rct[rows, :], ALU.mult), [bXacc, brct], [bmixT])

                    n_it = len(its)
                    for i in range(min(LV, n_it)):
                        stV(i)
                    for i in range(n_it + LA):
                        if i + LV < n_it:
                            stV(i + LV)
                        if i < n_it:
                            stS(i)
                        if i - LA >= 0:
                            stX(i - LA)
                    k.barrier()
                chk(3)

                with contextlib.ExitStack() as sc_:
                    gates = k.sb("gates", [128, 2, 1024], F32, sc_)
                    FG = k.sb("FG", [128, 1024], F32, sc_)
                    k.dma("sp", FG[:], fgd.unsqueeze(0).to_broadcast([128, 1024]), [], [bFG], bFG)
                    for gi, vec in enumerate((2, 5)):
                        for c in range(8):
                            pbi = gi * 2 + c // 4
                            k.mm(PB[pbi][:, (c % 4) * 128:(c % 4 + 1) * 128],
                                 modT[:, vec * 8 + c, 64 + b:65 + b].to_broadcast([128, 128]), T["ident_f"][:],
                                 [bmodT, bT], [bPB[pbi]], last=(c % 4 == 3))
                        for hf in range(2):
                            k.op("act", lambda e, gi=gi, hf=hf: e.activation(gates[:, gi, hf * 512:(hf + 1) * 512], PB[gi * 2 + hf][:], AF.Copy),
                                 [bPB[gi * 2 + hf]], [bgates])
                    X1 = [k.sb(f"X1_{i}", [128, 1024], F32, sc_) for i in range(8)]
                    bX1 = [Buf(f"X1_{i}") for i in range(8)]
                    Xr = [k.sb(f"Xr{i}", [128, 1024], F32, sc_) for i in range(2)]
                    bXr = [Buf(f"Xr{i}") for i in range(2)]
                    xn2s = [k.sb(f"xn2_{i}", [128, 1024], BF16, sc_) for i in range(2)]; bxn2s = [Buf(f"xn2_{i}") for i in range(2)]
                    jk2 = k.sb("jk2", [128, 1024], BF16, sc_); bjk2 = Buf("jk2")
                    h2Ts = [k.sb(f"h2T{i}", [128, 8, 512], BF16, sc_) for i in range(2)]
                    bh2Ts = [Buf(f"h2T{i}") for i in range(2)]
                    ffT = k.sb("ffT", [128, NJ, 512], BF16, sc_); bffT = Buf("ffT")
                    ring = [(k.sb(f"gu{i}", [128, 2, 8, 128], BF16, sc_), k.sb(f"dd{i}", [128, 1024], BF16, sc_)) for i in range(3)]
                    bring = [Buf(f"ring{i}") for i in range(3)]
                    rcnt = [0]

                    def nslot():
                        i = rcnt[0] % len(ring)
                        rcnt[0] += 1
                        return ring[i][0], ring[i][1], bring[i]

                    def pro(g, ti):
                        t = g * 4 + ti
                        tcs = slice(t * 128, (t + 1) * 128)
                        tpb = 4
                        x1, bx1 = X1[(g % 2) * 4 + ti], bX1[(g % 2) * 4 + ti]
                        h2T, bh2T = h2Ts[g % 2], bh2Ts[g % 2]
                        xn2, bxn2 = xn2s[ti % 2], bxn2s[ti % 2]
                        for nb in range(2):
                            for kc_ in range(8):
                                k.mm(PB[2 + nb][:], mixT[:, kc_, tcs], Wo[:, kc_, nb * 512:(nb + 1) * 512], [bmixT, bWo], [bPB[2 + nb]],
                                     start=(kc_ == 0), stop=(kc_ == 7), last=(kc_ == 7))
                        xr, bxr = Xr[ti % 2], bXr[ti % 2]
                        k.dma("sp", xr[:], xp[b, tcs, :], [], [bxr], bxr)
                        for nb in range(2):
                            cs = slice(nb * 512, (nb + 1) * 512)
                            k.op("dve", lambda e, nb=nb, cs=cs: e.tensor_tensor(x1[:, cs], PB[2 + nb][:], gates[:, 0, cs], ALU.mult),
                                 [bPB[2 + nb], bgates], [bx1])
                        k.op("dve", lambda e: e.tensor_tensor(x1[:], x1[:], xr[:], ALU.add), [bx1, bxr], [bx1])
                        rms_stats(x1[:], bx1, 128, 1, xn2, bxn2)
                        k.op("dve", lambda e: e.tensor_scalar_mul(xn2[:], x1[:], ss_t[:, 1:2]), [bx1, bss], [bxn2])

                    def pro_b(g, ti):
                        tpb = 4
                        h2T, bh2T = h2Ts[g % 2], bh2Ts[g % 2]
                        xn2, bxn2 = xn2s[ti % 2], bxn2s[ti % 2]
                        tp = pbf(tpb)
                        for c in range(8):
                            k.tr(tp[:, c * 128:(c + 1) * 128], xn2[:, c * 128:(c + 1) * 128], T["ident_b"][:], [bxn2, bT], [bPB[tpb]], last=(c == 7))
                        for c in range(8):
                            k.op("act", lambda e, c=c: e.activation(h2T[:, c, ti * 128:(ti + 1) * 128], tp[:, c * 128:(c + 1) * 128], AF.Identity,
                                                                     scale=G2T[:, c, 64 + b:65 + b], bias=modT[:, 24 + c, 64 + b:65 + b]),
                                 [bPB[tpb], bG2T, bmodT], [bh2T])

                    def gu_(g, j):
                        h2T, bh2T = h2Ts[g % 2], bh2Ts[g % 2]
                        gu, dd, bsl = nslot()
                        k.dma("sp", gu[:].rearrange("p a c n -> p (a c n)"), sc_gu[j], [bsc[j]], [bsl], bsl)
                        for a in range(2):
                            for dc in range(8):
                                k.mm(PB[a][:], gu[:, a, dc, :], h2T[:, dc, :], [bsl, bh2T], [bPB[a]],
                                     start=(dc == 0), stop=(dc == 7), last=(dc == 7))
                        k.op("act", lambda e: e.activation(ffT[:, j, :], PB[0][:], AF.Silu), [bPB[0]], [bffT])
                        k.op("dve", lambda e: e.tensor_tensor(ffT[:, j, :], ffT[:, j, :], PB[1][:], ALU.mult), [bffT, bPB[1]], [bffT])

                    def dn_(g, j):
                        gu, dd, bsl = nslot()
                        k.dma("sp", dd[:], sc_d[j], [bsc[j]], [bsl], bsl)
                        for ti in range(4):
                            for nb in range(2):
                                k.mm(PB[2 * ti + nb][:], ffT[:, j, ti * 128:(ti + 1) * 128], dd[:, nb * 512:(nb + 1) * 512],
                                     [bsl, bffT], [bPB[2 * ti + nb]], start=(j == 0), stop=(j == NJ - 1), last=(j == NJ - 1 or (ti == 3 and nb == 1)))

                    def epi(g, ti):
                        t = g * 4 + ti
                        x1, bx1 = X1[(g % 2) * 4 + ti], bX1[(g % 2) * 4 + ti]
                        for nb in range(2):
                            cs = slice(nb * 512, (nb + 1) * 512)
                            pbx = PB[2 * ti + nb]
                            k.op("dve", lambda e, cs=cs, pbx=pbx: e.tensor_tensor(pbx[:], pbx[:], gates[:, 1, cs], ALU.mult), [bPB[2 * ti + nb], bgates], [bPB[2 * ti + nb]])
                            k.op("dve", lambda e, cs=cs, pbx=pbx: e.tensor_tensor(x1[:, cs], x1[:, cs], pbx[:], ALU.add), [bx1, bPB[2 * ti + nb]], [bx1])
                        rms_stats(x1[:], bx1, 128, 3, jk2, bjk2)
                        k.op("dve", lambda e: e.scalar_tensor_tensor(x1[:], x1[:], ss_t[:, 3:4], FG[:], op0=ALU.mult, op1=ALU.mult), [bx1, bss, bFG], [bx1])
                        k.dma("pool", yp[b, t * 128:(t + 1) * 128, :], x1[:], [bx1], [], bx1, final=True)

                    pro(0, 0)
                    pro(0, 1)
                    pro_b(0, 0)
                    pro(0, 2)
                    pro_b(0, 1)
                    pro(0, 3)
                    pro_b(0, 2)
                    pro_b(0, 3)
                    pend = []
                    for g in range(4):
                        for j in range(NJ):
                            gu_(g, j)
                            if pend:
                                epi(g - 1, pend.pop(0))
                            if g + 1 < 4 and j in (3, 8, 13, 18):
                                pro(g + 1, (j - 3) // 5)
                            if g + 1 < 4 and j in (5, 10, 15, 20):
                                pro_b(g + 1, (j - 5) // 5)
                        for j in range(NJ):
                            dn_(g, j)
                        epi(g, 0)
                        pend = [1, 2, 3]
                        if g == 3:
                            for ti in pend:
                                epi(g, ti)
                            pend = []
                    k.barrier()

            if do_sample:
                TS = {}
                with contextlib.ExitStack() as ss_:
                    for nm in ("cos_sa", "sin_sa", "cos_sr", "sin_sr", "zeta_s", "xit_s", "gc_s", "intra_s", "onehot_s", "smask", "nmask"):
                        TS[nm] = k.sb("TS_" + nm, tabs[nm].shape, _TAB_DT.get(nm, F32), ss_)
                        k.dma("sp", TS[nm][:], td[nm], [], [bT], bT)
                    mods = k.sb("mods", [64, 2, 1024], F32, ss_); bmods = Buf("mods")
                    Xs = k.sb("Xs", [64, 1024], F32, ss_); bXs = Buf("Xs")
                    hs = k.sb("hs", [64, 1024], BF16, ss_); bhs = Buf("hs")
                    hTs = k.sb("hTs", [128, 8, 64], BF16, ss_); bhTs = Buf("hTs")
                    rqk_s = k.sb("rqk_s", [64, 512], BF16, ss_); brqk_s = Buf("rqk_s")
                    rts = [k.sb(f"rts{i}", [64, 8, 32], F32, ss_) for i in range(4)]
                    brts = Buf("rts")
                    aqkf_s = k.sb("aqkf_s", [64, 512], F32, ss_); baqkf_s = Buf("aqkf_s")
                    aqkb_s = k.sb("aqkb_s", [64, 1024], BF16, ss_); baqkb_s = Buf("aqkb_s")
                    avf_s = k.sb("avf_s", [64, 512], F32, ss_); bavf_s = Buf("avf_s")
                    rv_s = k.sb("rv_s", [64, 512], BF16, ss_); brv_s = Buf("rv_s")
                    sg_s = k.sb("sg_s", [64, 512], BF16, ss_); bsg_s = Buf("sg_s")
                    kz_s = k.sb("kz_s", [64, 256], BF16, ss_); bkz_s = Buf("kz_s")
                    kzm = [k.sb(f"kzm{i}", [64, 256], BF16, ss_) for i in range(2)]
                    bkzm = [Buf(f"kzm{i}") for i in range(2)]
                    rqkT_s = k.sb("rqkT_s", [128, 4, 64], BF16, ss_); brqkT_s = Buf("rqkT_s")
                    rqm_s = k.sb("rqm_s", [128, 2, 2, 64], BF16, ss_); brqm_s = Buf("rqm_s")
                    rqx_s = k.sb("rqx_s", [128, 2, 2, 64], BF16, ss_); brqx_s = Buf("rqx_s")
                    scm_s = k.sb("scm_s", [64, 256], BF16, ss_); bscm_s = Buf("scm_s")
                    oT_s = k.sb("oT_s", [128, 256], F32, ss_); boT_s = Buf("oT_s")
                    rety_s = k.sb("rety_s", [64, 512], BF16, ss_); brety_s = Buf("rety_s")
                    Rsf = [k.sb(f"Rsf{i}", [128, 2, 128], F32, ss_) for i in range(2)]
                    bRsf = [Buf(f"Rsf{i}") for i in range(2)]
                    Rsb = k.sb("Rsb", [128, 2, 16, 128], BF16, ss_); bRsb = Buf("Rsb")
                    aqm_s = k.sb("aqm_s", [128, 2, 4, 64], BF16, ss_); baqm_s = Buf("aqm_s")
                    akT_s = k.sb("akT_s", [128, 4, 64], BF16, ss_); bakT_s = Buf("akT_s")
                    van = k.sb("van", [64, 8, 128], BF16, ss_); bvan = Buf("van")
                    Es = k.sb("Es", [128, 9, 8, 4], BF16, ss_); bEs = Buf("Es")
                    En = k.sb("En", [64, 8, 4], BF16, ss_); bEn = Buf("En")
                    rcs = k.sb("rcs", [128, 64], F32, ss_); brcs = Buf("rcs")

                    def tm_mod(dst_slot, src_fn, dst, bdst):
                        for c in range(8):
                            pbi = c // 4
                            k.tr(PB[pbi][0:64, (c % 4) * 128:(c % 4 + 1) * 128], src_fn(c), T["ident_f"][:], [bmodT, bG1T, bG2T, bT], [bPB[pbi]], last=(c % 4 == 3))
                        for pbi in range(2):
                            k.op("act", lambda e, pbi=pbi: e.activation(dst[:, dst_slot, pbi * 512:(pbi + 1) * 512], PB[pbi][0:64, :], AF.Copy), [bPB[pbi]], [bdst])
                    tm_mod(0, lambda c: modT[:, c, 0:64], mods, bmods)
                    tm_mod(1, lambda c: G1T[:, c, 0:64], mods, bmods)
                    k.op("dve", lambda e: e.memset(van[:], 1.0), [], [bvan])
                    k.op("dve", lambda e: e.memset(rqm_s[:], 0.0), [], [brqm_s])
                    k.op("dve", lambda e: e.memset(aqm_s[:], 0.0), [], [baqm_s])

                    k.dma("sp", Xs[:], xs, [], [bXs], bXs)
                    rms_stats(Xs[:], bXs, 64, 0, hs, bhs)
                    k.op("dve", lambda e: e.scalar_tensor_tensor(Xs[:], Xs[:], ss_t[0:64, 0:1], mods[:, 1, :], op0=ALU.mult, op1=ALU.mult), [bXs, bss, bmods], [bXs])
                    k.op("dve", lambda e: e.tensor_tensor(hs[:], Xs[:], mods[:, 0, :], ALU.add), [bXs, bmods], [bhs])
                    tp = pbf(7)
                    for c in range(8):
                        k.tr(tp[:, c * 64:(c + 1) * 64], hs[:, c * 128:(c + 1) * 128], T["ident_b"][0:64, 0:64], [bhs, bT], [bPB[7]], last=(c == 7))
                    k.op("act", lambda e: e.activation(hTs[:].rearrange("p c n -> p (c n)"), tp[:, 0:512], AF.Copy), [bPB[7]], [bhTs])
                    with contextlib.ExitStack() as sw_:
                        Wi = k.sb("Wi_s", [128, 8, 3072], BF16, sw_)
                        bWis = [Buf(f"Wis{i}") for i in range(6)]
                        for i in range(6):
                            k.dma("sp", Wi[:, :, i * 512:(i + 1) * 512], sc_wi[:, :, i * 512:(i + 1) * 512].rearrange("c p n -> p c n"),
                                  [bscwi], [bWis[i]], bWis[i])
                        for nb in range(6):
                            for dc in range(8):
                                k.mm(PB[nb][0:64, :], hTs[:, dc, :], Wi[:, dc, nb * 512:(nb + 1) * 512], [bhTs, bWis[nb]], [bPB[nb]],
                                     start=(dc == 0), stop=(dc == 7), last=(dc == 7))
                        k.barrier()
                    NR = 4
                    Ktb = [k.sb(f"Ktf{i}", [128, 512], F32, ss_) for i in range(NR)]
                    bKtb = [Buf(f"Ktf{i}") for i in range(NR)]
                    Vtf = [k.sb(f"Vtf{i}", [128, 512], F32, ss_) for i in range(NR)]
                    bVtf = [Buf(f"Vtf{i}") for i in range(NR)]
                    Kcb = [k.sb(f"Kcb{i}", [128, 512], BF16, ss_) for i in range(NR)]
                    bKcb = [Buf(f"Kcb{i}") for i in range(NR)]
                    KTs = [k.sb(f"KTs{i}", [128, 4, 128], BF16, ss_) for i in range(NR)]
                    bKTs = [Buf(f"KTs{i}") for i in range(NR)]
                    vas = [k.sb(f"vas{i}", [128, 8, 128], BF16, ss_) for i in range(18)]
                    bvas = [Buf(f"vas{i}") for i in range(18)]
                    for i in range(18):
                        k.op("pool", lambda e, i=i: e.memset(vas[i][:], 1.0), [], [bvas[i]])

                    def rotary(src_ap, dst_ap, cosn, sinn, bsrc, bdst):
                        zv = src_ap.rearrange("p (h two f) -> p h two f", two=2, f=32)
                        x1v, x2v = zv[:, :, 0, :], zv[:, :, 1, :]
                        ov = dst_ap.rearrange("p (h two f) -> p h two f", two=2, f=32)
                        cb = TS[cosn][:].unsqueeze(1).to_broadcast([64, 8, 32])
                        sb_ = TS[sinn][:].unsqueeze(1).to_broadcast([64, 8, 32])
                        r0, r1, r2, r3 = (rts[i][:] for i in range(4))
                        k.op("dve", lambda e: e.tensor_tensor(r0, x1v, cb, ALU.mult), [bsrc, bT], [brts])
                        k.op("dve", lambda e: e.tensor_tensor(r1, x2v, sb_, ALU.mult), [bsrc, bT], [brts])
                        k.op("dve", lambda e: e.tensor_tensor(r2, x1v, sb_, ALU.mult), [bsrc, bT], [brts])
                        k.op("dve", lambda e: e.tensor_tensor(r3, x2v, cb, ALU.mult), [bsrc, bT], [brts])
                        k.op("dve", lambda e: e.tensor_tensor(ov[:, :, 0, :], r0, r1, ALU.subtract), [brts], [bdst])
                        k.op("dve", lambda e: e.tensor_tensor(ov[:, :, 1, :], r2, r3, ALU.add), [brts], [bdst])
                    rotary(PB[0][0:64, :], rqk_s[:], "cos_sr", "sin_sr", bPB[0], brqk_s)
                    rotary(PB[3][0:64, :], aqkb_s[:, 0:512], "cos_sa", "sin_sa", bPB[3], baqkb_s)
                    rotary(PB[4][0:64, :], aqkf_s[:, 0:512], "cos_sa", "sin_sa", bPB[4], baqkf_s)
                    k.dma("sp", kso, aqkf_s[:, 0:512], [baqkf_s], [], baqkf_s, final=True)
                    k.op("act", lambda e: e.activation(aqkb_s[:, 512:1024], aqkf_s[:, 0:512], AF.Copy), [baqkf_s], [baqkb_s])
                    k.op("act", lambda e: e.activation(avf_s[:], PB[5][0:64, :], AF.Copy), [bPB[5]], [bavf_s])
                    k.dma("sp", vso, avf_s[:], [bavf_s], [], bavf_s, final=True)
                    avv = avf_s[:].rearrange("p (h f) -> p h f", f=64)
                    k.op("dve", lambda e: e.tensor_copy(van[:, 0:8:2, 0:64], avv[:, 0:8:2, :]), [bavf_s], [bvan])
                    k.op("dve", lambda e: e.tensor_copy(van[:, 1:8:2, 64:128], avv[:, 1:8:2, :]), [bavf_s], [bvan])
                    k.op("act", lambda e: e.activation(rv_s[:], PB[1][0:64, :], AF.Copy), [bPB[1]], [brv_s])
                    k.op("act", lambda e: e.activation(sg_s[:], PB[2][0:64, :], AF.Silu), [bPB[2]], [bsg_s])
                    k.op("dve", lambda e: e.tensor_tensor(kz_s[:].rearrange("p (h f) -> p h f", f=64),
                                                          rqk_s[:, 256:512].rearrange("p (h f) -> p h f", f=64),
                                                          TS["zeta_s"][:].unsqueeze(2).to_broadcast([64, 4, 64]), ALU.mult), [brqk_s, bT], [bkz_s])
                    tp = pbf(7)
                    for c in range(4):
                        k.tr(tp[:, c * 64:(c + 1) * 64], rqk_s[:, c * 128:(c + 1) * 128], T["ident_b"][0:64, 0:64], [brqk_s, bT], [bPB[7]], last=(c == 3))
                    k.op("act", lambda e: e.activation(rqkT_s[:].rearrange("p c n -> p (c n)"), tp[:, 0:256], AF.Copy), [bPB[7]], [brqkT_s])
                    k.op("act", lambda e: e.activation(rqm_s[0:64, 0, :, :].rearrange("p c n -> p (c n)"), tp[0:64, 0:128], AF.Copy), [bPB[7]], [brqm_s])
                    k.op("act", lambda e: e.activation(rqm_s[64:128, 1, :, :].rearrange("p c n -> p (c n)"), tp[64:128, 0:128], AF.Copy), [bPB[7]], [brqm_s])
                    for vv in range(2):
                        k.op("dve", lambda e, vv=vv: e.tensor_tensor(rqx_s[:, vv, :, :], rqm_s[:, vv, :, :], TS["xit_s"][:], ALU.mult), [brqm_s, bT], [brqx_s])
                    tp = pbf(6)
                    for c in range(8):
                        k.tr(tp[:, c * 64:(c + 1) * 64], aqkb_s[:, c * 128:(c + 1) * 128], T["ident_b"][0:64, 0:64], [baqkb_s, bT], [bPB[6]], last=(c == 7))
                    k.op("act", lambda e: e.activation(aqm_s[0:64, 0, :, :].rearrange("p c n -> p (c n)"), tp[0:64, 0:256], AF.Copy), [bPB[6]], [baqm_s])
                    k.op("act", lambda e: e.activation(aqm_s[64:128, 1, :, :].rearrange("p c n -> p (c n)"), tp[64:128, 0:256], AF.Copy), [bPB[6]], [baqm_s])
                    k.op("act", lambda e: e.activation(akT_s[:].rearrange("p c n -> p (c n)"), tp[:, 256:512], AF.Copy), [bPB[6]], [bakT_s])

                    for hd in range(4):
                        c, hh = hd // 2, hd % 2
                        k.mm(PB[0][0:64, hd * 64:(hd + 1) * 64], rqkT_s[:, 2 + c, :], rqm_s[:, hh, c, :], [brqkT_s, brqm_s], [bPB[0]], last=(hd == 3))
                    k.op("dve", lambda e: e.tensor_tensor(scm_s[:], PB[0][0:64, 0:256], TS["intra_s"][:].rearrange("p h n -> p (h n)"), ALU.mult), [bPB[0], bT], [bscm_s])
                    srv = sr.rearrange("b (c hh) kk v -> (hh kk) c b v", hh=2)
                    for bb in range(16):
                        rf, brf = Rsf[bb % 2], bRsf[bb % 2]
                        k.dma("sp", rf[:], srv[:, :, bb, :], [], [brf], brf)
                        k.op("act", lambda e, bb=bb: e.activation(Rsb[:, :, bb, :], rf[:], AF.Copy), [brf], [bRsb])
                    for hd in range(4):
                        c, hh = hd // 2, hd % 2
                        k.mm(PB[1][:, hd * 64:(hd + 1) * 64], rv_s[:, hd * 128:(hd + 1) * 128], scm_s[:, hd * 64:(hd + 1) * 64], [brv_s, bscm_s], [bPB[1]],
                             start=True, stop=False, last=False)
                        for bb in range(16):
                            k.mm(PB[1][:, hd * 64 + bb * 4:hd * 64 + bb * 4 + 4], Rsb[:, c, bb, :], rqx_s[:, hh, c, bb * 4:(bb + 1) * 4], [bRsb, brqx_s], [bPB[1]],
                                 start=False, stop=(bb == 15), last=(hd == 3 and bb == 15))
                    k.op("act", lambda e: e.activation(oT_s[:], PB[1][:, 0:256], AF.Copy), [bPB[1]], [boT_s])
                    for hd in range(4):
                        k.tr(PB[2][0:64, hd * 128:(hd + 1) * 128], oT_s[:, hd * 64:(hd + 1) * 64], T["ident_f"][:], [boT_s, bT], [bPB[2]], last=(hd == 3))
                    for hd in range(4):
                        cs = slice(hd * 128, (hd + 1) * 128)
                        k.op("dve", lambda e, hd=hd: e.memset(ss_t[0:64, 4 + hd:5 + hd], 0.0), [], [bss])
                        k.op("act", lambda e, hd=hd, cs=cs: e.activation(rety_s[0:64, cs], PB[2][0:64, cs], AF.Square, accum_out=ss_t[0:64, 4 + hd:5 + hd]), [bPB[2]], [brety_s, bss])
                    k.op("act", lambda e: e.activation(ss_t[0:64, 4:8], ss_t[0:64, 4:8], AF.Sqrt, bias=epsT[0:64, :], scale=1.0 / 128), [bss, bT], [bss])
                    k.op("dve", lambda e: e.reciprocal(ss_t[0:64, 4:8], ss_t[0:64, 4:8]), [bss], [bss])
                    for hd in range(4):
                        cs = slice(hd * 128, (hd + 1) * 128)
                        k.op("dve", lambda e, hd=hd, cs=cs: e.scalar_tensor_tensor(rety_s[:, cs], PB[2][0:64, cs], ss_t[0:64, 4 + hd:5 + hd], sg_s[:, cs],
                                                                                    op0=ALU.mult, op1=ALU.mult), [bPB[2], bss, bsg_s], [brety_s])
                    tp = pbf(7)
                    for c in range(4):
                        k.tr(tp[:, c * 64:(c + 1) * 64], rety_s[:, c * 128:(c + 1) * 128], T["ident_b"][0:64, 0:64], [brety_s, bT], [bPB[7]], last=(c == 3))
                    k.op("act", lambda e: e.activation(mixT[:, 0:4, 0:64], tp[:, 0:256].rearrange("p (c n) -> p c n", n=64), AF.Copy), [bPB[7]], [bmixT])
                    rsov = rso.rearrange("b (c hh) kk v -> (hh kk) c b v", hh=2)
                    for bb in range(16):
                        rf, brf = Rsf[bb % 2], bRsf[bb % 2]
                        km, bkm = kzm[bb % 2], bkzm[bb % 2]
                        pbi = 3 + bb % 2
                        k.dma("sp", rf[:], srv[:, :, bb, :], [], [brf], brf)
                        k.op("dve", lambda e, bb=bb: e.tensor_scalar_mul(km[:], kz_s[:], TS["onehot_s"][:, bb:bb + 1]), [bkz_s, bT], [bkm])
                        for c in range(2):
                            k.mm(PB[pbi][:, c * 256:(c + 1) * 256], km[:, c * 128:(c + 1) * 128], rv_s[:, c * 256:(c + 1) * 256], [bkm, brv_s], [bPB[pbi]], last=(c == 1))
                        for c in range(2):
                            for hh in range(2):
                                rows = slice(hh * 64, (hh + 1) * 64)
                                k.op("dve", lambda e, c=c, hh=hh, rows=rows: e.scalar_tensor_tensor(
                                    rf[rows, c, :], rf[rows, c, :], TS["gc_s"][rows, c:c + 1], PB[pbi][rows, c * 256 + hh * 128:c * 256 + (hh + 1) * 128],
                                    op0=ALU.mult, op1=ALU.add), [brf, bT, bPB[pbi]], [brf])
                        k.dma("sp", rsov[:, :, bb, :], rf[:], [brf], [], brf, final=True)

                    for bb in range(16):
                        rowsl = [slice(1920, 2048)] + [slice(1536 + t_, 1536 + t_ + 4 * 127 + 1, 4) for t_ in range(4)] \
                            + [slice(t_, t_ + 16 * 127 + 1, 16) for t_ in range(4)]
                        for ti_, rs_ in enumerate(rowsl):
                            ri = (bb * 9 + ti_) % NR
                            vi_ = (bb % 2) * 9 + ti_
                            kt, bkt = Ktb[ri], bKtb[ri]
                            kT, bkT = KTs[ri], bKTs[ri]
                            k.dma("sp", kt[:], ck[bb, rs_, :], [], [bkt], bkt)
                            vt, bvt = Vtf[ri], bVtf[ri]
                            k.dma("sp", vt[:], cv[bb, rs_, :], [], [bvt], bvt)
                            vtv = vt[:].rearrange("p (h f) -> p h f", f=64)
                            k.op("pool", lambda e: e.tensor_copy(vas[vi_][:, 0:8:2, 0:64], vtv[:, 0:8:2, :]), [bvt], [bvas[vi_]])
                            k.op("dve", lambda e: e.tensor_copy(vas[vi_][:, 1:8:2, 64:128], vtv[:, 1:8:2, :]), [bvt], [bvas[vi_]])
                            kcb, bkcb = Kcb[ri], bKcb[ri]
                            k.op("dve", lambda e: e.tensor_copy(kcb[:], kt[:]), [bkt], [bkcb])
                            tpi = 6 + ti_ % 2
                            tp = pbf(tpi)
                            for c in range(4):
                                k.tr(tp[:, c * 128:(c + 1) * 128], kcb[:, c * 128:(c + 1) * 128], T["ident_b"][:], [bkcb, bT], [bPB[tpi]], last=(c == 3))
                            k.op("act", lambda e: e.activation(kT[:].rearrange("p c n -> p (c n)"), tp[:, 0:512], AF.Copy), [bPB[tpi]], [bkT])
                            for hd in range(8):
                                c, hh = hd // 2, hd % 2
                                k.mm(PB[0][:, ti_ * 32 + hd * 4:ti_ * 32 + hd * 4 + 4], kT[:, c, :], aqm_s[:, hh, c, bb * 4:(bb + 1) * 4], [bkT, baqm_s], [bPB[0]], last=(hd == 7))
                        for hd in range(8):
                            c, hh = hd // 2, hd % 2
                            k.mm(PB[1][0:64, hd * 4:hd * 4 + 4], akT_s[:, c, :], aqm_s[:, hh, c, bb * 4:(bb + 1) * 4], [bakT_s, baqm_s], [bPB[1]], last=(hd == 7))
                        k.op("act", lambda e: e.activation(Es[:].rearrange("p a h t -> p (a h t)"), PB[0][:, 0:288], AF.Exp, scale=0.125), [bPB[0]], [bEs])
                        k.op("dve", lambda e: e.tensor_tensor(Es[:], Es[:], TS["smask"][:].unsqueeze(2).to_broadcast([128, 9, 8, 4]), ALU.mult), [bEs, bT], [bEs])
                        k.op("act", lambda e: e.activation(En[:].rearrange("p h t -> p (h t)"), PB[1][0:64, 0:32], AF.Exp, scale=0.125), [bPB[1]], [bEn])
                        k.op("dve", lambda e, bb=bb: e.tensor_tensor(En[:], En[:], TS["nmask"][:, bb, :].unsqueeze(1).to_broadcast([64, 8, 4]), ALU.mult), [bEn, bT], [bEn])
                        for hd in range(8):
                            oc = slice(hd * 64 + bb * 4, hd * 64 + bb * 4 + 4)
                            for ti_ in range(9):
                                k.mm(PB[5][:, oc], vas[(bb % 2) * 9 + ti_][:, hd, :], Es[:, ti_, hd, :], [bvas[(bb % 2) * 9 + ti_], bEs], [bPB[5]], start=(ti_ == 0), stop=False, last=False)
                            k.mm(PB[5][:, oc], van[:, hd, :], En[:, hd, :], [bvan, bEn], [bPB[5]], start=False, stop=True, last=(hd == 7))
                    for hd in range(8):
                        c, hh = hd // 2, hd % 2
                        rows = slice(hh * 64, (hh + 1) * 64)
                        drows = slice((1 - hh) * 64, (2 - hh) * 64)
                        cs = slice(hd * 64, (hd + 1) * 64)
                        k.op("dve", lambda e: e.reciprocal(rcs[rows, :], PB[5][drows, cs]), [bPB[5]], [brcs])
                        k.op("dve", lambda e: e.tensor_tensor(mixT[rows, 4 + c, 0:64], PB[5][rows, cs], rcs[rows, :], ALU.mult), [bPB[5], brcs], [bmixT])
                    k.barrier()

                with contextlib.ExitStack() as sc_:
                    mods = k.sb("mods2", [64, 4, 1024], F32, sc_); bmods = Buf("mods2")
                    FG = k.sb("FGs", [128, 1024], F32, sc_)
                    k.dma("sp", FG[:], fgd.unsqueeze(0).to_broadcast([128, 1024]), [], [bFG], bFG)
                    tm_mod(0, lambda c: modT[:, 16 + c, 0:64], mods, bmods)
                    tm_mod(1, lambda c: modT[:, 24 + c, 0:64], mods, bmods)
                    tm_mod(2, lambda c: G2T[:, c, 0:64], mods, bmods)
                    tm_mod(3, lambda c: modT[:, 40 + c, 0:64], mods, bmods)
                    Xs = k.sb("Xs2", [128, 1024], F32, sc_); bXs = Buf("Xs2")
                    X1 = [k.sb("X1s", [128, 1024], F32, sc_)]; bX1 = [Buf("X1s")]
                    xn2 = k.sb("xn2s", [64, 1024], F32, sc_); bxn2 = Buf("xn2s")
                    h2s = k.sb("h2s", [64, 1024], BF16, sc_); bh2s = Buf("h2s")
                    h2T = k.sb("h2Ts", [128, 8, 64], BF16, sc_); bh2T = Buf("h2Ts")
                    ffT = k.sb("ffTs", [128, NJ, 64], BF16, sc_); bffT = Buf("ffTs")
                    ring = [(k.sb(f"gus{i}", [128, 2, 8, 128], BF16, sc_), k.sb(f"dds{i}", [128, 1024], BF16, sc_)) for i in range(2)]
                    bring = [Buf(f"rings{i}") for i in range(2)]
                    for nb in range(2):
                        for kc_ in range(8):
                            k.mm(PB[4 + nb][0:64, :], mixT[:, kc_, 0:64], Wo[:, kc_, nb * 512:(nb + 1) * 512], [bmixT, bWo], [bPB[4 + nb]],
                                 start=(kc_ == 0), stop=(kc_ == 7), last=(kc_ == 7))
                    k.dma("sp", Xs[0:64, :], xs, [], [bXs], bXs)
                    x1 = X1[0]
                    for nb in range(2):
                        cs = slice(nb * 512, (nb + 1) * 512)
                        k.op("dve", lambda e, nb=nb, cs=cs: e.tensor_tensor(x1[0:64, cs], PB[4 + nb][0:64, :], mods[:, 0, cs], ALU.mult),
                             [bPB[4 + nb], bmods], [bX1[0]])
                    k.op("dve", lambda e: e.tensor_tensor(x1[0:64, :], x1[0:64, :], Xs[0:64, :], ALU.add), [bX1[0], bXs], [bX1[0]])
                    rms_stats(x1[0:64, :], bX1[0], 64, 1, h2s, bh2s)
                    k.op("dve", lambda e: e.scalar_tensor_tensor(xn2[:], x1[0:64, :], ss_t[0:64, 1:2], mods[:, 2, :], op0=ALU.mult, op1=ALU.mult), [bX1[0], bss, bmods], [bxn2])
                    k.op("dve", lambda e: e.tensor_tensor(h2s[:], xn2[:], mods[:, 1, :], ALU.add), [bxn2, bmods], [bh2s])
                    tp = pbf(7)
                    for c in range(8):
                        k.tr(tp[:, c * 64:(c + 1) * 64], h2s[:, c * 128:(c + 1) * 128], T["ident_b"][0:64, 0:64], [bh2s, bT], [bPB[7]], last=(c == 7))
                    k.op("act", lambda e: e.activation(h2T[:].rearrange("p c n -> p (c n)"), tp[:, 0:512], AF.Copy), [bPB[7]], [bh2T])

                    def out_fn_s(ti, ytile, bytile, np_):
                        k.dma("sp", ys, ytile[0:64, :], [bytile], [], bytile, final=True)
                    ffn_group(sc_, 1, 64, X1, bX1, h2T, bh2T, ffT, bffT, ring, bring, mods[:, 3, :], bmods, out_fn_s, FG, Xs, bXs, h2s, bh2s)
                    k.barrier()

        except _Stop:
            pass
        k.finish()
    return nc, tabs


_CACHE = {}


def kernel(x_prompt, x_sample, cache_attn_k, cache_attn_v, state_ret, c_prompt, c_sample,
           w_ada, b_ada, norm1_g, w_in, w_out, norm2_g, w_gate, w_up, w_down, final_g):
    f = lambda a: np.ascontiguousarray(np.asarray(a, dtype=np.float32))
    x_prompt, x_sample = f(x_prompt), f(x_sample)
    ck, cv, srr = f(cache_attn_k)[0], f(cache_attn_v)[0], f(state_ret)[0]
    c_prompt, c_sample = f(c_prompt), f(c_sample)
    if "nc" not in _CACHE:
        _CACHE["nc"] = build()
    nc, tabs = _CACHE["nc"]
    shared = {"w_ada": f(w_ada)[0], "b_ada": f(b_ada)[0], "n1g": f(norm1_g)[0], "w_in": f(w_in)[0],
              "w_out": f(w_out)[0], "n2g": f(norm2_g)[0], "w_gate": f(w_gate)[0], "w_up": f(w_up)[0],
              "w_down": f(w_down)[0], "fg": f(final_g)}
    for kk, v in tabs.items():
        shared["t_" + kk] = v
    in_maps = []
    for c in range(NCORES):
        m = dict(shared)
        m["xp"] = x_prompt[4 * c:4 * c + 4]
        m["xs"] = x_sample[16 * c:16 * c + 16].reshape(64, D)
        m["ck"] = ck[16 * c:16 * c + 16].reshape(16, S, 512)
        m["cv"] = cv[16 * c:16 * c + 16].reshape(16, S, 512)
        m["sr"] = srr[16 * c:16 * c + 16]
        m["call"] = np.concatenate([np.repeat(c_sample[16 * c:16 * c + 16], 4, axis=0), c_prompt[4 * c:4 * c + 4]], axis=0)
        in_maps.append(m)
    res = run_bass_kernel_spmd(nc, in_maps, core_ids=list(range(NCORES)))
    R = res.results
    y_prompt = np.concatenate([r["yp"] for r in R], 0)
    y_sample = np.concatenate([r["ys"].reshape(16, 4, D) for r in R], 0)
    nkp = np.concatenate([r["kp"].reshape(4, S, 8, 64) for r in R], 0)[None]
    nvp = np.concatenate([r["vp"].reshape(4, S, 8, 64) for r in R], 0)[None]
    nrp = np.concatenate([r["rp"] for r in R], 0)[None]
    nks = np.concatenate([r["kso"].reshape(16, 4, 8, 64) for r in R], 0)[None]
    nvs = np.concatenate([r["vso"].reshape(16, 4, 8, 64) for r in R], 0)[None]
    nrs = np.concatenate([r["rso"] for r in R], 0)[None]
    return (y_prompt, y_sample, nkp, nvp, nrp, nks, nvs, nrs)
```
